# Optimizing a Trainium2 kernel written in Bass

```python
import math
import jax, jax.numpy as jnp
from jax import lax
import numpy as np

D_MODEL = 1024
BATCH = 2
SEQ = 8192
DEPTH = 1

CHUNK = 64
D_MIX = 2 * D_MODEL
SSD_WIDTH = D_MIX // 2
SSD_HEAD_DIM = 64
SSD_HEADS = SSD_WIDTH // SSD_HEAD_DIM
SSD_GROUPS = 2
SSD_HPG = SSD_HEADS // SSD_GROUPS
SSD_STATE = 128
SSD_CONV = 4
SSD_CONV_CH = SSD_WIDTH + 2 * SSD_GROUPS * SSD_STATE
SC_WIDTH = D_MIX - SSD_WIDTH
SC_GROUP_DIM = 64
SC_GROUPS = SC_WIDTH // SC_GROUP_DIM
SC_CONV = 3
IN_COLS = SSD_WIDTH + SSD_CONV_CH + SSD_HEADS + 3 * SC_WIDTH
N_EXPERT_GROUPS = 4
EXPERTS_PER_GROUP = 8
N_EXPERTS = N_EXPERT_GROUPS * EXPERTS_PER_GROUP
TOP_K_IN_GROUP = 2
D_FF_EXPERT = D_MODEL // 2
EXPERT_BLOCK = 128
EPS = 1e-6

kernel_name = "hymba_ssd_shortconv_hiermoe_layer"


def rmsnorm(x, g):
    xf = x.astype(jnp.float32)
    y = xf * lax.rsqrt(jnp.mean(xf * xf, axis=-1, keepdims=True) + EPS)
    return (y * g.astype(jnp.float32)).astype(x.dtype)


def grouped_rmsnorm(x, g, n_groups):
    shp = x.shape
    xf = x.astype(jnp.float32).reshape(*shp[:-1], n_groups, shp[-1] // n_groups)
    y = xf * lax.rsqrt(jnp.mean(xf * xf, axis=-1, keepdims=True) + EPS)
    return (y.reshape(shp) * g.astype(jnp.float32)).astype(x.dtype)


def causal_depthwise_conv(x, w):
    k = w.shape[0]
    return lax.conv_general_dilated(
        x, w[:, None, :].astype(x.dtype), window_strides=(1,), padding=[(k - 1, 0)],
        dimension_numbers=("NWC", "WIO", "NWC"), feature_group_count=x.shape[-1])


def ssd_chunked_scan(x, dt, a, bmat, cmat):
    bsz, seqlen = x.shape[0], x.shape[1]
    nc = seqlen // CHUNK
    f32 = jnp.float32
    xdt = (x.astype(f32) * dt[..., None]).reshape(bsz, nc, CHUNK, SSD_GROUPS, SSD_HPG, SSD_HEAD_DIM)
    bc = bmat.astype(f32).reshape(bsz, nc, CHUNK, SSD_GROUPS, SSD_STATE)
    cc = cmat.astype(f32).reshape(bsz, nc, CHUNK, SSD_GROUPS, SSD_STATE)
    a_dt = (dt * a.astype(f32)).reshape(bsz, nc, CHUNK, SSD_GROUPS, SSD_HPG)
    ac = jnp.cumsum(a_dt, axis=2).transpose(0, 1, 3, 4, 2)
    seg = ac[..., :, None] - ac[..., None, :]
    causal = jnp.tril(jnp.ones((CHUNK, CHUNK), dtype=bool))
    decay_in = jnp.exp(jnp.where(causal, seg, -jnp.inf))
    cb = jnp.einsum("bclgn,bcsgn->bcgls", cc, bc)
    y_diag = jnp.einsum("bcgls,bcgrls,bcsgrp->bclgrp", cb, decay_in, xdt)
    decay_to_end = jnp.exp(ac[..., -1:] - ac)
    states = jnp.einsum("bclgn,bcgrl,bclgrp->bcgrpn", bc, decay_to_end, xdt)
    chunk_decay = jnp.exp(ac[..., -1])

    def step(carry, inp):
        st, dec = inp
        return carry * dec[..., None, None] + st, carry

    init = jnp.zeros((bsz, SSD_GROUPS, SSD_HPG, SSD_HEAD_DIM, SSD_STATE), f32)
    _, prev = lax.scan(step, init, (jnp.moveaxis(states, 1, 0), jnp.moveaxis(chunk_decay, 1, 0)))
    prev = jnp.moveaxis(prev, 0, 1)
    y_off = jnp.einsum("bclgn,bcgrpn,bcgrl->bclgrp", cc, prev, jnp.exp(ac))
    return (y_diag + y_off).reshape(bsz, seqlen, SSD_HEADS, SSD_HEAD_DIM)


def hybrid_mixer(hn, w_in, ssd_conv_w, ssd_conv_b, dt_bias, a_log, d_skip, ssd_norm,
                 sc_conv_w, sc_norm, w_out):
    bsz, seqlen, _ = hn.shape
    proj = hn @ w_in
    o1 = SSD_WIDTH
    o2 = o1 + SSD_CONV_CH
    o3 = o2 + SSD_HEADS
    o4 = o3 + SC_WIDTH
    o5 = o4 + SC_WIDTH
    z, xbc, dt_raw, sc_b, sc_c, sc_v = jnp.split(proj, [o1, o2, o3, o4, o5], axis=-1)
    xbc = jax.nn.silu(causal_depthwise_conv(xbc, ssd_conv_w) + ssd_conv_b)
    xs, bm, cm = jnp.split(xbc, [SSD_WIDTH, SSD_WIDTH + SSD_GROUPS * SSD_STATE], axis=-1)
    dt = jax.nn.softplus(dt_raw.astype(jnp.float32) + dt_bias.astype(jnp.float32))
    a = -jnp.exp(a_log.astype(jnp.float32))
    xh = xs.reshape(bsz, seqlen, SSD_HEADS, SSD_HEAD_DIM)
    y = ssd_chunked_scan(xh, dt, a,
                         bm.reshape(bsz, seqlen, SSD_GROUPS, SSD_STATE),
                         cm.reshape(bsz, seqlen, SSD_GROUPS, SSD_STATE))
    y = y + d_skip.astype(jnp.float32)[:, None] * xh.astype(jnp.float32)
    y = y.reshape(bsz, seqlen, SSD_WIDTH).astype(hn.dtype)
    ssd_out = grouped_rmsnorm(y * jax.nn.silu(z), ssd_norm, SSD_GROUPS)
    sc = sc_b * causal_depthwise_conv(sc_c * sc_v, sc_conv_w)
    sc_out = grouped_rmsnorm(sc, sc_norm, SC_GROUPS)
    return jnp.concatenate([ssd_out, sc_out], axis=-1) @ w_out


def hierarchical_moe(hn, w_router_group, w_router_expert, w_gate, w_up, w_down):
    bsz, seqlen, d = hn.shape
    tokens = hn.reshape(-1, d)
    n = tokens.shape[0]
    g_logits = (tokens @ w_router_group).astype(jnp.float32)
    g_probs = jax.nn.softmax(g_logits, axis=-1)
    g_idx = jnp.argmax(g_logits, axis=-1)
    g_w = jnp.take_along_axis(g_probs, g_idx[:, None], axis=1)
    e_logits = jnp.einsum("nd,gde->nge", tokens, w_router_expert).astype(jnp.float32)
    e_sel = jnp.take_along_axis(e_logits, g_idx[:, None, None], axis=1)[:, 0]
    top_v, top_i = lax.top_k(e_sel, TOP_K_IN_GROUP)
    gates = g_w * jax.nn.softmax(top_v, axis=-1)
    expert_id = (g_idx[:, None] * EXPERTS_PER_GROUP + top_i).reshape(-1).astype(jnp.int32)
    token_id = jnp.repeat(jnp.arange(n, dtype=jnp.int32), TOP_K_IN_GROUP)
    gate_flat = gates.reshape(-1)
    m = n * TOP_K_IN_GROUP
    order = jnp.argsort(expert_id)
    sorted_e = expert_id[order]
    counts = jnp.bincount(expert_id, length=N_EXPERTS)
    padded_counts = ((counts + EXPERT_BLOCK - 1) // EXPERT_BLOCK) * EXPERT_BLOCK
    padded_end = jnp.cumsum(padded_counts)
    padded_start = padded_end - padded_counts
    start = jnp.cumsum(counts) - counts
    rank = jnp.arange(m, dtype=jnp.int32) - start[sorted_e]
    dest = padded_start[sorted_e] + rank
    p_rows = ((m + N_EXPERTS * EXPERT_BLOCK + EXPERT_BLOCK - 1) // EXPERT_BLOCK) * EXPERT_BLOCK
    n_blocks = p_rows // EXPERT_BLOCK
    row_token = jnp.zeros((p_rows,), jnp.int32).at[dest].set(token_id[order])
    row_gate = jnp.zeros((p_rows,), jnp.float32).at[dest].set(gate_flat[order])
    block_expert = jnp.clip(
        jnp.searchsorted(padded_end, jnp.arange(n_blocks) * EXPERT_BLOCK, side="right"),
        0, N_EXPERTS - 1).astype(jnp.int32)
    xs = tokens[row_token].reshape(n_blocks, EXPERT_BLOCK, d)

    def run_block(args):
        xb, e = args
        return (jax.nn.silu(xb @ w_gate[e]) * (xb @ w_up[e])) @ w_down[e]

    ys = lax.map(run_block, (xs, block_expert)).reshape(p_rows, d)
    out = jnp.zeros((n, d), jnp.float32).at[row_token].add(ys.astype(jnp.float32) * row_gate[:, None])
    return out.reshape(bsz, seqlen, d).astype(hn.dtype)


def setup_inputs(seed: int = 0) -> dict:
    key = jax.random.key(seed)
    ks = jax.random.split(key, 24)
    f32 = jnp.float32

    def nrm(k, shape, scale):
        return jax.random.normal(k, shape, f32) * scale

    x = nrm(ks[0], (BATCH, SEQ, D_MODEL), 1.0)
    norm_mix = 1.0 + nrm(ks[1], (DEPTH, D_MODEL), 0.02)
    w_in = nrm(ks[2], (DEPTH, D_MODEL, IN_COLS), D_MODEL ** -0.5)
    ssd_conv_w = nrm(ks[3], (DEPTH, SSD_CONV, SSD_CONV_CH), SSD_CONV ** -0.5)
    ssd_conv_b = nrm(ks[4], (DEPTH, SSD_CONV_CH), 0.01)
    dt0 = jnp.exp(jax.random.uniform(ks[5], (DEPTH, SSD_HEADS), f32,
                                     minval=math.log(1e-3), maxval=math.log(1e-1)))
    dt_bias = dt0 + jnp.log(-jnp.expm1(-dt0))
    a_log = jnp.log(jax.random.uniform(ks[6], (DEPTH, SSD_HEADS), f32, minval=1.0, maxval=16.0))
    d_skip = 1.0 + nrm(ks[7], (DEPTH, SSD_HEADS), 0.1)
    ssd_norm = 1.0 + nrm(ks[8], (DEPTH, SSD_WIDTH), 0.02)
    sc_conv_w = nrm(ks[9], (DEPTH, SC_CONV, SC_WIDTH), SC_CONV ** -0.5)
    sc_norm = 1.0 + nrm(ks[10], (DEPTH, SC_WIDTH), 0.02)
    w_out = nrm(ks[11], (DEPTH, D_MIX, D_MODEL), D_MIX ** -0.5)
    norm_ffn = 1.0 + nrm(ks[12], (DEPTH, D_MODEL), 0.02)
    w_router_group = nrm(ks[13], (DEPTH, D_MODEL, N_EXPERT_GROUPS), D_MODEL ** -0.5)
    w_router_expert = nrm(ks[14], (DEPTH, N_EXPERT_GROUPS, D_MODEL, EXPERTS_PER_GROUP), D_MODEL ** -0.5)
    w_gate = nrm(ks[15], (DEPTH, N_EXPERTS, D_MODEL, D_FF_EXPERT), D_MODEL ** -0.5)
    w_up = nrm(ks[16], (DEPTH, N_EXPERTS, D_MODEL, D_FF_EXPERT), D_MODEL ** -0.5)
    w_down = nrm(ks[17], (DEPTH, N_EXPERTS, D_FF_EXPERT, D_MODEL), D_FF_EXPERT ** -0.5)
    final_norm = 1.0 + nrm(ks[18], (D_MODEL,), 0.02)
    return {"x": x, "norm_mix": norm_mix, "w_in": w_in, "ssd_conv_w": ssd_conv_w,
            "ssd_conv_b": ssd_conv_b, "dt_bias": dt_bias, "a_log": a_log, "d_skip": d_skip,
            "ssd_norm": ssd_norm, "sc_conv_w": sc_conv_w, "sc_norm": sc_norm, "w_out": w_out,
            "norm_ffn": norm_ffn, "w_router_group": w_router_group,
            "w_router_expert": w_router_expert, "w_gate": w_gate, "w_up": w_up,
            "w_down": w_down, "final_norm": final_norm}


def reference(x, norm_mix, w_in, ssd_conv_w, ssd_conv_b, dt_bias, a_log, d_skip, ssd_norm,
              sc_conv_w, sc_norm, w_out, norm_ffn, w_router_group, w_router_expert,
              w_gate, w_up, w_down, final_norm):
    h = x
    for i in range(DEPTH):
        hn = rmsnorm(h, norm_mix[i])
        h = h + hybrid_mixer(hn, w_in[i], ssd_conv_w[i], ssd_conv_b[i], dt_bias[i], a_log[i],
                             d_skip[i], ssd_norm[i], sc_conv_w[i], sc_norm[i], w_out[i])
        hn = rmsnorm(h, norm_ffn[i])
        h = h + hierarchical_moe(hn, w_router_group[i], w_router_expert[i],
                                 w_gate[i], w_up[i], w_down[i])
    return rmsnorm(h, final_norm)
```

```python
from contextlib import ExitStack
import numpy as np
import concourse.bass as bass
import concourse.mybir as mybir
from concourse.bass_utils import run_bass_kernel_spmd

F32 = mybir.dt.float32
BF16 = mybir.dt.bfloat16
ALU = mybir.AluOpType
AF = mybir.ActivationFunctionType
AX = mybir.AxisListType

ENGS = ("pe", "act", "dve", "pool", "sp")
SEM_LIMIT = 12000
NCORES = 8
SPAN = 2048
NPREV = 3
HALO = 8
NTOK_EXT = HALO + (NPREV + 1) * SPAN
D = 1024
EPS = 1e-6
MOE_CAP = 384
I32 = mybir.dt.int32


class T:
    __slots__ = ("name", "w", "r", "rd")

    def __init__(self, name=""):
        self.name = name
        self.w = None
        self.r = {}
        self.rd = []


class Prog:
    def __init__(self, nc):
        self.nc = nc
        self.ops = []
        self.dma_sems = {}
        self.last = {}
        self.dmas = []

    def _deps(self, reads, writes):
        deps = set()
        for t in reads:
            if t.w is not None:
                deps.add(t.w)
        for t in writes:
            if t.w is not None:
                deps.add(t.w)
            deps.update(t.r.values())
            deps.update(t.rd)
        return deps

    def op(self, eng, fn, reads=(), writes=()):
        i = len(self.ops)
        self.ops.append(dict(eng=eng, fn=fn, deps=self._deps(reads, writes), kind="c"))
        self.last[eng] = i
        for t in reads:
            t.r[eng] = i
        for t in writes:
            t.w = i
            t.r = {}
            t.rd = []
        return i

    def dma(self, eng, fn, sem, reads=(), writes=(), inc=16):
        i = len(self.ops)
        n = self.dma_sems.get(sem, 0) + inc
        self.dma_sems[sem] = n
        self.ops.append(dict(eng=eng, fn=fn, deps=self._deps(reads, writes), kind="d", sem=sem, val=n, inc=inc))
        self.dmas.append(i)
        for t in reads:
            t.rd.append(i)
        for t in writes:
            t.w = i
            t.r = {}
            t.rd = []
        return i

    def barrier(self, engs=ENGS):
        deps = set(self.last.values()) | set(self.dmas)
        self.dmas = []
        for e in engs:
            self.ops.append(dict(eng=e, fn=None, deps=set(deps), kind="c"))

    def emit(self, stack):
        nc = self.nc
        ops = self.ops
        needed = set()
        for o in ops:
            for d in o["deps"]:
                if ops[d]["kind"] == "c":
                    needed.add(d)
        cnt = {e: 0 for e in ENGS}
        for i, o in enumerate(ops):
            if o["kind"] == "c" and i in needed and o["fn"] is not None:
                cnt[o["eng"]] += 1
                o["cnt"] = cnt[o["eng"]]
        esems = {}
        for e in ENGS:
            n = cnt[e] // SEM_LIMIT + 1
            esems[e] = [stack.enter_context(nc.semaphore(f"s_{e}{k}")) for k in range(n)]
        dsems = {k: stack.enter_context(nc.semaphore(f"d_{k}")) for k in self.dma_sems}
        per = {e: [] for e in ENGS}
        for i, o in enumerate(ops):
            per[o["eng"]].append(i)

        def section(e):
            def body(eng):
                seen = {}
                for i in per[e]:
                    o = ops[i]
                    w = {}
                    for d in o["deps"]:
                        od = ops[d]
                        if od["kind"] == "c":
                            if od["fn"] is None or "cnt" not in od:
                                continue
                            if od["eng"] == e and e == "pe":
                                continue
                            c = od["cnt"]
                            key = ("e", od["eng"], (c - 1) // SEM_LIMIT)
                            val = (c - 1) % SEM_LIMIT + 1
                        else:
                            key = ("d", od["sem"])
                            val = od["val"]
                        if w.get(key, 0) < val:
                            w[key] = val
                    for key, val in sorted(w.items(), key=lambda kv: str(kv[0])):
                        if seen.get(key, 0) >= val:
                            continue
                        seen[key] = val
                        if key[0] == "e":
                            sem = esems[key[1]][key[2]]
                            for kk in range(key[2]):
                                seen[("e", key[1], kk)] = SEM_LIMIT
                        else:
                            sem = dsems[key[1]]
                        eng.wait_ge(sem, val)
                    if o["fn"] is None:
                        continue
                    ins = o["fn"](eng)
                    if o["kind"] == "c":
                        if "cnt" in o:
                            c = o["cnt"]
                            ins.then_inc(esems[e][(c - 1) // SEM_LIMIT], 1)
                    else:
                        ins.then_inc(dsems[o["sem"]], o["inc"])
            return body

        with nc.Block() as block:
            block.tensor(section("pe"))
            block.scalar(section("act"))
            block.vector(section("dve"))
            block.gpsimd(section("pool"))
            block.sync(section("sp"))


CP = {}
_o = 0
for _n, _w in (("ident", 128), ("tri", 128), ("lstrict", 128), ("ones", 128), ("bd64", 128),
               ("gmix", 8), ("gffn", 8), ("gain16", 16), ("cwssd", 48), ("cbssd", 12), ("cwsc", 24),
               ("dtb", 16), ("alog", 16), ("dsk", 16), ("wr", 288), ("bmask", 4), ("ecap", 32), ("tokid", 16)):
    CP[_n] = (_o, _w)
    _o += _w
NCP = _o


def make_cp(inp, core):
    cp = np.zeros((128, NCP), np.float32)

    def put(name, arr):
        o, w = CP[name]
        cp[:, o:o + w] = np.asarray(arr, np.float32).reshape(128, w)

    idx = np.arange(128)
    put("ident", np.eye(128))
    put("tri", (idx[:, None] <= idx[None, :]))
    put("lstrict", (idx[:, None] > idx[None, :]))
    put("ones", np.ones((128, 128)))
    put("bd64", (idx[:, None] // 64 == idx[None, :] // 64))
    put("gmix", inp["norm_mix"][0].reshape(8, 128).T)
    put("gffn", inp["norm_ffn"][0].reshape(8, 128).T)
    put("gain16", np.concatenate([inp["ssd_norm"][0].reshape(8, 128).T, inp["sc_norm"][0].reshape(8, 128).T], 1))
    put("cwssd", inp["ssd_conv_w"][0].reshape(4, 12, 128).transpose(2, 1, 0))
    put("cbssd", inp["ssd_conv_b"][0].reshape(12, 128).T)
    put("cwsc", inp["sc_conv_w"][0].reshape(3, 8, 128).transpose(2, 1, 0))
    put("dtb", np.broadcast_to(inp["dt_bias"][0][None, :], (128, 16)))
    put("alog", np.broadcast_to(inp["a_log"][0][None, :], (128, 16)))
    put("dsk", np.broadcast_to(inp["d_skip"][0][None, :], (128, 16)))
    wr = np.concatenate([inp["w_router_group"][0],
                         inp["w_router_expert"][0].transpose(1, 0, 2).reshape(1024, 32)], 1)
    put("wr", wr.reshape(8, 128, 36).transpose(1, 0, 2))
    s = core % 4
    put("bmask", np.broadcast_to(np.array([1.0 if b >= NPREV - s else 0.0 for b in range(NPREV + 1)],
                                          np.float32)[None, :], (128, 4)))
    put("ecap", np.broadcast_to((np.arange(32, dtype=np.float32) * MOE_CAP)[None, :], (128, 32)))
    put("tokid", (np.arange(16)[None, :] * 128 + np.arange(128)[:, None]).astype(np.float32))
    return cp


def build(dbg=False, stop_after=None, n_experts=32):
    nc = bass.Bass("TRN2", target_bir_lowering=False)

    def dram(name, shape, kind="ExternalInput"):
        return nc.dram_tensor(name, shape, F32, kind=kind).ap()

    xe = dram("xe", [NTOK_EXT, D])
    cpd = dram("cp", [128, NCP])
    fnd = dram("fnorm", [128, D])
    gfd = dram("gfb", [128, D])
    w_in = dram("w_in", [D, 5648])
    w_out = dram("w_out", [2048, D])
    w_gate = dram("w_gate", [32, D, 512])
    w_up = dram("w_up", [32, D, 512])
    w_down = dram("w_down", [32, 512, D])
    out = dram("out", [SPAN, D], kind="ExternalOutput")
    dbg_out = {}

    P = Prog(nc)
    st = ExitStack()
    with st:
        ARENA_N = 53200
        arena = st.enter_context(nc.sbuf_tensor("arena", [128, ARENA_N], F32))
        pbt = [st.enter_context(nc.psum_tensor(f"pb{i}", [128, 512], F32)) for i in range(7)]
        pTb = st.enter_context(nc.psum_tensor("pTb", [128, 8, 128], BF16))
        pb = [t[:] for t in pbt]
        Tpb = [T(f"pb{i}") for i in range(7)]
        TpTb = T("pTb")

        a_f32 = arena[:]
        a_bf = arena[:].bitcast(BF16)
        a_i32 = arena[:].bitcast(mybir.dt.int32)

        class Alloc:
            def __init__(self, start, end):
                self.p = start
                self.end = end

            def get(self, n_elems, dt, shape=None):
                nb = n_elems * (2 if dt == BF16 else 4)
                nb_al = (nb + 63) // 64 * 64
                off = self.p
                assert off + nb_al <= self.end, ("arena overflow", off, nb_al, self.end)
                self.p += nb_al
                if dt == F32:
                    ap = a_f32[:, off // 4: off // 4 + n_elems]
                elif dt == mybir.dt.int32:
                    ap = a_i32[:, off // 4: off // 4 + n_elems]
                else:
                    ap = a_bf[:, off // 2: off // 2 + n_elems]
                if shape is not None and len(shape) == 2:
                    ap = ap.rearrange("p (a b) -> p a b", b=shape[1])
                return ap

        PERS0 = 0
        PERS1 = 13824
        RX0, RX1 = PERS1, PERS1 + 32768
        RH0, RH1 = RX1, RX1 + 65792
        RBC0, RBC1 = RH1, RH1 + 16384
        RSC0, RSC1 = RBC1, RBC1 + 32768
        RS20, RS21 = RSC1, ARENA_N * 4

        pers = Alloc(PERS0, PERS1)
        cp = pers.get(NCP, F32)
        Tcp = T("cp")

        def cpv(name, shape=None):
            o, w = CP[name]
            ap = cp[:, o:o + w]
            if shape is not None:
                ap = ap.rearrange("p (a b) -> p a b", b=shape[1])
            return ap

        identb = pers.get(128, BF16)
        bd64b = pers.get(128, BF16)
        Sst = pers.get(1024, F32)
        Sb = pers.get(1024, BF16)
        dtt = pers.get(256, F32, (16, 16))
        adt = pers.get(256, F32, (16, 16))
        avec = pers.get(16, F32)
        epsc = pers.get(1, F32)
        pers_stats = pers.get(64, F32)
        Tconst = T("const")
        TS_, TSb, Tdt, Tstats = T("S"), T("Sb"), T("dt"), T("stats")

        def act(out_, in_, func, reads, writes, **kw):
            P.op("act", lambda e: e.activation(out=out_, in_=in_, func=func, **kw), reads, writes)

        def tt(eng, out_, in0, in1, op, reads, writes):
            P.op(eng, lambda e: e.tensor_tensor(out=out_, in0=in0, in1=in1, op=op), reads, writes)

        def ts(eng, out_, in0, s1, op0, reads, writes, s2=None, op1=None):
            if op1 is None:
                P.op(eng, lambda e: e.tensor_scalar(out=out_, in0=in0, scalar1=s1, scalar2=None, op0=op0), reads, writes)
            else:
                P.op(eng, lambda e: e.tensor_scalar(out=out_, in0=in0, scalar1=s1, scalar2=s2, op0=op0, op1=op1), reads, writes)

        def stt(eng, out_, in0, scalar, in1, op0, op1, reads, writes):
            P.op(eng, lambda e: e.scalar_tensor_tensor(out=out_, in0=in0, scalar=scalar, in1=in1, op0=op0, op1=op1), reads, writes)

        def cpy(eng, out_, in_, reads, writes):
            if eng == "act":
                P.op("act", lambda e: e.copy(out=out_, in_=in_), reads, writes)
            else:
                P.op(eng, lambda e: e.tensor_copy(out=out_, in_=in_), reads, writes)

        def mm(out_, lhsT, rhs, start, stop, reads, writes):
            P.op("pe", lambda e: e.matmul(out_, lhsT=lhsT, rhs=rhs, start=start, stop=stop), reads, writes)

        def tr(out_, in_, ident, reads, writes):
            P.op("pe", lambda e: e.transpose(out=out_, in_=in_, identity=ident), reads, writes)

        stat_tiles = [pers_stats[:, 8 * q: 8 * q + 8] for q in range(8)]
        Tstat_tiles = [T(f"stat{q}") for q in range(8)]
        stc = [0]

        def stat_slot():
            q = stc[0] % 8
            stc[0] += 1
            return stat_tiles[q], Tstat_tiles[q]

        def rmsnorm_stats(x_ap, junk_ap, n, reads, Tjunk):
            sl_, Tsl_ = stat_slot()
            act(junk_ap, x_ap, AF.Square, reads, [Tjunk, Tsl_], accum_out=sl_[:n, 0:1])
            act(sl_[:n, 1:2], sl_[:n, 0:1], AF.Ln, [Tsl_, Tconst], [Tsl_], bias=epsc[:n, 0:1], scale=1.0 / D)
            act(sl_[:n, 2:3], sl_[:n, 1:2], AF.Exp, [Tsl_], [Tsl_], scale=-0.5)
            return sl_, Tsl_

        Tdump = T("dump")

        def dump(name, ap, shp, dt):
            if not dbg:
                return
            d = nc.dram_tensor("dbg_" + name, [128] + shp, dt, kind="ExternalOutput").ap()
            P.barrier()
            P.dma("sp", lambda e, d=d, ap=ap: e.dma_start(out=d, in_=ap), "dump", writes=[Tdump])
            P.barrier()

        P.dma("sp", lambda e: e.dma_start(out=cp, in_=cpd), "cp", writes=[Tcp])
        P.op("pool", lambda e: e.memset(epsc, EPS), writes=[Tconst])
        cpy("dve", identb, cpv("ident"), [Tcp], [Tconst])
        cpy("dve", bd64b, cpv("bd64"), [Tcp], [Tconst])
        act(avec, cpv("alog"), AF.Exp, [Tcp], [Tconst])
        ts("dve", avec, avec, -1.0, ALU.mult, [Tconst], [Tconst])
        P.op("pool", lambda e: e.memset(Sst, 0.0), writes=[TS_])

        rx = Alloc(RX0, RX1)
        xsT = rx.get(8 * 2048, BF16, (8, 2048))
        TxsT = [T(f"xsT{i}") for i in range(16)]
        rh = Alloc(RH0, RH1)
        siluz = rh.get(16 * 1024, BF16, (16, 1024))
        Tsiluz = [T(f"sz{i}") for i in range(16)]
        hnT = rh.get(8 * 2056, BF16, (8, 2056))
        ThnT = [T(f"hnT{i}") for i in range(17)]
        rbc = Alloc(RBC0, RBC1)
        BT = rbc.get(2 * 2048, BF16, (2, 2048))
        CT = rbc.get(2 * 2048, BF16, (2, 2048))
        TBT, TCT = T("BT"), T("CT")

        rs2 = Alloc(RS20, RS21)
        wts = [rs2.get(8 * 256, BF16, (8, 256)) for _ in range(4)]
        Twts = [T(f"wt{i}") for i in range(4)]
        U = [rs2.get(2056, BF16) for _ in range(3)]
        TU = [T(f"U{i}") for i in range(3)]
        xt = [rs2.get(1024, F32) for _ in range(2)]
        Txt = [T(f"xt{i}") for i in range(2)]
        dg = [rs2.get(4 * 128, BF16, (4, 128)) for _ in range(2)]
        Tdg = [T("dg0"), T("dg1")]
        acs_all = rs2.get(512, F32)
        dd_all = rs2.get(256, F32, (16, 16))
        ed_all = rs2.get(256, F32, (16, 16))
        eq_all = rs2.get(256, F32, (16, 16))
        ea_all = rs2.get(256, F32, (16, 16))
        LT = rs2.get(256, F32, (16, 16))
        wgt_all = rs2.get(256, F32, (16, 16))
        Tcum = T("cum")
        rsc = Alloc(RSC0, RSC1)
        xn = [rsc.get(1024, BF16) for _ in range(2)]
        Txn = [T(f"xn{i}") for i in range(2)]
        wdt = rsc.get(8 * 16, BF16, (8, 16))
        Twdt = T("wdt")
        dtmp = rsc.get(256, F32)
        Tdtmp = T("dtmp")
        xdt = [rsc.get(1024, BF16) for _ in range(2)]
        Txdt = [T(f"xdt{i}") for i in range(2)]
        xdtdec = rsc.get(1024, BF16)
        Txdtdec = T("xdtdec")
        xsD_ = [rsc.get(1024, BF16), rs2.get(1024, BF16)]
        TxsD_ = [T("xsD0"), T("xsD1")]
        Btok = [rsc.get(256, BF16) for _ in range(2)]
        TBtok = [T(f"Btok{i}") for i in range(2)]
        CBm = rsc.get(256, F32, (2, 128))
        TCBm = T("CBm")
        Rb = [rsc.get(512, F32, (4, 128)) for _ in range(2)]
        TRb = [T(f"R{i}") for i in range(2)]
        Eb = [rsc.get(512, BF16, (4, 128)) for _ in range(2)]
        TEb = [T(f"E{i}") for i in range(2)]
        MT = rsc.get(16 * 128, BF16, (16, 128))
        TMT = [T(f"MT{i}") for i in range(4)]
        ybuf = [rsc.get(1024, F32), a_f32[:, RSC0 // 4: RSC0 // 4 + 1024]]
        Tybuf = [T("y0"), T("y1")]
        ygn = rsc.get(1024, BF16)
        Tygn = T("ygn")

        pTs = [pTb[:], pb[6].bitcast(BF16).rearrange("p (k t) -> p k t", t=128),
               pb[3].bitcast(BF16).rearrange("p (k t) -> p k t", t=128),
               pb[2].bitcast(BF16).rearrange("p (k t) -> p k t", t=128)]
        TpTs = [TpTb, Tpb[6], Tpb[3], Tpb[2]]
        ptc = [0]
        pt_depth = [4]

        def next_pT():
            j = ptc[0] % pt_depth[0]
            ptc[0] += 1
            return pTs[j], TpTs[j]

        wctr = [0]

        def load_w(cols0, ncols):
            j = wctr[0] % 4
            wctr[0] += 1
            dst = wts[j][:, :, 0:ncols]
            src = w_in[:, cols0:cols0 + ncols].rearrange("(k p) c -> p k c", p=128)
            P.dma("pool", lambda e: e.dma_start(out=dst, in_=src), f"wt{j}", writes=[Twts[j]])
            return wts[j], Twts[j]

        xctr = [0]
        uctr = [0]
        dgc = [0]
        cvc = [0]

        def next_u():
            j = uctr[0] % 3
            uctr[0] += 1
            return U[j], TU[j]

        gmix_b = cpv("gmix").unsqueeze(2)
        identf = cpv("ident")

        def hn_tiles(tt4):
            return ThnT[1 + 4 * tt4: 5 + 4 * tt4]

        def proj_chunk(wt_ap, Tw, lc):
            for k in range(8):
                mm(pb[4][:, 0:HALO], wt_ap[:, k, lc * 128:(lc + 1) * 128], hnT[:, k, 0:HALO], k == 0, k == 7,
                   [Tw, ThnT[0]], [Tpb[4]])
            for t4 in range(4):
                for k in range(8):
                    mm(pb[t4], wt_ap[:, k, lc * 128:(lc + 1) * 128],
                       hnT[:, k, HALO + t4 * 512: HALO + (t4 + 1) * 512], k == 0, k == 7,
                       [Tw] + hn_tiles(t4), [Tpb[t4]])

        def evac_to_u(u, Tu, all_act=False):
            cpy("act", u[:, 0:HALO], pb[4][:, 0:HALO], [Tpb[4]], [Tu])
            for t4 in range(4):
                cpy("act" if (t4 % 2 == 0 or all_act) else "dve", u[:, HALO + t4 * 512: HALO + (t4 + 1) * 512], pb[t4],
                    [Tpb[t4]], [Tu])

        cacc = [a_f32[:, RH0 // 4 + q * 2048: RH0 // 4 + (q + 1) * 2048] for q in range(2)]
        Tcacc = [T("cacc0"), T("cacc1")]

        def make_diag(wcol, ntap):
            j = dgc[0] % 2
            dgc[0] += 1
            for jt in range(ntap):
                act(dg[j][:, jt, :], identf, AF.Copy, [Tcp], [Tdg[j]], scale=wcol(jt))
            return dg[j], Tdg[j]

        def conv_pe_bank():
            j = 5 + cvc[0] % 2
            cvc[0] += 1
            return pb[j], Tpb[j]

        def conv_pe(u, Tu, d, Td, ntap, t4):
            j = 5 + cvc[0] % 2
            cvc[0] += 1
            base = HALO - (ntap - 1) + t4 * 512
            for jt in range(ntap):
                mm(pb[j], d[:, jt, :], u[:, base + jt: base + jt + 512], jt == 0, jt == ntap - 1, [Td, Tu], [Tpb[j]])
            return pb[j], Tpb[j]

        a1_state = {}

        def a1_front(blk_, ti):
            row0_ = HALO + blk_ * SPAN
            n = HALO if ti == 0 else 128
            r0 = row0_ - HALO if ti == 0 else row0_ + (ti - 1) * 128
            j = xctr[0] % 2
            xctr[0] += 1
            xtj, xnj = xt[j], xn[j]
            P.dma("sp", lambda e, xtj=xtj, r0=r0, n=n: e.dma_start(out=xtj[:n, :], in_=xe[r0:r0 + n, :]),
                  f"xt{j}", writes=[Txt[j]])
            sc_, Tsc_ = rmsnorm_stats(xtj[:n, :], xnj[:n, :], n, [Txt[j]], Txn[j])
            ts("dve", xnj[:n, :], xtj[:n, :], sc_[:n, 2:3], ALU.mult, [Txt[j], Tsc_], [Txn[j]])
            a1_state[(blk_, ti)] = (j, n)

        def a1_back(blk_, ti):
            j, n = a1_state.pop((blk_, ti))
            xnj = xn[j]
            c0 = 0 if ti == 0 else HALO + (ti - 1) * 128
            pT, TpT = next_pT()
            for k in range(8):
                tr(pT[:, k, :n], xnj[:n, k * 128:(k + 1) * 128], identb[:n, :n], [Txn[j], Tconst], [TpT])
            tt("dve", hnT[:, :, c0:c0 + n], pT[:, :, :n], gmix_b.to_broadcast([128, 8, n]),
               ALU.mult, [TpT, Tcp], [ThnT[ti]])

        a1_front(0, 0)
        for ti in range(17):
            if ti + 1 < 17:
                a1_front(0, ti + 1)
            a1_back(0, ti)

        for blk in range(NPREV + 1):
            own = blk == NPREV
            row0 = HALO + blk * SPAN
            cwssd = cpv("cwssd")
            cbssd = cpv("cbssd")
            conv_chunks = list(range(8)) + [8, 9] + ([10, 11] if own else [])

            def conv_silu(c, u, Tu):
                acc, Tacc = cacc[c % 2], Tcacc[c % 2]
                base = HALO - 3
                ts("dve", acc, u[:, base:base + SPAN], cwssd[:, c * 4:c * 4 + 1], ALU.mult, [Tu, Tcp], [Tacc])
                for jt in range(1, 4):
                    stt("dve", acc, u[:, base + jt:base + jt + SPAN], cwssd[:, c * 4 + jt:c * 4 + jt + 1], acc,
                        ALU.mult, ALU.add, [Tu, Tcp, Tacc], [Tacc])
                if c < 8:
                    dst, Tdst = xsT[:, c, :], TxsT
                elif c < 10:
                    dst, Tdst = BT[:, c - 8, :], [TBT]
                else:
                    dst, Tdst = CT[:, c - 10, :], [TCT]
                act(dst, acc, AF.Silu, [Tacc, Tcp], Tdst, bias=cbssd[:, c:c + 1])

            pending = None
            for g0 in range(0, len(conv_chunks), 2):
                cc0 = conv_chunks[g0]
                wt_ap, Tw = load_w(1024 + cc0 * 128, 256)
                for lc in range(2):
                    c = cc0 + lc
                    proj_chunk(wt_ap, Tw, lc)
                    u, Tu = next_u()
                    evac_to_u(u, Tu, all_act=True)
                    if pending is not None:
                        conv_silu(*pending)
                    pending = (c, u, Tu)
            conv_silu(*pending)

            src = w_in[:, 2560:2576].rearrange("(k p) c -> p k c", p=128)
            P.dma("pool", lambda e, src=src: e.dma_start(out=wdt, in_=src), "wdt", writes=[Twdt])
            for i in range(16):
                for k in range(8):
                    mm(pb[4][:, 16 + i * 16: 32 + i * 16], hnT[:, k, HALO + i * 128: HALO + (i + 1) * 128], wdt[:, k, :],
                       k == 0, k == 7, [ThnT[1 + i], Twdt], [Tpb[4]])
            dtb_b = cpv("dtb").unsqueeze(1).to_broadcast([128, 16, 16])
            dt3 = dtmp.rearrange("p (a b) -> p a b", b=16)
            tt("dve", dt3, pb[4][:, 16:272].rearrange("p (a b) -> p a b", b=16), dtb_b, ALU.add, [Tpb[4], Tcp], [Tdtmp])
            act(dtmp, dtmp, AF.Exp, [Tdtmp], [Tdtmp])
            act(dtmp, dtmp, AF.Ln, [Tdtmp], [Tdtmp], bias=1.0)
            bm = cpv("bmask")
            ts("dve", dtt, dt3, bm[:, blk:blk + 1], ALU.mult, [Tdtmp, Tcp], [Tdt])
            tt("dve", adt, dtt, avec.unsqueeze(1).to_broadcast([128, 16, 16]), ALU.mult, [Tdt, Tconst], [Tdt])
            adt_f = adt.rearrange("p a b -> p (a b)")
            mm(pb[4][:, 0:256], cpv("tri"), adt_f, True, True, [Tcp, Tdt], [Tpb[4]])
            mm(pb[4][:, 256:512], cpv("ones"), adt_f, True, True, [Tcp, Tdt], [Tpb[4]])
            cpy("act", acs_all, pb[4], [Tpb[4]], [Tcum])
            ac3 = acs_all[:, 0:256].rearrange("p (a b) -> p a b", b=16)
            aq3 = acs_all[:, 256:512].rearrange("p (a b) -> p a b", b=16)
            tt("dve", dd_all, aq3, ac3, ALU.subtract, [Tcum], [Tcum])

            tri = cpv("tri")
            if not own:
                P.op("pool", lambda e: e.memset(LT[:, 15, :], 0.0), [Tcum], [Tcum])
                for i in range(14, -1, -1):
                    tt("pool", LT[:, i, :], LT[:, i + 1, :], aq3[:, i + 1, :], ALU.add, [Tcum], [Tcum])
                tt("dve", dd_all, dd_all, LT, ALU.add, [Tcum], [Tcum])
                act(ed_all, dd_all, AF.Exp, [Tcum], [Tcum])
                tt("dve", wgt_all, ed_all, dtt, ALU.mult, [Tcum, Tdt], [Tcum])
                tt("pool", eq_all[:, 0, :], LT[:, 0, :], aq3[:, 0, :], ALU.add, [Tcum], [Tcum])
                act(eq_all[:, 0, :], eq_all[:, 0, :], AF.Exp, [Tcum], [Tcum])
                for i in range(16):
                    tok = slice(i * 128, (i + 1) * 128)
                    xd, Txd = xdt[i % 2], Txdt[i % 2]
                    bt, Tbt = Btok[i % 2], TBtok[i % 2]
                    pT, TpT = next_pT()
                    for c in range(8):
                        tr(pT[:, c, :], xsT[:, c, tok], identb, [TxsT[i], Tconst], [TpT])
                    pX = pT.rearrange("p c t -> p (c t)").rearrange("p (h d) -> p h d", d=64)
                    tt("dve", xd.rearrange("p (h d) -> p h d", d=64), pX,
                       wgt_all[:, i, :].unsqueeze(2).to_broadcast([128, 16, 64]), ALU.mult, [TpT, Tcum], [Txd])
                    pT2, TpT2 = next_pT()
                    for g in range(2):
                        tr(pT2[:, g, :], BT[:, g, tok], identb, [TBT, Tconst], [TpT2])
                    cpy("act", bt.rearrange("p (g n) -> p g n", n=128), pT2[:, 0:2, :], [TpT2], [Tbt])
                    for g in range(2):
                        mm(pb[g], bt[:, g * 128:(g + 1) * 128], xd[:, g * 512:(g + 1) * 512], i == 0, i == 15,
                           [Tbt, Txd], [Tpb[g]])
                    if i == 0:
                        a1_front(blk + 1, 0)
                    a1_front(blk + 1, i + 1)
                    a1_back(blk + 1, i)
                a1_back(blk + 1, 16)
                tt("pool", Sst.rearrange("p (h d) -> p h d", d=64), Sst.rearrange("p (h d) -> p h d", d=64),
                   eq_all[:, 0, :].unsqueeze(2).to_broadcast([128, 16, 64]), ALU.mult, [TS_, Tcum], [TS_])
                for g in range(2):
                    tt("dve", Sst[:, g * 512:(g + 1) * 512], Sst[:, g * 512:(g + 1) * 512], pb[g], ALU.add,
                       [TS_, Tpb[g]], [TS_])
                continue

            act(ed_all, dd_all, AF.Exp, [Tcum], [Tcum])
            act(eq_all, aq3, AF.Exp, [Tcum], [Tcum])
            act(ea_all, ac3, AF.Exp, [Tcum], [Tcum])
            for q in range(4):
                wt_ap, Tw = load_w(q * 256, 256)
                for i in range(16):
                    pz, Tpz = pb[5 + i % 2], Tpb[5 + i % 2]
                    for k in range(8):
                        mm(pz[:, 0:256], hnT[:, k, HALO + i * 128: HALO + (i + 1) * 128], wt_ap[:, k, :], k == 0, k == 7,
                           [ThnT[1 + i], Tw], [Tpz])
                    act(siluz[:, i, q * 256:(q + 1) * 256], pz[:, 0:256], AF.Silu, [Tpz], [Tsiluz[i], Tcacc[0], Tcacc[1]])
            cpy("act", Sb, Sst, [TS_], [TSb])

            pt_depth[0] = 2
            xdtdec_ = [xdtdec, U[2][:, 0:1024]]
            Txdtdec_ = [Txdtdec, T("xdtdec1")]
            MT_ = [MT, U[1][:, 0:2048].rearrange("p (h l) -> p h l", l=128)]
            TMT_ = [TMT, [T(f"MTb{q}") for q in range(4)]]

            def stageA(i):
                tok = slice(i * 128, (i + 1) * 128)
                xd, Txd = xdt[i % 2], Txdt[i % 2]
                bt, Tbt = Btok[i % 2], TBtok[i % 2]
                xsD, TxsD = xsD_[i % 2], TxsD_[i % 2]
                xdd, Txdd = xdtdec_[i % 2], Txdtdec_[i % 2]
                pT, TpT = next_pT()
                for c in range(8):
                    tr(pT[:, c, :], xsT[:, c, tok], identb, [TxsT[i], Tconst], [TpT])
                pX = pT.rearrange("p c t -> p (c t)").rearrange("p (h d) -> p h d", d=64)
                tt("dve", xd.rearrange("p (h d) -> p h d", d=64), pX, dtt[:, i, :].unsqueeze(2).to_broadcast([128, 16, 64]),
                   ALU.mult, [TpT, Tdt], [Txd])
                tt("dve", xsD.rearrange("p (h d) -> p h d", d=64), pX,
                   cpv("dsk").unsqueeze(2).to_broadcast([128, 16, 64]), ALU.mult, [TpT, Tcp], [TxsD])
                pT2, TpT2 = next_pT()
                for g in range(2):
                    tr(pT2[:, g, :], BT[:, g, tok], identb, [TBT, Tconst], [TpT2])
                cpy("act", bt.rearrange("p (g n) -> p g n", n=128), pT2[:, 0:2, :], [TpT2], [Tbt])
                tt("dve", xdd.rearrange("p (h d) -> p h d", d=64), xd.rearrange("p (h d) -> p h d", d=64),
                   ed_all[:, i, :].unsqueeze(2).to_broadcast([128, 16, 64]), ALU.mult, [Txd, Tcum], [Txdd])
                pc, Tpc = pb[5], Tpb[5]
                for g in range(2):
                    mm(pc[:, g * 128:(g + 1) * 128], BT[:, g, tok], CT[:, g, tok], True, True, [TBT, TCT], [Tpc])
                tt("dve", CBm, pc[:, 0:256].rearrange("p (g l) -> p g l", l=128),
                   tri.unsqueeze(1).to_broadcast([128, 2, 128]), ALU.mult, [Tpc, Tcp], [TCBm])
                for hq in range(4):
                    g = hq // 2
                    R, TR = Rb[hq % 2], TRb[hq % 2]
                    E, TE = Eb[hq % 2], TEb[hq % 2]
                    pg, Tpg = (pb[4], Tpb[4]) if hq % 2 == 0 else (pb[5], Tpb[5])
                    tt("pool", R, tri.unsqueeze(1).to_broadcast([128, 4, 128]),
                       adt[:, i, hq * 4:(hq + 1) * 4].unsqueeze(2).to_broadcast([128, 4, 128]), ALU.mult,
                       [Tcp, Tdt], [TR])
                    mm(pg, cpv("lstrict"), R.rearrange("p h l -> p (h l)"), True, True, [Tcp, TR], [Tpg])
                    act(E.rearrange("p h l -> p (h l)"), pg, AF.Exp, [Tpg], [TE])
                    tt("dve", MT_[i % 2][:, hq * 4:(hq + 1) * 4, :], E, CBm[:, g, :].unsqueeze(1).to_broadcast([128, 4, 128]),
                       ALU.mult, [TE, TCBm], [TMT_[i % 2][hq]])

            def stageB(i):
                tok = slice(i * 128, (i + 1) * 128)
                bt, Tbt = Btok[i % 2], TBtok[i % 2]
                xdd, Txdd = xdtdec_[i % 2], Txdtdec_[i % 2]
                y, Ty = ybuf[i % 2], Tybuf[i % 2]
                xd, Txd = xdt[i % 2], Txdt[i % 2]
                for g in range(2):
                    py, Tpy = pb[2 + g], Tpb[2 + g]
                    for r in range(8):
                        hh = g * 8 + r
                        mm(py[:, r * 64:(r + 1) * 64], MT_[i % 2][:, hh, :], xd[:, hh * 64:(hh + 1) * 64], True, True,
                           [TMT_[i % 2][hh // 4], Txd], [Tpy])
                for g in range(2):
                    mm(pb[1] if g == 0 else pb[0], CT[:, g, tok], Sb[:, g * 512:(g + 1) * 512], True, True,
                       [TCT, TSb], [Tpb[1] if g == 0 else Tpb[0]])
                for g in range(2):
                    tt("dve", y[:, g * 512:(g + 1) * 512].rearrange("p (h d) -> p h d", d=64),
                       (pb[1] if g == 0 else pb[0]).rearrange("p (h d) -> p h d", d=64),
                       ea_all[:, i, g * 8:(g + 1) * 8].unsqueeze(2).to_broadcast([128, 8, 64]), ALU.mult,
                       [Tpb[1] if g == 0 else Tpb[0], Tcum], [Ty])
                for g in range(2):
                    mm(pb[g], bt[:, g * 128:(g + 1) * 128], xdd[:, g * 512:(g + 1) * 512], True, True,
                       [Tbt, Txdd], [Tpb[g]])
                tt("pool", Sst.rearrange("p (h d) -> p h d", d=64), Sst.rearrange("p (h d) -> p h d", d=64),
                   eq_all[:, i, :].unsqueeze(2).to_broadcast([128, 16, 64]), ALU.mult, [TS_, Tcum], [TS_])
                for g in range(2):
                    tt("dve", Sst[:, g * 512:(g + 1) * 512], Sst[:, g * 512:(g + 1) * 512], pb[g], ALU.add,
                       [TS_, Tpb[g]], [TS_])
                cpy("act", Sb, Sst, [TS_, TSb], [TSb])
                for g in range(2):
                    py, Tpy = pb[2 + g], Tpb[2 + g]
                    tt("dve", y[:, g * 512:(g + 1) * 512], y[:, g * 512:(g + 1) * 512], py, ALU.add, [Ty, Tpy], [Ty])

            def stageC(i):
                tok = slice(i * 128, (i + 1) * 128)
                xsD, TxsD = xsD_[i % 2], TxsD_[i % 2]
                y, Ty = ybuf[i % 2], Tybuf[i % 2]
                tt("dve", y, y, xsD, ALU.add, [Ty, TxsD], [Ty])
                tt("dve", y, y, siluz[:, i, :], ALU.mult, [Ty, Tsiluz[i]], [Ty])
                sc_, Tsc_ = stat_slot()
                for g in range(2):
                    act(ygn[:, g * 512:(g + 1) * 512], y[:, g * 512:(g + 1) * 512], AF.Square, [Ty], [Tygn, Tsc_],
                        accum_out=sc_[:, g:g + 1])
                act(sc_[:, 2:4], sc_[:, 0:2], AF.Ln, [Tsc_, Tconst], [Tsc_], bias=epsc[:, 0:1], scale=1.0 / 512)
                act(sc_[:, 4:6], sc_[:, 2:4], AF.Exp, [Tsc_], [Tsc_], scale=-0.5)
                for g in range(2):
                    act(ygn[:, g * 512:(g + 1) * 512], y[:, g * 512:(g + 1) * 512], AF.Copy, [Ty, Tsc_], [Tygn],
                        scale=sc_[:, 4 + g:5 + g])
                pT3, TpT3 = next_pT()
                for c in range(8):
                    tr(pT3[:, c, :], ygn[:, c * 128:(c + 1) * 128], identb, [Tygn, Tconst], [TpT3])
                cpy("act", xsT[:, :, tok], pT3, [TpT3], [TxsT[i]])

            stageA(0)
            for i in range(16):
                if i + 1 < 16:
                    stageA(i + 1)
                stageB(i)
                stageC(i)

        dump("mixT_ssd", xsT, [8, 2048], BF16)
        dump("S", Sst, [1024], F32)

        P.barrier()
        rsc = Alloc(RSC0, RSC1)
        scT = rsc.get(8 * 2048, BF16, (8, 2048))
        TscT = [T(f"scT{i}") for i in range(16)]
        rbc = Alloc(RBC0, RBC1)
        sq = [rbc.get(512, BF16) for _ in range(2)]
        Tsq = [T("sq0"), T("sq1")]
        rt = [rbc.get(512, F32) for _ in range(2)]
        Trt = [T("rt0"), T("rt1")]
        cvt = [rbc.get(512, F32) for _ in range(2)]
        Tcvt = [T("cvt0"), T("cvt1")]
        scb = [rbc.get(512, F32) for _ in range(2)]
        Tscb = [T("scb0"), T("scb1")]
        cwsc = cpv("cwsc")
        o3 = 1024 + 1536 + 16
        rhs_ = Alloc(RH0, RH0 + 32768)
        scw = [rhs_.get(8 * 256, BF16, (8, 256)) for _ in range(6)]
        Tscw = [T(f"scw{q}") for q in range(6)]

        def load_sc(jp):
            res = []
            for q, c0_ in enumerate((o3 + jp * 256, o3 + 2048 + jp * 256, o3 + 1024 + jp * 256)):
                bi = (jp % 2) * 3 + q
                src_ = w_in[:, c0_:c0_ + 256].rearrange("(k p) c -> p k c", p=128)
                P.dma("pool", lambda e, bi=bi, src_=src_: e.dma_start(out=scw[bi], in_=src_), f"scw{bi}", writes=[Tscw[bi]])
                res.append((scw[bi], Tscw[bi]))
            return res

        sc_loaded = {0: load_sc(0)}
        for jp in range(4):
            if jp + 1 < 4:
                sc_loaded[jp + 1] = load_sc(jp + 1)
            (wb, Twb), (wv, Twv), (wc, Twc) = sc_loaded[jp]
            for lc in range(2):
                j = jp * 2 + lc
                proj_chunk(wb, Twb, lc)
                ub, Tub = next_u()
                evac_to_u(ub, Tub)
                proj_chunk(wv, Twv, lc)
                u, Tu = next_u()
                evac_to_u(u, Tu)
                proj_chunk(wc, Twc, lc)
                tt("dve", u[:, 0:HALO], u[:, 0:HALO], pb[4][:, 0:HALO], ALU.mult, [Tu, Tpb[4]], [Tu])
                for t4 in range(4):
                    sl = slice(HALO + t4 * 512, HALO + (t4 + 1) * 512)
                    tt("dve", u[:, sl], u[:, sl], pb[t4], ALU.mult, [Tu, Tpb[t4]], [Tu])
                d, Td = make_diag(lambda jt, j=j: cwsc[:, j * 3 + jt:j * 3 + jt + 1], 3)
                for t4 in range(4):
                    sl = slice(t4 * 512, (t4 + 1) * 512)
                    slh = slice(HALO + t4 * 512, HALO + (t4 + 1) * 512)
                    b2 = t4 % 2
                    pc_, Tpc_ = conv_pe(u, Tu, d, Td, 3, t4)
                    tt("dve", scb[b2], pc_, ub[:, slh], ALU.mult, [Tpc_, Tub], [Tscb[b2]])
                    act(sq[b2], scb[b2], AF.Square, [Tscb[b2]], [Tsq[b2]])
                    pz, Tpz = conv_pe_bank()
                    mm(pz, bd64b, sq[b2], True, True, [Tconst, Tsq[b2]], [Tpz])
                    act(rt[b2], pz, AF.Ln, [Tpz, Tconst], [Trt[b2]], bias=epsc[:, 0:1], scale=1.0 / 64)
                    act(rt[b2], rt[b2], AF.Exp, [Trt[b2]], [Trt[b2]], scale=-0.5)
                    tt("pool", scT[:, j, sl], scb[b2], rt[b2], ALU.mult, [Tscb[b2], Trt[b2]], TscT[4 * t4:4 * t4 + 4])
        dump("scT", scT, [8, 2048], BF16)

        P.barrier()
        CAP = MOE_CAP
        NS = CAP // 128
        hn2d = nc.dram_tensor("hn2d", [SPAN + 128, D], BF16).ap()
        hacc = nc.dram_tensor("hacc", [SPAN + 128, D], F32).ap()
        listd = nc.dram_tensor("listd", [32 * CAP + 128, 16], F32).ap()
        Thacc, Thn2d, Tlistd = T("hacc"), T("hn2d"), T("listd")
        rh = Alloc(RH0, RH1)
        h = rh.get(16 * 1024, F32, (16, 1024))
        Th = [T(f"h{i}") for i in range(16)]
        rs2 = Alloc(RS20, RS21)
        wo = [rs2.get(16 * 512, BF16, (16, 512)) for _ in range(2)]
        Two = [T("wo0"), T("wo1")]
        xr = [rs2.get(1024, F32) for _ in range(2)]
        Txr = [T("xr0"), T("xr1")]
        RS2_ROUTE_END = rs2.p
        hnb = [rs2.get(1024, BF16) for _ in range(2)]
        Thnb = [T("hnb0"), T("hnb1")]
        gfb = rs2.get(1024, BF16)
        Tgfb = T("gfb")
        L = rs2.get(16 * 36, F32, (16, 36))
        TL = T("L")
        rbc = Alloc(RBC0, RBC1)
        hnf = [rbc.get(1024, F32) for _ in range(2)]
        Thnf = [T("hnf0"), T("hnf1")]
        hTf = [rbc.get(1024, F32, (8, 128)) for _ in range(2)]
        ThTf = [T("hTf0"), T("hTf1")]
        gffn_b = cpv("gffn").unsqueeze(2).to_broadcast([128, 8, 128])
        wr = cpv("wr", (8, 36))
        gain_b = cpv("gain16").unsqueeze(2).to_broadcast([128, 16, 512])
        own_row0 = HALO + NPREV * SPAN
        for half in range(2):
            src = w_out[:, half * 512:(half + 1) * 512].rearrange("(k p) c -> p k c", p=128)
            P.dma("pool", lambda e, src=src, half=half: e.dma_start(out=wo[half], in_=src), f"wo{half}", writes=[Two[half]])
            if half == 0:
                tt("dve", wo[half], wo[half], gain_b, ALU.mult, [Two[half], Tcp], [Two[half]])
            else:
                g16 = cpv("gain16")
                for k in range(16):
                    act(wo[half][:, k, :], wo[half][:, k, :], AF.Copy, [Two[half], Tcp], [Two[half]], scale=g16[:, k:k + 1])
        P.dma("pool", lambda e: e.dma_start(out=gfb, in_=gfd), "gfb", writes=[Tgfb])
        P.op("pool", lambda e: e.memset(hnb[1], 0.0), writes=[Thnb[1]])
        P.dma("sp", lambda e: e.dma_start(out=hn2d[SPAN:SPAN + 128, :], in_=hnb[1]), "hn2d0", reads=[Thnb[1]], writes=[])

        def n2_stage1(i):
            j = i % 2
            P.dma("sp", lambda e, i=i: e.dma_start(out=hacc[i * 128:(i + 1) * 128, :], in_=h[:, i, :]), "hacc0",
                  reads=[Th[i]], writes=[])
            sc_, Tsc_ = rmsnorm_stats(h[:, i, :], hnf[j], 128, [Th[i]], Thnf[j])
            ts("dve", hnf[j], h[:, i, :], sc_[:, 2:3], ALU.mult, [Th[i], Tsc_], [Thnf[j]])
            tt("pool", hnb[j], hnf[j], gfb, ALU.mult, [Thnf[j], Tgfb], [Thnb[j]])
            P.dma("sp", lambda e, i=i, j=j: e.dma_start(out=hn2d[i * 128:(i + 1) * 128, :], in_=hnb[j]), "hn2d0",
                  reads=[Thnb[j]], writes=[])

        def n2_stage2(i):
            j = i % 2
            for hf in range(2):
                pz, Tpz = pb[4 + hf], Tpb[4 + hf]
                for k4 in range(4):
                    k = hf * 4 + k4
                    tr(pz[:, k4 * 128:(k4 + 1) * 128], hnf[j][:, k * 128:(k + 1) * 128], identf, [Thnf[j], Tcp], [Tpz])
                tt("dve", hTf[j][:, hf * 4:(hf + 1) * 4, :], pz.rearrange("p (k t) -> p k t", t=128),
                   gffn_b[:, hf * 4:(hf + 1) * 4, :], ALU.mult, [Tpz, Tcp], [ThTf[j]])

        def n2_stage3(i):
            j = i % 2
            pr, Tpr = pb[6], Tpb[6]
            for k in range(8):
                mm(pr[:, 0:36], hTf[j][:, k, :], wr[:, k, :], k == 0, k == 7, [ThTf[j], Tcp], [Tpr])
            cpy("act", L[:, i, :], pr[:, 0:36], [Tpr], [TL])

        for i in range(16):
            tok = slice(i * 128, (i + 1) * 128)
            pzs = [(pb[(2 * i) % 4], Tpb[(2 * i) % 4]), (pb[(2 * i + 1) % 4], Tpb[(2 * i + 1) % 4])]
            for k in range(16):
                lhs = xsT[:, k, tok] if k < 8 else scT[:, k - 8, tok]
                for half in range(2):
                    mm(pzs[half][0], lhs, wo[half][:, k, :], k == 0, k == 15, [TxsT[i], TscT[i], Two[half]], [pzs[half][1]])
            jx = i % 2
            P.dma("sp", lambda e, jx=jx, i=i: e.dma_start(
                out=xr[jx], in_=xe[own_row0 + i * 128: own_row0 + (i + 1) * 128, :]), f"xr{jx}", writes=[Txr[jx]])
            for half in range(2):
                tt("dve", h[:, i, half * 512:(half + 1) * 512], pzs[half][0], xr[jx][:, half * 512:(half + 1) * 512], ALU.add,
                   [pzs[half][1], Txr[jx]], [Th[i]])
            n2_stage1(i)
            if i >= 1:
                n2_stage2(i - 1)
            if i >= 2:
                n2_stage3(i - 2)
        n2_stage2(15)
        n2_stage3(14)
        n2_stage3(15)
        dump("h", h, [16, 1024], F32)

        P.barrier()
        rs2 = Alloc(RS20, RS2_ROUTE_END)
        r16 = [rs2.get(16, F32) for _ in range(10)]
        r64 = [rs2.get(64, F32, (16, 4)) for _ in range(3)]
        r512 = [rs2.get(512, F32, (16, 32)) for _ in range(7)]
        idxi = [rs2.get(16, I32) for _ in range(2)]
        rows = [rs2.get(256, F32, (16, 16)) for _ in range(2)]
        linit = rs2.get(32 * CAP // 128 * 16, F32)
        Trt_ = T("routetmp")
        nrow_init = 32 * CAP // 128
        li3 = linit[:, 0:nrow_init * 16].rearrange("p (a b) -> p a b", b=16)
        P.op("pool", lambda e: e.memset(li3, 0.0), writes=[Trt_])
        ts("pool", li3[:, :, 0:1], cpv("tokid")[:, 0:1].unsqueeze(1).to_broadcast([128, nrow_init, 1]), float(SPAN), ALU.add,
           [Trt_, Tcp], [Trt_])
        P.dma("sp", lambda e: e.dma_start(out=listd[0:32 * CAP, :].rearrange("(a p) c -> p a c", p=128), in_=li3), "linit",
              reads=[Trt_], writes=[Tlistd])
        gl = L[:, :, 0:4]
        el = L[:, :, 4:36]
        gmax, gsum, gw, m1, m2, dd2, p1, p2, ix1, ix2 = r16
        goh, gex, pen = r64
        msk, oh1, oh2, tmp5, posv, pre, oh_keep = r512
        dummyp = r16[0][:, 0:1] if False else rs2.get(1, F32)
        RT = [TL, Trt_]
        P.op("dve", lambda e: e.tensor_reduce(out=gmax, in_=gl, axis=AX.X, op=ALU.max), [TL], [Trt_])
        tt("dve", gex, gl, gmax.unsqueeze(2).to_broadcast([128, 16, 4]), ALU.subtract, RT, [Trt_])
        tt("dve", goh, gl, gmax.unsqueeze(2).to_broadcast([128, 16, 4]), ALU.is_equal, RT, [Trt_])
        act(gex, gex, AF.Exp, [Trt_], [Trt_])
        P.op("dve", lambda e: e.tensor_reduce(out=gsum, in_=gex, axis=AX.X, op=ALU.add), [Trt_], [Trt_])
        P.op("dve", lambda e: e.reciprocal(out=gw, in_=gsum), [Trt_], [Trt_])
        ts("dve", pen, goh, -1.0, ALU.add, [Trt_], [Trt_], s2=1e30, op1=ALU.mult)
        tt("dve", msk.rearrange("p a (g e) -> p a g e", e=8), el.rearrange("p a (g e) -> p a g e", e=8),
           pen.unsqueeze(3).to_broadcast([128, 16, 4, 8]), ALU.add, RT, [Trt_])
        P.op("dve", lambda e: e.tensor_reduce(out=m1, in_=msk, axis=AX.X, op=ALU.max), [Trt_], [Trt_])
        tt("dve", oh1, msk, m1.unsqueeze(2).to_broadcast([128, 16, 32]), ALU.is_equal, [Trt_], [Trt_])
        stt("dve", tmp5, oh1, -1e30, msk, ALU.mult, ALU.add, [Trt_], [Trt_])
        P.op("dve", lambda e: e.tensor_reduce(out=m2, in_=tmp5, axis=AX.X, op=ALU.max), [Trt_], [Trt_])
        tt("dve", oh2, tmp5, m2.unsqueeze(2).to_broadcast([128, 16, 32]), ALU.is_equal, [Trt_], [Trt_])
        tt("dve", dd2, m2, m1, ALU.subtract, [Trt_], [Trt_])
        act(dd2, dd2, AF.Exp, [Trt_], [Trt_])
        ts("dve", p1, dd2, 1.0, ALU.add, [Trt_], [Trt_])
        P.op("dve", lambda e: e.reciprocal(out=p1, in_=p1), [Trt_], [Trt_])
        tt("dve", p2, dd2, p1, ALU.mult, [Trt_], [Trt_])
        tt("dve", p1, p1, gw, ALU.mult, [Trt_], [Trt_])
        tt("dve", p2, p2, gw, ALU.mult, [Trt_], [Trt_])
        tt("dve", tmp5, oh1, oh2, ALU.add, [Trt_], [Trt_])
        oh_f = tmp5.rearrange("p a b -> p (a b)")
        mm(pb[4], cpv("tri"), oh_f, True, True, [Tcp, Trt_], [Tpb[4]])
        mm(pb[5], cpv("ones"), oh_f, True, True, [Tcp, Trt_], [Tpb[5]])
        cpy("act", msk.rearrange("p a b -> p (a b)"), pb[5], [Tpb[5]], [Trt_])
        P.op("pool", lambda e: e.memset(pre[:, 0, :], 0.0), [Trt_], [Trt_])
        for i in range(1, 16):
            tt("pool", pre[:, i, :], pre[:, i - 1, :], msk[:, i - 1, :], ALU.add, [Trt_], [Trt_])
        stt("dve", posv.rearrange("p a b -> p (a b)"), pb[4], -1.0, pre.rearrange("p a b -> p (a b)"), ALU.add, ALU.add,
            [Tpb[4], Trt_], [Trt_])
        ts("dve", msk, posv, float(CAP), ALU.is_ge, [Trt_], [Trt_])
        tt("dve", posv, posv, cpv("ecap").unsqueeze(1).to_broadcast([128, 16, 32]), ALU.add, [Trt_, Tcp], [Trt_])
        ts("dve", oh_keep, msk, -1.0, ALU.mult, [Trt_], [Trt_], s2=1.0, op1=ALU.add)
        tt("dve", posv, posv, oh_keep, ALU.mult, [Trt_], [Trt_])
        ts("pool", dummyp, cpv("tokid")[:, 0:1], float(32 * CAP), ALU.add, [Tcp], [Trt_])
        stt("dve", posv, msk, dummyp, posv, ALU.mult, ALU.add, [Trt_], [Trt_])
        for kk, (ohk, ixk, pk) in enumerate(((oh1, ix1, p1), (oh2, ix2, p2))):
            tt("dve", msk, ohk, posv, ALU.mult, [Trt_], [Trt_])
            P.op("dve", lambda e, ixk=ixk: e.tensor_reduce(out=ixk, in_=msk, axis=AX.X, op=ALU.add), [Trt_], [Trt_])
            cpy("dve", idxi[kk], ixk, [Trt_], [Trt_])
            P.op("pool", lambda e, kk=kk: e.memset(rows[kk], 0.0), [Trt_], [Trt_])
            cpy("pool", rows[kk][:, :, 0:1], cpv("tokid").unsqueeze(2), [Trt_, Tcp], [Trt_])
            cpy("pool", rows[kk][:, :, 1:2], pk.unsqueeze(2), [Trt_], [Trt_])
        for kk in range(2):
            for i in range(16):
                P.dma("pool", lambda e, kk=kk, i=i: e.indirect_dma_start(
                    out=listd, out_offset=bass.IndirectOffsetOnAxis(ap=idxi[kk][:, i:i + 1], axis=0),
                    in_=rows[kk][:, i, :], in_offset=None),
                    "lsc", reads=[Trt_, Tlistd], writes=[])
        dump("L", L, [16, 36], F32)

        P.barrier()
        rsc = Alloc(RSC0, RSC1)
        wgu_region = rsc.get(8 * 2048, BF16, (8, 2048))
        wgb = [wgu_region[:, :, 0:512], wgu_region[:, :, 512:1024]]
        wub = [wgu_region[:, :, 1024:1536], wgu_region[:, :, 1536:2048]]
        Twg, Twu = [T("wg0"), T("wg1")], [T("wu0"), T("wu1")]
        rbc = Alloc(RBC0, RBC1)
        wdb = [rbc.get(4 * 1024, BF16, (4, 1024)) for _ in range(2)]
        Twd = [T("wd0"), T("wd1")]
        rh = Alloc(RH0, RH1)
        sli = [rh.get(NS * 16, F32, (NS, 16)) for _ in range(4)]
        Tsli = [T(f"sli{q}") for q in range(4)]
        tki = [rh.get(NS, I32) for _ in range(4)]
        Ttki = [T(f"tki{q}") for q in range(4)]
        Xe = [rh.get(NS * 1024, BF16, (NS, 1024)) for _ in range(2)]
        TXe = [T("Xe0"), T("Xe1")]
        XgT = [rh.get(8 * CAP, BF16, (8, CAP)) for _ in range(2)]
        TXgT = [T("XgT0"), T("XgT1")]
        sg = [rh.get(CAP, F32) for _ in range(2)]
        Tsg = [T("sg0"), T("sg1")]
        hT = [rh.get(4 * CAP, BF16, (4, CAP)) for _ in range(2)]
        ThT = [T("hT0"), T("hT1")]
        yb = [rh.get(1024, F32) for _ in range(3)]
        Tyb = [T("yb0"), T("yb1"), T("yb2")]
        for b in range(2):
            P.op("pool", lambda e, b=b: e.memset(Xe[b], 0.0), writes=[TXe[b]])
        rx = Alloc(RX0, RX1)
        stg = [rx.get(4096, F32), rx.get(4096, F32), rh.get(4096, F32)]
        Tstg = [T("stg0"), T("stg1"), T("stg2")]
        cnt = 0
        ycnt = 0

        def prefetch(ex):
            b = ex % 2
            b4 = ex % 4
            P.dma("sp", lambda e, ex=ex, b4=b4: e.dma_start(
                out=sli[b4], in_=listd[ex * CAP:(ex + 1) * CAP, :].rearrange("(s p) c -> p s c", p=128)),
                f"sli{b4}", writes=[Tsli[b4]])
            srcs = (w_gate[ex].rearrange("(k p) f -> p k f", p=128), w_up[ex].rearrange("(k p) f -> p k f", p=128),
                    w_down[ex].rearrange("(k p) d -> p k d", p=128))
            views = (stg[0].rearrange("p (k f) -> p k f", f=512), stg[1].rearrange("p (k f) -> p k f", f=512),
                     stg[2].rearrange("p (k f) -> p k f", f=1024))
            for q in range(3):
                P.dma("sp", lambda e, q=q, srcs=srcs, views=views: e.dma_start(out=views[q], in_=srcs[q]), f"stg{q}",
                      writes=[Tstg[q]])
            cpy("dve", tki[b4], sli[b4][:, :, 0], [Tsli[b4]], [Ttki[b4]])
            for s in range(NS):
                P.dma("pool", lambda e, b=b, b4=b4, s=s: e.indirect_dma_start(
                    out=Xe[b][:, s, :], out_offset=None, in_=hn2d,
                    in_offset=bass.IndirectOffsetOnAxis(ap=tki[b4][:, s:s + 1], axis=0)),
                    f"xg{b}", reads=[Ttki[b4]], writes=[TXe[b]])

        def casts(ex):
            b = ex % 2
            v0 = stg[0].rearrange("p (k f) -> p k f", f=512)
            v1 = stg[1].rearrange("p (k f) -> p k f", f=512)
            v2 = stg[2].rearrange("p (k f) -> p k f", f=1024)
            cpy("act", wgb[b][:, 0:4, :], v0[:, 0:4, :], [Tstg[0]], [Twg[b]])
            cpy("dve", wgb[b][:, 4:8, :], v0[:, 4:8, :], [Tstg[0]], [Twg[b]])
            cpy("act", wub[b][:, 0:4, :], v1[:, 0:4, :], [Tstg[1]], [Twu[b]])
            cpy("dve", wub[b][:, 4:8, :], v1[:, 4:8, :], [Tstg[1]], [Twu[b]])
            cpy("act", wdb[b][:, 0:2, :], v2[:, 0:2, :], [Tstg[2]], [Twd[b]])
            cpy("dve", wdb[b][:, 2:4, :], v2[:, 2:4, :], [Tstg[2]], [Twd[b]])

        if n_experts > 0:
            prefetch(0)
            casts(0)
        for ex in range(n_experts):
            b = ex % 2
            b4 = ex % 4
            if ex + 1 < n_experts:
                prefetch(ex + 1)
            for s in range(NS):
                pT, TpT = next_pT()
                for k in range(8):
                    tr(pT[:, k, :], Xe[b][:, s, k * 128:(k + 1) * 128], identb, [TXe[b], Tconst], [TpT])
                cpy("act" if s % 2 == 0 else "dve", XgT[b][:, :, s * 128:(s + 1) * 128], pT, [TpT], [TXgT[b]])
            for f in range(4):
                pg, Tpg = pb[cnt % 2], Tpb[cnt % 2]
                pu, Tpu = pb[2 + cnt % 2], Tpb[2 + cnt % 2]
                s_, Ts_ = sg[cnt % 2], Tsg[cnt % 2]
                cnt += 1
                for k in range(8):
                    mm(pg[:, 0:CAP], wgb[b][:, k, f * 128:(f + 1) * 128], XgT[b][:, k, :], k == 0, k == 7,
                       [Twg[b], TXgT[b]], [Tpg])
                for k in range(8):
                    mm(pu[:, 0:CAP], wub[b][:, k, f * 128:(f + 1) * 128], XgT[b][:, k, :], k == 0, k == 7,
                       [Twu[b], TXgT[b]], [Tpu])
                act(s_, pg[:, 0:CAP], AF.Silu, [Tpg], [Ts_])
                tt("dve", hT[b][:, f, :], s_, pu[:, 0:CAP], ALU.mult, [Ts_, Tpu], [ThT[b]])
            ys = []
            for s in range(NS):
                yy, Tyy = yb[ycnt % 3], Tyb[ycnt % 3]
                ycnt += 1
                for half in range(2):
                    pd, Tpd = pb[4 + half], Tpb[4 + half]
                    for f in range(4):
                        mm(pd, hT[b][:, f, s * 128:(s + 1) * 128], wdb[b][:, f, half * 512:(half + 1) * 512],
                           f == 0, f == 3, [ThT[b], Twd[b]], [Tpd])
                    if half == 0:
                        act(yy[:, 0:512], pd, AF.Copy, [Tpd, Tsli[b4]], [Tyy], scale=sli[b4][:, s, 1:2])
                    else:
                        ts("dve", yy[:, 512:1024], pd, sli[b4][:, s, 1:2], ALU.mult, [Tpd, Tsli[b4]], [Tyy])
                ys.append((yy, Tyy, s))
            if ex + 1 < n_experts:
                casts(ex + 1)
            for yy, Tyy, s in ys:
                P.dma("pool", lambda e, b4=b4, s=s, yy=yy: e.indirect_dma_start(
                    out=hacc, out_offset=bass.IndirectOffsetOnAxis(ap=tki[b4][:, s:s + 1], axis=0),
                    in_=yy, in_offset=None, compute_op=ALU.add),
                    "ysc", reads=[Ttki[b4], Tyy], writes=[Thacc])

        rs2 = Alloc(RS20, RS21)
        fn = rs2.get(1024, F32)
        Tfn = T("fn")
        hb = [rs2.get(1024, F32) for _ in range(3)]
        Thb = [T("hb0"), T("hb1"), T("hb2")]
        ob = [rs2.get(1024, F32) for _ in range(2)]
        Tob = [T("ob0"), T("ob1")]
        Tout = T("out")
        P.barrier()
        P.dma("sp", lambda e: e.dma_start(out=fn, in_=fnd), "fn", writes=[Tfn])
        for i in range(16):
            j = i % 2
            j3 = i % 3
            P.dma("sp", lambda e, i=i, j3=j3: e.dma_start(out=hb[j3], in_=hacc[i * 128:(i + 1) * 128, :]), f"hb{j3}",
                  reads=[Thacc], writes=[Thb[j3]])
            sc_, Tsc_ = rmsnorm_stats(hb[j3], ob[j], 128, [Thb[j3]], Tob[j])
            stt("dve", ob[j], hb[j3], sc_[:, 2:3], fn, ALU.mult, ALU.mult, [Thb[j3], Tsc_, Tfn], [Tob[j]])
            P.dma("sp", lambda e, i=i, j=j: e.dma_start(out=out[i * 128:(i + 1) * 128, :], in_=ob[j]), "out",
                  reads=[Tob[j]], writes=[Tout])
        P.barrier(engs=("sp",))
        P.emit(st)
    return nc


_NC_CACHE = {}


def make_inputs(inputs):
    x = np.asarray(inputs["x"], np.float32)
    in_maps = []
    shared = {
        "w_in": np.ascontiguousarray(np.asarray(inputs["w_in"], np.float32)[0]),
        "w_out": np.ascontiguousarray(np.asarray(inputs["w_out"], np.float32)[0]),
        "w_gate": np.ascontiguousarray(np.asarray(inputs["w_gate"], np.float32)[0]),
        "w_up": np.ascontiguousarray(np.asarray(inputs["w_up"], np.float32)[0]),
        "w_down": np.ascontiguousarray(np.asarray(inputs["w_down"], np.float32)[0]),
        "fnorm": np.ascontiguousarray(np.broadcast_to(np.asarray(inputs["final_norm"], np.float32)[None, :], (128, D))),
        "gfb": np.ascontiguousarray(np.broadcast_to(np.asarray(inputs["norm_ffn"], np.float32)[0][None, :], (128, D))),
    }
    inp_np = {k: np.asarray(v, np.float32) for k, v in inputs.items() if k not in ("x", "w_in", "w_out", "w_gate", "w_up", "w_down")}
    for c in range(NCORES):
        b, s = c // 4, c % 4
        start = s * SPAN
        lo = start - NPREV * SPAN - HALO
        xe = np.zeros((NTOK_EXT, D), np.float32)
        src_lo = max(lo, 0)
        xe[src_lo - lo:, :] = x[b, src_lo:start + SPAN, :]
        m = dict(shared)
        m["xe"] = xe
        m["cp"] = make_cp(inp_np, c)
        in_maps.append(m)
    return in_maps


def kernel(**inputs):
    if "nc" not in _NC_CACHE:
        _NC_CACHE["nc"] = build()
    nc = _NC_CACHE["nc"]
    in_maps = make_inputs(inputs)
    res = run_bass_kernel_spmd(nc, in_maps, core_ids=list(range(NCORES)))
    outs = [np.asarray(res.results[c]["out"], np.float32) for c in range(NCORES)]
    full = np.stack(outs, 0).reshape(2, 4 * SPAN, D)
    return full
```

```python
from contextlib import ExitStack
import numpy as np
import concourse.bass as bass
import concourse.mybir as mybir
from concourse.bass_utils import run_bass_kernel_spmd

F32 = mybir.dt.float32
BF16 = mybir.dt.bfloat16
ALU = mybir.AluOpType
AF = mybir.ActivationFunctionType
AX = mybir.AxisListType

ENGS = ("pe", "act", "dve", "pool", "sp")
SEM_LIMIT = 12000
NCORES = 8
SPAN = 2048
NPREV = 3
HALO = 8
NTOK_EXT = HALO + (NPREV + 1) * SPAN
D = 1024
EPS = 1e-6
MOE_CAP = 256
I32 = mybir.dt.int32


class T:
    __slots__ = ("name", "w", "r", "rd")

    def __init__(self, name=""):
        self.name = name
        self.w = None
        self.r = {}
        self.rd = []


class Prog:
    def __init__(self, nc):
        self.nc = nc
        self.ops = []
        self.dma_sems = {}
        self.last = {}
        self.dmas = []

    def _deps(self, reads, writes):
        deps = set()
        raw = set()
        for t in reads:
            if t.w is not None:
                deps.add(t.w)
                raw.add(t.w)
        for t in writes:
            if t.w is not None:
                deps.add(t.w)
            deps.update(t.r.values())
            deps.update(t.rd)
        self._last_raw = raw
        return deps

    def op(self, eng, fn, reads=(), writes=()):
        i = len(self.ops)
        self.ops.append(dict(eng=eng, fn=fn, deps=self._deps(reads, writes), kind="c"))
        self.ops[-1]["raw"] = self._last_raw
        self.last[eng] = i
        for t in reads:
            t.r[eng] = i
        for t in writes:
            t.w = i
            t.r = {}
            t.rd = []
        return i

    def dma(self, eng, fn, sem, reads=(), writes=(), inc=16):
        i = len(self.ops)
        n = self.dma_sems.get(sem, 0) + inc
        self.dma_sems[sem] = n
        self.ops.append(dict(eng=eng, fn=fn, deps=self._deps(reads, writes), kind="d", sem=sem, val=n, inc=inc))
        self.dmas.append(i)
        for t in reads:
            t.rd.append(i)
        for t in writes:
            t.w = i
            t.r = {}
            t.rd = []
        return i

    def barrier(self, engs=ENGS):
        deps = set(self.last.values()) | set(self.dmas)
        self.dmas = []
        for e in engs:
            self.ops.append(dict(eng=e, fn=None, deps=set(deps), kind="c"))

    def emit(self, stack):
        nc = self.nc
        ops = self.ops
        needed = set()
        for o in ops:
            for d in o["deps"]:
                if ops[d]["kind"] == "c":
                    needed.add(d)
        cnt = {e: 0 for e in ENGS}
        for i, o in enumerate(ops):
            if o["kind"] == "c" and i in needed and o["fn"] is not None:
                cnt[o["eng"]] += 1
                o["cnt"] = cnt[o["eng"]]
        esems = {}
        for e in ENGS:
            n = cnt[e] // SEM_LIMIT + 1
            esems[e] = [stack.enter_context(nc.semaphore(f"s_{e}{k}")) for k in range(n)]
        dsems = {k: stack.enter_context(nc.semaphore(f"d_{k}")) for k in self.dma_sems}
        per = {e: [] for e in ENGS}
        for i, o in enumerate(ops):
            per[o["eng"]].append(i)

        def section(e):
            def body(eng):
                seen = {}
                for i in per[e]:
                    o = ops[i]
                    w = {}
                    for d in o["deps"]:
                        od = ops[d]
                        if od["kind"] == "c":
                            if od["fn"] is None or "cnt" not in od:
                                continue
                            if od["eng"] == e and e == "pe":
                                continue
                            if od["eng"] == e and "raw" in o and d not in o["raw"]:
                                continue
                            c = od["cnt"]
                            key = ("e", od["eng"], (c - 1) // SEM_LIMIT)
                            val = (c - 1) % SEM_LIMIT + 1
                        else:
                            key = ("d", od["sem"])
                            val = od["val"]
                        if w.get(key, 0) < val:
                            w[key] = val
                    for key, val in sorted(w.items(), key=lambda kv: str(kv[0])):
                        if seen.get(key, 0) >= val:
                            continue
                        seen[key] = val
                        if key[0] == "e":
                            sem = esems[key[1]][key[2]]
                            for kk in range(key[2]):
                                seen[("e", key[1], kk)] = SEM_LIMIT
                        else:
                            sem = dsems[key[1]]
                        eng.wait_ge(sem, val)
                    if o["fn"] is None:
                        continue
                    ins = o["fn"](eng)
                    if o["kind"] == "c":
                        if "cnt" in o:
                            c = o["cnt"]
                            ins.then_inc(esems[e][(c - 1) // SEM_LIMIT], 1)
                    else:
                        ins.then_inc(dsems[o["sem"]], o["inc"])
            return body

        with nc.Block() as block:
            block.tensor(section("pe"))
            block.scalar(section("act"))
            block.vector(section("dve"))
            block.gpsimd(section("pool"))
            block.sync(section("sp"))


CP = {}
_o = 0
for _n, _w in (("ident", 128), ("tri", 128), ("lstrict", 128), ("ones", 128), ("bd64", 128),
               ("gmix", 8), ("gffn", 8), ("gain16", 16), ("cwssd", 48), ("cbssd", 12), ("cwsc", 24),
               ("dtb", 16), ("alog", 16), ("dsk", 16), ("wr", 288), ("bmask", 4), ("ecap", 32), ("tokid", 16)):
    CP[_n] = (_o, _w)
    _o += _w
NCP = _o


def make_cp(inp, core):
    cp = np.zeros((128, NCP), np.float32)

    def put(name, arr):
        o, w = CP[name]
        cp[:, o:o + w] = np.asarray(arr, np.float32).reshape(128, w)

    idx = np.arange(128)
    put("ident", np.eye(128))
    put("tri", (idx[:, None] <= idx[None, :]))
    put("lstrict", (idx[:, None] > idx[None, :]))
    put("ones", np.ones((128, 128)))
    put("bd64", (idx[:, None] // 64 == idx[None, :] // 64))
    put("gmix", inp["norm_mix"][0].reshape(8, 128).T)
    put("gffn", inp["norm_ffn"][0].reshape(8, 128).T)
    put("gain16", np.concatenate([inp["ssd_norm"][0].reshape(8, 128).T, inp["sc_norm"][0].reshape(8, 128).T], 1))
    put("cwssd", inp["ssd_conv_w"][0].reshape(4, 12, 128).transpose(2, 1, 0))
    put("cbssd", inp["ssd_conv_b"][0].reshape(12, 128).T)
    put("cwsc", inp["sc_conv_w"][0].reshape(3, 8, 128).transpose(2, 1, 0))
    put("dtb", np.broadcast_to(inp["dt_bias"][0][None, :], (128, 16)))
    put("alog", np.broadcast_to(inp["a_log"][0][None, :], (128, 16)))
    put("dsk", np.broadcast_to(inp["d_skip"][0][None, :], (128, 16)))
    wr = np.concatenate([inp["w_router_group"][0],
                         inp["w_router_expert"][0].transpose(1, 0, 2).reshape(1024, 32)], 1)
    put("wr", wr.reshape(8, 128, 36).transpose(1, 0, 2))
    s = core % 4
    put("bmask", np.broadcast_to(np.array([1.0 if b >= NPREV - s else 0.0 for b in range(NPREV + 1)],
                                          np.float32)[None, :], (128, 4)))
    put("ecap", np.broadcast_to((np.arange(32, dtype=np.float32) * MOE_CAP)[None, :], (128, 32)))
    put("tokid", (np.arange(16)[None, :] * 128 + np.arange(128)[:, None]).astype(np.float32))
    return cp


def build(dbg=False, stop_after=None, n_experts=32):
    nc = bass.Bass("TRN2", target_bir_lowering=False)

    def dram(name, shape, kind="ExternalInput"):
        return nc.dram_tensor(name, shape, F32, kind=kind).ap()

    xe = dram("xe", [NTOK_EXT, D])
    cpd = dram("cp", [128, NCP])
    fnd = dram("fnorm", [128, D])
    gfd = dram("gfb", [128, D])
    w_in = dram("w_in", [D, 5648])
    w_out = dram("w_out", [2048, D])
    w_gate = dram("w_gate", [32, D, 512])
    w_up = dram("w_up", [32, D, 512])
    w_down = dram("w_down", [32, 512, D])
    out = dram("out", [SPAN, D], kind="ExternalOutput")
    dbg_out = {}

    P = Prog(nc)
    st = ExitStack()
    with st:
        ARENA_N = 53200
        arena = st.enter_context(nc.sbuf_tensor("arena", [128, ARENA_N], F32))
        pbt = [st.enter_context(nc.psum_tensor(f"pb{i}", [128, 512], F32)) for i in range(7)]
        pTb = st.enter_context(nc.psum_tensor("pTb", [128, 8, 128], BF16))
        pb = [t[:] for t in pbt]
        Tpb = [T(f"pb{i}") for i in range(7)]
        TpTb = T("pTb")

        a_f32 = arena[:]
        a_bf = arena[:].bitcast(BF16)
        a_i32 = arena[:].bitcast(mybir.dt.int32)

        class Alloc:
            def __init__(self, start, end):
                self.p = start
                self.end = end

            def get(self, n_elems, dt, shape=None):
                nb = n_elems * (2 if dt == BF16 else 4)
                nb_al = (nb + 63) // 64 * 64
                off = self.p
                assert off + nb_al <= self.end, ("arena overflow", off, nb_al, self.end)
                self.p += nb_al
                if dt == F32:
                    ap = a_f32[:, off // 4: off // 4 + n_elems]
                elif dt == mybir.dt.int32:
                    ap = a_i32[:, off // 4: off // 4 + n_elems]
                else:
                    ap = a_bf[:, off // 2: off // 2 + n_elems]
                if shape is not None and len(shape) == 2:
                    ap = ap.rearrange("p (a b) -> p a b", b=shape[1])
                return ap

        PERS0 = 0
        PERS1 = 13824
        RX0, RX1 = PERS1, PERS1 + 32768
        RH0, RH1 = RX1, RX1 + 65792
        RBC0, RBC1 = RH1, RH1 + 16384
        RSC0, RSC1 = RBC1, RBC1 + 32768
        RS20, RS21 = RSC1, ARENA_N * 4

        pers = Alloc(PERS0, PERS1)
        cp = pers.get(NCP, F32)
        Tcp = T("cp")

        def cpv(name, shape=None):
            o, w = CP[name]
            ap = cp[:, o:o + w]
            if shape is not None:
                ap = ap.rearrange("p (a b) -> p a b", b=shape[1])
            return ap

        identb = pers.get(128, BF16)
        bd64b = pers.get(128, BF16)
        Sst = pers.get(1024, F32)
        Sb = pers.get(1024, BF16)
        dtt = pers.get(256, F32, (16, 16))
        adt = pers.get(256, F32, (16, 16))
        avec = pers.get(16, F32)
        epsc = pers.get(1, F32)
        pers_stats = pers.get(64, F32)
        Tconst = T("const")
        TS_, TSb, Tdt, Tstats = T("S"), T("Sb"), T("dt"), T("stats")

        def act(out_, in_, func, reads, writes, **kw):
            P.op("act", lambda e: e.activation(out=out_, in_=in_, func=func, **kw), reads, writes)

        def tt(eng, out_, in0, in1, op, reads, writes):
            P.op(eng, lambda e: e.tensor_tensor(out=out_, in0=in0, in1=in1, op=op), reads, writes)

        def ts(eng, out_, in0, s1, op0, reads, writes, s2=None, op1=None):
            if op1 is None:
                P.op(eng, lambda e: e.tensor_scalar(out=out_, in0=in0, scalar1=s1, scalar2=None, op0=op0), reads, writes)
            else:
                P.op(eng, lambda e: e.tensor_scalar(out=out_, in0=in0, scalar1=s1, scalar2=s2, op0=op0, op1=op1), reads, writes)

        def stt(eng, out_, in0, scalar, in1, op0, op1, reads, writes):
            P.op(eng, lambda e: e.scalar_tensor_tensor(out=out_, in0=in0, scalar=scalar, in1=in1, op0=op0, op1=op1), reads, writes)

        def cpy(eng, out_, in_, reads, writes):
            if eng == "act":
                P.op("act", lambda e: e.copy(out=out_, in_=in_), reads, writes)
            else:
                P.op(eng, lambda e: e.tensor_copy(out=out_, in_=in_), reads, writes)

        def mm(out_, lhsT, rhs, start, stop, reads, writes):
            P.op("pe", lambda e: e.matmul(out_, lhsT=lhsT, rhs=rhs, start=start, stop=stop), reads, writes)

        def tr(out_, in_, ident, reads, writes):
            P.op("pe", lambda e: e.transpose(out=out_, in_=in_, identity=ident), reads, writes)

        stat_tiles = [pers_stats[:, 8 * q: 8 * q + 8] for q in range(8)]
        Tstat_tiles = [T(f"stat{q}") for q in range(8)]
        stc = [0]

        def stat_slot():
            q = stc[0] % 8
            stc[0] += 1
            return stat_tiles[q], Tstat_tiles[q]

        def rmsnorm_stats(x_ap, junk_ap, n, reads, Tjunk):
            sl_, Tsl_ = stat_slot()
            act(junk_ap, x_ap, AF.Square, reads, [Tjunk, Tsl_], accum_out=sl_[:n, 0:1])
            act(sl_[:n, 1:2], sl_[:n, 0:1], AF.Ln, [Tsl_, Tconst], [Tsl_], bias=epsc[:n, 0:1], scale=1.0 / D)
            act(sl_[:n, 2:3], sl_[:n, 1:2], AF.Exp, [Tsl_], [Tsl_], scale=-0.5)
            return sl_, Tsl_

        Tdump = T("dump")

        def dump(name, ap, shp, dt):
            if not dbg:
                return
            d = nc.dram_tensor("dbg_" + name, [128] + shp, dt, kind="ExternalOutput").ap()
            P.barrier()
            P.dma("sp", lambda e, d=d, ap=ap: e.dma_start(out=d, in_=ap), "dump", writes=[Tdump])
            P.barrier()

        P.dma("sp", lambda e: e.dma_start(out=cp, in_=cpd), "cp", writes=[Tcp])
        P.op("pool", lambda e: e.memset(epsc, EPS), writes=[Tconst])
        cpy("dve", identb, cpv("ident"), [Tcp], [Tconst])
        cpy("dve", bd64b, cpv("bd64"), [Tcp], [Tconst])
        act(avec, cpv("alog"), AF.Exp, [Tcp], [Tconst])
        ts("dve", avec, avec, -1.0, ALU.mult, [Tconst], [Tconst])
        P.op("pool", lambda e: e.memset(Sst, 0.0), writes=[TS_])

        rx = Alloc(RX0, RX1)
        xsT = rx.get(8 * 2048, BF16, (8, 2048))
        TxsT = [T(f"xsT{i}") for i in range(16)]
        rh = Alloc(RH0, RH1)
        siluz = rh.get(16 * 1024, BF16, (16, 1024))
        Tsiluz = [T(f"sz{i}") for i in range(16)]
        hnT = rh.get(8 * 2056, BF16, (8, 2056))
        ThnT = [T(f"hnT{i}") for i in range(17)]
        rbc = Alloc(RBC0, RBC1)
        BT = rbc.get(2 * 2048, BF16, (2, 2048))
        CT = rbc.get(2 * 2048, BF16, (2, 2048))
        TBT, TCT = T("BT"), T("CT")

        rs2 = Alloc(RS20, RS21)
        wts = [rs2.get(8 * 256, BF16, (8, 256)) for _ in range(4)]
        Twts = [T(f"wt{i}") for i in range(4)]
        U = [rs2.get(2056, BF16) for _ in range(3)]
        TU = [T(f"U{i}") for i in range(3)]
        xt = [rs2.get(1024, F32) for _ in range(2)]
        Txt = [T(f"xt{i}") for i in range(2)]
        dg = [rs2.get(4 * 128, BF16, (4, 128)) for _ in range(2)]
        Tdg = [T("dg0"), T("dg1")]
        acs_all = rs2.get(512, F32)
        dd_all = rs2.get(256, F32, (16, 16))
        ed_all = rs2.get(256, F32, (16, 16))
        eq_all = rs2.get(256, F32, (16, 16))
        ea_all = rs2.get(256, F32, (16, 16))
        LT = rs2.get(256, F32, (16, 16))
        wgt_all = rs2.get(256, F32, (16, 16))
        Tcum = T("cum")
        rsc = Alloc(RSC0, RSC1)
        xn = [rsc.get(1024, BF16) for _ in range(2)]
        Txn = [T(f"xn{i}") for i in range(2)]
        wdt = rsc.get(8 * 16, BF16, (8, 16))
        Twdt = T("wdt")
        dtmp = rsc.get(256, F32)
        Tdtmp = T("dtmp")
        xdt = [rsc.get(1024, BF16) for _ in range(2)]
        Txdt = [T(f"xdt{i}") for i in range(2)]
        xdtdec = rsc.get(1024, BF16)
        Txdtdec = T("xdtdec")
        xsD_ = [rsc.get(1024, BF16), rs2.get(1024, BF16)]
        TxsD_ = [T("xsD0"), T("xsD1")]
        Btok = [rsc.get(256, BF16) for _ in range(2)]
        TBtok = [T(f"Btok{i}") for i in range(2)]
        CBm = rsc.get(256, F32, (2, 128))
        TCBm = T("CBm")
        Rb = [rsc.get(512, F32, (4, 128)) for _ in range(2)]
        TRb = [T(f"R{i}") for i in range(2)]
        Eb = [rsc.get(512, BF16, (4, 128)) for _ in range(2)]
        TEb = [T(f"E{i}") for i in range(2)]
        MT = rsc.get(16 * 128, BF16, (16, 128))
        TMT = [T(f"MT{i}") for i in range(4)]
        ybuf = [rsc.get(1024, F32), a_f32[:, RSC0 // 4: RSC0 // 4 + 1024]]
        Tybuf = [T("y0"), T("y1")]
        ygn = rsc.get(1024, BF16)
        Tygn = T("ygn")

        pTs = [pTb[:], pb[6].bitcast(BF16).rearrange("p (k t) -> p k t", t=128),
               pb[3].bitcast(BF16).rearrange("p (k t) -> p k t", t=128),
               pb[2].bitcast(BF16).rearrange("p (k t) -> p k t", t=128)]
        TpTs = [TpTb, Tpb[6], Tpb[3], Tpb[2]]
        ptc = [0]
        pt_depth = [4]

        def next_pT():
            j = ptc[0] % pt_depth[0]
            ptc[0] += 1
            return pTs[j], TpTs[j]

        wctr = [0]

        def load_w(cols0, ncols):
            j = wctr[0] % 4
            wctr[0] += 1
            dst = wts[j][:, :, 0:ncols]
            src = w_in[:, cols0:cols0 + ncols].rearrange("(k p) c -> p k c", p=128)
            P.dma("pool", lambda e: e.dma_start(out=dst, in_=src), f"wt{j}", writes=[Twts[j]])
            return wts[j], Twts[j]

        xctr = [0]
        uctr = [0]
        dgc = [0]
        cvc = [0]

        def next_u():
            j = uctr[0] % 3
            uctr[0] += 1
            return U[j], TU[j]

        gmix_b = cpv("gmix").unsqueeze(2)
        identf = cpv("ident")

        def hn_tiles(tt4):
            return ThnT[1 + 4 * tt4: 5 + 4 * tt4]

        def proj_chunk(wt_ap, Tw, lc):
            for k in range(8):
                mm(pb[4][:, 0:HALO], wt_ap[:, k, lc * 128:(lc + 1) * 128], hnT[:, k, 0:HALO], k == 0, k == 7,
                   [Tw, ThnT[0]], [Tpb[4]])
            for t4 in range(4):
                for k in range(8):
                    mm(pb[t4], wt_ap[:, k, lc * 128:(lc + 1) * 128],
                       hnT[:, k, HALO + t4 * 512: HALO + (t4 + 1) * 512], k == 0, k == 7,
                       [Tw] + hn_tiles(t4), [Tpb[t4]])

        def evac_to_u(u, Tu, all_act=False):
            cpy("act", u[:, 0:HALO], pb[4][:, 0:HALO], [Tpb[4]], [Tu])
            for t4 in range(4):
                cpy("act" if (t4 % 2 == 0 or all_act) else "dve", u[:, HALO + t4 * 512: HALO + (t4 + 1) * 512], pb[t4],
                    [Tpb[t4]], [Tu])

        cacc = [a_f32[:, RH0 // 4 + q * 2048: RH0 // 4 + (q + 1) * 2048] for q in range(2)]
        Tcacc = [T("cacc0"), T("cacc1")]

        def make_diag(wcol, ntap):
            j = dgc[0] % 2
            dgc[0] += 1
            for jt in range(ntap):
                act(dg[j][:, jt, :], identf, AF.Copy, [Tcp], [Tdg[j]], scale=wcol(jt))
            return dg[j], Tdg[j]

        def conv_pe_bank():
            j = 5 + cvc[0] % 2
            cvc[0] += 1
            return pb[j], Tpb[j]

        def conv_pe(u, Tu, d, Td, ntap, t4):
            j = 5 + cvc[0] % 2
            cvc[0] += 1
            base = HALO - (ntap - 1) + t4 * 512
            for jt in range(ntap):
                mm(pb[j], d[:, jt, :], u[:, base + jt: base + jt + 512], jt == 0, jt == ntap - 1, [Td, Tu], [Tpb[j]])
            return pb[j], Tpb[j]

        a1_state = {}

        def a1_front(blk_, ti):
            row0_ = HALO + blk_ * SPAN
            n = HALO if ti == 0 else 128
            r0 = row0_ - HALO if ti == 0 else row0_ + (ti - 1) * 128
            j = xctr[0] % 2
            xctr[0] += 1
            xtj, xnj = xt[j], xn[j]
            P.dma("sp", lambda e, xtj=xtj, r0=r0, n=n: e.dma_start(out=xtj[:n, :], in_=xe[r0:r0 + n, :]),
                  f"xt{j}", writes=[Txt[j]])
            sc_, Tsc_ = rmsnorm_stats(xtj[:n, :], xnj[:n, :], n, [Txt[j]], Txn[j])
            ts("dve", xnj[:n, :], xtj[:n, :], sc_[:n, 2:3], ALU.mult, [Txt[j], Tsc_], [Txn[j]])
            a1_state[(blk_, ti)] = (j, n)

        def a1_back(blk_, ti):
            j, n = a1_state.pop((blk_, ti))
            xnj = xn[j]
            c0 = 0 if ti == 0 else HALO + (ti - 1) * 128
            pT, TpT = next_pT()
            for k in range(8):
                tr(pT[:, k, :n], xnj[:n, k * 128:(k + 1) * 128], identb[:n, :n], [Txn[j], Tconst], [TpT])
            tt("dve", hnT[:, :, c0:c0 + n], pT[:, :, :n], gmix_b.to_broadcast([128, 8, n]),
               ALU.mult, [TpT, Tcp], [ThnT[ti]])

        a1_front(0, 0)
        for ti in range(17):
            if ti + 1 < 17:
                a1_front(0, ti + 1)
            a1_back(0, ti)

        for blk in range(NPREV + 1):
            own = blk == NPREV
            row0 = HALO + blk * SPAN
            cwssd = cpv("cwssd")
            cbssd = cpv("cbssd")
            conv_chunks = list(range(8)) + [8, 9] + ([10, 11] if own else [])

            def conv_silu(c, u, Tu):
                acc, Tacc = cacc[c % 2], Tcacc[c % 2]
                base = HALO - 3
                ts("dve", acc, u[:, base:base + SPAN], cwssd[:, c * 4:c * 4 + 1], ALU.mult, [Tu, Tcp], [Tacc])
                for jt in range(1, 4):
                    stt("dve", acc, u[:, base + jt:base + jt + SPAN], cwssd[:, c * 4 + jt:c * 4 + jt + 1], acc,
                        ALU.mult, ALU.add, [Tu, Tcp, Tacc], [Tacc])
                if c < 8:
                    dst, Tdst = xsT[:, c, :], TxsT
                elif c < 10:
                    dst, Tdst = BT[:, c - 8, :], [TBT]
                else:
                    dst, Tdst = CT[:, c - 10, :], [TCT]
                act(dst, acc, AF.Silu, [Tacc, Tcp], Tdst, bias=cbssd[:, c:c + 1])

            pending = None
            for g0 in range(0, len(conv_chunks), 2):
                cc0 = conv_chunks[g0]
                wt_ap, Tw = load_w(1024 + cc0 * 128, 256)
                for lc in range(2):
                    c = cc0 + lc
                    proj_chunk(wt_ap, Tw, lc)
                    u, Tu = next_u()
                    evac_to_u(u, Tu, all_act=True)
                    if pending is not None:
                        conv_silu(*pending)
                    pending = (c, u, Tu)
            conv_silu(*pending)

            src = w_in[:, 2560:2576].rearrange("(k p) c -> p k c", p=128)
            P.dma("pool", lambda e, src=src: e.dma_start(out=wdt, in_=src), "wdt", writes=[Twdt])
            for i in range(16):
                for k in range(8):
                    mm(pb[4][:, 16 + i * 16: 32 + i * 16], hnT[:, k, HALO + i * 128: HALO + (i + 1) * 128], wdt[:, k, :],
                       k == 0, k == 7, [ThnT[1 + i], Twdt], [Tpb[4]])
            dtb_b = cpv("dtb").unsqueeze(1).to_broadcast([128, 16, 16])
            dt3 = dtmp.rearrange("p (a b) -> p a b", b=16)
            tt("dve", dt3, pb[4][:, 16:272].rearrange("p (a b) -> p a b", b=16), dtb_b, ALU.add, [Tpb[4], Tcp], [Tdtmp])
            act(dtmp, dtmp, AF.Exp, [Tdtmp], [Tdtmp])
            act(dtmp, dtmp, AF.Ln, [Tdtmp], [Tdtmp], bias=1.0)
            bm = cpv("bmask")
            ts("dve", dtt, dt3, bm[:, blk:blk + 1], ALU.mult, [Tdtmp, Tcp], [Tdt])
            tt("dve", adt, dtt, avec.unsqueeze(1).to_broadcast([128, 16, 16]), ALU.mult, [Tdt, Tconst], [Tdt])
            adt_f = adt.rearrange("p a b -> p (a b)")
            mm(pb[4][:, 0:256], cpv("tri"), adt_f, True, True, [Tcp, Tdt], [Tpb[4]])
            mm(pb[4][:, 256:512], cpv("ones"), adt_f, True, True, [Tcp, Tdt], [Tpb[4]])
            cpy("act", acs_all, pb[4], [Tpb[4]], [Tcum])
            ac3 = acs_all[:, 0:256].rearrange("p (a b) -> p a b", b=16)
            aq3 = acs_all[:, 256:512].rearrange("p (a b) -> p a b", b=16)
            tt("dve", dd_all, aq3, ac3, ALU.subtract, [Tcum], [Tcum])

            tri = cpv("tri")
            if not own:
                P.op("pool", lambda e: e.memset(LT[:, 15, :], 0.0), [Tcum], [Tcum])
                for i in range(14, -1, -1):
                    tt("pool", LT[:, i, :], LT[:, i + 1, :], aq3[:, i + 1, :], ALU.add, [Tcum], [Tcum])
                tt("dve", dd_all, dd_all, LT, ALU.add, [Tcum], [Tcum])
                act(ed_all, dd_all, AF.Exp, [Tcum], [Tcum])
                tt("dve", wgt_all, ed_all, dtt, ALU.mult, [Tcum, Tdt], [Tcum])
                tt("pool", eq_all[:, 0, :], LT[:, 0, :], aq3[:, 0, :], ALU.add, [Tcum], [Tcum])
                act(eq_all[:, 0, :], eq_all[:, 0, :], AF.Exp, [Tcum], [Tcum])
                for i in range(16):
                    tok = slice(i * 128, (i + 1) * 128)
                    xd, Txd = xdt[i % 2], Txdt[i % 2]
                    bt, Tbt = Btok[i % 2], TBtok[i % 2]
                    pT, TpT = next_pT()
                    for c in range(8):
                        tr(pT[:, c, :], xsT[:, c, tok], identb, [TxsT[i], Tconst], [TpT])
                    pX = pT.rearrange("p c t -> p (c t)").rearrange("p (h d) -> p h d", d=64)
                    tt("dve", xd.rearrange("p (h d) -> p h d", d=64), pX,
                       wgt_all[:, i, :].unsqueeze(2).to_broadcast([128, 16, 64]), ALU.mult, [TpT, Tcum], [Txd])
                    pT2, TpT2 = next_pT()
                    for g in range(2):
                        tr(pT2[:, g, :], BT[:, g, tok], identb, [TBT, Tconst], [TpT2])
                    cpy("act", bt.rearrange("p (g n) -> p g n", n=128), pT2[:, 0:2, :], [TpT2], [Tbt])
                    for g in range(2):
                        mm(pb[g], bt[:, g * 128:(g + 1) * 128], xd[:, g * 512:(g + 1) * 512], i == 0, i == 15,
                           [Tbt, Txd], [Tpb[g]])
                    if i == 0:
                        a1_front(blk + 1, 0)
                    a1_front(blk + 1, i + 1)
                    a1_back(blk + 1, i)
                a1_back(blk + 1, 16)
                tt("pool", Sst.rearrange("p (h d) -> p h d", d=64), Sst.rearrange("p (h d) -> p h d", d=64),
                   eq_all[:, 0, :].unsqueeze(2).to_broadcast([128, 16, 64]), ALU.mult, [TS_, Tcum], [TS_])
                for g in range(2):
                    tt("dve", Sst[:, g * 512:(g + 1) * 512], Sst[:, g * 512:(g + 1) * 512], pb[g], ALU.add,
                       [TS_, Tpb[g]], [TS_])
                continue

            act(ed_all, dd_all, AF.Exp, [Tcum], [Tcum])
            act(eq_all, aq3, AF.Exp, [Tcum], [Tcum])
            act(ea_all, ac3, AF.Exp, [Tcum], [Tcum])
            for q in range(4):
                wt_ap, Tw = load_w(q * 256, 256)
                for i in range(16):
                    pz, Tpz = pb[5 + i % 2], Tpb[5 + i % 2]
                    for k in range(8):
                        mm(pz[:, 0:256], hnT[:, k, HALO + i * 128: HALO + (i + 1) * 128], wt_ap[:, k, :], k == 0, k == 7,
                           [ThnT[1 + i], Tw], [Tpz])
                    act(siluz[:, i, q * 256:(q + 1) * 256], pz[:, 0:256], AF.Silu, [Tpz], [Tsiluz[i], Tcacc[0], Tcacc[1]])
            cpy("act", Sb, Sst, [TS_], [TSb])

            pt_depth[0] = 2
            xdtdec_ = [xdtdec, U[2][:, 0:1024]]
            Txdtdec_ = [Txdtdec, T("xdtdec1")]
            MT_ = [MT, U[1][:, 0:2048].rearrange("p (h l) -> p h l", l=128)]
            TMT_ = [TMT, [T(f"MTb{q}") for q in range(4)]]

            def stageA(i):
                tok = slice(i * 128, (i + 1) * 128)
                xd, Txd = xdt[i % 2], Txdt[i % 2]
                bt, Tbt = Btok[i % 2], TBtok[i % 2]
                xsD, TxsD = xsD_[i % 2], TxsD_[i % 2]
                xdd, Txdd = xdtdec_[i % 2], Txdtdec_[i % 2]
                pT, TpT = next_pT()
                for c in range(8):
                    tr(pT[:, c, :], xsT[:, c, tok], identb, [TxsT[i], Tconst], [TpT])
                pX = pT.rearrange("p c t -> p (c t)").rearrange("p (h d) -> p h d", d=64)
                tt("dve", xd.rearrange("p (h d) -> p h d", d=64), pX, dtt[:, i, :].unsqueeze(2).to_broadcast([128, 16, 64]),
                   ALU.mult, [TpT, Tdt], [Txd])
                tt("dve", xsD.rearrange("p (h d) -> p h d", d=64), pX,
                   cpv("dsk").unsqueeze(2).to_broadcast([128, 16, 64]), ALU.mult, [TpT, Tcp], [TxsD])
                pT2, TpT2 = next_pT()
                for g in range(2):
                    tr(pT2[:, g, :], BT[:, g, tok], identb, [TBT, Tconst], [TpT2])
                cpy("act", bt.rearrange("p (g n) -> p g n", n=128), pT2[:, 0:2, :], [TpT2], [Tbt])
                tt("dve", xdd.rearrange("p (h d) -> p h d", d=64), xd.rearrange("p (h d) -> p h d", d=64),
                   ed_all[:, i, :].unsqueeze(2).to_broadcast([128, 16, 64]), ALU.mult, [Txd, Tcum], [Txdd])
                pc, Tpc = pb[5], Tpb[5]
                for g in range(2):
                    mm(pc[:, g * 128:(g + 1) * 128], BT[:, g, tok], CT[:, g, tok], True, True, [TBT, TCT], [Tpc])
                tt("dve", CBm, pc[:, 0:256].rearrange("p (g l) -> p g l", l=128),
                   tri.unsqueeze(1).to_broadcast([128, 2, 128]), ALU.mult, [Tpc, Tcp], [TCBm])
                for hq in range(4):
                    g = hq // 2
                    R, TR = Rb[hq % 2], TRb[hq % 2]
                    E, TE = Eb[hq % 2], TEb[hq % 2]
                    pg, Tpg = (pb[4], Tpb[4]) if hq % 2 == 0 else (pb[5], Tpb[5])
                    tt("pool", R, tri.unsqueeze(1).to_broadcast([128, 4, 128]),
                       adt[:, i, hq * 4:(hq + 1) * 4].unsqueeze(2).to_broadcast([128, 4, 128]), ALU.mult,
                       [Tcp, Tdt], [TR])
                    mm(pg, cpv("lstrict"), R.rearrange("p h l -> p (h l)"), True, True, [Tcp, TR], [Tpg])
                    act(E.rearrange("p h l -> p (h l)"), pg, AF.Exp, [Tpg], [TE])
                    tt("dve", MT_[i % 2][:, hq * 4:(hq + 1) * 4, :], E, CBm[:, g, :].unsqueeze(1).to_broadcast([128, 4, 128]),
                       ALU.mult, [TE, TCBm], [TMT_[i % 2][hq]])

            def stageB(i):
                tok = slice(i * 128, (i + 1) * 128)
                bt, Tbt = Btok[i % 2], TBtok[i % 2]
                xdd, Txdd = xdtdec_[i % 2], Txdtdec_[i % 2]
                y, Ty = ybuf[i % 2], Tybuf[i % 2]
                xd, Txd = xdt[i % 2], Txdt[i % 2]
                for g in range(2):
                    py, Tpy = pb[2 + g], Tpb[2 + g]
                    for r in range(8):
                        hh = g * 8 + r
                        mm(py[:, r * 64:(r + 1) * 64], MT_[i % 2][:, hh, :], xd[:, hh * 64:(hh + 1) * 64], True, True,
                           [TMT_[i % 2][hh // 4], Txd], [Tpy])
                for g in range(2):
                    mm(pb[1] if g == 0 else pb[0], CT[:, g, tok], Sb[:, g * 512:(g + 1) * 512], True, True,
                       [TCT, TSb], [Tpb[1] if g == 0 else Tpb[0]])
                for g in range(2):
                    tt("dve", y[:, g * 512:(g + 1) * 512].rearrange("p (h d) -> p h d", d=64),
                       (pb[1] if g == 0 else pb[0]).rearrange("p (h d) -> p h d", d=64),
                       ea_all[:, i, g * 8:(g + 1) * 8].unsqueeze(2).to_broadcast([128, 8, 64]), ALU.mult,
                       [Tpb[1] if g == 0 else Tpb[0], Tcum], [Ty])
                for g in range(2):
                    mm(pb[g], bt[:, g * 128:(g + 1) * 128], xdd[:, g * 512:(g + 1) * 512], True, True,
                       [Tbt, Txdd], [Tpb[g]])
                tt("pool", Sst.rearrange("p (h d) -> p h d", d=64), Sst.rearrange("p (h d) -> p h d", d=64),
                   eq_all[:, i, :].unsqueeze(2).to_broadcast([128, 16, 64]), ALU.mult, [TS_, Tcum], [TS_])
                for g in range(2):
                    tt("dve", Sst[:, g * 512:(g + 1) * 512], Sst[:, g * 512:(g + 1) * 512], pb[g], ALU.add,
                       [TS_, Tpb[g]], [TS_])
                cpy("act", Sb, Sst, [TS_, TSb], [TSb])
                for g in range(2):
                    py, Tpy = pb[2 + g], Tpb[2 + g]
                    tt("dve", y[:, g * 512:(g + 1) * 512], y[:, g * 512:(g + 1) * 512], py, ALU.add, [Ty, Tpy], [Ty])

            def stageC(i):
                tok = slice(i * 128, (i + 1) * 128)
                xsD, TxsD = xsD_[i % 2], TxsD_[i % 2]
                y, Ty = ybuf[i % 2], Tybuf[i % 2]
                tt("dve", y, y, xsD, ALU.add, [Ty, TxsD], [Ty])
                tt("dve", y, y, siluz[:, i, :], ALU.mult, [Ty, Tsiluz[i]], [Ty])
                sc_, Tsc_ = stat_slot()
                for g in range(2):
                    act(ygn[:, g * 512:(g + 1) * 512], y[:, g * 512:(g + 1) * 512], AF.Square, [Ty], [Tygn, Tsc_],
                        accum_out=sc_[:, g:g + 1])
                act(sc_[:, 2:4], sc_[:, 0:2], AF.Ln, [Tsc_, Tconst], [Tsc_], bias=epsc[:, 0:1], scale=1.0 / 512)
                act(sc_[:, 4:6], sc_[:, 2:4], AF.Exp, [Tsc_], [Tsc_], scale=-0.5)
                for g in range(2):
                    act(ygn[:, g * 512:(g + 1) * 512], y[:, g * 512:(g + 1) * 512], AF.Copy, [Ty, Tsc_], [Tygn],
                        scale=sc_[:, 4 + g:5 + g])
                pT3, TpT3 = next_pT()
                for c in range(8):
                    tr(pT3[:, c, :], ygn[:, c * 128:(c + 1) * 128], identb, [Tygn, Tconst], [TpT3])
                cpy("act", xsT[:, :, tok], pT3, [TpT3], [TxsT[i]])

            stageA(0)
            for i in range(16):
                if i + 1 < 16:
                    stageA(i + 1)
                stageB(i)
                stageC(i)

        dump("mixT_ssd", xsT, [8, 2048], BF16)
        dump("S", Sst, [1024], F32)

        P.barrier()
        rsc = Alloc(RSC0, RSC1)
        scT = rsc.get(8 * 2048, BF16, (8, 2048))
        TscT = [T(f"scT{i}") for i in range(16)]
        rbc = Alloc(RBC0, RBC1)
        sq = [rbc.get(512, BF16) for _ in range(2)]
        Tsq = [T("sq0"), T("sq1")]
        rt = [rbc.get(512, F32) for _ in range(2)]
        Trt = [T("rt0"), T("rt1")]
        cvt = [rbc.get(512, F32) for _ in range(2)]
        Tcvt = [T("cvt0"), T("cvt1")]
        scb = [rbc.get(512, F32) for _ in range(2)]
        Tscb = [T("scb0"), T("scb1")]
        cwsc = cpv("cwsc")
        o3 = 1024 + 1536 + 16
        rhs_ = Alloc(RH0, RH0 + 32768)
        scw = [rhs_.get(8 * 256, BF16, (8, 256)) for _ in range(6)]
        Tscw = [T(f"scw{q}") for q in range(6)]

        def load_sc(jp):
            res = []
            for q, c0_ in enumerate((o3 + jp * 256, o3 + 2048 + jp * 256, o3 + 1024 + jp * 256)):
                bi = (jp % 2) * 3 + q
                src_ = w_in[:, c0_:c0_ + 256].rearrange("(k p) c -> p k c", p=128)
                P.dma("pool", lambda e, bi=bi, src_=src_: e.dma_start(out=scw[bi], in_=src_), f"scw{bi}", writes=[Tscw[bi]])
                res.append((scw[bi], Tscw[bi]))
            return res

        sc_loaded = {0: load_sc(0)}
        for jp in range(4):
            if jp + 1 < 4:
                sc_loaded[jp + 1] = load_sc(jp + 1)
            (wb, Twb), (wv, Twv), (wc, Twc) = sc_loaded[jp]
            for lc in range(2):
                j = jp * 2 + lc
                proj_chunk(wb, Twb, lc)
                ub, Tub = next_u()
                evac_to_u(ub, Tub)
                proj_chunk(wv, Twv, lc)
                u, Tu = next_u()
                evac_to_u(u, Tu)
                proj_chunk(wc, Twc, lc)
                tt("dve", u[:, 0:HALO], u[:, 0:HALO], pb[4][:, 0:HALO], ALU.mult, [Tu, Tpb[4]], [Tu])
                for t4 in range(4):
                    sl = slice(HALO + t4 * 512, HALO + (t4 + 1) * 512)
                    tt("dve", u[:, sl], u[:, sl], pb[t4], ALU.mult, [Tu, Tpb[t4]], [Tu])
                d, Td = make_diag(lambda jt, j=j: cwsc[:, j * 3 + jt:j * 3 + jt + 1], 3)
                for t4 in range(4):
                    sl = slice(t4 * 512, (t4 + 1) * 512)
                    slh = slice(HALO + t4 * 512, HALO + (t4 + 1) * 512)
                    b2 = t4 % 2
                    pc_, Tpc_ = conv_pe(u, Tu, d, Td, 3, t4)
                    tt("dve", scb[b2], pc_, ub[:, slh], ALU.mult, [Tpc_, Tub], [Tscb[b2]])
                    act(sq[b2], scb[b2], AF.Square, [Tscb[b2]], [Tsq[b2]])
                    pz, Tpz = conv_pe_bank()
                    mm(pz, bd64b, sq[b2], True, True, [Tconst, Tsq[b2]], [Tpz])
                    act(rt[b2], pz, AF.Ln, [Tpz, Tconst], [Trt[b2]], bias=epsc[:, 0:1], scale=1.0 / 64)
                    act(rt[b2], rt[b2], AF.Exp, [Trt[b2]], [Trt[b2]], scale=-0.5)
                    tt("pool", scT[:, j, sl], scb[b2], rt[b2], ALU.mult, [Tscb[b2], Trt[b2]], TscT[4 * t4:4 * t4 + 4])
        dump("scT", scT, [8, 2048], BF16)

        P.barrier()
        CAP = MOE_CAP
        NS = CAP // 128
        hn2d = nc.dram_tensor("hn2d", [SPAN + 128, D], BF16).ap()
        hacc = nc.dram_tensor("hacc", [SPAN + 128, D], F32).ap()
        listd = nc.dram_tensor("listd", [32 * CAP + 128, 16], F32).ap()
        Thacc, Thn2d, Tlistd = T("hacc"), T("hn2d"), T("listd")
        rh = Alloc(RH0, RH1)
        h = rh.get(16 * 1024, F32, (16, 1024))
        Th = [T(f"h{i}") for i in range(16)]
        rs2 = Alloc(RS20, RS21)
        wo = [rs2.get(16 * 512, BF16, (16, 512)) for _ in range(2)]
        Two = [T("wo0"), T("wo1")]
        xr = [rs2.get(1024, F32) for _ in range(2)]
        Txr = [T("xr0"), T("xr1")]
        RS2_ROUTE_END = rs2.p
        hnb = [rs2.get(1024, BF16) for _ in range(2)]
        Thnb = [T("hnb0"), T("hnb1")]
        gfb = rs2.get(1024, BF16)
        Tgfb = T("gfb")
        L = rs2.get(16 * 36, F32, (16, 36))
        TL = T("L")
        rbc = Alloc(RBC0, RBC1)
        hnf = [rbc.get(1024, F32) for _ in range(2)]
        Thnf = [T("hnf0"), T("hnf1")]
        hTf = [rbc.get(1024, F32, (8, 128)) for _ in range(2)]
        ThTf = [T("hTf0"), T("hTf1")]
        gffn_b = cpv("gffn").unsqueeze(2).to_broadcast([128, 8, 128])
        wr = cpv("wr", (8, 36))
        gain_b = cpv("gain16").unsqueeze(2).to_broadcast([128, 16, 512])
        own_row0 = HALO + NPREV * SPAN
        for half in range(2):
            src = w_out[:, half * 512:(half + 1) * 512].rearrange("(k p) c -> p k c", p=128)
            P.dma("pool", lambda e, src=src, half=half: e.dma_start(out=wo[half], in_=src), f"wo{half}", writes=[Two[half]])
            if half == 0:
                tt("dve", wo[half], wo[half], gain_b, ALU.mult, [Two[half], Tcp], [Two[half]])
            else:
                g16 = cpv("gain16")
                for k in range(16):
                    act(wo[half][:, k, :], wo[half][:, k, :], AF.Copy, [Two[half], Tcp], [Two[half]], scale=g16[:, k:k + 1])
        P.dma("pool", lambda e: e.dma_start(out=gfb, in_=gfd), "gfb", writes=[Tgfb])
        P.op("pool", lambda e: e.memset(hnb[1], 0.0), writes=[Thnb[1]])
        P.dma("sp", lambda e: e.dma_start(out=hn2d[SPAN:SPAN + 128, :], in_=hnb[1]), "hn2d0", reads=[Thnb[1]], writes=[])

        def n2_stage1(i):
            j = i % 2
            P.dma("sp", lambda e, i=i: e.dma_start(out=hacc[i * 128:(i + 1) * 128, :], in_=h[:, i, :]), "hacc0",
                  reads=[Th[i]], writes=[])
            sc_, Tsc_ = rmsnorm_stats(h[:, i, :], hnf[j], 128, [Th[i]], Thnf[j])
            ts("dve", hnf[j], h[:, i, :], sc_[:, 2:3], ALU.mult, [Th[i], Tsc_], [Thnf[j]])
            tt("pool", hnb[j], hnf[j], gfb, ALU.mult, [Thnf[j], Tgfb], [Thnb[j]])
            P.dma("sp", lambda e, i=i, j=j: e.dma_start(out=hn2d[i * 128:(i + 1) * 128, :], in_=hnb[j]), "hn2d0",
                  reads=[Thnb[j]], writes=[])

        def n2_stage2(i):
            j = i % 2
            for hf in range(2):
                pz, Tpz = pb[4 + hf], Tpb[4 + hf]
                for k4 in range(4):
                    k = hf * 4 + k4
                    tr(pz[:, k4 * 128:(k4 + 1) * 128], hnf[j][:, k * 128:(k + 1) * 128], identf, [Thnf[j], Tcp], [Tpz])
                tt("dve", hTf[j][:, hf * 4:(hf + 1) * 4, :], pz.rearrange("p (k t) -> p k t", t=128),
                   gffn_b[:, hf * 4:(hf + 1) * 4, :], ALU.mult, [Tpz, Tcp], [ThTf[j]])

        def n2_stage3(i):
            j = i % 2
            pr, Tpr = pb[6], Tpb[6]
            for k in range(8):
                mm(pr[:, 0:36], hTf[j][:, k, :], wr[:, k, :], k == 0, k == 7, [ThTf[j], Tcp], [Tpr])
            cpy("act", L[:, i, :], pr[:, 0:36], [Tpr], [TL])

        for i in range(16):
            tok = slice(i * 128, (i + 1) * 128)
            pzs = [(pb[(2 * i) % 4], Tpb[(2 * i) % 4]), (pb[(2 * i + 1) % 4], Tpb[(2 * i + 1) % 4])]
            for k in range(16):
                lhs = xsT[:, k, tok] if k < 8 else scT[:, k - 8, tok]
                for half in range(2):
                    mm(pzs[half][0], lhs, wo[half][:, k, :], k == 0, k == 15, [TxsT[i], TscT[i], Two[half]], [pzs[half][1]])
            jx = i % 2
            P.dma("sp", lambda e, jx=jx, i=i: e.dma_start(
                out=xr[jx], in_=xe[own_row0 + i * 128: own_row0 + (i + 1) * 128, :]), f"xr{jx}", writes=[Txr[jx]])
            for half in range(2):
                tt("dve", h[:, i, half * 512:(half + 1) * 512], pzs[half][0], xr[jx][:, half * 512:(half + 1) * 512], ALU.add,
                   [pzs[half][1], Txr[jx]], [Th[i]])
            n2_stage1(i)
            if i >= 1:
                n2_stage2(i - 1)
            if i >= 2:
                n2_stage3(i - 2)
        n2_stage2(15)
        n2_stage3(14)
        n2_stage3(15)
        dump("h", h, [16, 1024], F32)

        P.barrier()
        rs2 = Alloc(RS20, RS2_ROUTE_END)
        r16 = [rs2.get(16, F32) for _ in range(10)]
        r64 = [rs2.get(64, F32, (16, 4)) for _ in range(3)]
        r512 = [rs2.get(512, F32, (16, 32)) for _ in range(7)]
        idxi = [rs2.get(16, I32) for _ in range(2)]
        rows = [rs2.get(256, F32, (16, 16)) for _ in range(2)]
        linit = rs2.get(32 * CAP // 128 * 16, F32)
        Trt_ = T("routetmp")
        nrow_init = 32 * CAP // 128
        li3 = linit[:, 0:nrow_init * 16].rearrange("p (a b) -> p a b", b=16)
        P.op("pool", lambda e: e.memset(li3, 0.0), writes=[Trt_])
        ts("pool", li3[:, :, 0:1], cpv("tokid")[:, 0:1].unsqueeze(1).to_broadcast([128, nrow_init, 1]), float(SPAN), ALU.add,
           [Trt_, Tcp], [Trt_])
        P.dma("sp", lambda e: e.dma_start(out=listd[0:32 * CAP, :].rearrange("(a p) c -> p a c", p=128), in_=li3), "linit",
              reads=[Trt_], writes=[Tlistd])
        gl = L[:, :, 0:4]
        el = L[:, :, 4:36]
        gmax, gsum, gw, m1, m2, dd2, p1, p2, ix1, ix2 = r16
        goh, gex, pen = r64
        msk, oh1, oh2, tmp5, posv, pre, oh_keep = r512
        dummyp = r16[0][:, 0:1] if False else rs2.get(1, F32)
        RT = [TL, Trt_]
        P.op("dve", lambda e: e.tensor_reduce(out=gmax, in_=gl, axis=AX.X, op=ALU.max), [TL], [Trt_])
        tt("dve", gex, gl, gmax.unsqueeze(2).to_broadcast([128, 16, 4]), ALU.subtract, RT, [Trt_])
        tt("dve", goh, gl, gmax.unsqueeze(2).to_broadcast([128, 16, 4]), ALU.is_equal, RT, [Trt_])
        act(gex, gex, AF.Exp, [Trt_], [Trt_])
        P.op("dve", lambda e: e.tensor_reduce(out=gsum, in_=gex, axis=AX.X, op=ALU.add), [Trt_], [Trt_])
        P.op("dve", lambda e: e.reciprocal(out=gw, in_=gsum), [Trt_], [Trt_])
        ts("dve", pen, goh, -1.0, ALU.add, [Trt_], [Trt_], s2=1e30, op1=ALU.mult)
        tt("dve", msk.rearrange("p a (g e) -> p a g e", e=8), el.rearrange("p a (g e) -> p a g e", e=8),
           pen.unsqueeze(3).to_broadcast([128, 16, 4, 8]), ALU.add, RT, [Trt_])
        P.op("dve", lambda e: e.tensor_reduce(out=m1, in_=msk, axis=AX.X, op=ALU.max), [Trt_], [Trt_])
        tt("dve", oh1, msk, m1.unsqueeze(2).to_broadcast([128, 16, 32]), ALU.is_equal, [Trt_], [Trt_])
        stt("dve", tmp5, oh1, -1e30, msk, ALU.mult, ALU.add, [Trt_], [Trt_])
        P.op("dve", lambda e: e.tensor_reduce(out=m2, in_=tmp5, axis=AX.X, op=ALU.max), [Trt_], [Trt_])
        tt("dve", oh2, tmp5, m2.unsqueeze(2).to_broadcast([128, 16, 32]), ALU.is_equal, [Trt_], [Trt_])
        tt("dve", dd2, m2, m1, ALU.subtract, [Trt_], [Trt_])
        act(dd2, dd2, AF.Exp, [Trt_], [Trt_])
        ts("dve", p1, dd2, 1.0, ALU.add, [Trt_], [Trt_])
        P.op("dve", lambda e: e.reciprocal(out=p1, in_=p1), [Trt_], [Trt_])
        tt("dve", p2, dd2, p1, ALU.mult, [Trt_], [Trt_])
        tt("dve", p1, p1, gw, ALU.mult, [Trt_], [Trt_])
        tt("dve", p2, p2, gw, ALU.mult, [Trt_], [Trt_])
        tt("dve", tmp5, oh1, oh2, ALU.add, [Trt_], [Trt_])
        oh_f = tmp5.rearrange("p a b -> p (a b)")
        mm(pb[4], cpv("tri"), oh_f, True, True, [Tcp, Trt_], [Tpb[4]])
        mm(pb[5], cpv("ones"), oh_f, True, True, [Tcp, Trt_], [Tpb[5]])
        cpy("act", msk.rearrange("p a b -> p (a b)"), pb[5], [Tpb[5]], [Trt_])
        P.op("pool", lambda e: e.memset(pre[:, 0, :], 0.0), [Trt_], [Trt_])
        for i in range(1, 16):
            tt("pool", pre[:, i, :], pre[:, i - 1, :], msk[:, i - 1, :], ALU.add, [Trt_], [Trt_])
        stt("dve", posv.rearrange("p a b -> p (a b)"), pb[4], -1.0, pre.rearrange("p a b -> p (a b)"), ALU.add, ALU.add,
            [Tpb[4], Trt_], [Trt_])
        ts("dve", msk, posv, float(CAP), ALU.is_ge, [Trt_], [Trt_])
        tt("dve", posv, posv, cpv("ecap").unsqueeze(1).to_broadcast([128, 16, 32]), ALU.add, [Trt_, Tcp], [Trt_])
        ts("dve", oh_keep, msk, -1.0, ALU.mult, [Trt_], [Trt_], s2=1.0, op1=ALU.add)
        tt("dve", posv, posv, oh_keep, ALU.mult, [Trt_], [Trt_])
        ts("pool", dummyp, cpv("tokid")[:, 0:1], float(32 * CAP), ALU.add, [Tcp], [Trt_])
        stt("dve", posv, msk, dummyp, posv, ALU.mult, ALU.add, [Trt_], [Trt_])
        for kk, (ohk, ixk, pk) in enumerate(((oh1, ix1, p1), (oh2, ix2, p2))):
            tt("dve", msk, ohk, posv, ALU.mult, [Trt_], [Trt_])
            P.op("dve", lambda e, ixk=ixk: e.tensor_reduce(out=ixk, in_=msk, axis=AX.X, op=ALU.add), [Trt_], [Trt_])
            cpy("dve", idxi[kk], ixk, [Trt_], [Trt_])
            P.op("pool", lambda e, kk=kk: e.memset(rows[kk], 0.0), [Trt_], [Trt_])
            cpy("pool", rows[kk][:, :, 0:1], cpv("tokid").unsqueeze(2), [Trt_, Tcp], [Trt_])
            cpy("pool", rows[kk][:, :, 1:2], pk.unsqueeze(2), [Trt_], [Trt_])
        for kk in range(2):
            for i in range(16):
                P.dma("pool", lambda e, kk=kk, i=i: e.indirect_dma_start(
                    out=listd, out_offset=bass.IndirectOffsetOnAxis(ap=idxi[kk][:, i:i + 1], axis=0),
                    in_=rows[kk][:, i, :], in_offset=None),
                    "lsc", reads=[Trt_, Tlistd], writes=[])
        dump("L", L, [16, 36], F32)

        P.barrier()
        rsc = Alloc(RSC0, RSC1)
        wgu_region = rsc.get(8 * 2048, BF16, (8, 2048))
        wgb = [wgu_region[:, :, 0:512], wgu_region[:, :, 512:1024]]
        wub = [wgu_region[:, :, 1024:1536], wgu_region[:, :, 1536:2048]]
        Twg, Twu = [T("wg0"), T("wg1")], [T("wu0"), T("wu1")]
        rbc = Alloc(RBC0, RBC1)
        wdb = [rbc.get(4 * 1024, BF16, (4, 1024)) for _ in range(2)]
        Twd = [T("wd0"), T("wd1")]
        rh = Alloc(RH0, RH1)
        sli = [rh.get(NS * 16, F32, (NS, 16)) for _ in range(4)]
        Tsli = [T(f"sli{q}") for q in range(4)]
        tki = [rh.get(NS, I32) for _ in range(4)]
        Ttki = [T(f"tki{q}") for q in range(4)]
        Xe = [rh.get(NS * 1024, BF16, (NS, 1024)) for _ in range(2)]
        TXe = [T("Xe0"), T("Xe1")]
        XgT = [rh.get(8 * CAP, BF16, (8, CAP)) for _ in range(2)]
        TXgT = [T("XgT0"), T("XgT1")]
        sg = [rh.get(CAP, F32) for _ in range(2)]
        Tsg = [T("sg0"), T("sg1")]
        hT = [rh.get(4 * CAP, BF16, (4, CAP)) for _ in range(2)]
        ThT = [T("hT0"), T("hT1")]
        yb = [rh.get(1024, F32) for _ in range(3)]
        Tyb = [T("yb0"), T("yb1"), T("yb2")]
        for b in range(2):
            P.op("pool", lambda e, b=b: e.memset(Xe[b], 0.0), writes=[TXe[b]])
        rx = Alloc(RX0, RX1)
        rs2e = Alloc(RS20, RS21)
        stg_sets = [[rx.get(4096, F32), rx.get(4096, F32), rh.get(4096, F32)],
                    [rs2e.get(4096, F32), rs2e.get(4096, F32), rs2e.get(4096, F32)]]
        Tstg_sets = [[T("stg0"), T("stg1"), T("stg2")], [T("stg3"), T("stg4"), T("stg5")]]
        cnt = 0
        ycnt = 0

        def prefetch_x(ex):
            b = ex % 2
            b4 = ex % 4
            P.dma("sp", lambda e, ex=ex, b4=b4: e.dma_start(
                out=sli[b4], in_=listd[ex * CAP:(ex + 1) * CAP, :].rearrange("(s p) c -> p s c", p=128)),
                f"sli{b4}", writes=[Tsli[b4]])
            cpy("dve", tki[b4], sli[b4][:, :, 0], [Tsli[b4]], [Ttki[b4]])
            for s in range(NS):
                P.dma("pool", lambda e, b=b, b4=b4, s=s: e.indirect_dma_start(
                    out=Xe[b][:, s, :], out_offset=None, in_=hn2d,
                    in_offset=bass.IndirectOffsetOnAxis(ap=tki[b4][:, s:s + 1], axis=0)),
                    f"xg{b}", reads=[Ttki[b4]], writes=[TXe[b]])

        def prefetch_w(ex):
            stg, Tstg = stg_sets[ex % 2], Tstg_sets[ex % 2]
            srcs = (w_gate[ex].rearrange("(k p) f -> p k f", p=128), w_up[ex].rearrange("(k p) f -> p k f", p=128),
                    w_down[ex].rearrange("(k p) d -> p k d", p=128))
            views = (stg[0].rearrange("p (k f) -> p k f", f=512), stg[1].rearrange("p (k f) -> p k f", f=512),
                     stg[2].rearrange("p (k f) -> p k f", f=1024))
            for q in range(3):
                P.dma("sp", lambda e, q=q, srcs=srcs, views=views: e.dma_start(out=views[q], in_=srcs[q]),
                      f"stg{q + 3 * (ex % 2)}", writes=[Tstg[q]])

        def casts(ex):
            b = ex % 2
            stg, Tstg = stg_sets[ex % 2], Tstg_sets[ex % 2]
            v0 = stg[0].rearrange("p (k f) -> p k f", f=512)
            v1 = stg[1].rearrange("p (k f) -> p k f", f=512)
            v2 = stg[2].rearrange("p (k f) -> p k f", f=1024)
            cpy("act", wgb[b][:, 0:4, :], v0[:, 0:4, :], [Tstg[0]], [Twg[b]])
            cpy("dve", wgb[b][:, 4:8, :], v0[:, 4:8, :], [Tstg[0]], [Twg[b]])
            cpy("act", wub[b][:, 0:4, :], v1[:, 0:4, :], [Tstg[1]], [Twu[b]])
            cpy("dve", wub[b][:, 4:8, :], v1[:, 4:8, :], [Tstg[1]], [Twu[b]])
            cpy("act", wdb[b][:, 0:2, :], v2[:, 0:2, :], [Tstg[2]], [Twd[b]])
            cpy("dve", wdb[b][:, 2:4, :], v2[:, 2:4, :], [Tstg[2]], [Twd[b]])

        if n_experts > 0:
            prefetch_w(0)
            casts(0)
            prefetch_x(0)
        if n_experts > 1:
            prefetch_w(1)
        for ex in range(n_experts):
            b = ex % 2
            b4 = ex % 4
            if ex + 1 < n_experts:
                prefetch_x(ex + 1)
            if ex + 2 < n_experts:
                prefetch_w(ex + 2)
            for s in range(NS):
                pT, TpT = next_pT()
                for k in range(8):
                    tr(pT[:, k, :], Xe[b][:, s, k * 128:(k + 1) * 128], identb, [TXe[b], Tconst], [TpT])
                cpy("act" if s % 2 == 0 else "dve", XgT[b][:, :, s * 128:(s + 1) * 128], pT, [TpT], [TXgT[b]])
            for f in range(4):
                pg, Tpg = pb[cnt % 2], Tpb[cnt % 2]
                pu, Tpu = pb[2 + cnt % 2], Tpb[2 + cnt % 2]
                s_, Ts_ = sg[cnt % 2], Tsg[cnt % 2]
                cnt += 1
                for k in range(8):
                    mm(pg[:, 0:CAP], wgb[b][:, k, f * 128:(f + 1) * 128], XgT[b][:, k, :], k == 0, k == 7,
                       [Twg[b], TXgT[b]], [Tpg])
                for k in range(8):
                    mm(pu[:, 0:CAP], wub[b][:, k, f * 128:(f + 1) * 128], XgT[b][:, k, :], k == 0, k == 7,
                       [Twu[b], TXgT[b]], [Tpu])
                act(s_, pg[:, 0:CAP], AF.Silu, [Tpg], [Ts_])
                tt("dve", hT[b][:, f, :], s_, pu[:, 0:CAP], ALU.mult, [Ts_, Tpu], [ThT[b]])
            ys = []
            for s in range(NS):
                yy, Tyy = yb[ycnt % 3], Tyb[ycnt % 3]
                ycnt += 1
                for half in range(2):
                    pd, Tpd = pb[4 + half], Tpb[4 + half]
                    for f in range(4):
                        mm(pd, hT[b][:, f, s * 128:(s + 1) * 128], wdb[b][:, f, half * 512:(half + 1) * 512],
                           f == 0, f == 3, [ThT[b], Twd[b]], [Tpd])
                    if half == 0:
                        act(yy[:, 0:512], pd, AF.Copy, [Tpd, Tsli[b4]], [Tyy], scale=sli[b4][:, s, 1:2])
                    else:
                        ts("dve", yy[:, 512:1024], pd, sli[b4][:, s, 1:2], ALU.mult, [Tpd, Tsli[b4]], [Tyy])
                ys.append((yy, Tyy, s))
            if ex + 1 < n_experts:
                casts(ex + 1)
            for yy, Tyy, s in ys:
                P.dma("pool", lambda e, b4=b4, s=s, yy=yy: e.indirect_dma_start(
                    out=hacc, out_offset=bass.IndirectOffsetOnAxis(ap=tki[b4][:, s:s + 1], axis=0),
                    in_=yy, in_offset=None, compute_op=ALU.add),
                    "ysc", reads=[Ttki[b4], Tyy], writes=[Thacc])

        rs2 = Alloc(RS20, RS21)
        fn = rs2.get(1024, F32)
        Tfn = T("fn")
        hb = [rs2.get(1024, F32) for _ in range(3)]
        Thb = [T("hb0"), T("hb1"), T("hb2")]
        ob = [rs2.get(1024, F32) for _ in range(2)]
        Tob = [T("ob0"), T("ob1")]
        Tout = T("out")
        P.barrier()
        P.dma("sp", lambda e: e.dma_start(out=fn, in_=fnd), "fn", writes=[Tfn])
        for i in range(16):
            j = i % 2
            j3 = i % 3
            P.dma("sp", lambda e, i=i, j3=j3: e.dma_start(out=hb[j3], in_=hacc[i * 128:(i + 1) * 128, :]), f"hb{j3}",
                  reads=[Thacc], writes=[Thb[j3]])
            sc_, Tsc_ = rmsnorm_stats(hb[j3], ob[j], 128, [Thb[j3]], Tob[j])
            stt("dve", ob[j], hb[j3], sc_[:, 2:3], fn, ALU.mult, ALU.mult, [Thb[j3], Tsc_, Tfn], [Tob[j]])
            P.dma("sp", lambda e, i=i, j=j: e.dma_start(out=out[i * 128:(i + 1) * 128, :], in_=ob[j]), "out",
                  reads=[Tob[j]], writes=[Tout])
        P.barrier(engs=("sp",))
        P.emit(st)
    return nc


_NC_CACHE = {}


def make_inputs(inputs):
    x = np.asarray(inputs["x"], np.float32)
    in_maps = []
    shared = {
        "w_in": np.ascontiguousarray(np.asarray(inputs["w_in"], np.float32)[0]),
        "w_out": np.ascontiguousarray(np.asarray(inputs["w_out"], np.float32)[0]),
        "w_gate": np.ascontiguousarray(np.asarray(inputs["w_gate"], np.float32)[0]),
        "w_up": np.ascontiguousarray(np.asarray(inputs["w_up"], np.float32)[0]),
        "w_down": np.ascontiguousarray(np.asarray(inputs["w_down"], np.float32)[0]),
        "fnorm": np.ascontiguousarray(np.broadcast_to(np.asarray(inputs["final_norm"], np.float32)[None, :], (128, D))),
        "gfb": np.ascontiguousarray(np.broadcast_to(np.asarray(inputs["norm_ffn"], np.float32)[0][None, :], (128, D))),
    }
    inp_np = {k: np.asarray(v, np.float32) for k, v in inputs.items() if k not in ("x", "w_in", "w_out", "w_gate", "w_up", "w_down")}
    for c in range(NCORES):
        b, s = c // 4, c % 4
        start = s * SPAN
        lo = start - NPREV * SPAN - HALO
        xe = np.zeros((NTOK_EXT, D), np.float32)
        src_lo = max(lo, 0)
        xe[src_lo - lo:, :] = x[b, src_lo:start + SPAN, :]
        m = dict(shared)
        m["xe"] = xe
        m["cp"] = make_cp(inp_np, c)
        in_maps.append(m)
    return in_maps


def kernel(**inputs):
    if "nc" not in _NC_CACHE:
        _NC_CACHE["nc"] = build()
    nc = _NC_CACHE["nc"]
    in_maps = make_inputs(inputs)
    res = run_bass_kernel_spmd(nc, in_maps, core_ids=list(range(NCORES)))
    outs = [np.asarray(res.results[c]["out"], np.float32) for c in range(NCORES)]
    full = np.stack(outs, 0).reshape(2, 4 * SPAN, D)
    return full
```

```python
from contextlib import ExitStack
import numpy as np
import concourse.bass as bass
import concourse.mybir as mybir
from concourse.bass_utils import run_bass_kernel_spmd

F32 = mybir.dt.float32
BF16 = mybir.dt.bfloat16
ALU = mybir.AluOpType
AF = mybir.ActivationFunctionType
AX = mybir.AxisListType

ENGS = ("pe", "act", "dve", "pool", "sp")
SEM_LIMIT = 12000
NCORES = 8
SPAN = 2048
NPREV = 3
HALO = 8
NTOK_EXT = HALO + (NPREV + 1) * SPAN
D = 1024
EPS = 1e-6
MOE_CAP = 256
I32 = mybir.dt.int32


class T:
    __slots__ = ("name", "w", "r", "rd")

    def __init__(self, name=""):
        self.name = name
        self.w = None
        self.r = {}
        self.rd = []


class Prog:
    def __init__(self, nc):
        self.nc = nc
        self.ops = []
        self.dma_sems = {}
        self.last = {}
        self.dmas = []

    def _deps(self, reads, writes):
        deps = set()
        raw = set()
        for t in reads:
            if t.w is not None:
                deps.add(t.w)
                raw.add(t.w)
        for t in writes:
            if t.w is not None:
                deps.add(t.w)
            deps.update(t.r.values())
            deps.update(t.rd)
        self._last_raw = raw
        return deps

    def op(self, eng, fn, reads=(), writes=()):
        i = len(self.ops)
        self.ops.append(dict(eng=eng, fn=fn, deps=self._deps(reads, writes), kind="c"))
        self.ops[-1]["raw"] = self._last_raw
        self.last[eng] = i
        for t in reads:
            t.r[eng] = i
        for t in writes:
            t.w = i
            t.r = {}
            t.rd = []
        return i

    def dma(self, eng, fn, sem, reads=(), writes=(), inc=16):
        i = len(self.ops)
        n = self.dma_sems.get(sem, 0) + inc
        self.dma_sems[sem] = n
        self.ops.append(dict(eng=eng, fn=fn, deps=self._deps(reads, writes), kind="d", sem=sem, val=n, inc=inc))
        self.dmas.append(i)
        for t in reads:
            t.rd.append(i)
        for t in writes:
            t.w = i
            t.r = {}
            t.rd = []
        return i

    def barrier(self, engs=ENGS):
        deps = set(self.last.values()) | set(self.dmas)
        self.dmas = []
        for e in engs:
            self.ops.append(dict(eng=e, fn=None, deps=set(deps), kind="c"))

    def emit(self, stack):
        nc = self.nc
        ops = self.ops
        needed = set()
        for o in ops:
            for d in o["deps"]:
                if ops[d]["kind"] == "c":
                    needed.add(d)
        cnt = {e: 0 for e in ENGS}
        for i, o in enumerate(ops):
            if o["kind"] == "c" and i in needed and o["fn"] is not None:
                cnt[o["eng"]] += 1
                o["cnt"] = cnt[o["eng"]]
        esems = {}
        for e in ENGS:
            n = cnt[e] // SEM_LIMIT + 1
            esems[e] = [stack.enter_context(nc.semaphore(f"s_{e}{k}")) for k in range(n)]
        dsems = {k: stack.enter_context(nc.semaphore(f"d_{k}")) for k in self.dma_sems}
        per = {e: [] for e in ENGS}
        for i, o in enumerate(ops):
            per[o["eng"]].append(i)

        def section(e):
            def body(eng):
                seen = {}
                for i in per[e]:
                    o = ops[i]
                    w = {}
                    for d in o["deps"]:
                        od = ops[d]
                        if od["kind"] == "c":
                            if od["fn"] is None or "cnt" not in od:
                                continue
                            if od["eng"] == e and e == "pe":
                                continue
                            if od["eng"] == e and "raw" in o and d not in o["raw"]:
                                continue
                            c = od["cnt"]
                            key = ("e", od["eng"], (c - 1) // SEM_LIMIT)
                            val = (c - 1) % SEM_LIMIT + 1
                        else:
                            key = ("d", od["sem"])
                            val = od["val"]
                        if w.get(key, 0) < val:
                            w[key] = val
                    for key, val in sorted(w.items(), key=lambda kv: str(kv[0])):
                        if seen.get(key, 0) >= val:
                            continue
                        seen[key] = val
                        if key[0] == "e":
                            sem = esems[key[1]][key[2]]
                            for kk in range(key[2]):
                                seen[("e", key[1], kk)] = SEM_LIMIT
                        else:
                            sem = dsems[key[1]]
                        eng.wait_ge(sem, val)
                    if o["fn"] is None:
                        continue
                    ins = o["fn"](eng)
                    if o["kind"] == "c":
                        if "cnt" in o:
                            c = o["cnt"]
                            ins.then_inc(esems[e][(c - 1) // SEM_LIMIT], 1)
                    else:
                        ins.then_inc(dsems[o["sem"]], o["inc"])
            return body

        with nc.Block() as block:
            block.tensor(section("pe"))
            block.scalar(section("act"))
            block.vector(section("dve"))
            block.gpsimd(section("pool"))
            block.sync(section("sp"))


CP = {}
_o = 0
for _n, _w in (("ident", 128), ("tri", 128), ("lstrict", 128), ("ones", 128), ("bd64", 128),
               ("gmix", 8), ("gffn", 8), ("gain16", 16), ("cwssd", 48), ("cbssd", 12), ("cwsc", 24),
               ("dtb", 16), ("alog", 16), ("dsk", 16), ("wr", 288), ("bmask", 4), ("ecap", 32), ("tokid", 16)):
    CP[_n] = (_o, _w)
    _o += _w
NCP = _o


def make_cp(inp, core):
    cp = np.zeros((128, NCP), np.float32)

    def put(name, arr):
        o, w = CP[name]
        cp[:, o:o + w] = np.asarray(arr, np.float32).reshape(128, w)

    idx = np.arange(128)
    put("ident", np.eye(128))
    put("tri", (idx[:, None] <= idx[None, :]))
    put("lstrict", (idx[:, None] > idx[None, :]))
    put("ones", np.ones((128, 128)))
    put("bd64", (idx[:, None] // 64 == idx[None, :] // 64))
    put("gmix", inp["norm_mix"][0].reshape(8, 128).T)
    put("gffn", inp["norm_ffn"][0].reshape(8, 128).T)
    put("gain16", np.concatenate([inp["ssd_norm"][0].reshape(8, 128).T, inp["sc_norm"][0].reshape(8, 128).T], 1))
    put("cwssd", inp["ssd_conv_w"][0].reshape(4, 12, 128).transpose(2, 1, 0))
    put("cbssd", inp["ssd_conv_b"][0].reshape(12, 128).T)
    put("cwsc", inp["sc_conv_w"][0].reshape(3, 8, 128).transpose(2, 1, 0))
    put("dtb", np.broadcast_to(inp["dt_bias"][0][None, :], (128, 16)))
    put("alog", np.broadcast_to(inp["a_log"][0][None, :], (128, 16)))
    put("dsk", np.broadcast_to(inp["d_skip"][0][None, :], (128, 16)))
    wr = np.concatenate([inp["w_router_group"][0],
                         inp["w_router_expert"][0].transpose(1, 0, 2).reshape(1024, 32)], 1)
    put("wr", wr.reshape(8, 128, 36).transpose(1, 0, 2))
    s = core % 4
    put("bmask", np.broadcast_to(np.array([1.0 if b >= NPREV - s else 0.0 for b in range(NPREV + 1)],
                                          np.float32)[None, :], (128, 4)))
    put("ecap", np.broadcast_to((np.arange(32, dtype=np.float32) * MOE_CAP)[None, :], (128, 32)))
    put("tokid", (np.arange(16)[None, :] * 128 + np.arange(128)[:, None]).astype(np.float32))
    return cp


def build(dbg=False, stop_after=None, n_experts=32):
    nc = bass.Bass("TRN2", target_bir_lowering=False)

    def dram(name, shape, kind="ExternalInput"):
        return nc.dram_tensor(name, shape, F32, kind=kind).ap()

    xe = dram("xe", [NTOK_EXT, D])
    cpd = dram("cp", [128, NCP])
    fnd = dram("fnorm", [128, D])
    gfd = dram("gfb", [128, D])
    w_in = dram("w_in", [D, 5648])
    w_out = dram("w_out", [2048, D])
    w_gate = dram("w_gate", [32, D, 512])
    w_up = dram("w_up", [32, D, 512])
    w_down = dram("w_down", [32, 512, D])
    out = dram("out", [SPAN, D], kind="ExternalOutput")
    dbg_out = {}

    P = Prog(nc)
    st = ExitStack()
    with st:
        ARENA_N = 53200
        arena = st.enter_context(nc.sbuf_tensor("arena", [128, ARENA_N], F32))
        pbt = [st.enter_context(nc.psum_tensor(f"pb{i}", [128, 512], F32)) for i in range(7)]
        pTb = st.enter_context(nc.psum_tensor("pTb", [128, 8, 128], BF16))
        pb = [t[:] for t in pbt]
        Tpb = [T(f"pb{i}") for i in range(7)]
        TpTb = T("pTb")

        a_f32 = arena[:]
        a_bf = arena[:].bitcast(BF16)
        a_i32 = arena[:].bitcast(mybir.dt.int32)

        class Alloc:
            def __init__(self, start, end):
                self.p = start
                self.end = end

            def get(self, n_elems, dt, shape=None):
                nb = n_elems * (2 if dt == BF16 else 4)
                nb_al = (nb + 63) // 64 * 64
                off = self.p
                assert off + nb_al <= self.end, ("arena overflow", off, nb_al, self.end)
                self.p += nb_al
                if dt == F32:
                    ap = a_f32[:, off // 4: off // 4 + n_elems]
                elif dt == mybir.dt.int32:
                    ap = a_i32[:, off // 4: off // 4 + n_elems]
                else:
                    ap = a_bf[:, off // 2: off // 2 + n_elems]
                if shape is not None and len(shape) == 2:
                    ap = ap.rearrange("p (a b) -> p a b", b=shape[1])
                return ap

        PERS0 = 0
        PERS1 = 13824
        RX0, RX1 = PERS1, PERS1 + 32768
        RH0, RH1 = RX1, RX1 + 65792
        RBC0, RBC1 = RH1, RH1 + 16384
        RSC0, RSC1 = RBC1, RBC1 + 32768
        RS20, RS21 = RSC1, ARENA_N * 4

        pers = Alloc(PERS0, PERS1)
        cp = pers.get(NCP, F32)
        Tcp = T("cp")

        def cpv(name, shape=None):
            o, w = CP[name]
            ap = cp[:, o:o + w]
            if shape is not None:
                ap = ap.rearrange("p (a b) -> p a b", b=shape[1])
            return ap

        identb = pers.get(128, BF16)
        bd64b = pers.get(128, BF16)
        Sst = pers.get(1024, F32)
        Sb = pers.get(1024, BF16)
        dtt = pers.get(256, F32, (16, 16))
        adt = pers.get(256, F32, (16, 16))
        avec = pers.get(16, F32)
        epsc = pers.get(1, F32)
        pers_stats = pers.get(64, F32)
        Tconst = T("const")
        TS_, TSb, Tdt, Tstats = T("S"), T("Sb"), T("dt"), T("stats")

        def act(out_, in_, func, reads, writes, **kw):
            P.op("act", lambda e: e.activation(out=out_, in_=in_, func=func, **kw), reads, writes)

        def tt(eng, out_, in0, in1, op, reads, writes):
            P.op(eng, lambda e: e.tensor_tensor(out=out_, in0=in0, in1=in1, op=op), reads, writes)

        def ts(eng, out_, in0, s1, op0, reads, writes, s2=None, op1=None):
            if op1 is None:
                P.op(eng, lambda e: e.tensor_scalar(out=out_, in0=in0, scalar1=s1, scalar2=None, op0=op0), reads, writes)
            else:
                P.op(eng, lambda e: e.tensor_scalar(out=out_, in0=in0, scalar1=s1, scalar2=s2, op0=op0, op1=op1), reads, writes)

        def stt(eng, out_, in0, scalar, in1, op0, op1, reads, writes):
            P.op(eng, lambda e: e.scalar_tensor_tensor(out=out_, in0=in0, scalar=scalar, in1=in1, op0=op0, op1=op1), reads, writes)

        def cpy(eng, out_, in_, reads, writes):
            if eng == "act":
                P.op("act", lambda e: e.copy(out=out_, in_=in_), reads, writes)
            else:
                P.op(eng, lambda e: e.tensor_copy(out=out_, in_=in_), reads, writes)

        def mm(out_, lhsT, rhs, start, stop, reads, writes):
            P.op("pe", lambda e: e.matmul(out_, lhsT=lhsT, rhs=rhs, start=start, stop=stop), reads, writes)

        def tr(out_, in_, ident, reads, writes):
            P.op("pe", lambda e: e.transpose(out=out_, in_=in_, identity=ident), reads, writes)

        stat_tiles = [pers_stats[:, 8 * q: 8 * q + 8] for q in range(8)]
        Tstat_tiles = [T(f"stat{q}") for q in range(8)]
        stc = [0]

        def stat_slot():
            q = stc[0] % 8
            stc[0] += 1
            return stat_tiles[q], Tstat_tiles[q]

        def rmsnorm_stats(x_ap, junk_ap, n, reads, Tjunk):
            sl_, Tsl_ = stat_slot()
            act(junk_ap, x_ap, AF.Square, reads, [Tjunk, Tsl_], accum_out=sl_[:n, 0:1])
            act(sl_[:n, 1:2], sl_[:n, 0:1], AF.Ln, [Tsl_, Tconst], [Tsl_], bias=epsc[:n, 0:1], scale=1.0 / D)
            act(sl_[:n, 2:3], sl_[:n, 1:2], AF.Exp, [Tsl_], [Tsl_], scale=-0.5)
            return sl_, Tsl_

        Tdump = T("dump")

        def dump(name, ap, shp, dt):
            if not dbg:
                return
            d = nc.dram_tensor("dbg_" + name, [128] + shp, dt, kind="ExternalOutput").ap()
            P.barrier()
            P.dma("sp", lambda e, d=d, ap=ap: e.dma_start(out=d, in_=ap), "dump", writes=[Tdump])
            P.barrier()

        P.dma("sp", lambda e: e.dma_start(out=cp, in_=cpd), "cp", writes=[Tcp])
        P.op("pool", lambda e: e.memset(epsc, EPS), writes=[Tconst])
        cpy("dve", identb, cpv("ident"), [Tcp], [Tconst])
        cpy("dve", bd64b, cpv("bd64"), [Tcp], [Tconst])
        act(avec, cpv("alog"), AF.Exp, [Tcp], [Tconst])
        ts("dve", avec, avec, -1.0, ALU.mult, [Tconst], [Tconst])
        P.op("pool", lambda e: e.memset(Sst, 0.0), writes=[TS_])

        rx = Alloc(RX0, RX1)
        xsT = rx.get(8 * 2048, BF16, (8, 2048))
        TxsT = [T(f"xsT{i}") for i in range(16)]
        rh = Alloc(RH0, RH1)
        siluz = rh.get(16 * 1024, BF16, (16, 1024))
        Tsiluz = [T(f"sz{i}") for i in range(16)]
        hnT = rh.get(8 * 2056, BF16, (8, 2056))
        ThnT = [T(f"hnT{i}") for i in range(17)]
        rbc = Alloc(RBC0, RBC1)
        BT = rbc.get(2 * 2048, BF16, (2, 2048))
        CT = rbc.get(2 * 2048, BF16, (2, 2048))
        TBT, TCT = T("BT"), T("CT")

        rs2 = Alloc(RS20, RS21)
        wts = [rs2.get(8 * 256, BF16, (8, 256)) for _ in range(4)]
        Twts = [T(f"wt{i}") for i in range(4)]
        U = [rs2.get(2056, BF16) for _ in range(3)]
        TU = [T(f"U{i}") for i in range(3)]
        xt = [rs2.get(1024, F32) for _ in range(2)]
        Txt = [T(f"xt{i}") for i in range(2)]
        dg = [rs2.get(4 * 128, BF16, (4, 128)) for _ in range(2)]
        Tdg = [T("dg0"), T("dg1")]
        acs_all = rs2.get(512, F32)
        dd_all = rs2.get(256, F32, (16, 16))
        ed_all = rs2.get(256, F32, (16, 16))
        eq_all = rs2.get(256, F32, (16, 16))
        ea_all = rs2.get(256, F32, (16, 16))
        LT = rs2.get(256, F32, (16, 16))
        wgt_all = rs2.get(256, F32, (16, 16))
        Tcum = T("cum")
        rsc = Alloc(RSC0, RSC1)
        xn = [rsc.get(1024, BF16) for _ in range(2)]
        Txn = [T(f"xn{i}") for i in range(2)]
        wdt = rsc.get(8 * 16, BF16, (8, 16))
        Twdt = T("wdt")
        dtmp = rsc.get(256, F32)
        Tdtmp = T("dtmp")
        xdt = [rsc.get(1024, BF16) for _ in range(2)]
        Txdt = [T(f"xdt{i}") for i in range(2)]
        xdtdec = rsc.get(1024, BF16)
        Txdtdec = T("xdtdec")
        xsD_ = [rsc.get(1024, BF16), rs2.get(1024, BF16)]
        TxsD_ = [T("xsD0"), T("xsD1")]
        Btok = [rsc.get(256, BF16) for _ in range(2)]
        TBtok = [T(f"Btok{i}") for i in range(2)]
        CBm = rsc.get(256, F32, (2, 128))
        TCBm = T("CBm")
        Rb = [rsc.get(512, F32, (4, 128)) for _ in range(2)]
        TRb = [T(f"R{i}") for i in range(2)]
        Eb = [rsc.get(512, BF16, (4, 128)) for _ in range(2)]
        TEb = [T(f"E{i}") for i in range(2)]
        MT = rsc.get(16 * 128, BF16, (16, 128))
        TMT = [T(f"MT{i}") for i in range(4)]
        ybuf = [rsc.get(1024, F32), a_f32[:, RSC0 // 4: RSC0 // 4 + 1024]]
        Tybuf = [T("y0"), T("y1")]
        ygn = rsc.get(1024, BF16)
        Tygn = T("ygn")

        pTs = [pTb[:], pb[6].bitcast(BF16).rearrange("p (k t) -> p k t", t=128),
               pb[3].bitcast(BF16).rearrange("p (k t) -> p k t", t=128),
               pb[2].bitcast(BF16).rearrange("p (k t) -> p k t", t=128)]
        TpTs = [TpTb, Tpb[6], Tpb[3], Tpb[2]]
        ptc = [0]
        pt_depth = [4]

        def next_pT():
            j = ptc[0] % pt_depth[0]
            ptc[0] += 1
            return pTs[j], TpTs[j]

        wctr = [0]

        def load_w(cols0, ncols):
            j = wctr[0] % 4
            wctr[0] += 1
            dst = wts[j][:, :, 0:ncols]
            src = w_in[:, cols0:cols0 + ncols].rearrange("(k p) c -> p k c", p=128)
            P.dma("pool", lambda e: e.dma_start(out=dst, in_=src), f"wt{j}", writes=[Twts[j]])
            return wts[j], Twts[j]

        xctr = [0]
        uctr = [0]
        dgc = [0]
        cvc = [0]

        def next_u():
            j = uctr[0] % 3
            uctr[0] += 1
            return U[j], TU[j]

        gmix_b = cpv("gmix").unsqueeze(2)
        identf = cpv("ident")

        def hn_tiles(tt4):
            return ThnT[1 + 4 * tt4: 5 + 4 * tt4]

        def proj_chunk(wt_ap, Tw, lc):
            for k in range(8):
                mm(pb[4][:, 0:HALO], wt_ap[:, k, lc * 128:(lc + 1) * 128], hnT[:, k, 0:HALO], k == 0, k == 7,
                   [Tw, ThnT[0]], [Tpb[4]])
            for t4 in range(4):
                for k in range(8):
                    mm(pb[t4], wt_ap[:, k, lc * 128:(lc + 1) * 128],
                       hnT[:, k, HALO + t4 * 512: HALO + (t4 + 1) * 512], k == 0, k == 7,
                       [Tw] + hn_tiles(t4), [Tpb[t4]])

        def evac_to_u(u, Tu, all_act=False):
            cpy("act", u[:, 0:HALO], pb[4][:, 0:HALO], [Tpb[4]], [Tu])
            for t4 in range(4):
                cpy("act" if (t4 % 2 == 0 or all_act) else "dve", u[:, HALO + t4 * 512: HALO + (t4 + 1) * 512], pb[t4],
                    [Tpb[t4]], [Tu])

        cacc = [a_f32[:, RH0 // 4 + q * 2048: RH0 // 4 + (q + 1) * 2048] for q in range(2)]
        Tcacc = [T("cacc0"), T("cacc1")]

        def make_diag(wcol, ntap):
            j = dgc[0] % 2
            dgc[0] += 1
            for jt in range(ntap):
                act(dg[j][:, jt, :], identf, AF.Copy, [Tcp], [Tdg[j]], scale=wcol(jt))
            return dg[j], Tdg[j]

        def conv_pe_bank():
            j = 5 + cvc[0] % 2
            cvc[0] += 1
            return pb[j], Tpb[j]

        def conv_pe(u, Tu, d, Td, ntap, t4):
            j = 5 + cvc[0] % 2
            cvc[0] += 1
            base = HALO - (ntap - 1) + t4 * 512
            for jt in range(ntap):
                mm(pb[j], d[:, jt, :], u[:, base + jt: base + jt + 512], jt == 0, jt == ntap - 1, [Td, Tu], [Tpb[j]])
            return pb[j], Tpb[j]

        a1_state = {}

        def a1_front(blk_, ti):
            row0_ = HALO + blk_ * SPAN
            n = HALO if ti == 0 else 128
            r0 = row0_ - HALO if ti == 0 else row0_ + (ti - 1) * 128
            j = xctr[0] % 2
            xctr[0] += 1
            xtj, xnj = xt[j], xn[j]
            P.dma("sp", lambda e, xtj=xtj, r0=r0, n=n: e.dma_start(out=xtj[:n, :], in_=xe[r0:r0 + n, :]),
                  f"xt{j}", writes=[Txt[j]])
            sc_, Tsc_ = rmsnorm_stats(xtj[:n, :], xnj[:n, :], n, [Txt[j]], Txn[j])
            ts("dve", xnj[:n, :], xtj[:n, :], sc_[:n, 2:3], ALU.mult, [Txt[j], Tsc_], [Txn[j]])
            a1_state[(blk_, ti)] = (j, n)

        def a1_back(blk_, ti):
            j, n = a1_state.pop((blk_, ti))
            xnj = xn[j]
            c0 = 0 if ti == 0 else HALO + (ti - 1) * 128
            pT, TpT = next_pT()
            for k in range(8):
                tr(pT[:, k, :n], xnj[:n, k * 128:(k + 1) * 128], identb[:n, :n], [Txn[j], Tconst], [TpT])
            tt("dve", hnT[:, :, c0:c0 + n], pT[:, :, :n], gmix_b.to_broadcast([128, 8, n]),
               ALU.mult, [TpT, Tcp], [ThnT[ti]])

        a1_front(0, 0)
        for ti in range(17):
            if ti + 1 < 17:
                a1_front(0, ti + 1)
            a1_back(0, ti)

        for blk in range(NPREV + 1):
            own = blk == NPREV
            row0 = HALO + blk * SPAN
            cwssd = cpv("cwssd")
            cbssd = cpv("cbssd")
            conv_chunks = list(range(8)) + [8, 9] + ([10, 11] if own else [])

            def conv_silu(c, u, Tu):
                acc, Tacc = cacc[c % 2], Tcacc[c % 2]
                base = HALO - 3
                ts("dve", acc, u[:, base:base + SPAN], cwssd[:, c * 4:c * 4 + 1], ALU.mult, [Tu, Tcp], [Tacc])
                for jt in range(1, 4):
                    stt("dve", acc, u[:, base + jt:base + jt + SPAN], cwssd[:, c * 4 + jt:c * 4 + jt + 1], acc,
                        ALU.mult, ALU.add, [Tu, Tcp, Tacc], [Tacc])
                if c < 8:
                    dst, Tdst = xsT[:, c, :], TxsT
                elif c < 10:
                    dst, Tdst = BT[:, c - 8, :], [TBT]
                else:
                    dst, Tdst = CT[:, c - 10, :], [TCT]
                act(dst, acc, AF.Silu, [Tacc, Tcp], Tdst, bias=cbssd[:, c:c + 1])

            pending = None
            for g0 in range(0, len(conv_chunks), 2):
                cc0 = conv_chunks[g0]
                wt_ap, Tw = load_w(1024 + cc0 * 128, 256)
                for lc in range(2):
                    c = cc0 + lc
                    proj_chunk(wt_ap, Tw, lc)
                    u, Tu = next_u()
                    evac_to_u(u, Tu, all_act=True)
                    if pending is not None:
                        conv_silu(*pending)
                    pending = (c, u, Tu)
            conv_silu(*pending)

            src = w_in[:, 2560:2576].rearrange("(k p) c -> p k c", p=128)
            P.dma("pool", lambda e, src=src: e.dma_start(out=wdt, in_=src), "wdt", writes=[Twdt])
            for i in range(16):
                for k in range(8):
                    mm(pb[4][:, 16 + i * 16: 32 + i * 16], hnT[:, k, HALO + i * 128: HALO + (i + 1) * 128], wdt[:, k, :],
                       k == 0, k == 7, [ThnT[1 + i], Twdt], [Tpb[4]])
            dtb_b = cpv("dtb").unsqueeze(1).to_broadcast([128, 16, 16])
            dt3 = dtmp.rearrange("p (a b) -> p a b", b=16)
            tt("dve", dt3, pb[4][:, 16:272].rearrange("p (a b) -> p a b", b=16), dtb_b, ALU.add, [Tpb[4], Tcp], [Tdtmp])
            act(dtmp, dtmp, AF.Exp, [Tdtmp], [Tdtmp])
            act(dtmp, dtmp, AF.Ln, [Tdtmp], [Tdtmp], bias=1.0)
            bm = cpv("bmask")
            ts("dve", dtt, dt3, bm[:, blk:blk + 1], ALU.mult, [Tdtmp, Tcp], [Tdt])
            tt("dve", adt, dtt, avec.unsqueeze(1).to_broadcast([128, 16, 16]), ALU.mult, [Tdt, Tconst], [Tdt])
            adt_f = adt.rearrange("p a b -> p (a b)")
            mm(pb[4][:, 0:256], cpv("tri"), adt_f, True, True, [Tcp, Tdt], [Tpb[4]])
            mm(pb[4][:, 256:512], cpv("ones"), adt_f, True, True, [Tcp, Tdt], [Tpb[4]])
            cpy("act", acs_all, pb[4], [Tpb[4]], [Tcum])
            ac3 = acs_all[:, 0:256].rearrange("p (a b) -> p a b", b=16)
            aq3 = acs_all[:, 256:512].rearrange("p (a b) -> p a b", b=16)
            tt("dve", dd_all, aq3, ac3, ALU.subtract, [Tcum], [Tcum])

            tri = cpv("tri")
            if not own:
                P.op("pool", lambda e: e.memset(LT[:, 15, :], 0.0), [Tcum], [Tcum])
                for i in range(14, -1, -1):
                    tt("pool", LT[:, i, :], LT[:, i + 1, :], aq3[:, i + 1, :], ALU.add, [Tcum], [Tcum])
                tt("dve", dd_all, dd_all, LT, ALU.add, [Tcum], [Tcum])
                act(ed_all, dd_all, AF.Exp, [Tcum], [Tcum])
                tt("dve", wgt_all, ed_all, dtt, ALU.mult, [Tcum, Tdt], [Tcum])
                tt("pool", eq_all[:, 0, :], LT[:, 0, :], aq3[:, 0, :], ALU.add, [Tcum], [Tcum])
                act(eq_all[:, 0, :], eq_all[:, 0, :], AF.Exp, [Tcum], [Tcum])
                for i in range(16):
                    tok = slice(i * 128, (i + 1) * 128)
                    xd, Txd = xdt[i % 2], Txdt[i % 2]
                    bt, Tbt = Btok[i % 2], TBtok[i % 2]
                    pT, TpT = next_pT()
                    for c in range(8):
                        tr(pT[:, c, :], xsT[:, c, tok], identb, [TxsT[i], Tconst], [TpT])
                    pX = pT.rearrange("p c t -> p (c t)").rearrange("p (h d) -> p h d", d=64)
                    tt("dve", xd.rearrange("p (h d) -> p h d", d=64), pX,
                       wgt_all[:, i, :].unsqueeze(2).to_broadcast([128, 16, 64]), ALU.mult, [TpT, Tcum], [Txd])
                    pT2, TpT2 = next_pT()
                    for g in range(2):
                        tr(pT2[:, g, :], BT[:, g, tok], identb, [TBT, Tconst], [TpT2])
                    cpy("act", bt.rearrange("p (g n) -> p g n", n=128), pT2[:, 0:2, :], [TpT2], [Tbt])
                    for g in range(2):
                        mm(pb[g], bt[:, g * 128:(g + 1) * 128], xd[:, g * 512:(g + 1) * 512], i == 0, i == 15,
                           [Tbt, Txd], [Tpb[g]])
                    if i == 0:
                        a1_front(blk + 1, 0)
                    a1_front(blk + 1, i + 1)
                    a1_back(blk + 1, i)
                a1_back(blk + 1, 16)
                tt("pool", Sst.rearrange("p (h d) -> p h d", d=64), Sst.rearrange("p (h d) -> p h d", d=64),
                   eq_all[:, 0, :].unsqueeze(2).to_broadcast([128, 16, 64]), ALU.mult, [TS_, Tcum], [TS_])
                for g in range(2):
                    tt("dve", Sst[:, g * 512:(g + 1) * 512], Sst[:, g * 512:(g + 1) * 512], pb[g], ALU.add,
                       [TS_, Tpb[g]], [TS_])
                continue

            act(ed_all, dd_all, AF.Exp, [Tcum], [Tcum])
            act(eq_all, aq3, AF.Exp, [Tcum], [Tcum])
            act(ea_all, ac3, AF.Exp, [Tcum], [Tcum])
            for q in range(4):
                wt_ap, Tw = load_w(q * 256, 256)
                for i in range(16):
                    pz, Tpz = pb[5 + i % 2], Tpb[5 + i % 2]
                    for k in range(8):
                        mm(pz[:, 0:256], hnT[:, k, HALO + i * 128: HALO + (i + 1) * 128], wt_ap[:, k, :], k == 0, k == 7,
                           [ThnT[1 + i], Tw], [Tpz])
                    act(siluz[:, i, q * 256:(q + 1) * 256], pz[:, 0:256], AF.Silu, [Tpz], [Tsiluz[i], Tcacc[0], Tcacc[1]])
            cpy("act", Sb, Sst, [TS_], [TSb])

            pt_depth[0] = 2
            xdtdec_ = [xdtdec, U[2][:, 0:1024]]
            Txdtdec_ = [Txdtdec, T("xdtdec1")]
            MT_ = [MT, U[1][:, 0:2048].rearrange("p (h l) -> p h l", l=128)]
            TMT_ = [TMT, [T(f"MTb{q}") for q in range(4)]]

            def stageA(i):
                tok = slice(i * 128, (i + 1) * 128)
                xd, Txd = xdt[i % 2], Txdt[i % 2]
                bt, Tbt = Btok[i % 2], TBtok[i % 2]
                xsD, TxsD = xsD_[i % 2], TxsD_[i % 2]
                xdd, Txdd = xdtdec_[i % 2], Txdtdec_[i % 2]
                pT, TpT = next_pT()
                for c in range(8):
                    tr(pT[:, c, :], xsT[:, c, tok], identb, [TxsT[i], Tconst], [TpT])
                pX = pT.rearrange("p c t -> p (c t)").rearrange("p (h d) -> p h d", d=64)
                tt("dve", xd.rearrange("p (h d) -> p h d", d=64), pX, dtt[:, i, :].unsqueeze(2).to_broadcast([128, 16, 64]),
                   ALU.mult, [TpT, Tdt], [Txd])
                tt("dve", xsD.rearrange("p (h d) -> p h d", d=64), pX,
                   cpv("dsk").unsqueeze(2).to_broadcast([128, 16, 64]), ALU.mult, [TpT, Tcp], [TxsD])
                pT2, TpT2 = next_pT()
                for g in range(2):
                    tr(pT2[:, g, :], BT[:, g, tok], identb, [TBT, Tconst], [TpT2])
                cpy("act", bt.rearrange("p (g n) -> p g n", n=128), pT2[:, 0:2, :], [TpT2], [Tbt])
                tt("dve", xdd.rearrange("p (h d) -> p h d", d=64), xd.rearrange("p (h d) -> p h d", d=64),
                   ed_all[:, i, :].unsqueeze(2).to_broadcast([128, 16, 64]), ALU.mult, [Txd, Tcum], [Txdd])
                pc, Tpc = pb[5], Tpb[5]
                for g in range(2):
                    mm(pc[:, g * 128:(g + 1) * 128], BT[:, g, tok], CT[:, g, tok], True, True, [TBT, TCT], [Tpc])
                tt("dve", CBm, pc[:, 0:256].rearrange("p (g l) -> p g l", l=128),
                   tri.unsqueeze(1).to_broadcast([128, 2, 128]), ALU.mult, [Tpc, Tcp], [TCBm])
                for hq in range(4):
                    g = hq // 2
                    R, TR = Rb[hq % 2], TRb[hq % 2]
                    E, TE = Eb[hq % 2], TEb[hq % 2]
                    pg, Tpg = (pb[4], Tpb[4]) if hq % 2 == 0 else (pb[5], Tpb[5])
                    tt("pool", R, tri.unsqueeze(1).to_broadcast([128, 4, 128]),
                       adt[:, i, hq * 4:(hq + 1) * 4].unsqueeze(2).to_broadcast([128, 4, 128]), ALU.mult,
                       [Tcp, Tdt], [TR])
                    mm(pg, cpv("lstrict"), R.rearrange("p h l -> p (h l)"), True, True, [Tcp, TR], [Tpg])
                    act(E.rearrange("p h l -> p (h l)"), pg, AF.Exp, [Tpg], [TE])
                    tt("dve", MT_[i % 2][:, hq * 4:(hq + 1) * 4, :], E, CBm[:, g, :].unsqueeze(1).to_broadcast([128, 4, 128]),
                       ALU.mult, [TE, TCBm], [TMT_[i % 2][hq]])

            def stageB(i):
                tok = slice(i * 128, (i + 1) * 128)
                bt, Tbt = Btok[i % 2], TBtok[i % 2]
                xdd, Txdd = xdtdec_[i % 2], Txdtdec_[i % 2]
                y, Ty = ybuf[i % 2], Tybuf[i % 2]
                xd, Txd = xdt[i % 2], Txdt[i % 2]
                for g in range(2):
                    py, Tpy = pb[2 + g], Tpb[2 + g]
                    for r in range(8):
                        hh = g * 8 + r
                        mm(py[:, r * 64:(r + 1) * 64], MT_[i % 2][:, hh, :], xd[:, hh * 64:(hh + 1) * 64], True, True,
                           [TMT_[i % 2][hh // 4], Txd], [Tpy])
                for g in range(2):
                    mm(pb[1] if g == 0 else pb[0], CT[:, g, tok], Sb[:, g * 512:(g + 1) * 512], True, True,
                       [TCT, TSb], [Tpb[1] if g == 0 else Tpb[0]])
                for g in range(2):
                    tt("dve", y[:, g * 512:(g + 1) * 512].rearrange("p (h d) -> p h d", d=64),
                       (pb[1] if g == 0 else pb[0]).rearrange("p (h d) -> p h d", d=64),
                       ea_all[:, i, g * 8:(g + 1) * 8].unsqueeze(2).to_broadcast([128, 8, 64]), ALU.mult,
                       [Tpb[1] if g == 0 else Tpb[0], Tcum], [Ty])
                for g in range(2):
                    mm(pb[g], bt[:, g * 128:(g + 1) * 128], xdd[:, g * 512:(g + 1) * 512], True, True,
                       [Tbt, Txdd], [Tpb[g]])
                tt("pool", Sst.rearrange("p (h d) -> p h d", d=64), Sst.rearrange("p (h d) -> p h d", d=64),
                   eq_all[:, i, :].unsqueeze(2).to_broadcast([128, 16, 64]), ALU.mult, [TS_, Tcum], [TS_])
                for g in range(2):
                    tt("dve", Sst[:, g * 512:(g + 1) * 512], Sst[:, g * 512:(g + 1) * 512], pb[g], ALU.add,
                       [TS_, Tpb[g]], [TS_])
                cpy("act", Sb, Sst, [TS_, TSb], [TSb])
                for g in range(2):
                    py, Tpy = pb[2 + g], Tpb[2 + g]
                    tt("dve", y[:, g * 512:(g + 1) * 512], y[:, g * 512:(g + 1) * 512], py, ALU.add, [Ty, Tpy], [Ty])

            def stageC(i):
                tok = slice(i * 128, (i + 1) * 128)
                xsD, TxsD = xsD_[i % 2], TxsD_[i % 2]
                y, Ty = ybuf[i % 2], Tybuf[i % 2]
                tt("dve", y, y, xsD, ALU.add, [Ty, TxsD], [Ty])
                tt("dve", y, y, siluz[:, i, :], ALU.mult, [Ty, Tsiluz[i]], [Ty])
                sc_, Tsc_ = stat_slot()
                for g in range(2):
                    act(ygn[:, g * 512:(g + 1) * 512], y[:, g * 512:(g + 1) * 512], AF.Square, [Ty], [Tygn, Tsc_],
                        accum_out=sc_[:, g:g + 1])
                act(sc_[:, 2:4], sc_[:, 0:2], AF.Ln, [Tsc_, Tconst], [Tsc_], bias=epsc[:, 0:1], scale=1.0 / 512)
                act(sc_[:, 4:6], sc_[:, 2:4], AF.Exp, [Tsc_], [Tsc_], scale=-0.5)
                for g in range(2):
                    act(ygn[:, g * 512:(g + 1) * 512], y[:, g * 512:(g + 1) * 512], AF.Copy, [Ty, Tsc_], [Tygn],
                        scale=sc_[:, 4 + g:5 + g])
                pT3, TpT3 = next_pT()
                for c in range(8):
                    tr(pT3[:, c, :], ygn[:, c * 128:(c + 1) * 128], identb, [Tygn, Tconst], [TpT3])
                cpy("act", xsT[:, :, tok], pT3, [TpT3], [TxsT[i]])

            stageA(0)
            for i in range(16):
                if i + 1 < 16:
                    stageA(i + 1)
                stageB(i)
                stageC(i)

        dump("mixT_ssd", xsT, [8, 2048], BF16)
        dump("S", Sst, [1024], F32)

        P.barrier()
        rsc = Alloc(RSC0, RSC1)
        scT = rsc.get(8 * 2048, BF16, (8, 2048))
        TscT = [T(f"scT{i}") for i in range(16)]
        rbc = Alloc(RBC0, RBC1)
        sq = [rbc.get(512, BF16) for _ in range(2)]
        Tsq = [T("sq0"), T("sq1")]
        rt = [rbc.get(512, F32) for _ in range(2)]
        Trt = [T("rt0"), T("rt1")]
        cvt = [rbc.get(512, F32) for _ in range(2)]
        Tcvt = [T("cvt0"), T("cvt1")]
        scb = [rbc.get(512, F32) for _ in range(2)]
        Tscb = [T("scb0"), T("scb1")]
        cwsc = cpv("cwsc")
        o3 = 1024 + 1536 + 16
        rhs_ = Alloc(RH0, RH0 + 32768)
        scw = [rhs_.get(8 * 256, BF16, (8, 256)) for _ in range(6)]
        Tscw = [T(f"scw{q}") for q in range(6)]

        def load_sc(jp):
            res = []
            for q, c0_ in enumerate((o3 + jp * 256, o3 + 2048 + jp * 256, o3 + 1024 + jp * 256)):
                bi = (jp % 2) * 3 + q
                src_ = w_in[:, c0_:c0_ + 256].rearrange("(k p) c -> p k c", p=128)
                P.dma("pool", lambda e, bi=bi, src_=src_: e.dma_start(out=scw[bi], in_=src_), f"scw{bi}", writes=[Tscw[bi]])
                res.append((scw[bi], Tscw[bi]))
            return res

        sc_loaded = {0: load_sc(0)}
        for jp in range(4):
            if jp + 1 < 4:
                sc_loaded[jp + 1] = load_sc(jp + 1)
            (wb, Twb), (wv, Twv), (wc, Twc) = sc_loaded[jp]
            for lc in range(2):
                j = jp * 2 + lc
                proj_chunk(wb, Twb, lc)
                ub, Tub = next_u()
                evac_to_u(ub, Tub)
                proj_chunk(wv, Twv, lc)
                u, Tu = next_u()
                evac_to_u(u, Tu)
                proj_chunk(wc, Twc, lc)
                tt("dve", u[:, 0:HALO], u[:, 0:HALO], pb[4][:, 0:HALO], ALU.mult, [Tu, Tpb[4]], [Tu])
                for t4 in range(4):
                    sl = slice(HALO + t4 * 512, HALO + (t4 + 1) * 512)
                    tt("dve", u[:, sl], u[:, sl], pb[t4], ALU.mult, [Tu, Tpb[t4]], [Tu])
                d, Td = make_diag(lambda jt, j=j: cwsc[:, j * 3 + jt:j * 3 + jt + 1], 3)
                for t4 in range(4):
                    sl = slice(t4 * 512, (t4 + 1) * 512)
                    slh = slice(HALO + t4 * 512, HALO + (t4 + 1) * 512)
                    b2 = t4 % 2
                    pc_, Tpc_ = conv_pe(u, Tu, d, Td, 3, t4)
                    tt("dve", scb[b2], pc_, ub[:, slh], ALU.mult, [Tpc_, Tub], [Tscb[b2]])
                    act(sq[b2], scb[b2], AF.Square, [Tscb[b2]], [Tsq[b2]])
                    pz, Tpz = conv_pe_bank()
                    mm(pz, bd64b, sq[b2], True, True, [Tconst, Tsq[b2]], [Tpz])
                    act(rt[b2], pz, AF.Ln, [Tpz, Tconst], [Trt[b2]], bias=epsc[:, 0:1], scale=1.0 / 64)
                    act(rt[b2], rt[b2], AF.Exp, [Trt[b2]], [Trt[b2]], scale=-0.5)
                    tt("pool", scT[:, j, sl], scb[b2], rt[b2], ALU.mult, [Tscb[b2], Trt[b2]], TscT[4 * t4:4 * t4 + 4])
        dump("scT", scT, [8, 2048], BF16)

        P.barrier()
        CAP = MOE_CAP
        NS = CAP // 128
        hn2d = nc.dram_tensor("hn2d", [SPAN + 128, D], BF16).ap()
        hacc = nc.dram_tensor("hacc", [SPAN + 128, D], F32).ap()
        listd = nc.dram_tensor("listd", [32 * CAP + 128, 16], F32).ap()
        Thacc, Thn2d, Tlistd = T("hacc"), T("hn2d"), T("listd")
        rh = Alloc(RH0, RH1)
        h = rh.get(16 * 1024, F32, (16, 1024))
        Th = [T(f"h{i}") for i in range(16)]
        rs2 = Alloc(RS20, RS21)
        wo = [rs2.get(16 * 512, BF16, (16, 512)) for _ in range(2)]
        Two = [T("wo0"), T("wo1")]
        xr = [rs2.get(1024, F32) for _ in range(2)]
        Txr = [T("xr0"), T("xr1")]
        RS2_ROUTE_END = rs2.p
        hnb = [rs2.get(1024, BF16) for _ in range(2)]
        Thnb = [T("hnb0"), T("hnb1")]
        gfb = rs2.get(1024, BF16)
        Tgfb = T("gfb")
        L = rs2.get(16 * 36, F32, (16, 36))
        TL = T("L")
        rbc = Alloc(RBC0, RBC1)
        hnf = [rbc.get(1024, F32) for _ in range(2)]
        Thnf = [T("hnf0"), T("hnf1")]
        hTf = [rbc.get(1024, F32, (8, 128)) for _ in range(2)]
        ThTf = [T("hTf0"), T("hTf1")]
        gffn_b = cpv("gffn").unsqueeze(2).to_broadcast([128, 8, 128])
        wr = cpv("wr", (8, 36))
        gain_b = cpv("gain16").unsqueeze(2).to_broadcast([128, 16, 512])
        own_row0 = HALO + NPREV * SPAN
        for half in range(2):
            src = w_out[:, half * 512:(half + 1) * 512].rearrange("(k p) c -> p k c", p=128)
            P.dma("pool", lambda e, src=src, half=half: e.dma_start(out=wo[half], in_=src), f"wo{half}", writes=[Two[half]])
            if half == 0:
                tt("dve", wo[half], wo[half], gain_b, ALU.mult, [Two[half], Tcp], [Two[half]])
            else:
                g16 = cpv("gain16")
                for k in range(16):
                    act(wo[half][:, k, :], wo[half][:, k, :], AF.Copy, [Two[half], Tcp], [Two[half]], scale=g16[:, k:k + 1])
        P.dma("pool", lambda e: e.dma_start(out=gfb, in_=gfd), "gfb", writes=[Tgfb])
        P.op("pool", lambda e: e.memset(hnb[1], 0.0), writes=[Thnb[1]])
        P.dma("sp", lambda e: e.dma_start(out=hn2d[SPAN:SPAN + 128, :], in_=hnb[1]), "hn2dz", reads=[Thnb[1]], writes=[])

        def n2_stage1(i):
            j = i % 2
            P.dma("sp", lambda e, i=i: e.dma_start(out=hacc[i * 128:(i + 1) * 128, :], in_=h[:, i, :]), "hacc0",
                  reads=[Th[i]], writes=[])
            sc_, Tsc_ = rmsnorm_stats(h[:, i, :], hnf[j], 128, [Th[i]], Thnf[j])
            ts("dve", hnf[j], h[:, i, :], sc_[:, 2:3], ALU.mult, [Th[i], Tsc_], [Thnf[j]])
            tt("pool", hnb[j], hnf[j], gfb, ALU.mult, [Thnf[j], Tgfb], [Thnb[j]])
            P.dma("sp", lambda e, i=i, j=j: e.dma_start(out=hn2d[i * 128:(i + 1) * 128, :], in_=hnb[j]), f"hn2d{j}",
                  reads=[Thnb[j]], writes=[])

        def n2_stage2(i):
            j = i % 2
            for hf in range(2):
                pz, Tpz = pb[4 + hf], Tpb[4 + hf]
                for k4 in range(4):
                    k = hf * 4 + k4
                    tr(pz[:, k4 * 128:(k4 + 1) * 128], hnf[j][:, k * 128:(k + 1) * 128], identf, [Thnf[j], Tcp], [Tpz])
                tt("dve", hTf[j][:, hf * 4:(hf + 1) * 4, :], pz.rearrange("p (k t) -> p k t", t=128),
                   gffn_b[:, hf * 4:(hf + 1) * 4, :], ALU.mult, [Tpz, Tcp], [ThTf[j]])

        def n2_stage3(i):
            j = i % 2
            pr, Tpr = pb[6], Tpb[6]
            for k in range(8):
                mm(pr[:, 0:36], hTf[j][:, k, :], wr[:, k, :], k == 0, k == 7, [ThTf[j], Tcp], [Tpr])
            cpy("act", L[:, i, :], pr[:, 0:36], [Tpr], [TL])

        for i in range(16):
            tok = slice(i * 128, (i + 1) * 128)
            pzs = [(pb[(2 * i) % 4], Tpb[(2 * i) % 4]), (pb[(2 * i + 1) % 4], Tpb[(2 * i + 1) % 4])]
            for k in range(16):
                lhs = xsT[:, k, tok] if k < 8 else scT[:, k - 8, tok]
                for half in range(2):
                    mm(pzs[half][0], lhs, wo[half][:, k, :], k == 0, k == 15, [TxsT[i], TscT[i], Two[half]], [pzs[half][1]])
            jx = i % 2
            P.dma("sp", lambda e, jx=jx, i=i: e.dma_start(
                out=xr[jx], in_=xe[own_row0 + i * 128: own_row0 + (i + 1) * 128, :]), f"xr{jx}", writes=[Txr[jx]])
            for half in range(2):
                tt("dve", h[:, i, half * 512:(half + 1) * 512], pzs[half][0], xr[jx][:, half * 512:(half + 1) * 512], ALU.add,
                   [pzs[half][1], Txr[jx]], [Th[i]])
            n2_stage1(i)
            if i >= 1:
                n2_stage2(i - 1)
            if i >= 2:
                n2_stage3(i - 2)
        n2_stage2(15)
        n2_stage3(14)
        n2_stage3(15)
        dump("h", h, [16, 1024], F32)

        P.barrier()
        rs2 = Alloc(RS20, RS2_ROUTE_END)
        r16 = [rs2.get(16, F32) for _ in range(10)]
        r64 = [rs2.get(64, F32, (16, 4)) for _ in range(3)]
        r512 = [rs2.get(512, F32, (16, 32)) for _ in range(7)]
        idxi = [rs2.get(16, I32) for _ in range(2)]
        rows = [rs2.get(256, F32, (16, 16)) for _ in range(2)]
        linit = rs2.get(32 * CAP // 128 * 16, F32)
        Trt_ = T("routetmp")
        nrow_init = 32 * CAP // 128
        li3 = linit[:, 0:nrow_init * 16].rearrange("p (a b) -> p a b", b=16)
        P.op("pool", lambda e: e.memset(li3, 0.0), writes=[Trt_])
        ts("pool", li3[:, :, 0:1], cpv("tokid")[:, 0:1].unsqueeze(1).to_broadcast([128, nrow_init, 1]), float(SPAN), ALU.add,
           [Trt_, Tcp], [Trt_])
        P.dma("sp", lambda e: e.dma_start(out=listd[0:32 * CAP, :].rearrange("(a p) c -> p a c", p=128), in_=li3), "linit",
              reads=[Trt_], writes=[Tlistd])
        gl = L[:, :, 0:4]
        el = L[:, :, 4:36]
        gmax, gsum, gw, m1, m2, dd2, p1, p2, ix1, ix2 = r16
        goh, gex, pen = r64
        msk, oh1, oh2, tmp5, posv, pre, oh_keep = r512
        dummyp = r16[0][:, 0:1] if False else rs2.get(1, F32)
        RT = [TL, Trt_]
        P.op("dve", lambda e: e.tensor_reduce(out=gmax, in_=gl, axis=AX.X, op=ALU.max), [TL], [Trt_])
        tt("dve", gex, gl, gmax.unsqueeze(2).to_broadcast([128, 16, 4]), ALU.subtract, RT, [Trt_])
        tt("dve", goh, gl, gmax.unsqueeze(2).to_broadcast([128, 16, 4]), ALU.is_equal, RT, [Trt_])
        act(gex, gex, AF.Exp, [Trt_], [Trt_])
        P.op("dve", lambda e: e.tensor_reduce(out=gsum, in_=gex, axis=AX.X, op=ALU.add), [Trt_], [Trt_])
        P.op("dve", lambda e: e.reciprocal(out=gw, in_=gsum), [Trt_], [Trt_])
        ts("dve", pen, goh, -1.0, ALU.add, [Trt_], [Trt_], s2=1e30, op1=ALU.mult)
        tt("dve", msk.rearrange("p a (g e) -> p a g e", e=8), el.rearrange("p a (g e) -> p a g e", e=8),
           pen.unsqueeze(3).to_broadcast([128, 16, 4, 8]), ALU.add, RT, [Trt_])
        P.op("dve", lambda e: e.tensor_reduce(out=m1, in_=msk, axis=AX.X, op=ALU.max), [Trt_], [Trt_])
        tt("dve", oh1, msk, m1.unsqueeze(2).to_broadcast([128, 16, 32]), ALU.is_equal, [Trt_], [Trt_])
        stt("dve", tmp5, oh1, -1e30, msk, ALU.mult, ALU.add, [Trt_], [Trt_])
        P.op("dve", lambda e: e.tensor_reduce(out=m2, in_=tmp5, axis=AX.X, op=ALU.max), [Trt_], [Trt_])
        tt("dve", oh2, tmp5, m2.unsqueeze(2).to_broadcast([128, 16, 32]), ALU.is_equal, [Trt_], [Trt_])
        tt("dve", dd2, m2, m1, ALU.subtract, [Trt_], [Trt_])
        act(dd2, dd2, AF.Exp, [Trt_], [Trt_])
        ts("dve", p1, dd2, 1.0, ALU.add, [Trt_], [Trt_])
        P.op("dve", lambda e: e.reciprocal(out=p1, in_=p1), [Trt_], [Trt_])
        tt("dve", p2, dd2, p1, ALU.mult, [Trt_], [Trt_])
        tt("dve", p1, p1, gw, ALU.mult, [Trt_], [Trt_])
        tt("dve", p2, p2, gw, ALU.mult, [Trt_], [Trt_])
        tt("dve", tmp5, oh1, oh2, ALU.add, [Trt_], [Trt_])
        oh_f = tmp5.rearrange("p a b -> p (a b)")
        mm(pb[4], cpv("tri"), oh_f, True, True, [Tcp, Trt_], [Tpb[4]])
        mm(pb[5], cpv("ones"), oh_f, True, True, [Tcp, Trt_], [Tpb[5]])
        cpy("act", msk.rearrange("p a b -> p (a b)"), pb[5], [Tpb[5]], [Trt_])
        P.op("pool", lambda e: e.memset(pre[:, 0, :], 0.0), [Trt_], [Trt_])
        for i in range(1, 16):
            tt("pool", pre[:, i, :], pre[:, i - 1, :], msk[:, i - 1, :], ALU.add, [Trt_], [Trt_])
        stt("dve", posv.rearrange("p a b -> p (a b)"), pb[4], -1.0, pre.rearrange("p a b -> p (a b)"), ALU.add, ALU.add,
            [Tpb[4], Trt_], [Trt_])
        ts("dve", msk, posv, float(CAP), ALU.is_ge, [Trt_], [Trt_])
        tt("dve", posv, posv, cpv("ecap").unsqueeze(1).to_broadcast([128, 16, 32]), ALU.add, [Trt_, Tcp], [Trt_])
        ts("dve", oh_keep, msk, -1.0, ALU.mult, [Trt_], [Trt_], s2=1.0, op1=ALU.add)
        tt("dve", posv, posv, oh_keep, ALU.mult, [Trt_], [Trt_])
        ts("pool", dummyp, cpv("tokid")[:, 0:1], float(32 * CAP), ALU.add, [Tcp], [Trt_])
        stt("dve", posv, msk, dummyp, posv, ALU.mult, ALU.add, [Trt_], [Trt_])
        for kk, (ohk, ixk, pk) in enumerate(((oh1, ix1, p1), (oh2, ix2, p2))):
            tt("dve", msk, ohk, posv, ALU.mult, [Trt_], [Trt_])
            P.op("dve", lambda e, ixk=ixk: e.tensor_reduce(out=ixk, in_=msk, axis=AX.X, op=ALU.add), [Trt_], [Trt_])
            cpy("dve", idxi[kk], ixk, [Trt_], [Trt_])
            P.op("pool", lambda e, kk=kk: e.memset(rows[kk], 0.0), [Trt_], [Trt_])
            cpy("pool", rows[kk][:, :, 0:1], cpv("tokid").unsqueeze(2), [Trt_, Tcp], [Trt_])
            cpy("pool", rows[kk][:, :, 1:2], pk.unsqueeze(2), [Trt_], [Trt_])
        for kk in range(2):
            for i in range(16):
                P.dma("pool", lambda e, kk=kk, i=i: e.indirect_dma_start(
                    out=listd, out_offset=bass.IndirectOffsetOnAxis(ap=idxi[kk][:, i:i + 1], axis=0),
                    in_=rows[kk][:, i, :], in_offset=None),
                    "lsc", reads=[Trt_, Tlistd], writes=[])
        dump("L", L, [16, 36], F32)

        P.barrier()
        rsc = Alloc(RSC0, RSC1)
        wgu_region = rsc.get(8 * 2048, BF16, (8, 2048))
        wgb = [wgu_region[:, :, 0:512], wgu_region[:, :, 512:1024]]
        wub = [wgu_region[:, :, 1024:1536], wgu_region[:, :, 1536:2048]]
        Twg, Twu = [T("wg0"), T("wg1")], [T("wu0"), T("wu1")]
        rbc = Alloc(RBC0, RBC1)
        wdb = [rbc.get(4 * 1024, BF16, (4, 1024)) for _ in range(2)]
        Twd = [T("wd0"), T("wd1")]
        rh = Alloc(RH0, RH1)
        sli = [rh.get(NS * 16, F32, (NS, 16)) for _ in range(4)]
        Tsli = [T(f"sli{q}") for q in range(4)]
        tki = [rh.get(NS, I32) for _ in range(4)]
        Ttki = [T(f"tki{q}") for q in range(4)]
        Xe = [rh.get(NS * 1024, BF16, (NS, 1024)) for _ in range(2)]
        TXe = [T("Xe0"), T("Xe1")]
        XgT = [rh.get(8 * CAP, BF16, (8, CAP)) for _ in range(2)]
        TXgT = [T("XgT0"), T("XgT1")]
        sg = [rh.get(CAP, F32) for _ in range(2)]
        Tsg = [T("sg0"), T("sg1")]
        hT = [rh.get(4 * CAP, BF16, (4, CAP)) for _ in range(2)]
        ThT = [T("hT0"), T("hT1")]
        yb = [rh.get(1024, F32) for _ in range(3)]
        Tyb = [T("yb0"), T("yb1"), T("yb2")]
        for b in range(2):
            P.op("pool", lambda e, b=b: e.memset(Xe[b], 0.0), writes=[TXe[b]])
        rx = Alloc(RX0, RX1)
        stg = [rx.get(4096, F32), rx.get(4096, F32), rh.get(4096, F32)]
        Tstg = [T("stg0"), T("stg1"), T("stg2")]
        cnt = 0
        ycnt = 0

        def prefetch(ex):
            b = ex % 2
            b4 = ex % 4
            P.dma("sp", lambda e, ex=ex, b4=b4: e.dma_start(
                out=sli[b4], in_=listd[ex * CAP:(ex + 1) * CAP, :].rearrange("(s p) c -> p s c", p=128)),
                f"sli{b4}", writes=[Tsli[b4]])
            srcs = (w_gate[ex].rearrange("(k p) f -> p k f", p=128), w_up[ex].rearrange("(k p) f -> p k f", p=128),
                    w_down[ex].rearrange("(k p) d -> p k d", p=128))
            views = (stg[0].rearrange("p (k f) -> p k f", f=512), stg[1].rearrange("p (k f) -> p k f", f=512),
                     stg[2].rearrange("p (k f) -> p k f", f=1024))
            for q in range(3):
                P.dma("sp", lambda e, q=q, srcs=srcs, views=views: e.dma_start(out=views[q], in_=srcs[q]), f"stg{q}",
                      writes=[Tstg[q]])
            cpy("dve", tki[b4], sli[b4][:, :, 0], [Tsli[b4]], [Ttki[b4]])
            for s in range(NS):
                P.dma("pool", lambda e, b=b, b4=b4, s=s: e.indirect_dma_start(
                    out=Xe[b][:, s, :], out_offset=None, in_=hn2d,
                    in_offset=bass.IndirectOffsetOnAxis(ap=tki[b4][:, s:s + 1], axis=0)),
                    f"xg{b}", reads=[Ttki[b4]], writes=[TXe[b]])

        def casts(ex):
            b = ex % 2
            v0 = stg[0].rearrange("p (k f) -> p k f", f=512)
            v1 = stg[1].rearrange("p (k f) -> p k f", f=512)
            v2 = stg[2].rearrange("p (k f) -> p k f", f=1024)
            cpy("act", wgb[b][:, 0:4, :], v0[:, 0:4, :], [Tstg[0]], [Twg[b]])
            cpy("dve", wgb[b][:, 4:8, :], v0[:, 4:8, :], [Tstg[0]], [Twg[b]])
            cpy("act", wub[b][:, 0:4, :], v1[:, 0:4, :], [Tstg[1]], [Twu[b]])
            cpy("dve", wub[b][:, 4:8, :], v1[:, 4:8, :], [Tstg[1]], [Twu[b]])
            cpy("act", wdb[b][:, 0:2, :], v2[:, 0:2, :], [Tstg[2]], [Twd[b]])
            cpy("dve", wdb[b][:, 2:4, :], v2[:, 2:4, :], [Tstg[2]], [Twd[b]])

        if n_experts > 0:
            prefetch(0)
            casts(0)
        for ex in range(n_experts):
            b = ex % 2
            b4 = ex % 4
            if ex + 1 < n_experts:
                prefetch(ex + 1)
            for s in range(NS):
                pT, TpT = next_pT()
                for k in range(8):
                    tr(pT[:, k, :], Xe[b][:, s, k * 128:(k + 1) * 128], identb, [TXe[b], Tconst], [TpT])
                cpy("act" if s % 2 == 0 else "dve", XgT[b][:, :, s * 128:(s + 1) * 128], pT, [TpT], [TXgT[b]])
            for f in range(4):
                pg, Tpg = pb[cnt % 2], Tpb[cnt % 2]
                pu, Tpu = pb[2 + cnt % 2], Tpb[2 + cnt % 2]
                s_, Ts_ = sg[cnt % 2], Tsg[cnt % 2]
                cnt += 1
                for k in range(8):
                    mm(pg[:, 0:CAP], wgb[b][:, k, f * 128:(f + 1) * 128], XgT[b][:, k, :], k == 0, k == 7,
                       [Twg[b], TXgT[b]], [Tpg])
                for k in range(8):
                    mm(pu[:, 0:CAP], wub[b][:, k, f * 128:(f + 1) * 128], XgT[b][:, k, :], k == 0, k == 7,
                       [Twu[b], TXgT[b]], [Tpu])
                act(s_, pg[:, 0:CAP], AF.Silu, [Tpg], [Ts_])
                tt("dve", hT[b][:, f, :], s_, pu[:, 0:CAP], ALU.mult, [Ts_, Tpu], [ThT[b]])
            ys = []
            for s in range(NS):
                yy, Tyy = yb[ycnt % 3], Tyb[ycnt % 3]
                ycnt += 1
                for half in range(2):
                    pd, Tpd = pb[4 + half], Tpb[4 + half]
                    for f in range(4):
                        mm(pd, hT[b][:, f, s * 128:(s + 1) * 128], wdb[b][:, f, half * 512:(half + 1) * 512],
                           f == 0, f == 3, [ThT[b], Twd[b]], [Tpd])
                    if half == 0:
                        act(yy[:, 0:512], pd, AF.Copy, [Tpd, Tsli[b4]], [Tyy], scale=sli[b4][:, s, 1:2])
                    else:
                        ts("dve", yy[:, 512:1024], pd, sli[b4][:, s, 1:2], ALU.mult, [Tpd, Tsli[b4]], [Tyy])
                ys.append((yy, Tyy, s))
            if ex + 1 < n_experts:
                casts(ex + 1)
            for yy, Tyy, s in ys:
                P.dma("pool", lambda e, b4=b4, s=s, yy=yy: e.indirect_dma_start(
                    out=hacc, out_offset=bass.IndirectOffsetOnAxis(ap=tki[b4][:, s:s + 1], axis=0),
                    in_=yy, in_offset=None, compute_op=ALU.add),
                    "ysc", reads=[Ttki[b4], Tyy], writes=[Thacc])

        rs2 = Alloc(RS20, RS21)
        fn = rs2.get(1024, F32)
        Tfn = T("fn")
        hb = [rs2.get(1024, F32) for _ in range(3)]
        Thb = [T("hb0"), T("hb1"), T("hb2")]
        ob = [rs2.get(1024, F32) for _ in range(2)]
        Tob = [T("ob0"), T("ob1")]
        Tout = T("out")
        P.barrier()
        P.dma("sp", lambda e: e.dma_start(out=fn, in_=fnd), "fn", writes=[Tfn])
        for i in range(16):
            j = i % 2
            j3 = i % 3
            P.dma("sp", lambda e, i=i, j3=j3: e.dma_start(out=hb[j3], in_=hacc[i * 128:(i + 1) * 128, :]), f"hb{j3}",
                  reads=[Thacc], writes=[Thb[j3]])
            sc_, Tsc_ = rmsnorm_stats(hb[j3], ob[j], 128, [Thb[j3]], Tob[j])
            stt("dve", ob[j], hb[j3], sc_[:, 2:3], fn, ALU.mult, ALU.mult, [Thb[j3], Tsc_, Tfn], [Tob[j]])
            P.dma("sp", lambda e, i=i, j=j: e.dma_start(out=out[i * 128:(i + 1) * 128, :], in_=ob[j]), f"out{j}",
                  reads=[Tob[j]], writes=[Tout])
        P.barrier(engs=("sp",))
        P.emit(st)
    return nc


_NC_CACHE = {}


def make_inputs(inputs):
    x = np.asarray(inputs["x"], np.float32)
    in_maps = []
    shared = {
        "w_in": np.ascontiguousarray(np.asarray(inputs["w_in"], np.float32)[0]),
        "w_out": np.ascontiguousarray(np.asarray(inputs["w_out"], np.float32)[0]),
        "w_gate": np.ascontiguousarray(np.asarray(inputs["w_gate"], np.float32)[0]),
        "w_up": np.ascontiguousarray(np.asarray(inputs["w_up"], np.float32)[0]),
        "w_down": np.ascontiguousarray(np.asarray(inputs["w_down"], np.float32)[0]),
        "fnorm": np.ascontiguousarray(np.broadcast_to(np.asarray(inputs["final_norm"], np.float32)[None, :], (128, D))),
        "gfb": np.ascontiguousarray(np.broadcast_to(np.asarray(inputs["norm_ffn"], np.float32)[0][None, :], (128, D))),
    }
    inp_np = {k: np.asarray(v, np.float32) for k, v in inputs.items() if k not in ("x", "w_in", "w_out", "w_gate", "w_up", "w_down")}
    for c in range(NCORES):
        b, s = c // 4, c % 4
        start = s * SPAN
        lo = start - NPREV * SPAN - HALO
        xe = np.zeros((NTOK_EXT, D), np.float32)
        src_lo = max(lo, 0)
        xe[src_lo - lo:, :] = x[b, src_lo:start + SPAN, :]
        m = dict(shared)
        m["xe"] = xe
        m["cp"] = make_cp(inp_np, c)
        in_maps.append(m)
    return in_maps


def kernel(**inputs):
    if "nc" not in _NC_CACHE:
        _NC_CACHE["nc"] = build()
    nc = _NC_CACHE["nc"]
    in_maps = make_inputs(inputs)
    res = run_bass_kernel_spmd(nc, in_maps, core_ids=list(range(NCORES)))
    outs = [np.asarray(res.results[c]["out"], np.float32) for c in range(NCORES)]
    full = np.stack(outs, 0).reshape(2, 4 * SPAN, D)
    return full
```

```python
from contextlib import ExitStack
import numpy as np
import concourse.bass as bass
import concourse.mybir as mybir
from concourse.bass_utils import run_bass_kernel_spmd

F32 = mybir.dt.float32
BF16 = mybir.dt.bfloat16
ALU = mybir.AluOpType
AF = mybir.ActivationFunctionType
AX = mybir.AxisListType

ENGS = ("pe", "act", "dve", "pool", "sp")
SEM_LIMIT = 12000
NCORES = 8
SPAN = 2048
NPREV = 3
HALO = 8
NTOK_EXT = HALO + (NPREV + 1) * SPAN
D = 1024
EPS = 1e-6
MOE_CAP = 256
I32 = mybir.dt.int32


class T:
    __slots__ = ("name", "w", "r", "rd")

    def __init__(self, name=""):
        self.name = name
        self.w = None
        self.r = {}
        self.rd = []


class Prog:
    def __init__(self, nc):
        self.nc = nc
        self.ops = []
        self.dma_sems = {}
        self.last = {}
        self.dmas = []

    def _deps(self, reads, writes):
        deps = set()
        raw = set()
        for t in reads:
            if t.w is not None:
                deps.add(t.w)
                raw.add(t.w)
        for t in writes:
            if t.w is not None:
                deps.add(t.w)
            deps.update(t.r.values())
            deps.update(t.rd)
        self._last_raw = raw
        return deps

    def op(self, eng, fn, reads=(), writes=()):
        i = len(self.ops)
        self.ops.append(dict(eng=eng, fn=fn, deps=self._deps(reads, writes), kind="c"))
        self.ops[-1]["raw"] = self._last_raw
        self.last[eng] = i
        for t in reads:
            t.r[eng] = i
        for t in writes:
            t.w = i
            t.r = {}
            t.rd = []
        return i

    def dma(self, eng, fn, sem, reads=(), writes=(), inc=16):
        i = len(self.ops)
        n = self.dma_sems.get(sem, 0) + inc
        self.dma_sems[sem] = n
        self.ops.append(dict(eng=eng, fn=fn, deps=self._deps(reads, writes), kind="d", sem=sem, val=n, inc=inc))
        self.dmas.append(i)
        for t in reads:
            t.rd.append(i)
        for t in writes:
            t.w = i
            t.r = {}
            t.rd = []
        return i

    def barrier(self, engs=ENGS):
        deps = set(self.last.values()) | set(self.dmas)
        self.dmas = []
        for e in engs:
            self.ops.append(dict(eng=e, fn=None, deps=set(deps), kind="c"))

    def emit(self, stack):
        nc = self.nc
        ops = self.ops
        needed = set()
        for o in ops:
            for d in o["deps"]:
                if ops[d]["kind"] == "c":
                    needed.add(d)
        cnt = {e: 0 for e in ENGS}
        for i, o in enumerate(ops):
            if o["kind"] == "c" and i in needed and o["fn"] is not None:
                cnt[o["eng"]] += 1
                o["cnt"] = cnt[o["eng"]]
        esems = {}
        for e in ENGS:
            n = cnt[e] // SEM_LIMIT + 1
            esems[e] = [stack.enter_context(nc.semaphore(f"s_{e}{k}")) for k in range(n)]
        dsems = {k: stack.enter_context(nc.semaphore(f"d_{k}")) for k in self.dma_sems}
        per = {e: [] for e in ENGS}
        for i, o in enumerate(ops):
            per[o["eng"]].append(i)

        def section(e):
            def body(eng):
                seen = {}
                for i in per[e]:
                    o = ops[i]
                    w = {}
                    for d in o["deps"]:
                        od = ops[d]
                        if od["kind"] == "c":
                            if od["fn"] is None or "cnt" not in od:
                                continue
                            if od["eng"] == e and e == "pe":
                                continue
                            if od["eng"] == e and "raw" in o and d not in o["raw"]:
                                continue
                            c = od["cnt"]
                            key = ("e", od["eng"], (c - 1) // SEM_LIMIT)
                            val = (c - 1) % SEM_LIMIT + 1
                        else:
                            key = ("d", od["sem"])
                            val = od["val"]
                        if w.get(key, 0) < val:
                            w[key] = val
                    for key, val in sorted(w.items(), key=lambda kv: str(kv[0])):
                        if seen.get(key, 0) >= val:
                            continue
                        seen[key] = val
                        if key[0] == "e":
                            sem = esems[key[1]][key[2]]
                            for kk in range(key[2]):
                                seen[("e", key[1], kk)] = SEM_LIMIT
                        else:
                            sem = dsems[key[1]]
                        eng.wait_ge(sem, val)
                    if o["fn"] is None:
                        continue
                    ins = o["fn"](eng)
                    if o["kind"] == "c":
                        if "cnt" in o:
                            c = o["cnt"]
                            ins.then_inc(esems[e][(c - 1) // SEM_LIMIT], 1)
                    else:
                        ins.then_inc(dsems[o["sem"]], o["inc"])
            return body

        with nc.Block() as block:
            block.tensor(section("pe"))
            block.scalar(section("act"))
            block.vector(section("dve"))
            block.gpsimd(section("pool"))
            block.sync(section("sp"))


CP = {}
_o = 0
for _n, _w in (("ident", 128), ("tri", 128), ("lstrict", 128), ("ones", 128), ("bd64", 128),
               ("gmix", 8), ("gffn", 8), ("gain16", 16), ("cwssd", 48), ("cbssd", 12), ("cwsc", 24),
               ("dtb", 16), ("alog", 16), ("dsk", 16), ("wr", 288), ("bmask", 4), ("ecap", 32), ("tokid", 16)):
    CP[_n] = (_o, _w)
    _o += _w
NCP = _o


def make_cp(inp, core):
    cp = np.zeros((128, NCP), np.float32)

    def put(name, arr):
        o, w = CP[name]
        cp[:, o:o + w] = np.asarray(arr, np.float32).reshape(128, w)

    idx = np.arange(128)
    put("ident", np.eye(128))
    put("tri", (idx[:, None] <= idx[None, :]))
    put("lstrict", (idx[:, None] > idx[None, :]))
    put("ones", np.ones((128, 128)))
    put("bd64", (idx[:, None] // 64 == idx[None, :] // 64))
    put("gmix", inp["norm_mix"][0].reshape(8, 128).T)
    put("gffn", inp["norm_ffn"][0].reshape(8, 128).T)
    put("gain16", np.concatenate([inp["ssd_norm"][0].reshape(8, 128).T, inp["sc_norm"][0].reshape(8, 128).T], 1))
    put("cwssd", inp["ssd_conv_w"][0].reshape(4, 12, 128).transpose(2, 1, 0))
    put("cbssd", inp["ssd_conv_b"][0].reshape(12, 128).T)
    put("cwsc", inp["sc_conv_w"][0].reshape(3, 8, 128).transpose(2, 1, 0))
    put("dtb", np.broadcast_to(inp["dt_bias"][0][None, :], (128, 16)))
    put("alog", np.broadcast_to(inp["a_log"][0][None, :], (128, 16)))
    put("dsk", np.broadcast_to(inp["d_skip"][0][None, :], (128, 16)))
    wr = np.concatenate([inp["w_router_group"][0],
                         inp["w_router_expert"][0].transpose(1, 0, 2).reshape(1024, 32)], 1)
    put("wr", wr.reshape(8, 128, 36).transpose(1, 0, 2))
    s = core % 4
    put("bmask", np.broadcast_to(np.array([1.0 if b >= NPREV - s else 0.0 for b in range(NPREV + 1)],
                                          np.float32)[None, :], (128, 4)))
    put("ecap", np.broadcast_to((np.arange(32, dtype=np.float32) * MOE_CAP)[None, :], (128, 32)))
    put("tokid", (np.arange(16)[None, :] * 128 + np.arange(128)[:, None]).astype(np.float32))
    return cp


def build(dbg=False, stop_after=None, n_experts=32):
    nc = bass.Bass("TRN2", target_bir_lowering=False)

    def dram(name, shape, kind="ExternalInput"):
        return nc.dram_tensor(name, shape, F32, kind=kind).ap()

    xe = dram("xe", [NTOK_EXT, D])
    cpd = dram("cp", [128, NCP])
    fnd = dram("fnorm", [128, D])
    gfd = dram("gfb", [128, D])
    w_in = dram("w_in", [D, 5648])
    w_out = dram("w_out", [2048, D])
    w_gate = dram("w_gate", [32, D, 512])
    w_up = dram("w_up", [32, D, 512])
    w_down = dram("w_down", [32, 512, D])
    out = dram("out", [SPAN, D], kind="ExternalOutput")
    dbg_out = {}

    P = Prog(nc)
    st = ExitStack()
    with st:
        ARENA_N = 53200
        arena = st.enter_context(nc.sbuf_tensor("arena", [128, ARENA_N], F32))
        pbt = [st.enter_context(nc.psum_tensor(f"pb{i}", [128, 512], F32)) for i in range(7)]
        pTb = st.enter_context(nc.psum_tensor("pTb", [128, 8, 128], BF16))
        pb = [t[:] for t in pbt]
        Tpb = [T(f"pb{i}") for i in range(7)]
        TpTb = T("pTb")

        a_f32 = arena[:]
        a_bf = arena[:].bitcast(BF16)
        a_i32 = arena[:].bitcast(mybir.dt.int32)

        class Alloc:
            def __init__(self, start, end):
                self.p = start
                self.end = end

            def get(self, n_elems, dt, shape=None):
                nb = n_elems * (2 if dt == BF16 else 4)
                nb_al = (nb + 63) // 64 * 64
                off = self.p
                assert off + nb_al <= self.end, ("arena overflow", off, nb_al, self.end)
                self.p += nb_al
                if dt == F32:
                    ap = a_f32[:, off // 4: off // 4 + n_elems]
                elif dt == mybir.dt.int32:
                    ap = a_i32[:, off // 4: off // 4 + n_elems]
                else:
                    ap = a_bf[:, off // 2: off // 2 + n_elems]
                if shape is not None and len(shape) == 2:
                    ap = ap.rearrange("p (a b) -> p a b", b=shape[1])
                return ap

        PERS0 = 0
        PERS1 = 13824
        RX0, RX1 = PERS1, PERS1 + 32768
        RH0, RH1 = RX1, RX1 + 65792
        RBC0, RBC1 = RH1, RH1 + 16384
        RSC0, RSC1 = RBC1, RBC1 + 32768
        RS20, RS21 = RSC1, ARENA_N * 4

        pers = Alloc(PERS0, PERS1)
        cp = pers.get(NCP, F32)
        Tcp = T("cp")

        def cpv(name, shape=None):
            o, w = CP[name]
            ap = cp[:, o:o + w]
            if shape is not None:
                ap = ap.rearrange("p (a b) -> p a b", b=shape[1])
            return ap

        identb = pers.get(128, BF16)
        bd64b = pers.get(128, BF16)
        Sst = pers.get(1024, F32)
        Sb = pers.get(1024, BF16)
        dtt = pers.get(256, F32, (16, 16))
        adt = pers.get(256, F32, (16, 16))
        avec = pers.get(16, F32)
        epsc = pers.get(1, F32)
        pers_stats = pers.get(64, F32)
        Tconst = T("const")
        TS_, TSb, Tdt, Tstats = T("S"), T("Sb"), T("dt"), T("stats")

        def act(out_, in_, func, reads, writes, **kw):
            P.op("act", lambda e: e.activation(out=out_, in_=in_, func=func, **kw), reads, writes)

        def tt(eng, out_, in0, in1, op, reads, writes):
            P.op(eng, lambda e: e.tensor_tensor(out=out_, in0=in0, in1=in1, op=op), reads, writes)

        def ts(eng, out_, in0, s1, op0, reads, writes, s2=None, op1=None):
            if op1 is None:
                P.op(eng, lambda e: e.tensor_scalar(out=out_, in0=in0, scalar1=s1, scalar2=None, op0=op0), reads, writes)
            else:
                P.op(eng, lambda e: e.tensor_scalar(out=out_, in0=in0, scalar1=s1, scalar2=s2, op0=op0, op1=op1), reads, writes)

        def stt(eng, out_, in0, scalar, in1, op0, op1, reads, writes):
            P.op(eng, lambda e: e.scalar_tensor_tensor(out=out_, in0=in0, scalar=scalar, in1=in1, op0=op0, op1=op1), reads, writes)

        def cpy(eng, out_, in_, reads, writes):
            if eng == "act":
                P.op("act", lambda e: e.copy(out=out_, in_=in_), reads, writes)
            else:
                P.op(eng, lambda e: e.tensor_copy(out=out_, in_=in_), reads, writes)

        def mm(out_, lhsT, rhs, start, stop, reads, writes):
            P.op("pe", lambda e: e.matmul(out_, lhsT=lhsT, rhs=rhs, start=start, stop=stop), reads, writes)

        def tr(out_, in_, ident, reads, writes):
            P.op("pe", lambda e: e.transpose(out=out_, in_=in_, identity=ident), reads, writes)

        stat_tiles = [pers_stats[:, 8 * q: 8 * q + 8] for q in range(8)]
        Tstat_tiles = [T(f"stat{q}") for q in range(8)]
        stc = [0]

        def stat_slot():
            q = stc[0] % 8
            stc[0] += 1
            return stat_tiles[q], Tstat_tiles[q]

        def rmsnorm_stats(x_ap, junk_ap, n, reads, Tjunk):
            sl_, Tsl_ = stat_slot()
            act(junk_ap, x_ap, AF.Square, reads, [Tjunk, Tsl_], accum_out=sl_[:n, 0:1])
            act(sl_[:n, 1:2], sl_[:n, 0:1], AF.Ln, [Tsl_, Tconst], [Tsl_], bias=epsc[:n, 0:1], scale=1.0 / D)
            act(sl_[:n, 2:3], sl_[:n, 1:2], AF.Exp, [Tsl_], [Tsl_], scale=-0.5)
            return sl_, Tsl_

        Tdump = T("dump")

        def dump(name, ap, shp, dt):
            if not dbg:
                return
            d = nc.dram_tensor("dbg_" + name, [128] + shp, dt, kind="ExternalOutput").ap()
            P.barrier()
            P.dma("sp", lambda e, d=d, ap=ap: e.dma_start(out=d, in_=ap), "dump", writes=[Tdump])
            P.barrier()

        P.dma("sp", lambda e: e.dma_start(out=cp, in_=cpd), "cp", writes=[Tcp])
        P.op("pool", lambda e: e.memset(epsc, EPS), writes=[Tconst])
        cpy("dve", identb, cpv("ident"), [Tcp], [Tconst])
        cpy("dve", bd64b, cpv("bd64"), [Tcp], [Tconst])
        act(avec, cpv("alog"), AF.Exp, [Tcp], [Tconst])
        ts("dve", avec, avec, -1.0, ALU.mult, [Tconst], [Tconst])
        P.op("pool", lambda e: e.memset(Sst, 0.0), writes=[TS_])

        rx = Alloc(RX0, RX1)
        xsT = rx.get(8 * 2048, BF16, (8, 2048))
        TxsT = [T(f"xsT{i}") for i in range(16)]
        rh = Alloc(RH0, RH1)
        siluz = rh.get(16 * 1024, BF16, (16, 1024))
        Tsiluz = [T(f"sz{i}") for i in range(16)]
        hnT = rh.get(8 * 2056, BF16, (8, 2056))
        ThnT = [T(f"hnT{i}") for i in range(17)]
        rbc = Alloc(RBC0, RBC1)
        BT = rbc.get(2 * 2048, BF16, (2, 2048))
        CT = rbc.get(2 * 2048, BF16, (2, 2048))
        TBT, TCT = T("BT"), T("CT")

        rs2 = Alloc(RS20, RS21)
        wts = [rs2.get(8 * 256, BF16, (8, 256)) for _ in range(4)]
        Twts = [T(f"wt{i}") for i in range(4)]
        U = [rs2.get(2056, BF16) for _ in range(3)]
        TU = [T(f"U{i}") for i in range(3)]
        xt = [rs2.get(1024, F32) for _ in range(2)]
        Txt = [T(f"xt{i}") for i in range(2)]
        dg = [rs2.get(4 * 128, BF16, (4, 128)) for _ in range(2)]
        Tdg = [T("dg0"), T("dg1")]
        acs_all = rs2.get(512, F32)
        dd_all = rs2.get(256, F32, (16, 16))
        ed_all = rs2.get(256, F32, (16, 16))
        eq_all = rs2.get(256, F32, (16, 16))
        ea_all = rs2.get(256, F32, (16, 16))
        LT = rs2.get(256, F32, (16, 16))
        wgt_all = rs2.get(256, F32, (16, 16))
        Tcum = T("cum")
        rsc = Alloc(RSC0, RSC1)
        xn = [rsc.get(1024, BF16) for _ in range(2)]
        Txn = [T(f"xn{i}") for i in range(2)]
        wdt = rsc.get(8 * 16, BF16, (8, 16))
        Twdt = T("wdt")
        dtmp = rsc.get(256, F32)
        Tdtmp = T("dtmp")
        xdt = [rsc.get(1024, BF16) for _ in range(2)]
        Txdt = [T(f"xdt{i}") for i in range(2)]
        xdtdec = rsc.get(1024, BF16)
        Txdtdec = T("xdtdec")
        xsD_ = [rsc.get(1024, BF16), rs2.get(1024, BF16)]
        TxsD_ = [T("xsD0"), T("xsD1")]
        Btok = [rsc.get(256, BF16) for _ in range(2)]
        TBtok = [T(f"Btok{i}") for i in range(2)]
        CBm = rsc.get(256, F32, (2, 128))
        TCBm = T("CBm")
        Rb = [rsc.get(512, F32, (4, 128)) for _ in range(2)]
        TRb = [T(f"R{i}") for i in range(2)]
        Eb = [rsc.get(512, BF16, (4, 128)) for _ in range(2)]
        TEb = [T(f"E{i}") for i in range(2)]
        MT = rsc.get(16 * 128, BF16, (16, 128))
        TMT = [T(f"MT{i}") for i in range(4)]
        ybuf = [rsc.get(1024, F32), a_f32[:, RSC0 // 4: RSC0 // 4 + 1024]]
        Tybuf = [T("y0"), T("y1")]
        ygn = rsc.get(1024, BF16)
        Tygn = T("ygn")

        pTs = [pTb[:], pb[6].bitcast(BF16).rearrange("p (k t) -> p k t", t=128),
               pb[3].bitcast(BF16).rearrange("p (k t) -> p k t", t=128),
               pb[2].bitcast(BF16).rearrange("p (k t) -> p k t", t=128)]
        TpTs = [TpTb, Tpb[6], Tpb[3], Tpb[2]]
        ptc = [0]
        pt_depth = [4]

        def next_pT():
            j = ptc[0] % pt_depth[0]
            ptc[0] += 1
            return pTs[j], TpTs[j]

        wctr = [0]

        def load_w(cols0, ncols):
            j = wctr[0] % 4
            wctr[0] += 1
            dst = wts[j][:, :, 0:ncols]
            src = w_in[:, cols0:cols0 + ncols].rearrange("(k p) c -> p k c", p=128)
            P.dma("pool", lambda e: e.dma_start(out=dst, in_=src), f"wt{j}", writes=[Twts[j]])
            return wts[j], Twts[j]

        xctr = [0]
        uctr = [0]
        dgc = [0]
        cvc = [0]

        def next_u():
            j = uctr[0] % 3
            uctr[0] += 1
            return U[j], TU[j]

        gmix_b = cpv("gmix").unsqueeze(2)
        identf = cpv("ident")

        def hn_tiles(tt4):
            return ThnT[1 + 4 * tt4: 5 + 4 * tt4]

        def proj_chunk(wt_ap, Tw, lc):
            for k in range(8):
                mm(pb[4][:, 0:HALO], wt_ap[:, k, lc * 128:(lc + 1) * 128], hnT[:, k, 0:HALO], k == 0, k == 7,
                   [Tw, ThnT[0]], [Tpb[4]])
            for t4 in range(4):
                for k in range(8):
                    mm(pb[t4], wt_ap[:, k, lc * 128:(lc + 1) * 128],
                       hnT[:, k, HALO + t4 * 512: HALO + (t4 + 1) * 512], k == 0, k == 7,
                       [Tw] + hn_tiles(t4), [Tpb[t4]])

        def evac_to_u(u, Tu, all_act=False):
            cpy("act", u[:, 0:HALO], pb[4][:, 0:HALO], [Tpb[4]], [Tu])
            for t4 in range(4):
                cpy("act" if (t4 % 2 == 0 or all_act) else "dve", u[:, HALO + t4 * 512: HALO + (t4 + 1) * 512], pb[t4],
                    [Tpb[t4]], [Tu])

        cacc = [a_f32[:, RH0 // 4 + q * 2048: RH0 // 4 + (q + 1) * 2048] for q in range(2)]
        Tcacc = [T("cacc0"), T("cacc1")]

        def make_diag(wcol, ntap):
            j = dgc[0] % 2
            dgc[0] += 1
            for jt in range(ntap):
                act(dg[j][:, jt, :], identf, AF.Copy, [Tcp], [Tdg[j]], scale=wcol(jt))
            return dg[j], Tdg[j]

        def conv_pe_bank():
            j = 5 + cvc[0] % 2
            cvc[0] += 1
            return pb[j], Tpb[j]

        def conv_pe(u, Tu, d, Td, ntap, t4):
            j = 5 + cvc[0] % 2
            cvc[0] += 1
            base = HALO - (ntap - 1) + t4 * 512
            for jt in range(ntap):
                mm(pb[j], d[:, jt, :], u[:, base + jt: base + jt + 512], jt == 0, jt == ntap - 1, [Td, Tu], [Tpb[j]])
            return pb[j], Tpb[j]

        a1_state = {}

        def a1_front(blk_, ti):
            row0_ = HALO + blk_ * SPAN
            n = HALO if ti == 0 else 128
            r0 = row0_ - HALO if ti == 0 else row0_ + (ti - 1) * 128
            j = xctr[0] % 2
            xctr[0] += 1
            xtj, xnj = xt[j], xn[j]
            P.dma("sp", lambda e, xtj=xtj, r0=r0, n=n: e.dma_start(out=xtj[:n, :], in_=xe[r0:r0 + n, :]),
                  f"xt{j}", writes=[Txt[j]])
            sc_, Tsc_ = rmsnorm_stats(xtj[:n, :], xnj[:n, :], n, [Txt[j]], Txn[j])
            act(xnj[:n, :], xtj[:n, :], AF.Copy, [Txt[j], Tsc_], [Txn[j]], scale=sc_[:n, 2:3])
            a1_state[(blk_, ti)] = (j, n)

        def a1_back(blk_, ti):
            j, n = a1_state.pop((blk_, ti))
            xnj = xn[j]
            c0 = 0 if ti == 0 else HALO + (ti - 1) * 128
            pT, TpT = next_pT()
            for k in range(8):
                tr(pT[:, k, :n], xnj[:n, k * 128:(k + 1) * 128], identb[:n, :n], [Txn[j], Tconst], [TpT])
            tt("dve", hnT[:, :, c0:c0 + n], pT[:, :, :n], gmix_b.to_broadcast([128, 8, n]),
               ALU.mult, [TpT, Tcp], [ThnT[ti]])

        a1_front(0, 0)
        for ti in range(17):
            if ti + 1 < 17:
                a1_front(0, ti + 1)
            a1_back(0, ti)

        for blk in range(NPREV + 1):
            own = blk == NPREV
            row0 = HALO + blk * SPAN
            cwssd = cpv("cwssd")
            cbssd = cpv("cbssd")
            conv_chunks = list(range(8)) + [8, 9] + ([10, 11] if own else [])

            def conv_silu(c, u, Tu):
                acc, Tacc = cacc[c % 2], Tcacc[c % 2]
                base = HALO - 3
                ts("dve", acc, u[:, base:base + SPAN], cwssd[:, c * 4:c * 4 + 1], ALU.mult, [Tu, Tcp], [Tacc])
                for jt in range(1, 4):
                    stt("dve", acc, u[:, base + jt:base + jt + SPAN], cwssd[:, c * 4 + jt:c * 4 + jt + 1], acc,
                        ALU.mult, ALU.add, [Tu, Tcp, Tacc], [Tacc])
                if c < 8:
                    dst, Tdst = xsT[:, c, :], TxsT
                elif c < 10:
                    dst, Tdst = BT[:, c - 8, :], [TBT]
                else:
                    dst, Tdst = CT[:, c - 10, :], [TCT]
                act(dst, acc, AF.Silu, [Tacc, Tcp], Tdst, bias=cbssd[:, c:c + 1])

            pending = None
            for g0 in range(0, len(conv_chunks), 2):
                cc0 = conv_chunks[g0]
                wt_ap, Tw = load_w(1024 + cc0 * 128, 256)
                for lc in range(2):
                    c = cc0 + lc
                    proj_chunk(wt_ap, Tw, lc)
                    u, Tu = next_u()
                    evac_to_u(u, Tu, all_act=True)
                    if pending is not None:
                        conv_silu(*pending)
                    pending = (c, u, Tu)
            conv_silu(*pending)

            src = w_in[:, 2560:2576].rearrange("(k p) c -> p k c", p=128)
            P.dma("pool", lambda e, src=src: e.dma_start(out=wdt, in_=src), "wdt", writes=[Twdt])
            for i in range(16):
                for k in range(8):
                    mm(pb[4][:, 16 + i * 16: 32 + i * 16], hnT[:, k, HALO + i * 128: HALO + (i + 1) * 128], wdt[:, k, :],
                       k == 0, k == 7, [ThnT[1 + i], Twdt], [Tpb[4]])
            dtb_b = cpv("dtb").unsqueeze(1).to_broadcast([128, 16, 16])
            dt3 = dtmp.rearrange("p (a b) -> p a b", b=16)
            tt("dve", dt3, pb[4][:, 16:272].rearrange("p (a b) -> p a b", b=16), dtb_b, ALU.add, [Tpb[4], Tcp], [Tdtmp])
            act(dtmp, dtmp, AF.Exp, [Tdtmp], [Tdtmp])
            act(dtmp, dtmp, AF.Ln, [Tdtmp], [Tdtmp], bias=1.0)
            bm = cpv("bmask")
            ts("dve", dtt, dt3, bm[:, blk:blk + 1], ALU.mult, [Tdtmp, Tcp], [Tdt])
            tt("dve", adt, dtt, avec.unsqueeze(1).to_broadcast([128, 16, 16]), ALU.mult, [Tdt, Tconst], [Tdt])
            adt_f = adt.rearrange("p a b -> p (a b)")
            mm(pb[4][:, 0:256], cpv("tri"), adt_f, True, True, [Tcp, Tdt], [Tpb[4]])
            mm(pb[4][:, 256:512], cpv("ones"), adt_f, True, True, [Tcp, Tdt], [Tpb[4]])
            cpy("act", acs_all, pb[4], [Tpb[4]], [Tcum])
            ac3 = acs_all[:, 0:256].rearrange("p (a b) -> p a b", b=16)
            aq3 = acs_all[:, 256:512].rearrange("p (a b) -> p a b", b=16)
            tt("dve", dd_all, aq3, ac3, ALU.subtract, [Tcum], [Tcum])

            tri = cpv("tri")
            if not own:
                P.op("pool", lambda e: e.memset(LT[:, 15, :], 0.0), [Tcum], [Tcum])
                for i in range(14, -1, -1):
                    tt("pool", LT[:, i, :], LT[:, i + 1, :], aq3[:, i + 1, :], ALU.add, [Tcum], [Tcum])
                tt("dve", dd_all, dd_all, LT, ALU.add, [Tcum], [Tcum])
                act(ed_all, dd_all, AF.Exp, [Tcum], [Tcum])
                tt("dve", wgt_all, ed_all, dtt, ALU.mult, [Tcum, Tdt], [Tcum])
                tt("pool", eq_all[:, 0, :], LT[:, 0, :], aq3[:, 0, :], ALU.add, [Tcum], [Tcum])
                act(eq_all[:, 0, :], eq_all[:, 0, :], AF.Exp, [Tcum], [Tcum])
                for i in range(16):
                    tok = slice(i * 128, (i + 1) * 128)
                    xd, Txd = xdt[i % 2], Txdt[i % 2]
                    bt, Tbt = Btok[i % 2], TBtok[i % 2]
                    pT, TpT = next_pT()
                    for c in range(8):
                        tr(pT[:, c, :], xsT[:, c, tok], identb, [TxsT[i], Tconst], [TpT])
                    pX = pT.rearrange("p c t -> p (c t)").rearrange("p (h d) -> p h d", d=64)
                    tt("dve", xd.rearrange("p (h d) -> p h d", d=64), pX,
                       wgt_all[:, i, :].unsqueeze(2).to_broadcast([128, 16, 64]), ALU.mult, [TpT, Tcum], [Txd])
                    pT2, TpT2 = next_pT()
                    for g in range(2):
                        tr(pT2[:, g, :], BT[:, g, tok], identb, [TBT, Tconst], [TpT2])
                    cpy("act", bt.rearrange("p (g n) -> p g n", n=128), pT2[:, 0:2, :], [TpT2], [Tbt])
                    for g in range(2):
                        mm(pb[g], bt[:, g * 128:(g + 1) * 128], xd[:, g * 512:(g + 1) * 512], i == 0, i == 15,
                           [Tbt, Txd], [Tpb[g]])
                    if i == 0:
                        a1_front(blk + 1, 0)
                    a1_front(blk + 1, i + 1)
                    a1_back(blk + 1, i)
                a1_back(blk + 1, 16)
                tt("pool", Sst.rearrange("p (h d) -> p h d", d=64), Sst.rearrange("p (h d) -> p h d", d=64),
                   eq_all[:, 0, :].unsqueeze(2).to_broadcast([128, 16, 64]), ALU.mult, [TS_, Tcum], [TS_])
                for g in range(2):
                    tt("dve", Sst[:, g * 512:(g + 1) * 512], Sst[:, g * 512:(g + 1) * 512], pb[g], ALU.add,
                       [TS_, Tpb[g]], [TS_])
                continue

            act(ed_all, dd_all, AF.Exp, [Tcum], [Tcum])
            act(eq_all, aq3, AF.Exp, [Tcum], [Tcum])
            act(ea_all, ac3, AF.Exp, [Tcum], [Tcum])
            for q in range(4):
                wt_ap, Tw = load_w(q * 256, 256)
                for i in range(16):
                    pz, Tpz = pb[5 + i % 2], Tpb[5 + i % 2]
                    for k in range(8):
                        mm(pz[:, 0:256], hnT[:, k, HALO + i * 128: HALO + (i + 1) * 128], wt_ap[:, k, :], k == 0, k == 7,
                           [ThnT[1 + i], Tw], [Tpz])
                    act(siluz[:, i, q * 256:(q + 1) * 256], pz[:, 0:256], AF.Silu, [Tpz], [Tsiluz[i], Tcacc[0], Tcacc[1]])
            cpy("act", Sb, Sst, [TS_], [TSb])

            pt_depth[0] = 2
            xdtdec_ = [xdtdec, U[2][:, 0:1024]]
            Txdtdec_ = [Txdtdec, T("xdtdec1")]
            MT_ = [MT, U[1][:, 0:2048].rearrange("p (h l) -> p h l", l=128)]
            TMT_ = [TMT, [T(f"MTb{q}") for q in range(4)]]

            def stageA(i):
                tok = slice(i * 128, (i + 1) * 128)
                xd, Txd = xdt[i % 2], Txdt[i % 2]
                bt, Tbt = Btok[i % 2], TBtok[i % 2]
                xsD, TxsD = xsD_[i % 2], TxsD_[i % 2]
                xdd, Txdd = xdtdec_[i % 2], Txdtdec_[i % 2]
                pT, TpT = next_pT()
                for c in range(8):
                    tr(pT[:, c, :], xsT[:, c, tok], identb, [TxsT[i], Tconst], [TpT])
                pX = pT.rearrange("p c t -> p (c t)").rearrange("p (h d) -> p h d", d=64)
                tt("dve", xd.rearrange("p (h d) -> p h d", d=64), pX, dtt[:, i, :].unsqueeze(2).to_broadcast([128, 16, 64]),
                   ALU.mult, [TpT, Tdt], [Txd])
                tt("dve", xsD.rearrange("p (h d) -> p h d", d=64), pX,
                   cpv("dsk").unsqueeze(2).to_broadcast([128, 16, 64]), ALU.mult, [TpT, Tcp], [TxsD])
                pT2, TpT2 = next_pT()
                for g in range(2):
                    tr(pT2[:, g, :], BT[:, g, tok], identb, [TBT, Tconst], [TpT2])
                cpy("act", bt.rearrange("p (g n) -> p g n", n=128), pT2[:, 0:2, :], [TpT2], [Tbt])
                tt("dve", xdd.rearrange("p (h d) -> p h d", d=64), xd.rearrange("p (h d) -> p h d", d=64),
                   ed_all[:, i, :].unsqueeze(2).to_broadcast([128, 16, 64]), ALU.mult, [Txd, Tcum], [Txdd])
                pc, Tpc = pb[5], Tpb[5]
                for g in range(2):
                    mm(pc[:, g * 128:(g + 1) * 128], BT[:, g, tok], CT[:, g, tok], True, True, [TBT, TCT], [Tpc])
                tt("dve", CBm, pc[:, 0:256].rearrange("p (g l) -> p g l", l=128),
                   tri.unsqueeze(1).to_broadcast([128, 2, 128]), ALU.mult, [Tpc, Tcp], [TCBm])
                for hq in range(4):
                    g = hq // 2
                    R, TR = Rb[hq % 2], TRb[hq % 2]
                    E, TE = Eb[hq % 2], TEb[hq % 2]
                    pg, Tpg = (pb[4], Tpb[4]) if hq % 2 == 0 else (pb[5], Tpb[5])
                    tt("pool", R, tri.unsqueeze(1).to_broadcast([128, 4, 128]),
                       adt[:, i, hq * 4:(hq + 1) * 4].unsqueeze(2).to_broadcast([128, 4, 128]), ALU.mult,
                       [Tcp, Tdt], [TR])
                    mm(pg, cpv("lstrict"), R.rearrange("p h l -> p (h l)"), True, True, [Tcp, TR], [Tpg])
                    act(E.rearrange("p h l -> p (h l)"), pg, AF.Exp, [Tpg], [TE])
                    tt("dve", MT_[i % 2][:, hq * 4:(hq + 1) * 4, :], E, CBm[:, g, :].unsqueeze(1).to_broadcast([128, 4, 128]),
                       ALU.mult, [TE, TCBm], [TMT_[i % 2][hq]])

            def stageB(i):
                tok = slice(i * 128, (i + 1) * 128)
                bt, Tbt = Btok[i % 2], TBtok[i % 2]
                xdd, Txdd = xdtdec_[i % 2], Txdtdec_[i % 2]
                y, Ty = ybuf[i % 2], Tybuf[i % 2]
                xd, Txd = xdt[i % 2], Txdt[i % 2]
                for g in range(2):
                    py, Tpy = pb[2 + g], Tpb[2 + g]
                    for r in range(8):
                        hh = g * 8 + r
                        mm(py[:, r * 64:(r + 1) * 64], MT_[i % 2][:, hh, :], xd[:, hh * 64:(hh + 1) * 64], True, True,
                           [TMT_[i % 2][hh // 4], Txd], [Tpy])
                for g in range(2):
                    mm(pb[1] if g == 0 else pb[0], CT[:, g, tok], Sb[:, g * 512:(g + 1) * 512], True, True,
                       [TCT, TSb], [Tpb[1] if g == 0 else Tpb[0]])
                for g in range(2):
                    tt("dve", y[:, g * 512:(g + 1) * 512].rearrange("p (h d) -> p h d", d=64),
                       (pb[1] if g == 0 else pb[0]).rearrange("p (h d) -> p h d", d=64),
                       ea_all[:, i, g * 8:(g + 1) * 8].unsqueeze(2).to_broadcast([128, 8, 64]), ALU.mult,
                       [Tpb[1] if g == 0 else Tpb[0], Tcum], [Ty])
                for g in range(2):
                    mm(pb[g], bt[:, g * 128:(g + 1) * 128], xdd[:, g * 512:(g + 1) * 512], True, True,
                       [Tbt, Txdd], [Tpb[g]])
                tt("pool", Sst.rearrange("p (h d) -> p h d", d=64), Sst.rearrange("p (h d) -> p h d", d=64),
                   eq_all[:, i, :].unsqueeze(2).to_broadcast([128, 16, 64]), ALU.mult, [TS_, Tcum], [TS_])
                for g in range(2):
                    tt("dve", Sst[:, g * 512:(g + 1) * 512], Sst[:, g * 512:(g + 1) * 512], pb[g], ALU.add,
                       [TS_, Tpb[g]], [TS_])
                cpy("act", Sb, Sst, [TS_, TSb], [TSb])
                for g in range(2):
                    py, Tpy = pb[2 + g], Tpb[2 + g]
                    tt("dve", y[:, g * 512:(g + 1) * 512], y[:, g * 512:(g + 1) * 512], py, ALU.add, [Ty, Tpy], [Ty])

            def stageC(i):
                tok = slice(i * 128, (i + 1) * 128)
                xsD, TxsD = xsD_[i % 2], TxsD_[i % 2]
                y, Ty = ybuf[i % 2], Tybuf[i % 2]
                tt("dve", y, y, xsD, ALU.add, [Ty, TxsD], [Ty])
                tt("dve", y, y, siluz[:, i, :], ALU.mult, [Ty, Tsiluz[i]], [Ty])
                sc_, Tsc_ = stat_slot()
                for g in range(2):
                    act(ygn[:, g * 512:(g + 1) * 512], y[:, g * 512:(g + 1) * 512], AF.Square, [Ty], [Tygn, Tsc_],
                        accum_out=sc_[:, g:g + 1])
                act(sc_[:, 2:4], sc_[:, 0:2], AF.Ln, [Tsc_, Tconst], [Tsc_], bias=epsc[:, 0:1], scale=1.0 / 512)
                act(sc_[:, 4:6], sc_[:, 2:4], AF.Exp, [Tsc_], [Tsc_], scale=-0.5)
                for g in range(2):
                    act(ygn[:, g * 512:(g + 1) * 512], y[:, g * 512:(g + 1) * 512], AF.Copy, [Ty, Tsc_], [Tygn],
                        scale=sc_[:, 4 + g:5 + g])
                pT3, TpT3 = next_pT()
                for c in range(8):
                    tr(pT3[:, c, :], ygn[:, c * 128:(c + 1) * 128], identb, [Tygn, Tconst], [TpT3])
                cpy("act", xsT[:, :, tok], pT3, [TpT3], [TxsT[i]])

            stageA(0)
            for i in range(16):
                if i + 1 < 16:
                    stageA(i + 1)
                stageB(i)
                stageC(i)

        dump("mixT_ssd", xsT, [8, 2048], BF16)
        dump("S", Sst, [1024], F32)

        P.barrier()
        rsc = Alloc(RSC0, RSC1)
        scT = rsc.get(8 * 2048, BF16, (8, 2048))
        TscT = [T(f"scT{i}") for i in range(16)]
        rbc = Alloc(RBC0, RBC1)
        sq = [rbc.get(512, BF16) for _ in range(2)]
        Tsq = [T("sq0"), T("sq1")]
        rt = [rbc.get(512, F32) for _ in range(2)]
        Trt = [T("rt0"), T("rt1")]
        cvt = [rbc.get(512, F32) for _ in range(2)]
        Tcvt = [T("cvt0"), T("cvt1")]
        scb = [rbc.get(512, F32) for _ in range(2)]
        Tscb = [T("scb0"), T("scb1")]
        cwsc = cpv("cwsc")
        o3 = 1024 + 1536 + 16
        rhs_ = Alloc(RH0, RH0 + 32768)
        scw = [rhs_.get(8 * 256, BF16, (8, 256)) for _ in range(6)]
        Tscw = [T(f"scw{q}") for q in range(6)]

        def load_sc(jp):
            res = []
            for q, c0_ in enumerate((o3 + jp * 256, o3 + 2048 + jp * 256, o3 + 1024 + jp * 256)):
                bi = (jp % 2) * 3 + q
                src_ = w_in[:, c0_:c0_ + 256].rearrange("(k p) c -> p k c", p=128)
                P.dma("pool", lambda e, bi=bi, src_=src_: e.dma_start(out=scw[bi], in_=src_), f"scw{bi}", writes=[Tscw[bi]])
                res.append((scw[bi], Tscw[bi]))
            return res

        sc_loaded = {0: load_sc(0)}
        for jp in range(4):
            if jp + 1 < 4:
                sc_loaded[jp + 1] = load_sc(jp + 1)
            (wb, Twb), (wv, Twv), (wc, Twc) = sc_loaded[jp]
            for lc in range(2):
                j = jp * 2 + lc
                proj_chunk(wb, Twb, lc)
                ub, Tub = next_u()
                evac_to_u(ub, Tub)
                proj_chunk(wv, Twv, lc)
                u, Tu = next_u()
                evac_to_u(u, Tu)
                proj_chunk(wc, Twc, lc)
                tt("dve", u[:, 0:HALO], u[:, 0:HALO], pb[4][:, 0:HALO], ALU.mult, [Tu, Tpb[4]], [Tu])
                for t4 in range(4):
                    sl = slice(HALO + t4 * 512, HALO + (t4 + 1) * 512)
                    tt("dve", u[:, sl], u[:, sl], pb[t4], ALU.mult, [Tu, Tpb[t4]], [Tu])
                d, Td = make_diag(lambda jt, j=j: cwsc[:, j * 3 + jt:j * 3 + jt + 1], 3)
                for t4 in range(4):
                    sl = slice(t4 * 512, (t4 + 1) * 512)
                    slh = slice(HALO + t4 * 512, HALO + (t4 + 1) * 512)
                    b2 = t4 % 2
                    pc_, Tpc_ = conv_pe(u, Tu, d, Td, 3, t4)
                    tt("dve", scb[b2], pc_, ub[:, slh], ALU.mult, [Tpc_, Tub], [Tscb[b2]])
                    act(sq[b2], scb[b2], AF.Square, [Tscb[b2]], [Tsq[b2]])
                    pz, Tpz = conv_pe_bank()
                    mm(pz, bd64b, sq[b2], True, True, [Tconst, Tsq[b2]], [Tpz])
                    act(rt[b2], pz, AF.Ln, [Tpz, Tconst], [Trt[b2]], bias=epsc[:, 0:1], scale=1.0 / 64)
                    act(rt[b2], rt[b2], AF.Exp, [Trt[b2]], [Trt[b2]], scale=-0.5)
                    tt("pool", scT[:, j, sl], scb[b2], rt[b2], ALU.mult, [Tscb[b2], Trt[b2]], TscT[4 * t4:4 * t4 + 4])
        dump("scT", scT, [8, 2048], BF16)

        P.barrier()
        CAP = MOE_CAP
        NS = CAP // 128
        hn2d = nc.dram_tensor("hn2d", [SPAN + 128, D], BF16).ap()
        hacc = nc.dram_tensor("hacc", [SPAN + 128, D], F32).ap()
        listd = nc.dram_tensor("listd", [32 * CAP + 128, 16], F32).ap()
        Thacc, Thn2d, Tlistd = T("hacc"), T("hn2d"), T("listd")
        rh = Alloc(RH0, RH1)
        h = rh.get(16 * 1024, F32, (16, 1024))
        Th = [T(f"h{i}") for i in range(16)]
        rs2 = Alloc(RS20, RS21)
        wo = [rs2.get(16 * 512, BF16, (16, 512)) for _ in range(2)]
        Two = [T("wo0"), T("wo1")]
        xr = [rs2.get(1024, F32) for _ in range(2)]
        Txr = [T("xr0"), T("xr1")]
        RS2_ROUTE_END = rs2.p
        hnb = [rs2.get(1024, BF16) for _ in range(2)]
        Thnb = [T("hnb0"), T("hnb1")]
        gfb = rs2.get(1024, BF16)
        Tgfb = T("gfb")
        L = rs2.get(16 * 36, F32, (16, 36))
        TL = T("L")
        rbc = Alloc(RBC0, RBC1)
        hnf = [rbc.get(1024, F32) for _ in range(2)]
        Thnf = [T("hnf0"), T("hnf1")]
        hTf = [rbc.get(1024, F32, (8, 128)) for _ in range(2)]
        ThTf = [T("hTf0"), T("hTf1")]
        gffn_b = cpv("gffn").unsqueeze(2).to_broadcast([128, 8, 128])
        wr = cpv("wr", (8, 36))
        gain_b = cpv("gain16").unsqueeze(2).to_broadcast([128, 16, 512])
        own_row0 = HALO + NPREV * SPAN
        for half in range(2):
            src = w_out[:, half * 512:(half + 1) * 512].rearrange("(k p) c -> p k c", p=128)
            P.dma("pool", lambda e, src=src, half=half: e.dma_start(out=wo[half], in_=src), f"wo{half}", writes=[Two[half]])
            if half == 0:
                tt("dve", wo[half], wo[half], gain_b, ALU.mult, [Two[half], Tcp], [Two[half]])
            else:
                g16 = cpv("gain16")
                for k in range(16):
                    act(wo[half][:, k, :], wo[half][:, k, :], AF.Copy, [Two[half], Tcp], [Two[half]], scale=g16[:, k:k + 1])
        P.dma("pool", lambda e: e.dma_start(out=gfb, in_=gfd), "gfb", writes=[Tgfb])
        P.op("pool", lambda e: e.memset(hnb[1], 0.0), writes=[Thnb[1]])
        P.dma("sp", lambda e: e.dma_start(out=hn2d[SPAN:SPAN + 128, :], in_=hnb[1]), "hn2dz", reads=[Thnb[1]], writes=[])

        def n2_stage1(i):
            j = i % 2
            P.dma("sp", lambda e, i=i: e.dma_start(out=hacc[i * 128:(i + 1) * 128, :], in_=h[:, i, :]), "hacc0",
                  reads=[Th[i]], writes=[])
            sc_, Tsc_ = rmsnorm_stats(h[:, i, :], hnf[j], 128, [Th[i]], Thnf[j])
            ts("dve", hnf[j], h[:, i, :], sc_[:, 2:3], ALU.mult, [Th[i], Tsc_], [Thnf[j]])
            tt("pool", hnb[j], hnf[j], gfb, ALU.mult, [Thnf[j], Tgfb], [Thnb[j]])
            P.dma("sp", lambda e, i=i, j=j: e.dma_start(out=hn2d[i * 128:(i + 1) * 128, :], in_=hnb[j]), f"hn2d{j}",
                  reads=[Thnb[j]], writes=[])

        def n2_stage2(i):
            j = i % 2
            for hf in range(2):
                pz, Tpz = pb[4 + hf], Tpb[4 + hf]
                for k4 in range(4):
                    k = hf * 4 + k4
                    tr(pz[:, k4 * 128:(k4 + 1) * 128], hnf[j][:, k * 128:(k + 1) * 128], identf, [Thnf[j], Tcp], [Tpz])
                tt("dve", hTf[j][:, hf * 4:(hf + 1) * 4, :], pz.rearrange("p (k t) -> p k t", t=128),
                   gffn_b[:, hf * 4:(hf + 1) * 4, :], ALU.mult, [Tpz, Tcp], [ThTf[j]])

        def n2_stage3(i):
            j = i % 2
            pr, Tpr = pb[6], Tpb[6]
            for k in range(8):
                mm(pr[:, 0:36], hTf[j][:, k, :], wr[:, k, :], k == 0, k == 7, [ThTf[j], Tcp], [Tpr])
            cpy("act", L[:, i, :], pr[:, 0:36], [Tpr], [TL])

        for i in range(16):
            tok = slice(i * 128, (i + 1) * 128)
            pzs = [(pb[(2 * i) % 4], Tpb[(2 * i) % 4]), (pb[(2 * i + 1) % 4], Tpb[(2 * i + 1) % 4])]
            for k in range(16):
                lhs = xsT[:, k, tok] if k < 8 else scT[:, k - 8, tok]
                for half in range(2):
                    mm(pzs[half][0], lhs, wo[half][:, k, :], k == 0, k == 15, [TxsT[i], TscT[i], Two[half]], [pzs[half][1]])
            jx = i % 2
            P.dma("sp", lambda e, jx=jx, i=i: e.dma_start(
                out=xr[jx], in_=xe[own_row0 + i * 128: own_row0 + (i + 1) * 128, :]), f"xr{jx}", writes=[Txr[jx]])
            for half in range(2):
                tt("dve", h[:, i, half * 512:(half + 1) * 512], pzs[half][0], xr[jx][:, half * 512:(half + 1) * 512], ALU.add,
                   [pzs[half][1], Txr[jx]], [Th[i]])
            n2_stage1(i)
            if i >= 1:
                n2_stage2(i - 1)
            if i >= 2:
                n2_stage3(i - 2)
        n2_stage2(15)
        n2_stage3(14)
        n2_stage3(15)
        dump("h", h, [16, 1024], F32)

        P.barrier()
        rs2 = Alloc(RS20, RS2_ROUTE_END)
        r16 = [rs2.get(16, F32) for _ in range(10)]
        r64 = [rs2.get(64, F32, (16, 4)) for _ in range(3)]
        r512 = [rs2.get(512, F32, (16, 32)) for _ in range(7)]
        idxi = [rs2.get(16, I32) for _ in range(2)]
        rows = [rs2.get(256, F32, (16, 16)) for _ in range(2)]
        linit = rs2.get(32 * CAP // 128 * 16, F32)
        Trt_ = T("routetmp")
        nrow_init = 32 * CAP // 128
        li3 = linit[:, 0:nrow_init * 16].rearrange("p (a b) -> p a b", b=16)
        P.op("pool", lambda e: e.memset(li3, 0.0), writes=[Trt_])
        ts("pool", li3[:, :, 0:1], cpv("tokid")[:, 0:1].unsqueeze(1).to_broadcast([128, nrow_init, 1]), float(SPAN), ALU.add,
           [Trt_, Tcp], [Trt_])
        P.dma("sp", lambda e: e.dma_start(out=listd[0:32 * CAP, :].rearrange("(a p) c -> p a c", p=128), in_=li3), "linit",
              reads=[Trt_], writes=[Tlistd])
        gl = L[:, :, 0:4]
        el = L[:, :, 4:36]
        gmax, gsum, gw, m1, m2, dd2, p1, p2, ix1, ix2 = r16
        goh, gex, pen = r64
        msk, oh1, oh2, tmp5, posv, pre, oh_keep = r512
        dummyp = r16[0][:, 0:1] if False else rs2.get(1, F32)
        RT = [TL, Trt_]
        P.op("dve", lambda e: e.tensor_reduce(out=gmax, in_=gl, axis=AX.X, op=ALU.max), [TL], [Trt_])
        tt("dve", gex, gl, gmax.unsqueeze(2).to_broadcast([128, 16, 4]), ALU.subtract, RT, [Trt_])
        tt("dve", goh, gl, gmax.unsqueeze(2).to_broadcast([128, 16, 4]), ALU.is_equal, RT, [Trt_])
        act(gex, gex, AF.Exp, [Trt_], [Trt_])
        P.op("dve", lambda e: e.tensor_reduce(out=gsum, in_=gex, axis=AX.X, op=ALU.add), [Trt_], [Trt_])
        P.op("dve", lambda e: e.reciprocal(out=gw, in_=gsum), [Trt_], [Trt_])
        ts("dve", pen, goh, -1.0, ALU.add, [Trt_], [Trt_], s2=1e30, op1=ALU.mult)
        tt("dve", msk.rearrange("p a (g e) -> p a g e", e=8), el.rearrange("p a (g e) -> p a g e", e=8),
           pen.unsqueeze(3).to_broadcast([128, 16, 4, 8]), ALU.add, RT, [Trt_])
        P.op("dve", lambda e: e.tensor_reduce(out=m1, in_=msk, axis=AX.X, op=ALU.max), [Trt_], [Trt_])
        tt("dve", oh1, msk, m1.unsqueeze(2).to_broadcast([128, 16, 32]), ALU.is_equal, [Trt_], [Trt_])
        stt("dve", tmp5, oh1, -1e30, msk, ALU.mult, ALU.add, [Trt_], [Trt_])
        P.op("dve", lambda e: e.tensor_reduce(out=m2, in_=tmp5, axis=AX.X, op=ALU.max), [Trt_], [Trt_])
        tt("dve", oh2, tmp5, m2.unsqueeze(2).to_broadcast([128, 16, 32]), ALU.is_equal, [Trt_], [Trt_])
        tt("dve", dd2, m2, m1, ALU.subtract, [Trt_], [Trt_])
        act(dd2, dd2, AF.Exp, [Trt_], [Trt_])
        ts("dve", p1, dd2, 1.0, ALU.add, [Trt_], [Trt_])
        P.op("dve", lambda e: e.reciprocal(out=p1, in_=p1), [Trt_], [Trt_])
        tt("dve", p2, dd2, p1, ALU.mult, [Trt_], [Trt_])
        tt("dve", p1, p1, gw, ALU.mult, [Trt_], [Trt_])
        tt("dve", p2, p2, gw, ALU.mult, [Trt_], [Trt_])
        tt("dve", tmp5, oh1, oh2, ALU.add, [Trt_], [Trt_])
        oh_f = tmp5.rearrange("p a b -> p (a b)")
        mm(pb[4], cpv("tri"), oh_f, True, True, [Tcp, Trt_], [Tpb[4]])
        mm(pb[5], cpv("ones"), oh_f, True, True, [Tcp, Trt_], [Tpb[5]])
        cpy("act", msk.rearrange("p a b -> p (a b)"), pb[5], [Tpb[5]], [Trt_])
        P.op("pool", lambda e: e.memset(pre[:, 0, :], 0.0), [Trt_], [Trt_])
        for i in range(1, 16):
            tt("pool", pre[:, i, :], pre[:, i - 1, :], msk[:, i - 1, :], ALU.add, [Trt_], [Trt_])
        stt("dve", posv.rearrange("p a b -> p (a b)"), pb[4], -1.0, pre.rearrange("p a b -> p (a b)"), ALU.add, ALU.add,
            [Tpb[4], Trt_], [Trt_])
        ts("dve", msk, posv, float(CAP), ALU.is_ge, [Trt_], [Trt_])
        tt("dve", posv, posv, cpv("ecap").unsqueeze(1).to_broadcast([128, 16, 32]), ALU.add, [Trt_, Tcp], [Trt_])
        ts("dve", oh_keep, msk, -1.0, ALU.mult, [Trt_], [Trt_], s2=1.0, op1=ALU.add)
        tt("dve", posv, posv, oh_keep, ALU.mult, [Trt_], [Trt_])
        ts("pool", dummyp, cpv("tokid")[:, 0:1], float(32 * CAP), ALU.add, [Tcp], [Trt_])
        stt("dve", posv, msk, dummyp, posv, ALU.mult, ALU.add, [Trt_], [Trt_])
        for kk, (ohk, ixk, pk) in enumerate(((oh1, ix1, p1), (oh2, ix2, p2))):
            tt("dve", msk, ohk, posv, ALU.mult, [Trt_], [Trt_])
            P.op("dve", lambda e, ixk=ixk: e.tensor_reduce(out=ixk, in_=msk, axis=AX.X, op=ALU.add), [Trt_], [Trt_])
            cpy("dve", idxi[kk], ixk, [Trt_], [Trt_])
            P.op("pool", lambda e, kk=kk: e.memset(rows[kk], 0.0), [Trt_], [Trt_])
            cpy("pool", rows[kk][:, :, 0:1], cpv("tokid").unsqueeze(2), [Trt_, Tcp], [Trt_])
            cpy("pool", rows[kk][:, :, 1:2], pk.unsqueeze(2), [Trt_], [Trt_])
        for kk in range(2):
            for i in range(16):
                P.dma("pool", lambda e, kk=kk, i=i: e.indirect_dma_start(
                    out=listd, out_offset=bass.IndirectOffsetOnAxis(ap=idxi[kk][:, i:i + 1], axis=0),
                    in_=rows[kk][:, i, :], in_offset=None),
                    "lsc", reads=[Trt_, Tlistd], writes=[])
        dump("L", L, [16, 36], F32)

        P.barrier()
        rsc = Alloc(RSC0, RSC1)
        wgu_region = rsc.get(8 * 2048, BF16, (8, 2048))
        wgb = [wgu_region[:, :, 0:512], wgu_region[:, :, 512:1024]]
        wub = [wgu_region[:, :, 1024:1536], wgu_region[:, :, 1536:2048]]
        Twg, Twu = [T("wg0"), T("wg1")], [T("wu0"), T("wu1")]
        rbc = Alloc(RBC0, RBC1)
        wdb = [rbc.get(4 * 1024, BF16, (4, 1024)) for _ in range(2)]
        Twd = [T("wd0"), T("wd1")]
        rh = Alloc(RH0, RH1)
        sli = [rh.get(NS * 16, F32, (NS, 16)) for _ in range(4)]
        Tsli = [T(f"sli{q}") for q in range(4)]
        tki = [rh.get(NS, I32) for _ in range(4)]
        Ttki = [T(f"tki{q}") for q in range(4)]
        Xe = [rh.get(NS * 1024, BF16, (NS, 1024)) for _ in range(2)]
        TXe = [T("Xe0"), T("Xe1")]
        XgT = [rh.get(8 * CAP, BF16, (8, CAP)) for _ in range(2)]
        TXgT = [T("XgT0"), T("XgT1")]
        sg = [rh.get(CAP, F32) for _ in range(2)]
        Tsg = [T("sg0"), T("sg1")]
        hT = [rh.get(4 * CAP, BF16, (4, CAP)) for _ in range(2)]
        ThT = [T("hT0"), T("hT1")]
        yb = [rh.get(1024, F32) for _ in range(3)]
        Tyb = [T("yb0"), T("yb1"), T("yb2")]
        for b in range(2):
            P.op("pool", lambda e, b=b: e.memset(Xe[b], 0.0), writes=[TXe[b]])
        rx = Alloc(RX0, RX1)
        stg = [rx.get(4096, F32), rx.get(4096, F32), rh.get(4096, F32)]
        Tstg = [T("stg0"), T("stg1"), T("stg2")]
        cnt = 0
        ycnt = 0

        def prefetch(ex):
            b = ex % 2
            b4 = ex % 4
            P.dma("sp", lambda e, ex=ex, b4=b4: e.dma_start(
                out=sli[b4], in_=listd[ex * CAP:(ex + 1) * CAP, :].rearrange("(s p) c -> p s c", p=128)),
                f"sli{b4}", writes=[Tsli[b4]])
            srcs = (w_gate[ex].rearrange("(k p) f -> p k f", p=128), w_up[ex].rearrange("(k p) f -> p k f", p=128),
                    w_down[ex].rearrange("(k p) d -> p k d", p=128))
            views = (stg[0].rearrange("p (k f) -> p k f", f=512), stg[1].rearrange("p (k f) -> p k f", f=512),
                     stg[2].rearrange("p (k f) -> p k f", f=1024))
            for q in range(3):
                P.dma("sp", lambda e, q=q, srcs=srcs, views=views: e.dma_start(out=views[q], in_=srcs[q]), f"stg{q}",
                      writes=[Tstg[q]])
            cpy("dve", tki[b4], sli[b4][:, :, 0], [Tsli[b4]], [Ttki[b4]])
            for s in range(NS):
                P.dma("pool", lambda e, b=b, b4=b4, s=s: e.indirect_dma_start(
                    out=Xe[b][:, s, :], out_offset=None, in_=hn2d,
                    in_offset=bass.IndirectOffsetOnAxis(ap=tki[b4][:, s:s + 1], axis=0)),
                    f"xg{b}", reads=[Ttki[b4]], writes=[TXe[b]])

        def casts(ex):
            b = ex % 2
            v0 = stg[0].rearrange("p (k f) -> p k f", f=512)
            v1 = stg[1].rearrange("p (k f) -> p k f", f=512)
            v2 = stg[2].rearrange("p (k f) -> p k f", f=1024)
            cpy("act", wgb[b][:, 0:4, :], v0[:, 0:4, :], [Tstg[0]], [Twg[b]])
            cpy("dve", wgb[b][:, 4:8, :], v0[:, 4:8, :], [Tstg[0]], [Twg[b]])
            cpy("act", wub[b][:, 0:4, :], v1[:, 0:4, :], [Tstg[1]], [Twu[b]])
            cpy("dve", wub[b][:, 4:8, :], v1[:, 4:8, :], [Tstg[1]], [Twu[b]])
            cpy("act", wdb[b][:, 0:2, :], v2[:, 0:2, :], [Tstg[2]], [Twd[b]])
            cpy("dve", wdb[b][:, 2:4, :], v2[:, 2:4, :], [Tstg[2]], [Twd[b]])

        if n_experts > 0:
            prefetch(0)
            casts(0)
        for ex in range(n_experts):
            b = ex % 2
            b4 = ex % 4
            if ex + 1 < n_experts:
                prefetch(ex + 1)
            for s in range(NS):
                pT, TpT = next_pT()
                for k in range(8):
                    tr(pT[:, k, :], Xe[b][:, s, k * 128:(k + 1) * 128], identb, [TXe[b], Tconst], [TpT])
                cpy("act" if s % 2 == 0 else "dve", XgT[b][:, :, s * 128:(s + 1) * 128], pT, [TpT], [TXgT[b]])
            for f in range(4):
                pg, Tpg = pb[cnt % 2], Tpb[cnt % 2]
                pu, Tpu = pb[2 + cnt % 2], Tpb[2 + cnt % 2]
                s_, Ts_ = sg[cnt % 2], Tsg[cnt % 2]
                cnt += 1
                for k in range(8):
                    mm(pg[:, 0:CAP], wgb[b][:, k, f * 128:(f + 1) * 128], XgT[b][:, k, :], k == 0, k == 7,
                       [Twg[b], TXgT[b]], [Tpg])
                for k in range(8):
                    mm(pu[:, 0:CAP], wub[b][:, k, f * 128:(f + 1) * 128], XgT[b][:, k, :], k == 0, k == 7,
                       [Twu[b], TXgT[b]], [Tpu])
                act(s_, pg[:, 0:CAP], AF.Silu, [Tpg], [Ts_])
                tt("dve", hT[b][:, f, :], s_, pu[:, 0:CAP], ALU.mult, [Ts_, Tpu], [ThT[b]])
            ys = []
            for s in range(NS):
                yy, Tyy = yb[ycnt % 3], Tyb[ycnt % 3]
                ycnt += 1
                for half in range(2):
                    pd, Tpd = pb[4 + half], Tpb[4 + half]
                    for f in range(4):
                        mm(pd, hT[b][:, f, s * 128:(s + 1) * 128], wdb[b][:, f, half * 512:(half + 1) * 512],
                           f == 0, f == 3, [ThT[b], Twd[b]], [Tpd])
                    if half == 0:
                        act(yy[:, 0:512], pd, AF.Copy, [Tpd, Tsli[b4]], [Tyy], scale=sli[b4][:, s, 1:2])
                    else:
                        ts("dve", yy[:, 512:1024], pd, sli[b4][:, s, 1:2], ALU.mult, [Tpd, Tsli[b4]], [Tyy])
                ys.append((yy, Tyy, s))
            if ex + 1 < n_experts:
                casts(ex + 1)
            for yy, Tyy, s in ys:
                P.dma("pool", lambda e, b4=b4, s=s, yy=yy: e.indirect_dma_start(
                    out=hacc, out_offset=bass.IndirectOffsetOnAxis(ap=tki[b4][:, s:s + 1], axis=0),
                    in_=yy, in_offset=None, compute_op=ALU.add),
                    "ysc", reads=[Ttki[b4], Tyy], writes=[Thacc])

        rs2 = Alloc(RS20, RS21)
        fn = rs2.get(1024, F32)
        Tfn = T("fn")
        hb = [rs2.get(1024, F32) for _ in range(3)]
        Thb = [T("hb0"), T("hb1"), T("hb2")]
        ob = [rs2.get(1024, F32) for _ in range(2)]
        Tob = [T("ob0"), T("ob1")]
        Tout = T("out")
        P.barrier()
        P.dma("sp", lambda e: e.dma_start(out=fn, in_=fnd), "fn", writes=[Tfn])
        for i in range(16):
            j = i % 2
            j3 = i % 3
            P.dma("sp", lambda e, i=i, j3=j3: e.dma_start(out=hb[j3], in_=hacc[i * 128:(i + 1) * 128, :]), f"hb{j3}",
                  reads=[Thacc], writes=[Thb[j3]])
            sc_, Tsc_ = rmsnorm_stats(hb[j3], ob[j], 128, [Thb[j3]], Tob[j])
            stt("dve", ob[j], hb[j3], sc_[:, 2:3], fn, ALU.mult, ALU.mult, [Thb[j3], Tsc_, Tfn], [Tob[j]])
            P.dma("sp", lambda e, i=i, j=j: e.dma_start(out=out[i * 128:(i + 1) * 128, :], in_=ob[j]), f"out{j}",
                  reads=[Tob[j]], writes=[Tout])
        P.barrier(engs=("sp",))
        P.emit(st)
    return nc


_NC_CACHE = {}


def make_inputs(inputs):
    x = np.asarray(inputs["x"], np.float32)
    in_maps = []
    shared = {
        "w_in": np.ascontiguousarray(np.asarray(inputs["w_in"], np.float32)[0]),
        "w_out": np.ascontiguousarray(np.asarray(inputs["w_out"], np.float32)[0]),
        "w_gate": np.ascontiguousarray(np.asarray(inputs["w_gate"], np.float32)[0]),
        "w_up": np.ascontiguousarray(np.asarray(inputs["w_up"], np.float32)[0]),
        "w_down": np.ascontiguousarray(np.asarray(inputs["w_down"], np.float32)[0]),
        "fnorm": np.ascontiguousarray(np.broadcast_to(np.asarray(inputs["final_norm"], np.float32)[None, :], (128, D))),
        "gfb": np.ascontiguousarray(np.broadcast_to(np.asarray(inputs["norm_ffn"], np.float32)[0][None, :], (128, D))),
    }
    inp_np = {k: np.asarray(v, np.float32) for k, v in inputs.items() if k not in ("x", "w_in", "w_out", "w_gate", "w_up", "w_down")}
    for c in range(NCORES):
        b, s = c // 4, c % 4
        start = s * SPAN
        lo = start - NPREV * SPAN - HALO
        xe = np.zeros((NTOK_EXT, D), np.float32)
        src_lo = max(lo, 0)
        xe[src_lo - lo:, :] = x[b, src_lo:start + SPAN, :]
        m = dict(shared)
        m["xe"] = xe
        m["cp"] = make_cp(inp_np, c)
        in_maps.append(m)
    return in_maps


def kernel(**inputs):
    if "nc" not in _NC_CACHE:
        _NC_CACHE["nc"] = build()
    nc = _NC_CACHE["nc"]
    in_maps = make_inputs(inputs)
    res = run_bass_kernel_spmd(nc, in_maps, core_ids=list(range(NCORES)))
    outs = [np.asarray(res.results[c]["out"], np.float32) for c in range(NCORES)]
    full = np.stack(outs, 0).reshape(2, 4 * SPAN, D)
    return full
```

```python
from contextlib import ExitStack
import numpy as np
import concourse.bass as bass
import concourse.mybir as mybir
from concourse.bass_utils import run_bass_kernel_spmd

F32 = mybir.dt.float32
BF16 = mybir.dt.bfloat16
ALU = mybir.AluOpType
AF = mybir.ActivationFunctionType
AX = mybir.AxisListType

ENGS = ("pe", "act", "dve", "pool", "sp")
SEM_LIMIT = 12000
NCORES = 8
SPAN = 2048
NPREV = 3
HALO = 8
NTOK_EXT = HALO + (NPREV + 1) * SPAN
D = 1024
EPS = 1e-6
MOE_CAP = 256
I32 = mybir.dt.int32


class T:
    __slots__ = ("name", "w", "r", "rd")

    def __init__(self, name=""):
        self.name = name
        self.w = None
        self.r = {}
        self.rd = []


class Prog:
    def __init__(self, nc):
        self.nc = nc
        self.ops = []
        self.dma_sems = {}
        self.last = {}
        self.dmas = []

    def _deps(self, reads, writes):
        deps = set()
        raw = set()
        for t in reads:
            if t.w is not None:
                deps.add(t.w)
                raw.add(t.w)
        for t in writes:
            if t.w is not None:
                deps.add(t.w)
            deps.update(t.r.values())
            deps.update(t.rd)
        self._last_raw = raw
        return deps

    def op(self, eng, fn, reads=(), writes=()):
        i = len(self.ops)
        self.ops.append(dict(eng=eng, fn=fn, deps=self._deps(reads, writes), kind="c"))
        self.ops[-1]["raw"] = self._last_raw
        self.last[eng] = i
        for t in reads:
            t.r[eng] = i
        for t in writes:
            t.w = i
            t.r = {}
            t.rd = []
        return i

    def dma(self, eng, fn, sem, reads=(), writes=(), inc=16):
        i = len(self.ops)
        n = self.dma_sems.get(sem, 0) + inc
        self.dma_sems[sem] = n
        self.ops.append(dict(eng=eng, fn=fn, deps=self._deps(reads, writes), kind="d", sem=sem, val=n, inc=inc))
        self.dmas.append(i)
        for t in reads:
            t.rd.append(i)
        for t in writes:
            t.w = i
            t.r = {}
            t.rd = []
        return i

    def barrier(self, engs=ENGS):
        deps = set(self.last.values()) | set(self.dmas)
        self.dmas = []
        for e in engs:
            self.ops.append(dict(eng=e, fn=None, deps=set(deps), kind="c"))

    def emit(self, stack):
        nc = self.nc
        ops = self.ops
        needed = set()
        for o in ops:
            for d in o["deps"]:
                if ops[d]["kind"] == "c":
                    needed.add(d)
        cnt = {e: 0 for e in ENGS}
        for i, o in enumerate(ops):
            if o["kind"] == "c" and i in needed and o["fn"] is not None:
                cnt[o["eng"]] += 1
                o["cnt"] = cnt[o["eng"]]
        esems = {}
        for e in ENGS:
            n = cnt[e] // SEM_LIMIT + 1
            esems[e] = [stack.enter_context(nc.semaphore(f"s_{e}{k}")) for k in range(n)]
        dsems = {k: stack.enter_context(nc.semaphore(f"d_{k}")) for k in self.dma_sems}
        per = {e: [] for e in ENGS}
        for i, o in enumerate(ops):
            per[o["eng"]].append(i)

        def section(e):
            def body(eng):
                seen = {}
                for i in per[e]:
                    o = ops[i]
                    w = {}
                    for d in o["deps"]:
                        od = ops[d]
                        if od["kind"] == "c":
                            if od["fn"] is None or "cnt" not in od:
                                continue
                            if od["eng"] == e and e == "pe":
                                continue
                            if od["eng"] == e and "raw" in o and d not in o["raw"]:
                                continue
                            c = od["cnt"]
                            key = ("e", od["eng"], (c - 1) // SEM_LIMIT)
                            val = (c - 1) % SEM_LIMIT + 1
                        else:
                            key = ("d", od["sem"])
                            val = od["val"]
                        if w.get(key, 0) < val:
                            w[key] = val
                    for key, val in sorted(w.items(), key=lambda kv: str(kv[0])):
                        if seen.get(key, 0) >= val:
                            continue
                        seen[key] = val
                        if key[0] == "e":
                            sem = esems[key[1]][key[2]]
                            for kk in range(key[2]):
                                seen[("e", key[1], kk)] = SEM_LIMIT
                        else:
                            sem = dsems[key[1]]
                        eng.wait_ge(sem, val)
                    if o["fn"] is None:
                        continue
                    ins = o["fn"](eng)
                    if o["kind"] == "c":
                        if "cnt" in o:
                            c = o["cnt"]
                            ins.then_inc(esems[e][(c - 1) // SEM_LIMIT], 1)
                    else:
                        ins.then_inc(dsems[o["sem"]], o["inc"])
            return body

        with nc.Block() as block:
            block.tensor(section("pe"))
            block.scalar(section("act"))
            block.vector(section("dve"))
            block.gpsimd(section("pool"))
            block.sync(section("sp"))


CP = {}
_o = 0
for _n, _w in (("ident", 128), ("tri", 128), ("lstrict", 128), ("ones", 128), ("bd64", 128),
               ("gmix", 8), ("gffn", 8), ("gain16", 16), ("cwssd", 48), ("cbssd", 12), ("cwsc", 24),
               ("dtb", 16), ("alog", 16), ("dsk", 16), ("wr", 288), ("bmask", 4), ("ecap", 32), ("tokid", 16)):
    CP[_n] = (_o, _w)
    _o += _w
NCP = _o


def make_cp(inp, core):
    cp = np.zeros((128, NCP), np.float32)

    def put(name, arr):
        o, w = CP[name]
        cp[:, o:o + w] = np.asarray(arr, np.float32).reshape(128, w)

    idx = np.arange(128)
    put("ident", np.eye(128))
    put("tri", (idx[:, None] <= idx[None, :]))
    put("lstrict", (idx[:, None] > idx[None, :]))
    put("ones", np.ones((128, 128)))
    put("bd64", (idx[:, None] // 64 == idx[None, :] // 64))
    put("gmix", inp["norm_mix"][0].reshape(8, 128).T)
    put("gffn", inp["norm_ffn"][0].reshape(8, 128).T)
    put("gain16", np.concatenate([inp["ssd_norm"][0].reshape(8, 128).T, inp["sc_norm"][0].reshape(8, 128).T], 1))
    put("cwssd", inp["ssd_conv_w"][0].reshape(4, 12, 128).transpose(2, 1, 0))
    put("cbssd", inp["ssd_conv_b"][0].reshape(12, 128).T)
    put("cwsc", inp["sc_conv_w"][0].reshape(3, 8, 128).transpose(2, 1, 0))
    put("dtb", np.broadcast_to(inp["dt_bias"][0][None, :], (128, 16)))
    put("alog", np.broadcast_to(inp["a_log"][0][None, :], (128, 16)))
    put("dsk", np.broadcast_to(inp["d_skip"][0][None, :], (128, 16)))
    wr = np.concatenate([inp["w_router_group"][0],
                         inp["w_router_expert"][0].transpose(1, 0, 2).reshape(1024, 32)], 1)
    put("wr", wr.reshape(8, 128, 36).transpose(1, 0, 2))
    s = core % 4
    put("bmask", np.broadcast_to(np.array([1.0 if b >= NPREV - s else 0.0 for b in range(NPREV + 1)],
                                          np.float32)[None, :], (128, 4)))
    put("ecap", np.broadcast_to((np.arange(32, dtype=np.float32) * MOE_CAP)[None, :], (128, 32)))
    put("tokid", (np.arange(16)[None, :] * 128 + np.arange(128)[:, None]).astype(np.float32))
    return cp


def build(dbg=False, stop_after=None, n_experts=32):
    nc = bass.Bass("TRN2", target_bir_lowering=False)

    def dram(name, shape, kind="ExternalInput"):
        return nc.dram_tensor(name, shape, F32, kind=kind).ap()

    xe = dram("xe", [NTOK_EXT, D])
    cpd = dram("cp", [128, NCP])
    fnd = dram("fnorm", [128, D])
    gfd = dram("gfb", [128, D])
    w_in = dram("w_in", [D, 5648])
    w_out = dram("w_out", [2048, D])
    w_gate = dram("w_gate", [32, D, 512])
    w_up = dram("w_up", [32, D, 512])
    w_down = dram("w_down", [32, 512, D])
    out = dram("out", [SPAN, D], kind="ExternalOutput")
    dbg_out = {}

    P = Prog(nc)
    st = ExitStack()
    with st:
        ARENA_N = 53200
        arena = st.enter_context(nc.sbuf_tensor("arena", [128, ARENA_N], F32))
        pbt = [st.enter_context(nc.psum_tensor(f"pb{i}", [128, 512], F32)) for i in range(7)]
        pTb = st.enter_context(nc.psum_tensor("pTb", [128, 8, 128], BF16))
        pb = [t[:] for t in pbt]
        Tpb = [T(f"pb{i}") for i in range(7)]
        TpTb = T("pTb")

        a_f32 = arena[:]
        a_bf = arena[:].bitcast(BF16)
        a_i32 = arena[:].bitcast(mybir.dt.int32)

        class Alloc:
            def __init__(self, start, end):
                self.p = start
                self.end = end

            def get(self, n_elems, dt, shape=None):
                nb = n_elems * (2 if dt == BF16 else 4)
                nb_al = (nb + 63) // 64 * 64
                off = self.p
                assert off + nb_al <= self.end, ("arena overflow", off, nb_al, self.end)
                self.p += nb_al
                if dt == F32:
                    ap = a_f32[:, off // 4: off // 4 + n_elems]
                elif dt == mybir.dt.int32:
                    ap = a_i32[:, off // 4: off // 4 + n_elems]
                else:
                    ap = a_bf[:, off // 2: off // 2 + n_elems]
                if shape is not None and len(shape) == 2:
                    ap = ap.rearrange("p (a b) -> p a b", b=shape[1])
                return ap

        PERS0 = 0
        PERS1 = 13824
        RX0, RX1 = PERS1, PERS1 + 32768
        RH0, RH1 = RX1, RX1 + 65792
        RBC0, RBC1 = RH1, RH1 + 16384
        RSC0, RSC1 = RBC1, RBC1 + 32768
        RS20, RS21 = RSC1, ARENA_N * 4

        pers = Alloc(PERS0, PERS1)
        cp = pers.get(NCP, F32)
        Tcp = T("cp")

        def cpv(name, shape=None):
            o, w = CP[name]
            ap = cp[:, o:o + w]
            if shape is not None:
                ap = ap.rearrange("p (a b) -> p a b", b=shape[1])
            return ap

        identb = pers.get(128, BF16)
        bd64b = pers.get(128, BF16)
        Sst = pers.get(1024, F32)
        Sb = pers.get(1024, BF16)
        dtt = pers.get(256, F32, (16, 16))
        adt = pers.get(256, F32, (16, 16))
        avec = pers.get(16, F32)
        epsc = pers.get(1, F32)
        pers_stats = pers.get(64, F32)
        Tconst = T("const")
        TS_, TSb, Tdt, Tstats = T("S"), T("Sb"), T("dt"), T("stats")

        def act(out_, in_, func, reads, writes, **kw):
            P.op("act", lambda e: e.activation(out=out_, in_=in_, func=func, **kw), reads, writes)

        def tt(eng, out_, in0, in1, op, reads, writes):
            P.op(eng, lambda e: e.tensor_tensor(out=out_, in0=in0, in1=in1, op=op), reads, writes)

        def ts(eng, out_, in0, s1, op0, reads, writes, s2=None, op1=None):
            if op1 is None:
                P.op(eng, lambda e: e.tensor_scalar(out=out_, in0=in0, scalar1=s1, scalar2=None, op0=op0), reads, writes)
            else:
                P.op(eng, lambda e: e.tensor_scalar(out=out_, in0=in0, scalar1=s1, scalar2=s2, op0=op0, op1=op1), reads, writes)

        def stt(eng, out_, in0, scalar, in1, op0, op1, reads, writes):
            P.op(eng, lambda e: e.scalar_tensor_tensor(out=out_, in0=in0, scalar=scalar, in1=in1, op0=op0, op1=op1), reads, writes)

        def cpy(eng, out_, in_, reads, writes):
            if eng == "act":
                P.op("act", lambda e: e.copy(out=out_, in_=in_), reads, writes)
            else:
                P.op(eng, lambda e: e.tensor_copy(out=out_, in_=in_), reads, writes)

        def mm(out_, lhsT, rhs, start, stop, reads, writes):
            P.op("pe", lambda e: e.matmul(out_, lhsT=lhsT, rhs=rhs, start=start, stop=stop), reads, writes)

        def tr(out_, in_, ident, reads, writes):
            P.op("pe", lambda e: e.transpose(out=out_, in_=in_, identity=ident), reads, writes)

        stat_tiles = [pers_stats[:, 8 * q: 8 * q + 8] for q in range(8)]
        Tstat_tiles = [T(f"stat{q}") for q in range(8)]
        stc = [0]

        def stat_slot():
            q = stc[0] % 8
            stc[0] += 1
            return stat_tiles[q], Tstat_tiles[q]

        def rmsnorm_stats(x_ap, junk_ap, n, reads, Tjunk):
            sl_, Tsl_ = stat_slot()
            act(junk_ap, x_ap, AF.Square, reads, [Tjunk, Tsl_], accum_out=sl_[:n, 0:1])
            act(sl_[:n, 1:2], sl_[:n, 0:1], AF.Ln, [Tsl_, Tconst], [Tsl_], bias=epsc[:n, 0:1], scale=1.0 / D)
            act(sl_[:n, 2:3], sl_[:n, 1:2], AF.Exp, [Tsl_], [Tsl_], scale=-0.5)
            return sl_, Tsl_

        Tdump = T("dump")

        def dump(name, ap, shp, dt):
            if not dbg:
                return
            d = nc.dram_tensor("dbg_" + name, [128] + shp, dt, kind="ExternalOutput").ap()
            P.barrier()
            P.dma("sp", lambda e, d=d, ap=ap: e.dma_start(out=d, in_=ap), "dump", writes=[Tdump])
            P.barrier()

        P.dma("sp", lambda e: e.dma_start(out=cp, in_=cpd), "cp", writes=[Tcp])
        P.op("pool", lambda e: e.memset(epsc, EPS), writes=[Tconst])
        cpy("dve", identb, cpv("ident"), [Tcp], [Tconst])
        cpy("dve", bd64b, cpv("bd64"), [Tcp], [Tconst])
        act(avec, cpv("alog"), AF.Exp, [Tcp], [Tconst])
        ts("dve", avec, avec, -1.0, ALU.mult, [Tconst], [Tconst])
        P.op("pool", lambda e: e.memset(Sst, 0.0), writes=[TS_])

        rx = Alloc(RX0, RX1)
        xsT = rx.get(8 * 2048, BF16, (8, 2048))
        TxsT = [T(f"xsT{i}") for i in range(16)]
        rh = Alloc(RH0, RH1)
        siluz = rh.get(16 * 1024, BF16, (16, 1024))
        Tsiluz = [T(f"sz{i}") for i in range(16)]
        hnT = rh.get(8 * 2056, BF16, (8, 2056))
        ThnT = [T(f"hnT{i}") for i in range(17)]
        rbc = Alloc(RBC0, RBC1)
        BT = rbc.get(2 * 2048, BF16, (2, 2048))
        CT = rbc.get(2 * 2048, BF16, (2, 2048))
        TBT, TCT = T("BT"), T("CT")

        rs2 = Alloc(RS20, RS21)
        wts = [rs2.get(8 * 256, BF16, (8, 256)) for _ in range(4)]
        Twts = [T(f"wt{i}") for i in range(4)]
        U = [rs2.get(2056, BF16) for _ in range(3)]
        TU = [T(f"U{i}") for i in range(3)]
        xt = [rs2.get(1024, F32) for _ in range(2)]
        Txt = [T(f"xt{i}") for i in range(2)]
        dg = [rs2.get(4 * 128, BF16, (4, 128)) for _ in range(2)]
        Tdg = [T("dg0"), T("dg1")]
        acs_all = rs2.get(512, F32)
        dd_all = rs2.get(256, F32, (16, 16))
        ed_all = rs2.get(256, F32, (16, 16))
        eq_all = rs2.get(256, F32, (16, 16))
        ea_all = rs2.get(256, F32, (16, 16))
        LT = rs2.get(256, F32, (16, 16))
        wgt_all = rs2.get(256, F32, (16, 16))
        Tcum = T("cum")
        rsc = Alloc(RSC0, RSC1)
        xn = [rsc.get(1024, BF16) for _ in range(2)]
        Txn = [T(f"xn{i}") for i in range(2)]
        wdt = rsc.get(8 * 16, BF16, (8, 16))
        Twdt = T("wdt")
        dtmp = rsc.get(256, F32)
        Tdtmp = T("dtmp")
        xdt = [rsc.get(1024, BF16) for _ in range(2)]
        Txdt = [T(f"xdt{i}") for i in range(2)]
        xdtdec = rsc.get(1024, BF16)
        Txdtdec = T("xdtdec")
        xsD_ = [rsc.get(1024, BF16), rs2.get(1024, BF16)]
        TxsD_ = [T("xsD0"), T("xsD1")]
        Btok = [rsc.get(256, BF16) for _ in range(2)]
        TBtok = [T(f"Btok{i}") for i in range(2)]
        CBm = rsc.get(256, F32, (2, 128))
        TCBm = T("CBm")
        Rb = [rsc.get(512, F32, (4, 128)) for _ in range(2)]
        TRb = [T(f"R{i}") for i in range(2)]
        Eb = [rsc.get(512, BF16, (4, 128)) for _ in range(2)]
        TEb = [T(f"E{i}") for i in range(2)]
        MT = rsc.get(16 * 128, BF16, (16, 128))
        TMT = [T(f"MT{i}") for i in range(4)]
        ybuf = [rsc.get(1024, F32), a_f32[:, RSC0 // 4: RSC0 // 4 + 1024]]
        Tybuf = [T("y0"), T("y1")]
        ygn = rsc.get(1024, BF16)
        Tygn = T("ygn")

        pTs = [pTb[:], pb[6].bitcast(BF16).rearrange("p (k t) -> p k t", t=128),
               pb[3].bitcast(BF16).rearrange("p (k t) -> p k t", t=128),
               pb[2].bitcast(BF16).rearrange("p (k t) -> p k t", t=128)]
        TpTs = [TpTb, Tpb[6], Tpb[3], Tpb[2]]
        ptc = [0]
        pt_depth = [4]

        def next_pT():
            j = ptc[0] % pt_depth[0]
            ptc[0] += 1
            return pTs[j], TpTs[j]

        wctr = [0]

        def load_w(cols0, ncols):
            j = wctr[0] % 4
            wctr[0] += 1
            dst = wts[j][:, :, 0:ncols]
            src = w_in[:, cols0:cols0 + ncols].rearrange("(k p) c -> p k c", p=128)
            P.dma("pool", lambda e: e.dma_start(out=dst, in_=src), f"wt{j}", writes=[Twts[j]])
            return wts[j], Twts[j]

        xctr = [0]
        uctr = [0]
        dgc = [0]
        cvc = [0]

        def next_u():
            j = uctr[0] % 3
            uctr[0] += 1
            return U[j], TU[j]

        gmix_b = cpv("gmix").unsqueeze(2)
        identf = cpv("ident")

        def hn_tiles(tt4):
            return ThnT[1 + 4 * tt4: 5 + 4 * tt4]

        def proj_chunk(wt_ap, Tw, lc):
            for k in range(8):
                mm(pb[4][:, 0:HALO], wt_ap[:, k, lc * 128:(lc + 1) * 128], hnT[:, k, 0:HALO], k == 0, k == 7,
                   [Tw, ThnT[0]], [Tpb[4]])
            for t4 in range(4):
                for k in range(8):
                    mm(pb[t4], wt_ap[:, k, lc * 128:(lc + 1) * 128],
                       hnT[:, k, HALO + t4 * 512: HALO + (t4 + 1) * 512], k == 0, k == 7,
                       [Tw] + hn_tiles(t4), [Tpb[t4]])

        def evac_to_u(u, Tu, all_act=False):
            cpy("act", u[:, 0:HALO], pb[4][:, 0:HALO], [Tpb[4]], [Tu])
            for t4 in range(4):
                cpy("act" if (t4 % 2 == 0 or all_act) else "dve", u[:, HALO + t4 * 512: HALO + (t4 + 1) * 512], pb[t4],
                    [Tpb[t4]], [Tu])

        cacc = [a_f32[:, RH0 // 4 + q * 2048: RH0 // 4 + (q + 1) * 2048] for q in range(2)]
        Tcacc = [T("cacc0"), T("cacc1")]

        def make_diag(wcol, ntap):
            j = dgc[0] % 2
            dgc[0] += 1
            for jt in range(ntap):
                act(dg[j][:, jt, :], identf, AF.Copy, [Tcp], [Tdg[j]], scale=wcol(jt))
            return dg[j], Tdg[j]

        def conv_pe_bank():
            j = 5 + cvc[0] % 2
            cvc[0] += 1
            return pb[j], Tpb[j]

        def conv_pe(u, Tu, d, Td, ntap, t4):
            j = 5 + cvc[0] % 2
            cvc[0] += 1
            base = HALO - (ntap - 1) + t4 * 512
            for jt in range(ntap):
                mm(pb[j], d[:, jt, :], u[:, base + jt: base + jt + 512], jt == 0, jt == ntap - 1, [Td, Tu], [Tpb[j]])
            return pb[j], Tpb[j]

        a1_state = {}

        def a1_front(blk_, ti):
            row0_ = HALO + blk_ * SPAN
            n = HALO if ti == 0 else 128
            r0 = row0_ - HALO if ti == 0 else row0_ + (ti - 1) * 128
            j = xctr[0] % 2
            xctr[0] += 1
            xtj, xnj = xt[j], xn[j]
            P.dma("sp", lambda e, xtj=xtj, r0=r0, n=n: e.dma_start(out=xtj[:n, :], in_=xe[r0:r0 + n, :]),
                  f"xt{j}", writes=[Txt[j]])
            sc_, Tsc_ = rmsnorm_stats(xtj[:n, :], xnj[:n, :], n, [Txt[j]], Txn[j])
            ts("dve", xnj[:n, :], xtj[:n, :], sc_[:n, 2:3], ALU.mult, [Txt[j], Tsc_], [Txn[j]])
            a1_state[(blk_, ti)] = (j, n)

        def a1_back(blk_, ti):
            j, n = a1_state.pop((blk_, ti))
            xnj = xn[j]
            c0 = 0 if ti == 0 else HALO + (ti - 1) * 128
            pT, TpT = next_pT()
            for k in range(8):
                tr(pT[:, k, :n], xnj[:n, k * 128:(k + 1) * 128], identb[:n, :n], [Txn[j], Tconst], [TpT])
            tt("dve", hnT[:, :, c0:c0 + n], pT[:, :, :n], gmix_b.to_broadcast([128, 8, n]),
               ALU.mult, [TpT, Tcp], [ThnT[ti]])

        a1_front(0, 0)
        for ti in range(17):
            if ti + 1 < 17:
                a1_front(0, ti + 1)
            a1_back(0, ti)

        for blk in range(NPREV + 1):
            own = blk == NPREV
            row0 = HALO + blk * SPAN
            cwssd = cpv("cwssd")
            cbssd = cpv("cbssd")
            conv_chunks = list(range(8)) + [8, 9] + ([10, 11] if own else [])

            def conv_silu(c, u, Tu):
                acc, Tacc = cacc[c % 2], Tcacc[c % 2]
                base = HALO - 3
                ts("dve", acc, u[:, base:base + SPAN], cwssd[:, c * 4:c * 4 + 1], ALU.mult, [Tu, Tcp], [Tacc])
                for jt in range(1, 4):
                    stt("dve", acc, u[:, base + jt:base + jt + SPAN], cwssd[:, c * 4 + jt:c * 4 + jt + 1], acc,
                        ALU.mult, ALU.add, [Tu, Tcp, Tacc], [Tacc])
                if c < 8:
                    dst, Tdst = xsT[:, c, :], TxsT
                elif c < 10:
                    dst, Tdst = BT[:, c - 8, :], [TBT]
                else:
                    dst, Tdst = CT[:, c - 10, :], [TCT]
                act(dst, acc, AF.Silu, [Tacc, Tcp], Tdst, bias=cbssd[:, c:c + 1])

            pending = None
            for g0 in range(0, len(conv_chunks), 2):
                cc0 = conv_chunks[g0]
                wt_ap, Tw = load_w(1024 + cc0 * 128, 256)
                for lc in range(2):
                    c = cc0 + lc
                    proj_chunk(wt_ap, Tw, lc)
                    u, Tu = next_u()
                    evac_to_u(u, Tu, all_act=True)
                    if pending is not None:
                        conv_silu(*pending)
                    pending = (c, u, Tu)
            conv_silu(*pending)

            src = w_in[:, 2560:2576].rearrange("(k p) c -> p k c", p=128)
            P.dma("pool", lambda e, src=src: e.dma_start(out=wdt, in_=src), "wdt", writes=[Twdt])
            for i in range(16):
                for k in range(8):
                    mm(pb[4][:, 16 + i * 16: 32 + i * 16], hnT[:, k, HALO + i * 128: HALO + (i + 1) * 128], wdt[:, k, :],
                       k == 0, k == 7, [ThnT[1 + i], Twdt], [Tpb[4]])
            dtb_b = cpv("dtb").unsqueeze(1).to_broadcast([128, 16, 16])
            dt3 = dtmp.rearrange("p (a b) -> p a b", b=16)
            tt("dve", dt3, pb[4][:, 16:272].rearrange("p (a b) -> p a b", b=16), dtb_b, ALU.add, [Tpb[4], Tcp], [Tdtmp])
            act(dtmp, dtmp, AF.Exp, [Tdtmp], [Tdtmp])
            act(dtmp, dtmp, AF.Ln, [Tdtmp], [Tdtmp], bias=1.0)
            bm = cpv("bmask")
            ts("dve", dtt, dt3, bm[:, blk:blk + 1], ALU.mult, [Tdtmp, Tcp], [Tdt])
            tt("dve", adt, dtt, avec.unsqueeze(1).to_broadcast([128, 16, 16]), ALU.mult, [Tdt, Tconst], [Tdt])
            adt_f = adt.rearrange("p a b -> p (a b)")
            mm(pb[4][:, 0:256], cpv("tri"), adt_f, True, True, [Tcp, Tdt], [Tpb[4]])
            mm(pb[4][:, 256:512], cpv("ones"), adt_f, True, True, [Tcp, Tdt], [Tpb[4]])
            cpy("act", acs_all, pb[4], [Tpb[4]], [Tcum])
            ac3 = acs_all[:, 0:256].rearrange("p (a b) -> p a b", b=16)
            aq3 = acs_all[:, 256:512].rearrange("p (a b) -> p a b", b=16)
            tt("dve", dd_all, aq3, ac3, ALU.subtract, [Tcum], [Tcum])

            tri = cpv("tri")
            if not own:
                P.op("pool", lambda e: e.memset(LT[:, 15, :], 0.0), [Tcum], [Tcum])
                for i in range(14, -1, -1):
                    tt("pool", LT[:, i, :], LT[:, i + 1, :], aq3[:, i + 1, :], ALU.add, [Tcum], [Tcum])
                tt("dve", dd_all, dd_all, LT, ALU.add, [Tcum], [Tcum])
                act(ed_all, dd_all, AF.Exp, [Tcum], [Tcum])
                tt("dve", wgt_all, ed_all, dtt, ALU.mult, [Tcum, Tdt], [Tcum])
                tt("pool", eq_all[:, 0, :], LT[:, 0, :], aq3[:, 0, :], ALU.add, [Tcum], [Tcum])
                act(eq_all[:, 0, :], eq_all[:, 0, :], AF.Exp, [Tcum], [Tcum])
                for i in range(16):
                    tok = slice(i * 128, (i + 1) * 128)
                    xd, Txd = xdt[i % 2], Txdt[i % 2]
                    bt, Tbt = Btok[i % 2], TBtok[i % 2]
                    pT, TpT = next_pT()
                    for c in range(8):
                        tr(pT[:, c, :], xsT[:, c, tok], identb, [TxsT[i], Tconst], [TpT])
                    pX = pT.rearrange("p c t -> p (c t)").rearrange("p (h d) -> p h d", d=64)
                    tt("dve", xd.rearrange("p (h d) -> p h d", d=64), pX,
                       wgt_all[:, i, :].unsqueeze(2).to_broadcast([128, 16, 64]), ALU.mult, [TpT, Tcum], [Txd])
                    pT2, TpT2 = next_pT()
                    for g in range(2):
                        tr(pT2[:, g, :], BT[:, g, tok], identb, [TBT, Tconst], [TpT2])
                    cpy("act", bt.rearrange("p (g n) -> p g n", n=128), pT2[:, 0:2, :], [TpT2], [Tbt])
                    for g in range(2):
                        mm(pb[g], bt[:, g * 128:(g + 1) * 128], xd[:, g * 512:(g + 1) * 512], i == 0, i == 15,
                           [Tbt, Txd], [Tpb[g]])
                    if i == 0:
                        a1_front(blk + 1, 0)
                    a1_front(blk + 1, i + 1)
                    a1_back(blk + 1, i)
                a1_back(blk + 1, 16)
                tt("pool", Sst.rearrange("p (h d) -> p h d", d=64), Sst.rearrange("p (h d) -> p h d", d=64),
                   eq_all[:, 0, :].unsqueeze(2).to_broadcast([128, 16, 64]), ALU.mult, [TS_, Tcum], [TS_])
                for g in range(2):
                    tt("dve", Sst[:, g * 512:(g + 1) * 512], Sst[:, g * 512:(g + 1) * 512], pb[g], ALU.add,
                       [TS_, Tpb[g]], [TS_])
                continue

            act(ed_all, dd_all, AF.Exp, [Tcum], [Tcum])
            act(eq_all, aq3, AF.Exp, [Tcum], [Tcum])
            act(ea_all, ac3, AF.Exp, [Tcum], [Tcum])
            for q in range(4):
                wt_ap, Tw = load_w(q * 256, 256)
                for i in range(16):
                    pz, Tpz = pb[5 + i % 2], Tpb[5 + i % 2]
                    for k in range(8):
                        mm(pz[:, 0:256], hnT[:, k, HALO + i * 128: HALO + (i + 1) * 128], wt_ap[:, k, :], k == 0, k == 7,
                           [ThnT[1 + i], Tw], [Tpz])
                    act(siluz[:, i, q * 256:(q + 1) * 256], pz[:, 0:256], AF.Silu, [Tpz], [Tsiluz[i], Tcacc[0], Tcacc[1]])
            cpy("act", Sb, Sst, [TS_], [TSb])

            pt_depth[0] = 2
            xdtdec_ = [xdtdec, U[2][:, 0:1024]]
            Txdtdec_ = [Txdtdec, T("xdtdec1")]
            MT_ = [MT, U[1][:, 0:2048].rearrange("p (h l) -> p h l", l=128)]
            TMT_ = [TMT, [T(f"MTb{q}") for q in range(4)]]

            def stageA(i):
                tok = slice(i * 128, (i + 1) * 128)
                xd, Txd = xdt[i % 2], Txdt[i % 2]
                bt, Tbt = Btok[i % 2], TBtok[i % 2]
                xsD, TxsD = xsD_[i % 2], TxsD_[i % 2]
                xdd, Txdd = xdtdec_[i % 2], Txdtdec_[i % 2]
                pT, TpT = next_pT()
                for c in range(8):
                    tr(pT[:, c, :], xsT[:, c, tok], identb, [TxsT[i], Tconst], [TpT])
                pX = pT.rearrange("p c t -> p (c t)").rearrange("p (h d) -> p h d", d=64)
                tt("dve", xd.rearrange("p (h d) -> p h d", d=64), pX, dtt[:, i, :].unsqueeze(2).to_broadcast([128, 16, 64]),
                   ALU.mult, [TpT, Tdt], [Txd])
                tt("dve", xsD.rearrange("p (h d) -> p h d", d=64), pX,
                   cpv("dsk").unsqueeze(2).to_broadcast([128, 16, 64]), ALU.mult, [TpT, Tcp], [TxsD])
                pT2, TpT2 = next_pT()
                for g in range(2):
                    tr(pT2[:, g, :], BT[:, g, tok], identb, [TBT, Tconst], [TpT2])
                cpy("act", bt.rearrange("p (g n) -> p g n", n=128), pT2[:, 0:2, :], [TpT2], [Tbt])
                tt("dve", xdd.rearrange("p (h d) -> p h d", d=64), xd.rearrange("p (h d) -> p h d", d=64),
                   ed_all[:, i, :].unsqueeze(2).to_broadcast([128, 16, 64]), ALU.mult, [Txd, Tcum], [Txdd])
                pc, Tpc = pb[5], Tpb[5]
                for g in range(2):
                    mm(pc[:, g * 128:(g + 1) * 128], BT[:, g, tok], CT[:, g, tok], True, True, [TBT, TCT], [Tpc])
                tt("dve", CBm, pc[:, 0:256].rearrange("p (g l) -> p g l", l=128),
                   tri.unsqueeze(1).to_broadcast([128, 2, 128]), ALU.mult, [Tpc, Tcp], [TCBm])
                for hq in range(4):
                    g = hq // 2
                    R, TR = Rb[hq % 2], TRb[hq % 2]
                    E, TE = Eb[hq % 2], TEb[hq % 2]
                    pg, Tpg = (pb[4], Tpb[4]) if hq % 2 == 0 else (pb[5], Tpb[5])
                    tt("pool", R, tri.unsqueeze(1).to_broadcast([128, 4, 128]),
                       adt[:, i, hq * 4:(hq + 1) * 4].unsqueeze(2).to_broadcast([128, 4, 128]), ALU.mult,
                       [Tcp, Tdt], [TR])
                    mm(pg, cpv("lstrict"), R.rearrange("p h l -> p (h l)"), True, True, [Tcp, TR], [Tpg])
                    act(E.rearrange("p h l -> p (h l)"), pg, AF.Exp, [Tpg], [TE])
                    tt("dve", MT_[i % 2][:, hq * 4:(hq + 1) * 4, :], E, CBm[:, g, :].unsqueeze(1).to_broadcast([128, 4, 128]),
                       ALU.mult, [TE, TCBm], [TMT_[i % 2][hq]])

            def stageB(i):
                tok = slice(i * 128, (i + 1) * 128)
                bt, Tbt = Btok[i % 2], TBtok[i % 2]
                xdd, Txdd = xdtdec_[i % 2], Txdtdec_[i % 2]
                y, Ty = ybuf[i % 2], Tybuf[i % 2]
                xd, Txd = xdt[i % 2], Txdt[i % 2]
                for g in range(2):
                    py, Tpy = pb[2 + g], Tpb[2 + g]
                    for r in range(8):
                        hh = g * 8 + r
                        mm(py[:, r * 64:(r + 1) * 64], MT_[i % 2][:, hh, :], xd[:, hh * 64:(hh + 1) * 64], True, True,
                           [TMT_[i % 2][hh // 4], Txd], [Tpy])
                for g in range(2):
                    mm(pb[1] if g == 0 else pb[0], CT[:, g, tok], Sb[:, g * 512:(g + 1) * 512], True, True,
                       [TCT, TSb], [Tpb[1] if g == 0 else Tpb[0]])
                for g in range(2):
                    tt("dve", y[:, g * 512:(g + 1) * 512].rearrange("p (h d) -> p h d", d=64),
                       (pb[1] if g == 0 else pb[0]).rearrange("p (h d) -> p h d", d=64),
                       ea_all[:, i, g * 8:(g + 1) * 8].unsqueeze(2).to_broadcast([128, 8, 64]), ALU.mult,
                       [Tpb[1] if g == 0 else Tpb[0], Tcum], [Ty])
                for g in range(2):
                    mm(pb[g], bt[:, g * 128:(g + 1) * 128], xdd[:, g * 512:(g + 1) * 512], True, True,
                       [Tbt, Txdd], [Tpb[g]])
                tt("pool", Sst.rearrange("p (h d) -> p h d", d=64), Sst.rearrange("p (h d) -> p h d", d=64),
                   eq_all[:, i, :].unsqueeze(2).to_broadcast([128, 16, 64]), ALU.mult, [TS_, Tcum], [TS_])
                for g in range(2):
                    tt("dve", Sst[:, g * 512:(g + 1) * 512], Sst[:, g * 512:(g + 1) * 512], pb[g], ALU.add,
                       [TS_, Tpb[g]], [TS_])
                cpy("act", Sb, Sst, [TS_, TSb], [TSb])
                for g in range(2):
                    py, Tpy = pb[2 + g], Tpb[2 + g]
                    tt("dve", y[:, g * 512:(g + 1) * 512], y[:, g * 512:(g + 1) * 512], py, ALU.add, [Ty, Tpy], [Ty])

            def stageC(i):
                tok = slice(i * 128, (i + 1) * 128)
                xsD, TxsD = xsD_[i % 2], TxsD_[i % 2]
                y, Ty = ybuf[i % 2], Tybuf[i % 2]
                tt("dve", y, y, xsD, ALU.add, [Ty, TxsD], [Ty])
                tt("dve", y, y, siluz[:, i, :], ALU.mult, [Ty, Tsiluz[i]], [Ty])
                sc_, Tsc_ = stat_slot()
                for g in range(2):
                    act(ygn[:, g * 512:(g + 1) * 512], y[:, g * 512:(g + 1) * 512], AF.Square, [Ty], [Tygn, Tsc_],
                        accum_out=sc_[:, g:g + 1])
                act(sc_[:, 2:4], sc_[:, 0:2], AF.Ln, [Tsc_, Tconst], [Tsc_], bias=epsc[:, 0:1], scale=1.0 / 512)
                act(sc_[:, 4:6], sc_[:, 2:4], AF.Exp, [Tsc_], [Tsc_], scale=-0.5)
                for g in range(2):
                    act(ygn[:, g * 512:(g + 1) * 512], y[:, g * 512:(g + 1) * 512], AF.Copy, [Ty, Tsc_], [Tygn],
                        scale=sc_[:, 4 + g:5 + g])
                pT3, TpT3 = next_pT()
                for c in range(8):
                    tr(pT3[:, c, :], ygn[:, c * 128:(c + 1) * 128], identb, [Tygn, Tconst], [TpT3])
                cpy("act", xsT[:, :, tok], pT3, [TpT3], [TxsT[i]])

            stageA(0)
            for i in range(16):
                if i + 1 < 16:
                    stageA(i + 1)
                stageB(i)
                stageC(i)

        dump("mixT_ssd", xsT, [8, 2048], BF16)
        dump("S", Sst, [1024], F32)

        P.barrier()
        rsc = Alloc(RSC0, RSC1)
        scT = rsc.get(8 * 2048, BF16, (8, 2048))
        TscT = [T(f"scT{i}") for i in range(16)]
        rbc = Alloc(RBC0, RBC1)
        sq = [rbc.get(512, BF16) for _ in range(2)]
        Tsq = [T("sq0"), T("sq1")]
        rt = [rbc.get(512, F32) for _ in range(2)]
        Trt = [T("rt0"), T("rt1")]
        cvt = [rbc.get(512, F32) for _ in range(2)]
        Tcvt = [T("cvt0"), T("cvt1")]
        scb = [rbc.get(512, F32) for _ in range(2)]
        Tscb = [T("scb0"), T("scb1")]
        cwsc = cpv("cwsc")
        o3 = 1024 + 1536 + 16
        rhs_ = Alloc(RH0, RH0 + 32768)
        scw = [rhs_.get(8 * 256, BF16, (8, 256)) for _ in range(6)]
        Tscw = [T(f"scw{q}") for q in range(6)]

        def load_sc(jp):
            res = []
            for q, c0_ in enumerate((o3 + jp * 256, o3 + 2048 + jp * 256, o3 + 1024 + jp * 256)):
                bi = (jp % 2) * 3 + q
                src_ = w_in[:, c0_:c0_ + 256].rearrange("(k p) c -> p k c", p=128)
                P.dma("pool", lambda e, bi=bi, src_=src_: e.dma_start(out=scw[bi], in_=src_), f"scw{bi}", writes=[Tscw[bi]])
                res.append((scw[bi], Tscw[bi]))
            return res

        sc_loaded = {0: load_sc(0)}
        for jp in range(4):
            if jp + 1 < 4:
                sc_loaded[jp + 1] = load_sc(jp + 1)
            (wb, Twb), (wv, Twv), (wc, Twc) = sc_loaded[jp]
            for lc in range(2):
                j = jp * 2 + lc
                proj_chunk(wb, Twb, lc)
                ub, Tub = next_u()
                evac_to_u(ub, Tub)
                proj_chunk(wv, Twv, lc)
                u, Tu = next_u()
                evac_to_u(u, Tu)
                proj_chunk(wc, Twc, lc)
                tt("dve", u[:, 0:HALO], u[:, 0:HALO], pb[4][:, 0:HALO], ALU.mult, [Tu, Tpb[4]], [Tu])
                for t4 in range(4):
                    sl = slice(HALO + t4 * 512, HALO + (t4 + 1) * 512)
                    tt("dve", u[:, sl], u[:, sl], pb[t4], ALU.mult, [Tu, Tpb[t4]], [Tu])
                d, Td = make_diag(lambda jt, j=j: cwsc[:, j * 3 + jt:j * 3 + jt + 1], 3)
                for t4 in range(4):
                    sl = slice(t4 * 512, (t4 + 1) * 512)
                    slh = slice(HALO + t4 * 512, HALO + (t4 + 1) * 512)
                    b2 = t4 % 2
                    pc_, Tpc_ = conv_pe(u, Tu, d, Td, 3, t4)
                    tt("dve", scb[b2], pc_, ub[:, slh], ALU.mult, [Tpc_, Tub], [Tscb[b2]])
                    act(sq[b2], scb[b2], AF.Square, [Tscb[b2]], [Tsq[b2]])
                    pz, Tpz = conv_pe_bank()
                    mm(pz, bd64b, sq[b2], True, True, [Tconst, Tsq[b2]], [Tpz])
                    act(rt[b2], pz, AF.Ln, [Tpz, Tconst], [Trt[b2]], bias=epsc[:, 0:1], scale=1.0 / 64)
                    act(rt[b2], rt[b2], AF.Exp, [Trt[b2]], [Trt[b2]], scale=-0.5)
                    tt("pool", scT[:, j, sl], scb[b2], rt[b2], ALU.mult, [Tscb[b2], Trt[b2]], TscT[4 * t4:4 * t4 + 4])
        dump("scT", scT, [8, 2048], BF16)

        P.barrier()
        CAP = MOE_CAP
        NS = CAP // 128
        hn2d = nc.dram_tensor("hn2d", [SPAN + 128, D], BF16).ap()
        hacc = nc.dram_tensor("hacc", [SPAN + 128, D], F32).ap()
        listd = nc.dram_tensor("listd", [32 * CAP + 128, 16], F32).ap()
        Thacc, Thn2d, Tlistd = T("hacc"), T("hn2d"), T("listd")
        rh = Alloc(RH0, RH1)
        h = rh.get(16 * 1024, F32, (16, 1024))
        Th = [T(f"h{i}") for i in range(16)]
        rs2 = Alloc(RS20, RS21)
        wo = [rs2.get(16 * 512, BF16, (16, 512)) for _ in range(2)]
        Two = [T("wo0"), T("wo1")]
        xr = [rs2.get(1024, F32) for _ in range(2)]
        Txr = [T("xr0"), T("xr1")]
        RS2_ROUTE_END = rs2.p
        hnb = [rs2.get(1024, BF16) for _ in range(2)]
        Thnb = [T("hnb0"), T("hnb1")]
        gfb = rs2.get(1024, BF16)
        Tgfb = T("gfb")
        L = rs2.get(16 * 36, F32, (16, 36))
        TL = T("L")
        rbc = Alloc(RBC0, RBC1)
        hnf = [rbc.get(1024, F32) for _ in range(2)]
        Thnf = [T("hnf0"), T("hnf1")]
        hTf = [rbc.get(1024, F32, (8, 128)) for _ in range(2)]
        ThTf = [T("hTf0"), T("hTf1")]
        gffn_b = cpv("gffn").unsqueeze(2).to_broadcast([128, 8, 128])
        wr = cpv("wr", (8, 36))
        gain_b = cpv("gain16").unsqueeze(2).to_broadcast([128, 16, 512])
        own_row0 = HALO + NPREV * SPAN
        for half in range(2):
            src = w_out[:, half * 512:(half + 1) * 512].rearrange("(k p) c -> p k c", p=128)
            P.dma("pool", lambda e, src=src, half=half: e.dma_start(out=wo[half], in_=src), f"wo{half}", writes=[Two[half]])
            if half == 0:
                tt("dve", wo[half], wo[half], gain_b, ALU.mult, [Two[half], Tcp], [Two[half]])
            else:
                g16 = cpv("gain16")
                for k in range(16):
                    act(wo[half][:, k, :], wo[half][:, k, :], AF.Copy, [Two[half], Tcp], [Two[half]], scale=g16[:, k:k + 1])
        P.dma("pool", lambda e: e.dma_start(out=gfb, in_=gfd), "gfb", writes=[Tgfb])
        P.op("pool", lambda e: e.memset(hnb[1], 0.0), writes=[Thnb[1]])
        P.dma("sp", lambda e: e.dma_start(out=hn2d[SPAN:SPAN + 128, :], in_=hnb[1]), "hn2dz", reads=[Thnb[1]], writes=[])

        def n2_stage1(i):
            j = i % 2
            P.dma("sp", lambda e, i=i: e.dma_start(out=hacc[i * 128:(i + 1) * 128, :], in_=h[:, i, :]), "hacc0",
                  reads=[Th[i]], writes=[])
            sc_, Tsc_ = rmsnorm_stats(h[:, i, :], hnf[j], 128, [Th[i]], Thnf[j])
            ts("dve", hnf[j], h[:, i, :], sc_[:, 2:3], ALU.mult, [Th[i], Tsc_], [Thnf[j]])
            tt("pool", hnb[j], hnf[j], gfb, ALU.mult, [Thnf[j], Tgfb], [Thnb[j]])
            P.dma("sp", lambda e, i=i, j=j: e.dma_start(out=hn2d[i * 128:(i + 1) * 128, :], in_=hnb[j]), f"hn2d{j}",
                  reads=[Thnb[j]], writes=[])

        def n2_stage2(i):
            j = i % 2
            for hf in range(2):
                pz, Tpz = pb[4 + hf], Tpb[4 + hf]
                for k4 in range(4):
                    k = hf * 4 + k4
                    tr(pz[:, k4 * 128:(k4 + 1) * 128], hnf[j][:, k * 128:(k + 1) * 128], identf, [Thnf[j], Tcp], [Tpz])
                tt("dve", hTf[j][:, hf * 4:(hf + 1) * 4, :], pz.rearrange("p (k t) -> p k t", t=128),
                   gffn_b[:, hf * 4:(hf + 1) * 4, :], ALU.mult, [Tpz, Tcp], [ThTf[j]])

        def n2_stage3(i):
            j = i % 2
            pr, Tpr = pb[6], Tpb[6]
            for k in range(8):
                mm(pr[:, 0:36], hTf[j][:, k, :], wr[:, k, :], k == 0, k == 7, [ThTf[j], Tcp], [Tpr])
            cpy("act", L[:, i, :], pr[:, 0:36], [Tpr], [TL])

        for i in range(16):
            tok = slice(i * 128, (i + 1) * 128)
            pzs = [(pb[(2 * i) % 4], Tpb[(2 * i) % 4]), (pb[(2 * i + 1) % 4], Tpb[(2 * i + 1) % 4])]
            for k in range(16):
                lhs = xsT[:, k, tok] if k < 8 else scT[:, k - 8, tok]
                for half in range(2):
                    mm(pzs[half][0], lhs, wo[half][:, k, :], k == 0, k == 15, [TxsT[i], TscT[i], Two[half]], [pzs[half][1]])
            jx = i % 2
            P.dma("sp", lambda e, jx=jx, i=i: e.dma_start(
                out=xr[jx], in_=xe[own_row0 + i * 128: own_row0 + (i + 1) * 128, :]), f"xr{jx}", writes=[Txr[jx]])
            for half in range(2):
                tt("dve", h[:, i, half * 512:(half + 1) * 512], pzs[half][0], xr[jx][:, half * 512:(half + 1) * 512], ALU.add,
                   [pzs[half][1], Txr[jx]], [Th[i]])
            n2_stage1(i)
            if i >= 1:
                n2_stage2(i - 1)
            if i >= 2:
                n2_stage3(i - 2)
        n2_stage2(15)
        n2_stage3(14)
        n2_stage3(15)
        dump("h", h, [16, 1024], F32)

        P.barrier()
        rs2 = Alloc(RS20, RS2_ROUTE_END)
        r16 = [rs2.get(16, F32) for _ in range(10)]
        r64 = [rs2.get(64, F32, (16, 4)) for _ in range(3)]
        r512 = [rs2.get(512, F32, (16, 32)) for _ in range(7)]
        idxi = [rs2.get(16, I32) for _ in range(2)]
        rows = [rs2.get(256, F32, (16, 16)) for _ in range(2)]
        linit = rs2.get(32 * CAP // 128 * 16, F32)
        Trt_ = T("routetmp")
        nrow_init = 32 * CAP // 128
        li3 = linit[:, 0:nrow_init * 16].rearrange("p (a b) -> p a b", b=16)
        P.op("pool", lambda e: e.memset(li3, 0.0), writes=[Trt_])
        ts("pool", li3[:, :, 0:1], cpv("tokid")[:, 0:1].unsqueeze(1).to_broadcast([128, nrow_init, 1]), float(SPAN), ALU.add,
           [Trt_, Tcp], [Trt_])
        P.dma("sp", lambda e: e.dma_start(out=listd[0:32 * CAP, :].rearrange("(a p) c -> p a c", p=128), in_=li3), "linit",
              reads=[Trt_], writes=[Tlistd])
        gl = L[:, :, 0:4]
        el = L[:, :, 4:36]
        gmax, gsum, gw, m1, m2, dd2, p1, p2, ix1, ix2 = r16
        goh, gex, pen = r64
        msk, oh1, oh2, tmp5, posv, pre, oh_keep = r512
        dummyp = r16[0][:, 0:1] if False else rs2.get(1, F32)
        RT = [TL, Trt_]
        P.op("dve", lambda e: e.tensor_reduce(out=gmax, in_=gl, axis=AX.X, op=ALU.max), [TL], [Trt_])
        tt("dve", gex, gl, gmax.unsqueeze(2).to_broadcast([128, 16, 4]), ALU.subtract, RT, [Trt_])
        tt("dve", goh, gl, gmax.unsqueeze(2).to_broadcast([128, 16, 4]), ALU.is_equal, RT, [Trt_])
        act(gex, gex, AF.Exp, [Trt_], [Trt_])
        P.op("dve", lambda e: e.tensor_reduce(out=gsum, in_=gex, axis=AX.X, op=ALU.add), [Trt_], [Trt_])
        P.op("dve", lambda e: e.reciprocal(out=gw, in_=gsum), [Trt_], [Trt_])
        ts("dve", pen, goh, -1.0, ALU.add, [Trt_], [Trt_], s2=1e30, op1=ALU.mult)
        tt("dve", msk.rearrange("p a (g e) -> p a g e", e=8), el.rearrange("p a (g e) -> p a g e", e=8),
           pen.unsqueeze(3).to_broadcast([128, 16, 4, 8]), ALU.add, RT, [Trt_])
        P.op("dve", lambda e: e.tensor_reduce(out=m1, in_=msk, axis=AX.X, op=ALU.max), [Trt_], [Trt_])
        tt("dve", oh1, msk, m1.unsqueeze(2).to_broadcast([128, 16, 32]), ALU.is_equal, [Trt_], [Trt_])
        stt("dve", tmp5, oh1, -1e30, msk, ALU.mult, ALU.add, [Trt_], [Trt_])
        P.op("dve", lambda e: e.tensor_reduce(out=m2, in_=tmp5, axis=AX.X, op=ALU.max), [Trt_], [Trt_])
        tt("dve", oh2, tmp5, m2.unsqueeze(2).to_broadcast([128, 16, 32]), ALU.is_equal, [Trt_], [Trt_])
        tt("dve", dd2, m2, m1, ALU.subtract, [Trt_], [Trt_])
        act(dd2, dd2, AF.Exp, [Trt_], [Trt_])
        ts("dve", p1, dd2, 1.0, ALU.add, [Trt_], [Trt_])
        P.op("dve", lambda e: e.reciprocal(out=p1, in_=p1), [Trt_], [Trt_])
        tt("dve", p2, dd2, p1, ALU.mult, [Trt_], [Trt_])
        tt("dve", p1, p1, gw, ALU.mult, [Trt_], [Trt_])
        tt("dve", p2, p2, gw, ALU.mult, [Trt_], [Trt_])
        tt("dve", tmp5, oh1, oh2, ALU.add, [Trt_], [Trt_])
        oh_f = tmp5.rearrange("p a b -> p (a b)")
        mm(pb[4], cpv("tri"), oh_f, True, True, [Tcp, Trt_], [Tpb[4]])
        mm(pb[5], cpv("ones"), oh_f, True, True, [Tcp, Trt_], [Tpb[5]])
        cpy("act", msk.rearrange("p a b -> p (a b)"), pb[5], [Tpb[5]], [Trt_])
        P.op("pool", lambda e: e.memset(pre[:, 0, :], 0.0), [Trt_], [Trt_])
        for i in range(1, 16):
            tt("pool", pre[:, i, :], pre[:, i - 1, :], msk[:, i - 1, :], ALU.add, [Trt_], [Trt_])
        stt("dve", posv.rearrange("p a b -> p (a b)"), pb[4], -1.0, pre.rearrange("p a b -> p (a b)"), ALU.add, ALU.add,
            [Tpb[4], Trt_], [Trt_])
        ts("dve", msk, posv, float(CAP), ALU.is_ge, [Trt_], [Trt_])
        tt("dve", posv, posv, cpv("ecap").unsqueeze(1).to_broadcast([128, 16, 32]), ALU.add, [Trt_, Tcp], [Trt_])
        ts("dve", oh_keep, msk, -1.0, ALU.mult, [Trt_], [Trt_], s2=1.0, op1=ALU.add)
        tt("dve", posv, posv, oh_keep, ALU.mult, [Trt_], [Trt_])
        ts("pool", dummyp, cpv("tokid")[:, 0:1], float(32 * CAP), ALU.add, [Tcp], [Trt_])
        stt("dve", posv, msk, dummyp, posv, ALU.mult, ALU.add, [Trt_], [Trt_])
        for kk, (ohk, ixk, pk) in enumerate(((oh1, ix1, p1), (oh2, ix2, p2))):
            tt("dve", msk, ohk, posv, ALU.mult, [Trt_], [Trt_])
            P.op("dve", lambda e, ixk=ixk: e.tensor_reduce(out=ixk, in_=msk, axis=AX.X, op=ALU.add), [Trt_], [Trt_])
            cpy("dve", idxi[kk], ixk, [Trt_], [Trt_])
            P.op("pool", lambda e, kk=kk: e.memset(rows[kk], 0.0), [Trt_], [Trt_])
            cpy("pool", rows[kk][:, :, 0:1], cpv("tokid").unsqueeze(2), [Trt_, Tcp], [Trt_])
            cpy("pool", rows[kk][:, :, 1:2], pk.unsqueeze(2), [Trt_], [Trt_])
        for kk in range(2):
            for i in range(16):
                P.dma("pool", lambda e, kk=kk, i=i: e.indirect_dma_start(
                    out=listd, out_offset=bass.IndirectOffsetOnAxis(ap=idxi[kk][:, i:i + 1], axis=0),
                    in_=rows[kk][:, i, :], in_offset=None),
                    "lsc", reads=[Trt_, Tlistd], writes=[])
        dump("L", L, [16, 36], F32)

        P.barrier()
        rsc = Alloc(RSC0, RSC1)
        wgu_region = rsc.get(8 * 2048, BF16, (8, 2048))
        wgb = [wgu_region[:, :, 0:512], wgu_region[:, :, 512:1024]]
        wub = [wgu_region[:, :, 1024:1536], wgu_region[:, :, 1536:2048]]
        Twg, Twu = [T("wg0"), T("wg1")], [T("wu0"), T("wu1")]
        rbc = Alloc(RBC0, RBC1)
        wdb = [rbc.get(4 * 1024, BF16, (4, 1024)) for _ in range(2)]
        Twd = [T("wd0"), T("wd1")]
        rh = Alloc(RH0, RH1)
        sli = [rh.get(NS * 16, F32, (NS, 16)) for _ in range(4)]
        Tsli = [T(f"sli{q}") for q in range(4)]
        tki = [rh.get(NS, I32) for _ in range(4)]
        Ttki = [T(f"tki{q}") for q in range(4)]
        Xe = [rh.get(NS * 1024, BF16, (NS, 1024)) for _ in range(2)]
        TXe = [T("Xe0"), T("Xe1")]
        XgT = [rh.get(8 * CAP, BF16, (8, CAP)) for _ in range(2)]
        TXgT = [T("XgT0"), T("XgT1")]
        sg = [rh.get(CAP, F32) for _ in range(2)]
        Tsg = [T("sg0"), T("sg1")]
        hT = [rh.get(4 * CAP, BF16, (4, CAP)) for _ in range(2)]
        ThT = [T("hT0"), T("hT1")]
        yb = [rh.get(1024, F32) for _ in range(3)]
        Tyb = [T("yb0"), T("yb1"), T("yb2")]
        for b in range(2):
            P.op("pool", lambda e, b=b: e.memset(Xe[b], 0.0), writes=[TXe[b]])
        rx = Alloc(RX0, RX1)
        stg = [rx.get(4096, F32), rx.get(4096, F32), rh.get(4096, F32)]
        Tstg = [T("stg0"), T("stg1"), T("stg2")]
        cnt = 0
        ycnt = 0

        def prefetch(ex):
            b = ex % 2
            b4 = ex % 4
            P.dma("sp", lambda e, ex=ex, b4=b4: e.dma_start(
                out=sli[b4], in_=listd[ex * CAP:(ex + 1) * CAP, :].rearrange("(s p) c -> p s c", p=128)),
                f"sli{b4}", writes=[Tsli[b4]])
            srcs = (w_gate[ex].rearrange("(k p) f -> p k f", p=128), w_up[ex].rearrange("(k p) f -> p k f", p=128),
                    w_down[ex].rearrange("(k p) d -> p k d", p=128))
            views = (stg[0].rearrange("p (k f) -> p k f", f=512), stg[1].rearrange("p (k f) -> p k f", f=512),
                     stg[2].rearrange("p (k f) -> p k f", f=1024))
            for q in range(3):
                P.dma("sp", lambda e, q=q, srcs=srcs, views=views: e.dma_start(out=views[q], in_=srcs[q]), f"stg{q}",
                      writes=[Tstg[q]])
            cpy("dve", tki[b4], sli[b4][:, :, 0], [Tsli[b4]], [Ttki[b4]])
            for s in range(NS):
                P.dma("pool", lambda e, b=b, b4=b4, s=s: e.indirect_dma_start(
                    out=Xe[b][:, s, :], out_offset=None, in_=hn2d,
                    in_offset=bass.IndirectOffsetOnAxis(ap=tki[b4][:, s:s + 1], axis=0)),
                    f"xg{b}", reads=[Ttki[b4]], writes=[TXe[b]])

        def casts(ex):
            b = ex % 2
            v0 = stg[0].rearrange("p (k f) -> p k f", f=512)
            v1 = stg[1].rearrange("p (k f) -> p k f", f=512)
            v2 = stg[2].rearrange("p (k f) -> p k f", f=1024)
            cpy("act", wgb[b][:, 0:4, :], v0[:, 0:4, :], [Tstg[0]], [Twg[b]])
            cpy("dve", wgb[b][:, 4:8, :], v0[:, 4:8, :], [Tstg[0]], [Twg[b]])
            cpy("act", wub[b][:, 0:4, :], v1[:, 0:4, :], [Tstg[1]], [Twu[b]])
            cpy("dve", wub[b][:, 4:8, :], v1[:, 4:8, :], [Tstg[1]], [Twu[b]])
            cpy("act", wdb[b][:, 0:2, :], v2[:, 0:2, :], [Tstg[2]], [Twd[b]])
            cpy("dve", wdb[b][:, 2:4, :], v2[:, 2:4, :], [Tstg[2]], [Twd[b]])

        if n_experts > 0:
            prefetch(0)
            casts(0)
        for ex in range(n_experts):
            b = ex % 2
            b4 = ex % 4
            if ex + 1 < n_experts:
                prefetch(ex + 1)
            for s in range(NS):
                pT, TpT = next_pT()
                for k in range(8):
                    tr(pT[:, k, :], Xe[b][:, s, k * 128:(k + 1) * 128], identb, [TXe[b], Tconst], [TpT])
                cpy("act" if s % 2 == 0 else "dve", XgT[b][:, :, s * 128:(s + 1) * 128], pT, [TpT], [TXgT[b]])
            for f in range(4):
                pg, Tpg = pb[cnt % 2], Tpb[cnt % 2]
                pu, Tpu = pb[2 + cnt % 2], Tpb[2 + cnt % 2]
                s_, Ts_ = sg[cnt % 2], Tsg[cnt % 2]
                cnt += 1
                for k in range(8):
                    mm(pg[:, 0:CAP], wgb[b][:, k, f * 128:(f + 1) * 128], XgT[b][:, k, :], k == 0, k == 7,
                       [Twg[b], TXgT[b]], [Tpg])
                for k in range(8):
                    mm(pu[:, 0:CAP], wub[b][:, k, f * 128:(f + 1) * 128], XgT[b][:, k, :], k == 0, k == 7,
                       [Twu[b], TXgT[b]], [Tpu])
                act(s_, pg[:, 0:CAP], AF.Silu, [Tpg], [Ts_])
                tt("dve", hT[b][:, f, :], s_, pu[:, 0:CAP], ALU.mult, [Ts_, Tpu], [ThT[b]])
            ys = []
            for s in range(NS):
                yy, Tyy = yb[ycnt % 3], Tyb[ycnt % 3]
                ycnt += 1
                for half in range(2):
                    pd, Tpd = pb[4 + half], Tpb[4 + half]
                    for f in range(4):
                        mm(pd, hT[b][:, f, s * 128:(s + 1) * 128], wdb[b][:, f, half * 512:(half + 1) * 512],
                           f == 0, f == 3, [ThT[b], Twd[b]], [Tpd])
                    if half == 0:
                        act(yy[:, 0:512], pd, AF.Copy, [Tpd, Tsli[b4]], [Tyy], scale=sli[b4][:, s, 1:2])
                    else:
                        ts("dve", yy[:, 512:1024], pd, sli[b4][:, s, 1:2], ALU.mult, [Tpd, Tsli[b4]], [Tyy])
                ys.append((yy, Tyy, s))
            if ex + 1 < n_experts:
                casts(ex + 1)
            for yy, Tyy, s in ys:
                P.dma("pool", lambda e, b4=b4, s=s, yy=yy: e.indirect_dma_start(
                    out=hacc, out_offset=bass.IndirectOffsetOnAxis(ap=tki[b4][:, s:s + 1], axis=0),
                    in_=yy, in_offset=None, compute_op=ALU.add),
                    "ysc", reads=[Ttki[b4], Tyy], writes=[Thacc])

        rs2 = Alloc(RS20, RS21)
        fn = rs2.get(1024, F32)
        Tfn = T("fn")
        hb = [rs2.get(1024, F32) for _ in range(3)]
        Thb = [T("hb0"), T("hb1"), T("hb2")]
        ob = [rs2.get(1024, F32) for _ in range(2)]
        Tob = [T("ob0"), T("ob1")]
        Tout = T("out")
        P.barrier()
        P.dma("sp", lambda e: e.dma_start(out=fn, in_=fnd), "fn", writes=[Tfn])
        for i in range(16):
            j = i % 2
            j3 = i % 3
            P.dma("sp", lambda e, i=i, j3=j3: e.dma_start(out=hb[j3], in_=hacc[i * 128:(i + 1) * 128, :]), f"hb{j3}",
                  reads=[Thacc], writes=[Thb[j3]])
            sc_, Tsc_ = rmsnorm_stats(hb[j3], ob[j], 128, [Thb[j3]], Tob[j])
            stt("dve", ob[j], hb[j3], sc_[:, 2:3], fn, ALU.mult, ALU.mult, [Thb[j3], Tsc_, Tfn], [Tob[j]])
            P.dma("sp", lambda e, i=i, j=j: e.dma_start(out=out[i * 128:(i + 1) * 128, :], in_=ob[j]), f"out{j}",
                  reads=[Tob[j]], writes=[Tout])
        P.barrier(engs=("sp",))
        P.emit(st)
    return nc


_NC_CACHE = {}


def make_inputs(inputs):
    x = np.asarray(inputs["x"], np.float32)
    in_maps = []
    shared = {
        "w_in": np.ascontiguousarray(np.asarray(inputs["w_in"], np.float32)[0]),
        "w_out": np.ascontiguousarray(np.asarray(inputs["w_out"], np.float32)[0]),
        "w_gate": np.ascontiguousarray(np.asarray(inputs["w_gate"], np.float32)[0]),
        "w_up": np.ascontiguousarray(np.asarray(inputs["w_up"], np.float32)[0]),
        "w_down": np.ascontiguousarray(np.asarray(inputs["w_down"], np.float32)[0]),
        "fnorm": np.ascontiguousarray(np.broadcast_to(np.asarray(inputs["final_norm"], np.float32)[None, :], (128, D))),
        "gfb": np.ascontiguousarray(np.broadcast_to(np.asarray(inputs["norm_ffn"], np.float32)[0][None, :], (128, D))),
    }
    inp_np = {k: np.asarray(v, np.float32) for k, v in inputs.items() if k not in ("x", "w_in", "w_out", "w_gate", "w_up", "w_down")}
    for c in range(NCORES):
        b, s = c // 4, c % 4
        start = s * SPAN
        lo = start - NPREV * SPAN - HALO
        xe = np.zeros((NTOK_EXT, D), np.float32)
        src_lo = max(lo, 0)
        xe[src_lo - lo:, :] = x[b, src_lo:start + SPAN, :]
        m = dict(shared)
        m["xe"] = xe
        m["cp"] = make_cp(inp_np, c)
        in_maps.append(m)
    return in_maps


def kernel(**inputs):
    if "nc" not in _NC_CACHE:
        _NC_CACHE["nc"] = build()
    nc = _NC_CACHE["nc"]
    in_maps = make_inputs(inputs)
    res = run_bass_kernel_spmd(nc, in_maps, core_ids=list(range(NCORES)))
    outs = [np.asarray(res.results[c]["out"], np.float32) for c in range(NCORES)]
    full = np.stack(outs, 0).reshape(2, 4 * SPAN, D)
    return full
```

```python
from contextlib import ExitStack
import numpy as np
import concourse.bass as bass
import concourse.mybir as mybir
from concourse.bass_utils import run_bass_kernel_spmd

F32 = mybir.dt.float32
BF16 = mybir.dt.bfloat16
ALU = mybir.AluOpType
AF = mybir.ActivationFunctionType
AX = mybir.AxisListType

ENGS = ("pe", "act", "dve", "pool", "sp")
SEM_LIMIT = 12000
NCORES = 8
SPAN = 2048
NPREV = 3
HALO = 8
NTOK_EXT = HALO + (NPREV + 1) * SPAN
D = 1024
EPS = 1e-6
MOE_CAP = 256
I32 = mybir.dt.int32


class T:
    __slots__ = ("name", "w", "r", "rd")

    def __init__(self, name=""):
        self.name = name
        self.w = None
        self.r = {}
        self.rd = []


class Prog:
    def __init__(self, nc):
        self.nc = nc
        self.ops = []
        self.dma_sems = {}
        self.last = {}
        self.dmas = []

    def _deps(self, reads, writes):
        deps = set()
        raw = set()
        for t in reads:
            if t.w is not None:
                deps.add(t.w)
                raw.add(t.w)
        for t in writes:
            if t.w is not None:
                deps.add(t.w)
            deps.update(t.r.values())
            deps.update(t.rd)
        self._last_raw = raw
        return deps

    def op(self, eng, fn, reads=(), writes=()):
        i = len(self.ops)
        self.ops.append(dict(eng=eng, fn=fn, deps=self._deps(reads, writes), kind="c"))
        self.ops[-1]["raw"] = self._last_raw
        self.last[eng] = i
        for t in reads:
            t.r[eng] = i
        for t in writes:
            t.w = i
            t.r = {}
            t.rd = []
        return i

    def dma(self, eng, fn, sem, reads=(), writes=(), inc=16):
        i = len(self.ops)
        n = self.dma_sems.get(sem, 0) + inc
        self.dma_sems[sem] = n
        self.ops.append(dict(eng=eng, fn=fn, deps=self._deps(reads, writes), kind="d", sem=sem, val=n, inc=inc))
        self.dmas.append(i)
        for t in reads:
            t.rd.append(i)
        for t in writes:
            t.w = i
            t.r = {}
            t.rd = []
        return i

    def barrier(self, engs=ENGS):
        deps = set(self.last.values()) | set(self.dmas)
        self.dmas = []
        for e in engs:
            self.ops.append(dict(eng=e, fn=None, deps=set(deps), kind="c"))

    def emit(self, stack):
        nc = self.nc
        ops = self.ops
        needed = set()
        for o in ops:
            for d in o["deps"]:
                if ops[d]["kind"] == "c":
                    needed.add(d)
        cnt = {e: 0 for e in ENGS}
        for i, o in enumerate(ops):
            if o["kind"] == "c" and i in needed and o["fn"] is not None:
                cnt[o["eng"]] += 1
                o["cnt"] = cnt[o["eng"]]
        esems = {}
        for e in ENGS:
            n = cnt[e] // SEM_LIMIT + 1
            esems[e] = [stack.enter_context(nc.semaphore(f"s_{e}{k}")) for k in range(n)]
        dsems = {k: stack.enter_context(nc.semaphore(f"d_{k}")) for k in self.dma_sems}
        per = {e: [] for e in ENGS}
        for i, o in enumerate(ops):
            per[o["eng"]].append(i)

        def section(e):
            def body(eng):
                seen = {}
                for i in per[e]:
                    o = ops[i]
                    w = {}
                    for d in o["deps"]:
                        od = ops[d]
                        if od["kind"] == "c":
                            if od["fn"] is None or "cnt" not in od:
                                continue
                            if od["eng"] == e and e == "pe":
                                continue
                            if od["eng"] == e and "raw" in o and d not in o["raw"]:
                                continue
                            c = od["cnt"]
                            key = ("e", od["eng"], (c - 1) // SEM_LIMIT)
                            val = (c - 1) % SEM_LIMIT + 1
                        else:
                            key = ("d", od["sem"])
                            val = od["val"]
                        if w.get(key, 0) < val:
                            w[key] = val
                    for key, val in sorted(w.items(), key=lambda kv: str(kv[0])):
                        if seen.get(key, 0) >= val:
                            continue
                        seen[key] = val
                        if key[0] == "e":
                            sem = esems[key[1]][key[2]]
                            for kk in range(key[2]):
                                seen[("e", key[1], kk)] = SEM_LIMIT
                        else:
                            sem = dsems[key[1]]
                        eng.wait_ge(sem, val)
                    if o["fn"] is None:
                        continue
                    ins = o["fn"](eng)
                    if o["kind"] == "c":
                        if "cnt" in o:
                            c = o["cnt"]
                            ins.then_inc(esems[e][(c - 1) // SEM_LIMIT], 1)
                    else:
                        ins.then_inc(dsems[o["sem"]], o["inc"])
            return body

        with nc.Block() as block:
            block.tensor(section("pe"))
            block.scalar(section("act"))
            block.vector(section("dve"))
            block.gpsimd(section("pool"))
            block.sync(section("sp"))


CP = {}
_o = 0
for _n, _w in (("ident", 128), ("tri", 128), ("lstrict", 128), ("ones", 128), ("bd64", 128),
               ("gmix", 8), ("gffn", 8), ("gain16", 16), ("cwssd", 48), ("cbssd", 12), ("cwsc", 24),
               ("dtb", 16), ("alog", 16), ("dsk", 16), ("wr", 288), ("bmask", 4), ("ecap", 32), ("tokid", 16)):
    CP[_n] = (_o, _w)
    _o += _w
NCP = _o


def make_cp(inp, core):
    cp = np.zeros((128, NCP), np.float32)

    def put(name, arr):
        o, w = CP[name]
        cp[:, o:o + w] = np.asarray(arr, np.float32).reshape(128, w)

    idx = np.arange(128)
    put("ident", np.eye(128))
    put("tri", (idx[:, None] <= idx[None, :]))
    put("lstrict", (idx[:, None] > idx[None, :]))
    put("ones", np.ones((128, 128)))
    put("bd64", (idx[:, None] // 64 == idx[None, :] // 64))
    put("gmix", inp["norm_mix"][0].reshape(8, 128).T)
    put("gffn", inp["norm_ffn"][0].reshape(8, 128).T)
    put("gain16", np.concatenate([inp["ssd_norm"][0].reshape(8, 128).T, inp["sc_norm"][0].reshape(8, 128).T], 1))
    put("cwssd", inp["ssd_conv_w"][0].reshape(4, 12, 128).transpose(2, 1, 0))
    put("cbssd", inp["ssd_conv_b"][0].reshape(12, 128).T)
    put("cwsc", inp["sc_conv_w"][0].reshape(3, 8, 128).transpose(2, 1, 0))
    put("dtb", np.broadcast_to(inp["dt_bias"][0][None, :], (128, 16)))
    put("alog", np.broadcast_to(inp["a_log"][0][None, :], (128, 16)))
    put("dsk", np.broadcast_to(inp["d_skip"][0][None, :], (128, 16)))
    wr = np.concatenate([inp["w_router_group"][0],
                         inp["w_router_expert"][0].transpose(1, 0, 2).reshape(1024, 32)], 1)
    put("wr", wr.reshape(8, 128, 36).transpose(1, 0, 2))
    s = core % 4
    put("bmask", np.broadcast_to(np.array([1.0 if b >= NPREV - s else 0.0 for b in range(NPREV + 1)],
                                          np.float32)[None, :], (128, 4)))
    put("ecap", np.broadcast_to((np.arange(32, dtype=np.float32) * MOE_CAP)[None, :], (128, 32)))
    put("tokid", (np.arange(16)[None, :] * 128 + np.arange(128)[:, None]).astype(np.float32))
    return cp


def build(dbg=False, stop_after=None, n_experts=32):
    nc = bass.Bass("TRN2", target_bir_lowering=False)

    def dram(name, shape, kind="ExternalInput"):
        return nc.dram_tensor(name, shape, F32, kind=kind).ap()

    xe = dram("xe", [NTOK_EXT, D])
    cpd = dram("cp", [128, NCP])
    fnd = dram("fnorm", [128, D])
    gfd = dram("gfb", [128, D])
    w_in = dram("w_in", [D, 5648])
    w_out = dram("w_out", [2048, D])
    w_gate = dram("w_gate", [32, D, 512])
    w_up = dram("w_up", [32, D, 512])
    w_down = dram("w_down", [32, 512, D])
    out = dram("out", [SPAN, D], kind="ExternalOutput")
    dbg_out = {}

    P = Prog(nc)
    st = ExitStack()
    with st:
        ARENA_N = 53200
        arena = st.enter_context(nc.sbuf_tensor("arena", [128, ARENA_N], F32))
        pbt = [st.enter_context(nc.psum_tensor(f"pb{i}", [128, 512], F32)) for i in range(7)]
        pTb = st.enter_context(nc.psum_tensor("pTb", [128, 8, 128], BF16))
        pb = [t[:] for t in pbt]
        Tpb = [T(f"pb{i}") for i in range(7)]
        TpTb = T("pTb")

        a_f32 = arena[:]
        a_bf = arena[:].bitcast(BF16)
        a_i32 = arena[:].bitcast(mybir.dt.int32)

        class Alloc:
            def __init__(self, start, end):
                self.p = start
                self.end = end

            def get(self, n_elems, dt, shape=None):
                nb = n_elems * (2 if dt == BF16 else 4)
                nb_al = (nb + 63) // 64 * 64
                off = self.p
                assert off + nb_al <= self.end, ("arena overflow", off, nb_al, self.end)
                self.p += nb_al
                if dt == F32:
                    ap = a_f32[:, off // 4: off // 4 + n_elems]
                elif dt == mybir.dt.int32:
                    ap = a_i32[:, off // 4: off // 4 + n_elems]
                else:
                    ap = a_bf[:, off // 2: off // 2 + n_elems]
                if shape is not None and len(shape) == 2:
                    ap = ap.rearrange("p (a b) -> p a b", b=shape[1])
                return ap

        PERS0 = 0
        PERS1 = 13824
        RX0, RX1 = PERS1, PERS1 + 32768
        RH0, RH1 = RX1, RX1 + 65792
        RBC0, RBC1 = RH1, RH1 + 16384
        RSC0, RSC1 = RBC1, RBC1 + 32768
        RS20, RS21 = RSC1, ARENA_N * 4

        pers = Alloc(PERS0, PERS1)
        cp = pers.get(NCP, F32)
        Tcp = T("cp")

        def cpv(name, shape=None):
            o, w = CP[name]
            ap = cp[:, o:o + w]
            if shape is not None:
                ap = ap.rearrange("p (a b) -> p a b", b=shape[1])
            return ap

        identb = pers.get(128, BF16)
        bd64b = pers.get(128, BF16)
        Sst = pers.get(1024, F32)
        Sb = pers.get(1024, BF16)
        dtt = pers.get(256, F32, (16, 16))
        adt = pers.get(256, F32, (16, 16))
        avec = pers.get(16, F32)
        epsc = pers.get(1, F32)
        pers_stats = pers.get(64, F32)
        Tconst = T("const")
        TS_, TSb, Tdt, Tstats = T("S"), T("Sb"), T("dt"), T("stats")

        def act(out_, in_, func, reads, writes, **kw):
            P.op("act", lambda e: e.activation(out=out_, in_=in_, func=func, **kw), reads, writes)

        def tt(eng, out_, in0, in1, op, reads, writes):
            P.op(eng, lambda e: e.tensor_tensor(out=out_, in0=in0, in1=in1, op=op), reads, writes)

        def ts(eng, out_, in0, s1, op0, reads, writes, s2=None, op1=None):
            if op1 is None:
                P.op(eng, lambda e: e.tensor_scalar(out=out_, in0=in0, scalar1=s1, scalar2=None, op0=op0), reads, writes)
            else:
                P.op(eng, lambda e: e.tensor_scalar(out=out_, in0=in0, scalar1=s1, scalar2=s2, op0=op0, op1=op1), reads, writes)

        def stt(eng, out_, in0, scalar, in1, op0, op1, reads, writes):
            P.op(eng, lambda e: e.scalar_tensor_tensor(out=out_, in0=in0, scalar=scalar, in1=in1, op0=op0, op1=op1), reads, writes)

        def cpy(eng, out_, in_, reads, writes):
            if eng == "act":
                P.op("act", lambda e: e.copy(out=out_, in_=in_), reads, writes)
            else:
                P.op(eng, lambda e: e.tensor_copy(out=out_, in_=in_), reads, writes)

        def mm(out_, lhsT, rhs, start, stop, reads, writes):
            P.op("pe", lambda e: e.matmul(out_, lhsT=lhsT, rhs=rhs, start=start, stop=stop), reads, writes)

        def tr(out_, in_, ident, reads, writes):
            P.op("pe", lambda e: e.transpose(out=out_, in_=in_, identity=ident), reads, writes)

        stat_tiles = [pers_stats[:, 8 * q: 8 * q + 8] for q in range(8)]
        Tstat_tiles = [T(f"stat{q}") for q in range(8)]
        stc = [0]

        def stat_slot():
            q = stc[0] % 8
            stc[0] += 1
            return stat_tiles[q], Tstat_tiles[q]

        def rmsnorm_stats(x_ap, junk_ap, n, reads, Tjunk):
            sl_, Tsl_ = stat_slot()
            act(junk_ap, x_ap, AF.Square, reads, [Tjunk, Tsl_], accum_out=sl_[:n, 0:1])
            act(sl_[:n, 1:2], sl_[:n, 0:1], AF.Ln, [Tsl_, Tconst], [Tsl_], bias=epsc[:n, 0:1], scale=1.0 / D)
            act(sl_[:n, 2:3], sl_[:n, 1:2], AF.Exp, [Tsl_], [Tsl_], scale=-0.5)
            return sl_, Tsl_

        Tdump = T("dump")

        def dump(name, ap, shp, dt):
            if not dbg:
                return
            d = nc.dram_tensor("dbg_" + name, [128] + shp, dt, kind="ExternalOutput").ap()
            P.barrier()
            P.dma("sp", lambda e, d=d, ap=ap: e.dma_start(out=d, in_=ap), "dump", writes=[Tdump])
            P.barrier()

        P.dma("sp", lambda e: e.dma_start(out=cp, in_=cpd), "cp", writes=[Tcp])
        P.op("pool", lambda e: e.memset(epsc, EPS), writes=[Tconst])
        cpy("dve", identb, cpv("ident"), [Tcp], [Tconst])
        cpy("dve", bd64b, cpv("bd64"), [Tcp], [Tconst])
        act(avec, cpv("alog"), AF.Exp, [Tcp], [Tconst])
        ts("dve", avec, avec, -1.0, ALU.mult, [Tconst], [Tconst])
        P.op("pool", lambda e: e.memset(Sst, 0.0), writes=[TS_])

        rx = Alloc(RX0, RX1)
        xsT = rx.get(8 * 2048, BF16, (8, 2048))
        TxsT = [T(f"xsT{i}") for i in range(16)]
        rh = Alloc(RH0, RH1)
        siluz = rh.get(16 * 1024, BF16, (16, 1024))
        Tsiluz = [T(f"sz{i}") for i in range(16)]
        hnT = rh.get(8 * 2056, BF16, (8, 2056))
        ThnT = [T(f"hnT{i}") for i in range(17)]
        rbc = Alloc(RBC0, RBC1)
        BT = rbc.get(2 * 2048, BF16, (2, 2048))
        CT = rbc.get(2 * 2048, BF16, (2, 2048))
        TBT, TCT = T("BT"), T("CT")

        rs2 = Alloc(RS20, RS21)
        wts = [rs2.get(8 * 256, BF16, (8, 256)) for _ in range(4)]
        Twts = [T(f"wt{i}") for i in range(4)]
        U = [rs2.get(2056, BF16) for _ in range(3)]
        TU = [T(f"U{i}") for i in range(3)]
        xt = [rs2.get(1024, F32) for _ in range(2)]
        Txt = [T(f"xt{i}") for i in range(2)]
        dg = [rs2.get(4 * 128, BF16, (4, 128)) for _ in range(2)]
        Tdg = [T("dg0"), T("dg1")]
        acs_all = rs2.get(512, F32)
        dd_all = rs2.get(256, F32, (16, 16))
        ed_all = rs2.get(256, F32, (16, 16))
        eq_all = rs2.get(256, F32, (16, 16))
        ea_all = rs2.get(256, F32, (16, 16))
        LT = rs2.get(256, F32, (16, 16))
        wgt_all = rs2.get(256, F32, (16, 16))
        Tcum = T("cum")
        rsc = Alloc(RSC0, RSC1)
        xn = [rsc.get(1024, BF16) for _ in range(2)]
        Txn = [T(f"xn{i}") for i in range(2)]
        wdt = rsc.get(8 * 16, BF16, (8, 16))
        Twdt = T("wdt")
        dtmp = rsc.get(256, F32)
        Tdtmp = T("dtmp")
        xdt = [rsc.get(1024, BF16) for _ in range(2)]
        Txdt = [T(f"xdt{i}") for i in range(2)]
        xdtdec = rsc.get(1024, BF16)
        Txdtdec = T("xdtdec")
        xsD_ = [rsc.get(1024, BF16), rs2.get(1024, BF16)]
        TxsD_ = [T("xsD0"), T("xsD1")]
        Btok = [rsc.get(256, BF16) for _ in range(2)]
        TBtok = [T(f"Btok{i}") for i in range(2)]
        CBm = rsc.get(256, F32, (2, 128))
        TCBm = T("CBm")
        Rb = [rsc.get(512, F32, (4, 128)) for _ in range(2)]
        TRb = [T(f"R{i}") for i in range(2)]
        Eb = [rsc.get(512, BF16, (4, 128)) for _ in range(2)]
        TEb = [T(f"E{i}") for i in range(2)]
        MT = rsc.get(16 * 128, BF16, (16, 128))
        TMT = [T(f"MT{i}") for i in range(4)]
        ybuf = [rsc.get(1024, F32), a_f32[:, RSC0 // 4: RSC0 // 4 + 1024]]
        Tybuf = [T("y0"), T("y1")]
        ygn = rsc.get(1024, BF16)
        Tygn = T("ygn")

        pTs = [pTb[:], pb[6].bitcast(BF16).rearrange("p (k t) -> p k t", t=128),
               pb[3].bitcast(BF16).rearrange("p (k t) -> p k t", t=128),
               pb[2].bitcast(BF16).rearrange("p (k t) -> p k t", t=128)]
        TpTs = [TpTb, Tpb[6], Tpb[3], Tpb[2]]
        ptc = [0]
        pt_depth = [4]

        def next_pT():
            j = ptc[0] % pt_depth[0]
            ptc[0] += 1
            return pTs[j], TpTs[j]

        wctr = [0]

        def load_w(cols0, ncols):
            j = wctr[0] % 4
            wctr[0] += 1
            dst = wts[j][:, :, 0:ncols]
            src = w_in[:, cols0:cols0 + ncols].rearrange("(k p) c -> p k c", p=128)
            P.dma("pool", lambda e: e.dma_start(out=dst, in_=src), f"wt{j}", writes=[Twts[j]])
            return wts[j], Twts[j]

        xctr = [0]
        uctr = [0]
        dgc = [0]
        cvc = [0]

        def next_u():
            j = uctr[0] % 3
            uctr[0] += 1
            return U[j], TU[j]

        gmix_b = cpv("gmix").unsqueeze(2)
        identf = cpv("ident")

        def hn_tiles(tt4):
            return ThnT[1 + 4 * tt4: 5 + 4 * tt4]

        def proj_chunk(wt_ap, Tw, lc):
            for k in range(8):
                mm(pb[4][:, 0:HALO], wt_ap[:, k, lc * 128:(lc + 1) * 128], hnT[:, k, 0:HALO], k == 0, k == 7,
                   [Tw, ThnT[0]], [Tpb[4]])
            for t4 in range(4):
                for k in range(8):
                    mm(pb[t4], wt_ap[:, k, lc * 128:(lc + 1) * 128],
                       hnT[:, k, HALO + t4 * 512: HALO + (t4 + 1) * 512], k == 0, k == 7,
                       [Tw] + hn_tiles(t4), [Tpb[t4]])

        def evac_to_u(u, Tu, all_act=False):
            cpy("act", u[:, 0:HALO], pb[4][:, 0:HALO], [Tpb[4]], [Tu])
            for t4 in range(4):
                cpy("act" if (t4 % 2 == 0 or all_act) else "dve", u[:, HALO + t4 * 512: HALO + (t4 + 1) * 512], pb[t4],
                    [Tpb[t4]], [Tu])

        cacc = [a_f32[:, RH0 // 4 + q * 2048: RH0 // 4 + (q + 1) * 2048] for q in range(2)]
        Tcacc = [T("cacc0"), T("cacc1")]

        def make_diag(wcol, ntap):
            j = dgc[0] % 2
            dgc[0] += 1
            for jt in range(ntap):
                act(dg[j][:, jt, :], identf, AF.Copy, [Tcp], [Tdg[j]], scale=wcol(jt))
            return dg[j], Tdg[j]

        def conv_pe_bank():
            j = 5 + cvc[0] % 2
            cvc[0] += 1
            return pb[j], Tpb[j]

        def conv_pe(u, Tu, d, Td, ntap, t4):
            j = 5 + cvc[0] % 2
            cvc[0] += 1
            base = HALO - (ntap - 1) + t4 * 512
            for jt in range(ntap):
                mm(pb[j], d[:, jt, :], u[:, base + jt: base + jt + 512], jt == 0, jt == ntap - 1, [Td, Tu], [Tpb[j]])
            return pb[j], Tpb[j]

        a1_state = {}

        def a1_front(blk_, ti):
            row0_ = HALO + blk_ * SPAN
            n = HALO if ti == 0 else 128
            r0 = row0_ - HALO if ti == 0 else row0_ + (ti - 1) * 128
            j = xctr[0] % 2
            xctr[0] += 1
            xtj, xnj = xt[j], xn[j]
            P.dma("sp", lambda e, xtj=xtj, r0=r0, n=n: e.dma_start(out=xtj[:n, :], in_=xe[r0:r0 + n, :]),
                  f"xt{j}", writes=[Txt[j]])
            sc_, Tsc_ = rmsnorm_stats(xtj[:n, :], xnj[:n, :], n, [Txt[j]], Txn[j])
            ts("dve", xnj[:n, :], xtj[:n, :], sc_[:n, 2:3], ALU.mult, [Txt[j], Tsc_], [Txn[j]])
            a1_state[(blk_, ti)] = (j, n)

        def a1_back(blk_, ti):
            j, n = a1_state.pop((blk_, ti))
            xnj = xn[j]
            c0 = 0 if ti == 0 else HALO + (ti - 1) * 128
            pT, TpT = next_pT()
            for k in range(8):
                tr(pT[:, k, :n], xnj[:n, k * 128:(k + 1) * 128], identb[:n, :n], [Txn[j], Tconst], [TpT])
            tt("dve", hnT[:, :, c0:c0 + n], pT[:, :, :n], gmix_b.to_broadcast([128, 8, n]),
               ALU.mult, [TpT, Tcp], [ThnT[ti]])

        a1_front(0, 0)
        for ti in range(17):
            if ti + 1 < 17:
                a1_front(0, ti + 1)
            a1_back(0, ti)

        for blk in range(NPREV + 1):
            own = blk == NPREV
            row0 = HALO + blk * SPAN
            cwssd = cpv("cwssd")
            cbssd = cpv("cbssd")
            conv_chunks = list(range(8)) + [8, 9] + ([10, 11] if own else [])

            def conv_silu(c, u, Tu):
                acc, Tacc = cacc[c % 2], Tcacc[c % 2]
                base = HALO - 3
                ts("dve", acc, u[:, base:base + SPAN], cwssd[:, c * 4:c * 4 + 1], ALU.mult, [Tu, Tcp], [Tacc])
                for jt in range(1, 4):
                    stt("dve", acc, u[:, base + jt:base + jt + SPAN], cwssd[:, c * 4 + jt:c * 4 + jt + 1], acc,
                        ALU.mult, ALU.add, [Tu, Tcp, Tacc], [Tacc])
                if c < 8:
                    dst, Tdst = xsT[:, c, :], TxsT
                elif c < 10:
                    dst, Tdst = BT[:, c - 8, :], [TBT]
                else:
                    dst, Tdst = CT[:, c - 10, :], [TCT]
                act(dst, acc, AF.Silu, [Tacc, Tcp], Tdst, bias=cbssd[:, c:c + 1])

            pending = None
            for g0 in range(0, len(conv_chunks), 2):
                cc0 = conv_chunks[g0]
                wt_ap, Tw = load_w(1024 + cc0 * 128, 256)
                for lc in range(2):
                    c = cc0 + lc
                    proj_chunk(wt_ap, Tw, lc)
                    u, Tu = next_u()
                    evac_to_u(u, Tu, all_act=True)
                    if pending is not None:
                        conv_silu(*pending)
                    pending = (c, u, Tu)
            conv_silu(*pending)

            src = w_in[:, 2560:2576].rearrange("(k p) c -> p k c", p=128)
            P.dma("pool", lambda e, src=src: e.dma_start(out=wdt, in_=src), "wdt", writes=[Twdt])
            for i in range(16):
                for k in range(8):
                    mm(pb[4][:, 16 + i * 16: 32 + i * 16], hnT[:, k, HALO + i * 128: HALO + (i + 1) * 128], wdt[:, k, :],
                       k == 0, k == 7, [ThnT[1 + i], Twdt], [Tpb[4]])
            dtb_b = cpv("dtb").unsqueeze(1).to_broadcast([128, 16, 16])
            dt3 = dtmp.rearrange("p (a b) -> p a b", b=16)
            tt("dve", dt3, pb[4][:, 16:272].rearrange("p (a b) -> p a b", b=16), dtb_b, ALU.add, [Tpb[4], Tcp], [Tdtmp])
            act(dtmp, dtmp, AF.Exp, [Tdtmp], [Tdtmp])
            act(dtmp, dtmp, AF.Ln, [Tdtmp], [Tdtmp], bias=1.0)
            bm = cpv("bmask")
            ts("dve", dtt, dt3, bm[:, blk:blk + 1], ALU.mult, [Tdtmp, Tcp], [Tdt])
            tt("dve", adt, dtt, avec.unsqueeze(1).to_broadcast([128, 16, 16]), ALU.mult, [Tdt, Tconst], [Tdt])
            adt_f = adt.rearrange("p a b -> p (a b)")
            mm(pb[4][:, 0:256], cpv("tri"), adt_f, True, True, [Tcp, Tdt], [Tpb[4]])
            mm(pb[4][:, 256:512], cpv("ones"), adt_f, True, True, [Tcp, Tdt], [Tpb[4]])
            cpy("act", acs_all, pb[4], [Tpb[4]], [Tcum])
            ac3 = acs_all[:, 0:256].rearrange("p (a b) -> p a b", b=16)
            aq3 = acs_all[:, 256:512].rearrange("p (a b) -> p a b", b=16)
            tt("dve", dd_all, aq3, ac3, ALU.subtract, [Tcum], [Tcum])

            tri = cpv("tri")
            if not own:
                P.op("pool", lambda e: e.memset(LT[:, 15, :], 0.0), [Tcum], [Tcum])
                for i in range(14, -1, -1):
                    tt("pool", LT[:, i, :], LT[:, i + 1, :], aq3[:, i + 1, :], ALU.add, [Tcum], [Tcum])
                tt("dve", dd_all, dd_all, LT, ALU.add, [Tcum], [Tcum])
                act(ed_all, dd_all, AF.Exp, [Tcum], [Tcum])
                tt("dve", wgt_all, ed_all, dtt, ALU.mult, [Tcum, Tdt], [Tcum])
                tt("pool", eq_all[:, 0, :], LT[:, 0, :], aq3[:, 0, :], ALU.add, [Tcum], [Tcum])
                act(eq_all[:, 0, :], eq_all[:, 0, :], AF.Exp, [Tcum], [Tcum])
                for i in range(16):
                    tok = slice(i * 128, (i + 1) * 128)
                    xd, Txd = xdt[i % 2], Txdt[i % 2]
                    bt, Tbt = Btok[i % 2], TBtok[i % 2]
                    pT, TpT = next_pT()
                    for c in range(8):
                        tr(pT[:, c, :], xsT[:, c, tok], identb, [TxsT[i], Tconst], [TpT])
                    pX = pT.rearrange("p c t -> p (c t)").rearrange("p (h d) -> p h d", d=64)
                    tt("dve", xd.rearrange("p (h d) -> p h d", d=64), pX,
                       wgt_all[:, i, :].unsqueeze(2).to_broadcast([128, 16, 64]), ALU.mult, [TpT, Tcum], [Txd])
                    pT2, TpT2 = next_pT()
                    for g in range(2):
                        tr(pT2[:, g, :], BT[:, g, tok], identb, [TBT, Tconst], [TpT2])
                    cpy("act", bt.rearrange("p (g n) -> p g n", n=128), pT2[:, 0:2, :], [TpT2], [Tbt])
                    for g in range(2):
                        mm(pb[g], bt[:, g * 128:(g + 1) * 128], xd[:, g * 512:(g + 1) * 512], i == 0, i == 15,
                           [Tbt, Txd], [Tpb[g]])
                    if i == 0:
                        a1_front(blk + 1, 0)
                    a1_front(blk + 1, i + 1)
                    a1_back(blk + 1, i)
                a1_back(blk + 1, 16)
                tt("pool", Sst.rearrange("p (h d) -> p h d", d=64), Sst.rearrange("p (h d) -> p h d", d=64),
                   eq_all[:, 0, :].unsqueeze(2).to_broadcast([128, 16, 64]), ALU.mult, [TS_, Tcum], [TS_])
                for g in range(2):
                    tt("dve", Sst[:, g * 512:(g + 1) * 512], Sst[:, g * 512:(g + 1) * 512], pb[g], ALU.add,
                       [TS_, Tpb[g]], [TS_])
                continue

            act(ed_all, dd_all, AF.Exp, [Tcum], [Tcum])
            act(eq_all, aq3, AF.Exp, [Tcum], [Tcum])
            act(ea_all, ac3, AF.Exp, [Tcum], [Tcum])
            for q in range(4):
                wt_ap, Tw = load_w(q * 256, 256)
                for i in range(16):
                    pz, Tpz = pb[5 + i % 2], Tpb[5 + i % 2]
                    for k in range(8):
                        mm(pz[:, 0:256], hnT[:, k, HALO + i * 128: HALO + (i + 1) * 128], wt_ap[:, k, :], k == 0, k == 7,
                           [ThnT[1 + i], Tw], [Tpz])
                    act(siluz[:, i, q * 256:(q + 1) * 256], pz[:, 0:256], AF.Silu, [Tpz], [Tsiluz[i], Tcacc[0], Tcacc[1]])
            cpy("act", Sb, Sst, [TS_], [TSb])

            pt_depth[0] = 2
            xdtdec_ = [xdtdec, U[2][:, 0:1024]]
            Txdtdec_ = [Txdtdec, T("xdtdec1")]
            MT_ = [MT, U[1][:, 0:2048].rearrange("p (h l) -> p h l", l=128)]
            TMT_ = [TMT, [T(f"MTb{q}") for q in range(4)]]

            def stageA(i):
                tok = slice(i * 128, (i + 1) * 128)
                xd, Txd = xdt[i % 2], Txdt[i % 2]
                bt, Tbt = Btok[i % 2], TBtok[i % 2]
                xsD, TxsD = xsD_[i % 2], TxsD_[i % 2]
                xdd, Txdd = xdtdec_[i % 2], Txdtdec_[i % 2]
                pT, TpT = next_pT()
                for c in range(8):
                    tr(pT[:, c, :], xsT[:, c, tok], identb, [TxsT[i], Tconst], [TpT])
                pX = pT.rearrange("p c t -> p (c t)").rearrange("p (h d) -> p h d", d=64)
                tt("dve", xd.rearrange("p (h d) -> p h d", d=64), pX, dtt[:, i, :].unsqueeze(2).to_broadcast([128, 16, 64]),
                   ALU.mult, [TpT, Tdt], [Txd])
                tt("dve", xsD.rearrange("p (h d) -> p h d", d=64), pX,
                   cpv("dsk").unsqueeze(2).to_broadcast([128, 16, 64]), ALU.mult, [TpT, Tcp], [TxsD])
                pT2, TpT2 = next_pT()
                for g in range(2):
                    tr(pT2[:, g, :], BT[:, g, tok], identb, [TBT, Tconst], [TpT2])
                cpy("act", bt.rearrange("p (g n) -> p g n", n=128), pT2[:, 0:2, :], [TpT2], [Tbt])
                tt("dve", xdd.rearrange("p (h d) -> p h d", d=64), xd.rearrange("p (h d) -> p h d", d=64),
                   ed_all[:, i, :].unsqueeze(2).to_broadcast([128, 16, 64]), ALU.mult, [Txd, Tcum], [Txdd])
                pc, Tpc = pb[5], Tpb[5]
                for g in range(2):
                    mm(pc[:, g * 128:(g + 1) * 128], BT[:, g, tok], CT[:, g, tok], True, True, [TBT, TCT], [Tpc])
                tt("dve", CBm, pc[:, 0:256].rearrange("p (g l) -> p g l", l=128),
                   tri.unsqueeze(1).to_broadcast([128, 2, 128]), ALU.mult, [Tpc, Tcp], [TCBm])
                for hq in range(4):
                    g = hq // 2
                    R, TR = Rb[hq % 2], TRb[hq % 2]
                    E, TE = Eb[hq % 2], TEb[hq % 2]
                    pg, Tpg = (pb[4], Tpb[4]) if hq % 2 == 0 else (pb[5], Tpb[5])
                    tt("pool", R, tri.unsqueeze(1).to_broadcast([128, 4, 128]),
                       adt[:, i, hq * 4:(hq + 1) * 4].unsqueeze(2).to_broadcast([128, 4, 128]), ALU.mult,
                       [Tcp, Tdt], [TR])
                    mm(pg, cpv("lstrict"), R.rearrange("p h l -> p (h l)"), True, True, [Tcp, TR], [Tpg])
                    act(E.rearrange("p h l -> p (h l)"), pg, AF.Exp, [Tpg], [TE])
                    tt("dve", MT_[i % 2][:, hq * 4:(hq + 1) * 4, :], E, CBm[:, g, :].unsqueeze(1).to_broadcast([128, 4, 128]),
                       ALU.mult, [TE, TCBm], [TMT_[i % 2][hq]])

            def stageB(i):
                tok = slice(i * 128, (i + 1) * 128)
                bt, Tbt = Btok[i % 2], TBtok[i % 2]
                xdd, Txdd = xdtdec_[i % 2], Txdtdec_[i % 2]
                y, Ty = ybuf[i % 2], Tybuf[i % 2]
                xd, Txd = xdt[i % 2], Txdt[i % 2]
                for g in range(2):
                    py, Tpy = pb[2 + g], Tpb[2 + g]
                    for r in range(8):
                        hh = g * 8 + r
                        mm(py[:, r * 64:(r + 1) * 64], MT_[i % 2][:, hh, :], xd[:, hh * 64:(hh + 1) * 64], True, True,
                           [TMT_[i % 2][hh // 4], Txd], [Tpy])
                for g in range(2):
                    mm(pb[1] if g == 0 else pb[0], CT[:, g, tok], Sb[:, g * 512:(g + 1) * 512], True, True,
                       [TCT, TSb], [Tpb[1] if g == 0 else Tpb[0]])
                for g in range(2):
                    tt("dve", y[:, g * 512:(g + 1) * 512].rearrange("p (h d) -> p h d", d=64),
                       (pb[1] if g == 0 else pb[0]).rearrange("p (h d) -> p h d", d=64),
                       ea_all[:, i, g * 8:(g + 1) * 8].unsqueeze(2).to_broadcast([128, 8, 64]), ALU.mult,
                       [Tpb[1] if g == 0 else Tpb[0], Tcum], [Ty])
                for g in range(2):
                    mm(pb[g], bt[:, g * 128:(g + 1) * 128], xdd[:, g * 512:(g + 1) * 512], True, True,
                       [Tbt, Txdd], [Tpb[g]])
                tt("pool", Sst.rearrange("p (h d) -> p h d", d=64), Sst.rearrange("p (h d) -> p h d", d=64),
                   eq_all[:, i, :].unsqueeze(2).to_broadcast([128, 16, 64]), ALU.mult, [TS_, Tcum], [TS_])
                for g in range(2):
                    tt("dve", Sst[:, g * 512:(g + 1) * 512], Sst[:, g * 512:(g + 1) * 512], pb[g], ALU.add,
                       [TS_, Tpb[g]], [TS_])
                cpy("act", Sb, Sst, [TS_, TSb], [TSb])
                for g in range(2):
                    py, Tpy = pb[2 + g], Tpb[2 + g]
                    tt("dve", y[:, g * 512:(g + 1) * 512], y[:, g * 512:(g + 1) * 512], py, ALU.add, [Ty, Tpy], [Ty])

            def stageC(i):
                tok = slice(i * 128, (i + 1) * 128)
                xsD, TxsD = xsD_[i % 2], TxsD_[i % 2]
                y, Ty = ybuf[i % 2], Tybuf[i % 2]
                tt("dve", y, y, xsD, ALU.add, [Ty, TxsD], [Ty])
                tt("dve", y, y, siluz[:, i, :], ALU.mult, [Ty, Tsiluz[i]], [Ty])
                sc_, Tsc_ = stat_slot()
                for g in range(2):
                    act(ygn[:, g * 512:(g + 1) * 512], y[:, g * 512:(g + 1) * 512], AF.Square, [Ty], [Tygn, Tsc_],
                        accum_out=sc_[:, g:g + 1])
                act(sc_[:, 2:4], sc_[:, 0:2], AF.Ln, [Tsc_, Tconst], [Tsc_], bias=epsc[:, 0:1], scale=1.0 / 512)
                act(sc_[:, 4:6], sc_[:, 2:4], AF.Exp, [Tsc_], [Tsc_], scale=-0.5)
                for g in range(2):
                    act(ygn[:, g * 512:(g + 1) * 512], y[:, g * 512:(g + 1) * 512], AF.Copy, [Ty, Tsc_], [Tygn],
                        scale=sc_[:, 4 + g:5 + g])
                pT3, TpT3 = next_pT()
                for c in range(8):
                    tr(pT3[:, c, :], ygn[:, c * 128:(c + 1) * 128], identb, [Tygn, Tconst], [TpT3])
                cpy("act", xsT[:, :, tok], pT3, [TpT3], [TxsT[i]])

            stageA(0)
            for i in range(16):
                if i + 1 < 16:
                    stageA(i + 1)
                stageB(i)
                stageC(i)

        dump("mixT_ssd", xsT, [8, 2048], BF16)
        dump("S", Sst, [1024], F32)

        P.barrier()
        rsc = Alloc(RSC0, RSC1)
        scT = rsc.get(8 * 2048, BF16, (8, 2048))
        TscT = [T(f"scT{i}") for i in range(16)]
        rbc = Alloc(RBC0, RBC1)
        sq = [rbc.get(512, BF16) for _ in range(2)]
        Tsq = [T("sq0"), T("sq1")]
        rt = [rbc.get(512, F32) for _ in range(2)]
        Trt = [T("rt0"), T("rt1")]
        cvt = [rbc.get(512, F32) for _ in range(2)]
        Tcvt = [T("cvt0"), T("cvt1")]
        scb = [rbc.get(512, F32) for _ in range(2)]
        Tscb = [T("scb0"), T("scb1")]
        cwsc = cpv("cwsc")
        o3 = 1024 + 1536 + 16
        rhs_ = Alloc(RH0, RH0 + 32768)
        scw = [rhs_.get(8 * 256, BF16, (8, 256)) for _ in range(6)]
        Tscw = [T(f"scw{q}") for q in range(6)]

        def load_sc(jp):
            res = []
            for q, c0_ in enumerate((o3 + jp * 256, o3 + 2048 + jp * 256, o3 + 1024 + jp * 256)):
                bi = (jp % 2) * 3 + q
                src_ = w_in[:, c0_:c0_ + 256].rearrange("(k p) c -> p k c", p=128)
                P.dma("pool", lambda e, bi=bi, src_=src_: e.dma_start(out=scw[bi], in_=src_), f"scw{bi}", writes=[Tscw[bi]])
                res.append((scw[bi], Tscw[bi]))
            return res

        sc_loaded = {0: load_sc(0)}
        for jp in range(4):
            if jp + 1 < 4:
                sc_loaded[jp + 1] = load_sc(jp + 1)
            (wb, Twb), (wv, Twv), (wc, Twc) = sc_loaded[jp]
            for lc in range(2):
                j = jp * 2 + lc
                proj_chunk(wb, Twb, lc)
                ub, Tub = next_u()
                evac_to_u(ub, Tub)
                proj_chunk(wv, Twv, lc)
                u, Tu = next_u()
                evac_to_u(u, Tu)
                proj_chunk(wc, Twc, lc)
                tt("dve", u[:, 0:HALO], u[:, 0:HALO], pb[4][:, 0:HALO], ALU.mult, [Tu, Tpb[4]], [Tu])
                for t4 in range(4):
                    sl = slice(HALO + t4 * 512, HALO + (t4 + 1) * 512)
                    tt("dve", u[:, sl], u[:, sl], pb[t4], ALU.mult, [Tu, Tpb[t4]], [Tu])
                d, Td = make_diag(lambda jt, j=j: cwsc[:, j * 3 + jt:j * 3 + jt + 1], 3)
                scb4 = [scb[0], scb[1], cvt[0], cvt[1]]
                Tscb4 = [Tscb[0], Tscb[1], Tcvt[0], Tcvt[1]]

                def sc_stage1(t4, u=u, Tu=Tu, d=d, Td=Td, ub=ub, Tub=Tub):
                    slh = slice(HALO + t4 * 512, HALO + (t4 + 1) * 512)
                    b2 = t4 % 2
                    pc_, Tpc_ = conv_pe(u, Tu, d, Td, 3, t4)
                    tt("dve", scb4[t4], pc_, ub[:, slh], ALU.mult, [Tpc_, Tub], [Tscb4[t4]])
                    act(sq[b2], scb4[t4], AF.Square, [Tscb4[t4]], [Tsq[b2]])

                def sc_stage2(t4, j=j):
                    sl = slice(t4 * 512, (t4 + 1) * 512)
                    b2 = t4 % 2
                    pz, Tpz = conv_pe_bank()
                    mm(pz, bd64b, sq[b2], True, True, [Tconst, Tsq[b2]], [Tpz])
                    act(rt[b2], pz, AF.Ln, [Tpz, Tconst], [Trt[b2]], bias=epsc[:, 0:1], scale=1.0 / 64)
                    act(rt[b2], rt[b2], AF.Exp, [Trt[b2]], [Trt[b2]], scale=-0.5)
                    tt("pool", scT[:, j, sl], scb4[t4], rt[b2], ALU.mult, [Tscb4[t4], Trt[b2]], TscT[4 * t4:4 * t4 + 4])

                sc_stage1(0)
                sc_stage1(1)
                sc_stage2(0)
                sc_stage1(2)
                sc_stage2(1)
                sc_stage1(3)
                sc_stage2(2)
                sc_stage2(3)
        dump("scT", scT, [8, 2048], BF16)

        P.barrier()
        CAP = MOE_CAP
        NS = CAP // 128
        hn2d = nc.dram_tensor("hn2d", [SPAN + 128, D], BF16).ap()
        hacc = nc.dram_tensor("hacc", [SPAN + 128, D], F32).ap()
        listd = nc.dram_tensor("listd", [32 * CAP + 128, 16], F32).ap()
        Thacc, Thn2d, Tlistd = T("hacc"), T("hn2d"), T("listd")
        rh = Alloc(RH0, RH1)
        h = rh.get(16 * 1024, F32, (16, 1024))
        Th = [T(f"h{i}") for i in range(16)]
        rs2 = Alloc(RS20, RS21)
        wo = [rs2.get(16 * 512, BF16, (16, 512)) for _ in range(2)]
        Two = [T("wo0"), T("wo1")]
        xr = [rs2.get(1024, F32) for _ in range(2)]
        Txr = [T("xr0"), T("xr1")]
        RS2_ROUTE_END = rs2.p
        hnb = [rs2.get(1024, BF16) for _ in range(2)]
        Thnb = [T("hnb0"), T("hnb1")]
        gfb = rs2.get(1024, BF16)
        Tgfb = T("gfb")
        L = rs2.get(16 * 36, F32, (16, 36))
        TL = T("L")
        rbc = Alloc(RBC0, RBC1)
        hnf = [rbc.get(1024, F32) for _ in range(2)]
        Thnf = [T("hnf0"), T("hnf1")]
        hTf = [rbc.get(1024, F32, (8, 128)) for _ in range(2)]
        ThTf = [T("hTf0"), T("hTf1")]
        gffn_b = cpv("gffn").unsqueeze(2).to_broadcast([128, 8, 128])
        wr = cpv("wr", (8, 36))
        gain_b = cpv("gain16").unsqueeze(2).to_broadcast([128, 16, 512])
        own_row0 = HALO + NPREV * SPAN
        for half in range(2):
            src = w_out[:, half * 512:(half + 1) * 512].rearrange("(k p) c -> p k c", p=128)
            P.dma("pool", lambda e, src=src, half=half: e.dma_start(out=wo[half], in_=src), f"wo{half}", writes=[Two[half]])
            if half == 0:
                tt("dve", wo[half], wo[half], gain_b, ALU.mult, [Two[half], Tcp], [Two[half]])
            else:
                g16 = cpv("gain16")
                for k in range(16):
                    act(wo[half][:, k, :], wo[half][:, k, :], AF.Copy, [Two[half], Tcp], [Two[half]], scale=g16[:, k:k + 1])
        P.dma("pool", lambda e: e.dma_start(out=gfb, in_=gfd), "gfb", writes=[Tgfb])
        P.op("pool", lambda e: e.memset(hnb[1], 0.0), writes=[Thnb[1]])
        P.dma("sp", lambda e: e.dma_start(out=hn2d[SPAN:SPAN + 128, :], in_=hnb[1]), "hn2dz", reads=[Thnb[1]], writes=[])

        def n2_stage1(i):
            j = i % 2
            P.dma("sp", lambda e, i=i: e.dma_start(out=hacc[i * 128:(i + 1) * 128, :], in_=h[:, i, :]), "hacc0",
                  reads=[Th[i]], writes=[])
            sc_, Tsc_ = rmsnorm_stats(h[:, i, :], hnf[j], 128, [Th[i]], Thnf[j])
            ts("dve", hnf[j], h[:, i, :], sc_[:, 2:3], ALU.mult, [Th[i], Tsc_], [Thnf[j]])
            tt("pool", hnb[j], hnf[j], gfb, ALU.mult, [Thnf[j], Tgfb], [Thnb[j]])
            P.dma("sp", lambda e, i=i, j=j: e.dma_start(out=hn2d[i * 128:(i + 1) * 128, :], in_=hnb[j]), f"hn2d{j}",
                  reads=[Thnb[j]], writes=[])

        def n2_stage2(i):
            j = i % 2
            for hf in range(2):
                pz, Tpz = pb[4 + hf], Tpb[4 + hf]
                for k4 in range(4):
                    k = hf * 4 + k4
                    tr(pz[:, k4 * 128:(k4 + 1) * 128], hnf[j][:, k * 128:(k + 1) * 128], identf, [Thnf[j], Tcp], [Tpz])
                tt("dve", hTf[j][:, hf * 4:(hf + 1) * 4, :], pz.rearrange("p (k t) -> p k t", t=128),
                   gffn_b[:, hf * 4:(hf + 1) * 4, :], ALU.mult, [Tpz, Tcp], [ThTf[j]])

        def n2_stage3(i):
            j = i % 2
            pr, Tpr = pb[6], Tpb[6]
            for k in range(8):
                mm(pr[:, 0:36], hTf[j][:, k, :], wr[:, k, :], k == 0, k == 7, [ThTf[j], Tcp], [Tpr])
            cpy("act", L[:, i, :], pr[:, 0:36], [Tpr], [TL])

        for i in range(16):
            tok = slice(i * 128, (i + 1) * 128)
            pzs = [(pb[(2 * i) % 4], Tpb[(2 * i) % 4]), (pb[(2 * i + 1) % 4], Tpb[(2 * i + 1) % 4])]
            for k in range(16):
                lhs = xsT[:, k, tok] if k < 8 else scT[:, k - 8, tok]
                for half in range(2):
                    mm(pzs[half][0], lhs, wo[half][:, k, :], k == 0, k == 15, [TxsT[i], TscT[i], Two[half]], [pzs[half][1]])
            jx = i % 2
            P.dma("sp", lambda e, jx=jx, i=i: e.dma_start(
                out=xr[jx], in_=xe[own_row0 + i * 128: own_row0 + (i + 1) * 128, :]), f"xr{jx}", writes=[Txr[jx]])
            for half in range(2):
                tt("dve", h[:, i, half * 512:(half + 1) * 512], pzs[half][0], xr[jx][:, half * 512:(half + 1) * 512], ALU.add,
                   [pzs[half][1], Txr[jx]], [Th[i]])
            n2_stage1(i)
            if i >= 1:
                n2_stage2(i - 1)
            if i >= 2:
                n2_stage3(i - 2)
        n2_stage2(15)
        n2_stage3(14)
        n2_stage3(15)
        dump("h", h, [16, 1024], F32)

        P.barrier()
        rs2 = Alloc(RS20, RS2_ROUTE_END)
        r16 = [rs2.get(16, F32) for _ in range(10)]
        r64 = [rs2.get(64, F32, (16, 4)) for _ in range(3)]
        r512 = [rs2.get(512, F32, (16, 32)) for _ in range(7)]
        idxi = [rs2.get(16, I32) for _ in range(2)]
        rows = [rs2.get(256, F32, (16, 16)) for _ in range(2)]
        linit = rs2.get(32 * CAP // 128 * 16, F32)
        Trt_ = T("routetmp")
        nrow_init = 32 * CAP // 128
        li3 = linit[:, 0:nrow_init * 16].rearrange("p (a b) -> p a b", b=16)
        P.op("pool", lambda e: e.memset(li3, 0.0), writes=[Trt_])
        ts("pool", li3[:, :, 0:1], cpv("tokid")[:, 0:1].unsqueeze(1).to_broadcast([128, nrow_init, 1]), float(SPAN), ALU.add,
           [Trt_, Tcp], [Trt_])
        P.dma("sp", lambda e: e.dma_start(out=listd[0:32 * CAP, :].rearrange("(a p) c -> p a c", p=128), in_=li3), "linit",
              reads=[Trt_], writes=[Tlistd])
        gl = L[:, :, 0:4]
        el = L[:, :, 4:36]
        gmax, gsum, gw, m1, m2, dd2, p1, p2, ix1, ix2 = r16
        goh, gex, pen = r64
        msk, oh1, oh2, tmp5, posv, pre, oh_keep = r512
        dummyp = r16[0][:, 0:1] if False else rs2.get(1, F32)
        RT = [TL, Trt_]
        P.op("dve", lambda e: e.tensor_reduce(out=gmax, in_=gl, axis=AX.X, op=ALU.max), [TL], [Trt_])
        tt("dve", gex, gl, gmax.unsqueeze(2).to_broadcast([128, 16, 4]), ALU.subtract, RT, [Trt_])
        tt("dve", goh, gl, gmax.unsqueeze(2).to_broadcast([128, 16, 4]), ALU.is_equal, RT, [Trt_])
        act(gex, gex, AF.Exp, [Trt_], [Trt_])
        P.op("dve", lambda e: e.tensor_reduce(out=gsum, in_=gex, axis=AX.X, op=ALU.add), [Trt_], [Trt_])
        P.op("dve", lambda e: e.reciprocal(out=gw, in_=gsum), [Trt_], [Trt_])
        ts("dve", pen, goh, -1.0, ALU.add, [Trt_], [Trt_], s2=1e30, op1=ALU.mult)
        tt("dve", msk.rearrange("p a (g e) -> p a g e", e=8), el.rearrange("p a (g e) -> p a g e", e=8),
           pen.unsqueeze(3).to_broadcast([128, 16, 4, 8]), ALU.add, RT, [Trt_])
        P.op("dve", lambda e: e.tensor_reduce(out=m1, in_=msk, axis=AX.X, op=ALU.max), [Trt_], [Trt_])
        tt("dve", oh1, msk, m1.unsqueeze(2).to_broadcast([128, 16, 32]), ALU.is_equal, [Trt_], [Trt_])
        stt("dve", tmp5, oh1, -1e30, msk, ALU.mult, ALU.add, [Trt_], [Trt_])
        P.op("dve", lambda e: e.tensor_reduce(out=m2, in_=tmp5, axis=AX.X, op=ALU.max), [Trt_], [Trt_])
        tt("dve", oh2, tmp5, m2.unsqueeze(2).to_broadcast([128, 16, 32]), ALU.is_equal, [Trt_], [Trt_])
        tt("dve", dd2, m2, m1, ALU.subtract, [Trt_], [Trt_])
        act(dd2, dd2, AF.Exp, [Trt_], [Trt_])
        ts("dve", p1, dd2, 1.0, ALU.add, [Trt_], [Trt_])
        P.op("dve", lambda e: e.reciprocal(out=p1, in_=p1), [Trt_], [Trt_])
        tt("dve", p2, dd2, p1, ALU.mult, [Trt_], [Trt_])
        tt("dve", p1, p1, gw, ALU.mult, [Trt_], [Trt_])
        tt("dve", p2, p2, gw, ALU.mult, [Trt_], [Trt_])
        tt("dve", tmp5, oh1, oh2, ALU.add, [Trt_], [Trt_])
        oh_f = tmp5.rearrange("p a b -> p (a b)")
        mm(pb[4], cpv("tri"), oh_f, True, True, [Tcp, Trt_], [Tpb[4]])
        mm(pb[5], cpv("ones"), oh_f, True, True, [Tcp, Trt_], [Tpb[5]])
        cpy("act", msk.rearrange("p a b -> p (a b)"), pb[5], [Tpb[5]], [Trt_])
        P.op("pool", lambda e: e.memset(pre[:, 0, :], 0.0), [Trt_], [Trt_])
        for i in range(1, 16):
            tt("pool", pre[:, i, :], pre[:, i - 1, :], msk[:, i - 1, :], ALU.add, [Trt_], [Trt_])
        stt("dve", posv.rearrange("p a b -> p (a b)"), pb[4], -1.0, pre.rearrange("p a b -> p (a b)"), ALU.add, ALU.add,
            [Tpb[4], Trt_], [Trt_])
        ts("dve", msk, posv, float(CAP), ALU.is_ge, [Trt_], [Trt_])
        tt("dve", posv, posv, cpv("ecap").unsqueeze(1).to_broadcast([128, 16, 32]), ALU.add, [Trt_, Tcp], [Trt_])
        ts("dve", oh_keep, msk, -1.0, ALU.mult, [Trt_], [Trt_], s2=1.0, op1=ALU.add)
        tt("dve", posv, posv, oh_keep, ALU.mult, [Trt_], [Trt_])
        ts("pool", dummyp, cpv("tokid")[:, 0:1], float(32 * CAP), ALU.add, [Tcp], [Trt_])
        stt("dve", posv, msk, dummyp, posv, ALU.mult, ALU.add, [Trt_], [Trt_])
        for kk, (ohk, ixk, pk) in enumerate(((oh1, ix1, p1), (oh2, ix2, p2))):
            tt("dve", msk, ohk, posv, ALU.mult, [Trt_], [Trt_])
            P.op("dve", lambda e, ixk=ixk: e.tensor_reduce(out=ixk, in_=msk, axis=AX.X, op=ALU.add), [Trt_], [Trt_])
            cpy("dve", idxi[kk], ixk, [Trt_], [Trt_])
            P.op("pool", lambda e, kk=kk: e.memset(rows[kk], 0.0), [Trt_], [Trt_])
            cpy("pool", rows[kk][:, :, 0:1], cpv("tokid").unsqueeze(2), [Trt_, Tcp], [Trt_])
            cpy("pool", rows[kk][:, :, 1:2], pk.unsqueeze(2), [Trt_], [Trt_])
        for kk in range(2):
            for i in range(16):
                P.dma("pool", lambda e, kk=kk, i=i: e.indirect_dma_start(
                    out=listd, out_offset=bass.IndirectOffsetOnAxis(ap=idxi[kk][:, i:i + 1], axis=0),
                    in_=rows[kk][:, i, :], in_offset=None),
                    "lsc", reads=[Trt_, Tlistd], writes=[])
        dump("L", L, [16, 36], F32)

        P.barrier()
        rsc = Alloc(RSC0, RSC1)
        wgu_region = rsc.get(8 * 2048, BF16, (8, 2048))
        wgb = [wgu_region[:, :, 0:512], wgu_region[:, :, 512:1024]]
        wub = [wgu_region[:, :, 1024:1536], wgu_region[:, :, 1536:2048]]
        Twg, Twu = [T("wg0"), T("wg1")], [T("wu0"), T("wu1")]
        rbc = Alloc(RBC0, RBC1)
        wdb = [rbc.get(4 * 1024, BF16, (4, 1024)) for _ in range(2)]
        Twd = [T("wd0"), T("wd1")]
        rh = Alloc(RH0, RH1)
        sli = [rh.get(NS * 16, F32, (NS, 16)) for _ in range(4)]
        Tsli = [T(f"sli{q}") for q in range(4)]
        tki = [rh.get(NS, I32) for _ in range(4)]
        Ttki = [T(f"tki{q}") for q in range(4)]
        Xe = [rh.get(NS * 1024, BF16, (NS, 1024)) for _ in range(2)]
        TXe = [T("Xe0"), T("Xe1")]
        XgT = [rh.get(8 * CAP, BF16, (8, CAP)) for _ in range(2)]
        TXgT = [T("XgT0"), T("XgT1")]
        sg = [rh.get(CAP, F32) for _ in range(2)]
        Tsg = [T("sg0"), T("sg1")]
        hT = [rh.get(4 * CAP, BF16, (4, CAP)) for _ in range(2)]
        ThT = [T("hT0"), T("hT1")]
        yb = [rh.get(1024, F32) for _ in range(3)]
        Tyb = [T("yb0"), T("yb1"), T("yb2")]
        for b in range(2):
            P.op("pool", lambda e, b=b: e.memset(Xe[b], 0.0), writes=[TXe[b]])
        rx = Alloc(RX0, RX1)
        stg = [rx.get(4096, F32), rx.get(4096, F32), rh.get(4096, F32)]
        Tstg = [T("stg0"), T("stg1"), T("stg2")]
        cnt = 0
        ycnt = 0

        def prefetch(ex):
            b = ex % 2
            b4 = ex % 4
            P.dma("sp", lambda e, ex=ex, b4=b4: e.dma_start(
                out=sli[b4], in_=listd[ex * CAP:(ex + 1) * CAP, :].rearrange("(s p) c -> p s c", p=128)),
                f"sli{b4}", writes=[Tsli[b4]])
            srcs = (w_gate[ex].rearrange("(k p) f -> p k f", p=128), w_up[ex].rearrange("(k p) f -> p k f", p=128),
                    w_down[ex].rearrange("(k p) d -> p k d", p=128))
            views = (stg[0].rearrange("p (k f) -> p k f", f=512), stg[1].rearrange("p (k f) -> p k f", f=512),
                     stg[2].rearrange("p (k f) -> p k f", f=1024))
            for q in range(3):
                P.dma("sp", lambda e, q=q, srcs=srcs, views=views: e.dma_start(out=views[q], in_=srcs[q]), f"stg{q}",
                      writes=[Tstg[q]])
            cpy("dve", tki[b4], sli[b4][:, :, 0], [Tsli[b4]], [Ttki[b4]])
            for s in range(NS):
                P.dma("pool", lambda e, b=b, b4=b4, s=s: e.indirect_dma_start(
                    out=Xe[b][:, s, :], out_offset=None, in_=hn2d,
                    in_offset=bass.IndirectOffsetOnAxis(ap=tki[b4][:, s:s + 1], axis=0)),
                    f"xg{b}", reads=[Ttki[b4]], writes=[TXe[b]])

        def casts(ex):
            b = ex % 2
            v0 = stg[0].rearrange("p (k f) -> p k f", f=512)
            v1 = stg[1].rearrange("p (k f) -> p k f", f=512)
            v2 = stg[2].rearrange("p (k f) -> p k f", f=1024)
            cpy("act", wgb[b][:, 0:4, :], v0[:, 0:4, :], [Tstg[0]], [Twg[b]])
            cpy("dve", wgb[b][:, 4:8, :], v0[:, 4:8, :], [Tstg[0]], [Twg[b]])
            cpy("act", wub[b][:, 0:4, :], v1[:, 0:4, :], [Tstg[1]], [Twu[b]])
            cpy("dve", wub[b][:, 4:8, :], v1[:, 4:8, :], [Tstg[1]], [Twu[b]])
            cpy("act", wdb[b][:, 0:2, :], v2[:, 0:2, :], [Tstg[2]], [Twd[b]])
            cpy("dve", wdb[b][:, 2:4, :], v2[:, 2:4, :], [Tstg[2]], [Twd[b]])

        if n_experts > 0:
            prefetch(0)
            casts(0)
        for ex in range(n_experts):
            b = ex % 2
            b4 = ex % 4
            if ex + 1 < n_experts:
                prefetch(ex + 1)
            for s in range(NS):
                pT, TpT = next_pT()
                for k in range(8):
                    tr(pT[:, k, :], Xe[b][:, s, k * 128:(k + 1) * 128], identb, [TXe[b], Tconst], [TpT])
                cpy("act" if s % 2 == 0 else "dve", XgT[b][:, :, s * 128:(s + 1) * 128], pT, [TpT], [TXgT[b]])
            for f in range(4):
                pg, Tpg = pb[cnt % 2], Tpb[cnt % 2]
                pu, Tpu = pb[2 + cnt % 2], Tpb[2 + cnt % 2]
                s_, Ts_ = sg[cnt % 2], Tsg[cnt % 2]
                cnt += 1
                for k in range(8):
                    mm(pg[:, 0:CAP], wgb[b][:, k, f * 128:(f + 1) * 128], XgT[b][:, k, :], k == 0, k == 7,
                       [Twg[b], TXgT[b]], [Tpg])
                for k in range(8):
                    mm(pu[:, 0:CAP], wub[b][:, k, f * 128:(f + 1) * 128], XgT[b][:, k, :], k == 0, k == 7,
                       [Twu[b], TXgT[b]], [Tpu])
                act(s_, pg[:, 0:CAP], AF.Silu, [Tpg], [Ts_])
                tt("dve", hT[b][:, f, :], s_, pu[:, 0:CAP], ALU.mult, [Ts_, Tpu], [ThT[b]])
            ys = []
            for s in range(NS):
                yy, Tyy = yb[ycnt % 3], Tyb[ycnt % 3]
                ycnt += 1
                for half in range(2):
                    pd, Tpd = pb[4 + half], Tpb[4 + half]
                    for f in range(4):
                        mm(pd, hT[b][:, f, s * 128:(s + 1) * 128], wdb[b][:, f, half * 512:(half + 1) * 512],
                           f == 0, f == 3, [ThT[b], Twd[b]], [Tpd])
                    if half == 0:
                        act(yy[:, 0:512], pd, AF.Copy, [Tpd, Tsli[b4]], [Tyy], scale=sli[b4][:, s, 1:2])
                    else:
                        ts("dve", yy[:, 512:1024], pd, sli[b4][:, s, 1:2], ALU.mult, [Tpd, Tsli[b4]], [Tyy])
                ys.append((yy, Tyy, s))
            if ex + 1 < n_experts:
                casts(ex + 1)
            for yy, Tyy, s in ys:
                P.dma("pool", lambda e, b4=b4, s=s, yy=yy: e.indirect_dma_start(
                    out=hacc, out_offset=bass.IndirectOffsetOnAxis(ap=tki[b4][:, s:s + 1], axis=0),
                    in_=yy, in_offset=None, compute_op=ALU.add),
                    "ysc", reads=[Ttki[b4], Tyy], writes=[Thacc])

        rs2 = Alloc(RS20, RS21)
        fn = rs2.get(1024, F32)
        Tfn = T("fn")
        hb = [rs2.get(1024, F32) for _ in range(3)]
        Thb = [T("hb0"), T("hb1"), T("hb2")]
        ob = [rs2.get(1024, F32) for _ in range(2)]
        Tob = [T("ob0"), T("ob1")]
        Tout = T("out")
        P.barrier()
        P.dma("sp", lambda e: e.dma_start(out=fn, in_=fnd), "fn", writes=[Tfn])
        for i in range(16):
            j = i % 2
            j3 = i % 3
            P.dma("sp", lambda e, i=i, j3=j3: e.dma_start(out=hb[j3], in_=hacc[i * 128:(i + 1) * 128, :]), f"hb{j3}",
                  reads=[Thacc], writes=[Thb[j3]])
            sc_, Tsc_ = rmsnorm_stats(hb[j3], ob[j], 128, [Thb[j3]], Tob[j])
            stt("dve", ob[j], hb[j3], sc_[:, 2:3], fn, ALU.mult, ALU.mult, [Thb[j3], Tsc_, Tfn], [Tob[j]])
            P.dma("sp", lambda e, i=i, j=j: e.dma_start(out=out[i * 128:(i + 1) * 128, :], in_=ob[j]), f"out{j}",
                  reads=[Tob[j]], writes=[Tout])
        P.barrier(engs=("sp",))
        P.emit(st)
    return nc


_NC_CACHE = {}


def make_inputs(inputs):
    x = np.asarray(inputs["x"], np.float32)
    in_maps = []
    shared = {
        "w_in": np.ascontiguousarray(np.asarray(inputs["w_in"], np.float32)[0]),
        "w_out": np.ascontiguousarray(np.asarray(inputs["w_out"], np.float32)[0]),
        "w_gate": np.ascontiguousarray(np.asarray(inputs["w_gate"], np.float32)[0]),
        "w_up": np.ascontiguousarray(np.asarray(inputs["w_up"], np.float32)[0]),
        "w_down": np.ascontiguousarray(np.asarray(inputs["w_down"], np.float32)[0]),
        "fnorm": np.ascontiguousarray(np.broadcast_to(np.asarray(inputs["final_norm"], np.float32)[None, :], (128, D))),
        "gfb": np.ascontiguousarray(np.broadcast_to(np.asarray(inputs["norm_ffn"], np.float32)[0][None, :], (128, D))),
    }
    inp_np = {k: np.asarray(v, np.float32) for k, v in inputs.items() if k not in ("x", "w_in", "w_out", "w_gate", "w_up", "w_down")}
    for c in range(NCORES):
        b, s = c // 4, c % 4
        start = s * SPAN
        lo = start - NPREV * SPAN - HALO
        xe = np.zeros((NTOK_EXT, D), np.float32)
        src_lo = max(lo, 0)
        xe[src_lo - lo:, :] = x[b, src_lo:start + SPAN, :]
        m = dict(shared)
        m["xe"] = xe
        m["cp"] = make_cp(inp_np, c)
        in_maps.append(m)
    return in_maps


def kernel(**inputs):
    if "nc" not in _NC_CACHE:
        _NC_CACHE["nc"] = build()
    nc = _NC_CACHE["nc"]
    in_maps = make_inputs(inputs)
    res = run_bass_kernel_spmd(nc, in_maps, core_ids=list(range(NCORES)))
    outs = [np.asarray(res.results[c]["out"], np.float32) for c in range(NCORES)]
    full = np.stack(outs, 0).reshape(2, 4 * SPAN, D)
    return full
```
